# Optimizing a Trainium2 kernel written in Bass

```python
import math, functools
import jax, jax.numpy as jnp
from jax import lax
import numpy as np


D_MODEL = 1024
BATCH = 8
SEQ = 4096
DEPTH = 2

CTX_LEN = 256
GRID_W = 64
HEAD_DIM = 64
MIX_W = D_MODEL
ATT_W = MIX_W // 2
RET_W = MIX_W // 4
MLSTM_W = MIX_W - ATT_W - RET_W
ATT_HEADS = ATT_W // HEAD_DIM
ATT_KV_HEADS = ATT_HEADS // 4
KV_W = ATT_KV_HEADS * HEAD_DIM
RET_HEADS = RET_W // HEAD_DIM
MLSTM_HEADS = MLSTM_W // HEAD_DIM
N_GATES = 4
SPLIT_SIZES = (ATT_W, KV_W, KV_W, RET_W, RET_W, RET_W, RET_W, MLSTM_W, MLSTM_W, MLSTM_W, MLSTM_W, N_GATES * MLSTM_HEADS)
N_IN = sum(SPLIT_SIZES)
CHUNK = 128
Q_BLOCK = 128
CONV_W = 3
ROPE_THETA = 10000.0
D_FF = 2816
N_EXPERTS = 8
TOP_K = 2
D_FF_EXPERT = 3584
MOE_BLOCK = 256
N_DENSE = (DEPTH + 1) // 2
N_MOE = DEPTH // 2
EPS = 1e-6
F32 = jnp.float32

kernel_name = 'hybrid_dit_retention_gqa_mlstm_moe'


def rms_norm(x, w):
    xf = x.astype(F32)
    y = xf * lax.rsqrt(jnp.mean(jnp.square(xf), -1, keepdims=True) + EPS)
    return (y * w.astype(F32)).astype(x.dtype)


def head_layer_norm(x, w):
    xf = x.astype(F32)
    mu = jnp.mean(xf, -1, keepdims=True)
    var = jnp.mean(jnp.square(xf - mu), -1, keepdims=True)
    g = w.astype(F32).reshape(1, x.shape[1], 1, x.shape[3])
    return (xf - mu) * lax.rsqrt(var + EPS) * g


def modulate(x, shift, scale):
    return x * (1.0 + scale) + shift


def adaln_chunks(cond, w, b):
    return jnp.split(jax.nn.silu(cond) @ w + b, 6, axis=-1)


def to_heads(x):
    b, t, w = x.shape
    return x.reshape(b, t, w // HEAD_DIM, HEAD_DIM).transpose(0, 2, 1, 3)


def from_heads(x):
    b, h, t, d = x.shape
    return x.transpose(0, 2, 1, 3).reshape(b, t, h * d)


def axial_rope_tables(n_tokens):
    rows = n_tokens // GRID_W
    row = jnp.repeat(jnp.arange(rows, dtype=F32), GRID_W)
    col = jnp.tile(jnp.arange(GRID_W, dtype=F32), rows)
    n_freq = HEAD_DIM // 4
    inv_freq = ROPE_THETA ** (-jnp.arange(n_freq, dtype=F32) / n_freq)
    ang = jnp.concatenate([row[:, None] * inv_freq, col[:, None] * inv_freq], -1)
    return jnp.cos(ang), jnp.sin(ang)


def apply_rope(x, cos, sin):
    half = x.shape[-1] // 2
    x1, x2 = x[..., :half], x[..., half:]
    return jnp.concatenate([x1 * cos - x2 * sin, x1 * sin + x2 * cos], -1)


def conv_centered(x, w):
    ch = x.shape[-1]
    return lax.conv_general_dilated(x, w[:, None, :].astype(x.dtype), window_strides=(1,), padding='SAME',
                                    dimension_numbers=('NWC', 'WIO', 'NWC'), feature_group_count=ch)


def _flip(t):
    return jnp.flip(t, axis=2)


def _chunks(x):
    return x.reshape(x.shape[:2] + (x.shape[2] // CHUNK, CHUNK) + x.shape[3:])


def group_queries(q):
    b, hq, t, d = q.shape
    return q.reshape(b, ATT_KV_HEADS, hq // ATT_KV_HEADS, t, d)


def attend(qg, keys, vals):
    s = jnp.einsum('bhgqd,bhkd->bhgqk', qg, keys).astype(F32) * (HEAD_DIM ** -0.5)
    p = jax.nn.softmax(s, axis=-1).astype(vals.dtype)
    return jnp.einsum('bhgqk,bhkd->bhgqd', p, vals)


def latent_attention(q, k, v, k_ctx, v_ctx):
    b, hq, t, d = q.shape
    keys = jnp.concatenate([k_ctx, k], axis=2)
    vals = jnp.concatenate([v_ctx, v], axis=2)
    nb = t // Q_BLOCK
    qb = jnp.moveaxis(group_queries(q).reshape(b, ATT_KV_HEADS, hq // ATT_KV_HEADS, nb, Q_BLOCK, d), 3, 0)
    ob = lax.map(lambda qi: attend(qi, keys, vals), qb)
    return jnp.moveaxis(ob, 0, 3).reshape(b, hq, t, d)


def retention_states(k, v, log_gamma, s0):
    kc, vc = _chunks(k.astype(F32)), _chunks(v.astype(F32))
    j = jnp.arange(CHUNK, dtype=F32)
    k_dec = kc * jnp.exp((CHUNK - 1.0 - j)[None, :] * log_gamma[:, None])[None, :, None, :, None]
    s_inc = jnp.einsum('bhcsk,bhcsv->cbhkv', k_dec, vc)
    chunk_decay = jnp.exp(CHUNK * log_gamma)[None, :, None, None]

    def step(s, inc):
        return chunk_decay * s + inc, s

    s_fin, s_start = lax.scan(step, s0, s_inc)
    return s_start, s_fin


def retention_out(q, k, v, log_gamma, s_start):
    qc, kc, vc = (_chunks(t.astype(F32)) for t in (q, k, v))
    i = jnp.arange(CHUNK, dtype=F32)
    lag = i[:, None] - i[None, :]
    decay = jnp.where(lag >= 0, jnp.exp(jnp.maximum(lag, 0.0)[None] * log_gamma[:, None, None]), 0.0)
    scores = jnp.einsum('bhcld,bhcsd->bhcls', qc, kc) * decay[None, :, None]
    intra = jnp.einsum('bhcls,bhcsv->bhclv', scores, vc)
    q_dec = qc * jnp.exp((i + 1.0)[None, :] * log_gamma[:, None])[None, :, None, :, None]
    cross = jnp.einsum('bhcld,cbhdv->bhclv', q_dec, s_start)
    o = intra + cross
    return o.reshape(q.shape[0], q.shape[1], -1, o.shape[-1])


def bidir_retention_states(k, v, log_gamma, init_f, init_b):
    st_f, fin_f = retention_states(k, v, log_gamma[0], init_f)
    st_b, fin_b = retention_states(_flip(k), _flip(v), log_gamma[1], init_b)
    return (st_f, st_b), (fin_f, fin_b)


def bidir_retention_out(q, k, v, log_gamma, starts):
    st_f, st_b = starts
    fwd = retention_out(q, k, v, log_gamma[0], st_f)
    bwd = _flip(retention_out(_flip(q), _flip(k), _flip(v), log_gamma[1], st_b))
    return fwd + bwd


def _mlstm_logs(ig, lf):
    return _chunks(ig), jnp.cumsum(_chunks(lf), axis=-1)


def mlstm_states(k, v, ig, lf, state0):
    kc, vc = _chunks(k.astype(F32)), _chunks(v.astype(F32))
    igc, b = _mlstm_logs(ig, lf)
    b_end = b[..., -1]
    a = b_end[..., None] - b + igc
    m_loc = jnp.max(a, axis=-1)
    kw = kc * jnp.exp(a - m_loc[..., None])[..., None]
    c_inc = jnp.einsum('bhcsk,bhcsv->cbhkv', kw, vc)
    n_inc = jnp.moveaxis(jnp.sum(kw, axis=3), 2, 0)

    def step(state, inp):
        c, n, m = state
        be, ml, ci, ni = inp
        m_new = jnp.maximum(be + m, ml)
        w_prev = jnp.exp(be + m - m_new)
        w_inc = jnp.exp(ml - m_new)
        c_new = w_prev[..., None, None] * c + w_inc[..., None, None] * ci
        n_new = w_prev[..., None] * n + w_inc[..., None] * ni
        return (c_new, n_new, m_new), (c, n, m)

    xs = (jnp.moveaxis(b_end, 2, 0), jnp.moveaxis(m_loc, 2, 0), c_inc, n_inc)
    final, starts = lax.scan(step, state0, xs)
    return starts, final


def mlstm_out(q, k, v, ig, lf, starts):
    c_st, n_st, m_st = starts
    qc, kc, vc = (_chunks(t.astype(F32)) for t in (q, k, v))
    igc, b = _mlstm_logs(ig, lf)
    tri = jnp.tril(jnp.ones((CHUNK, CHUNK), dtype=bool))
    log_d = jnp.where(tri, b[..., :, None] - b[..., None, :] + igc[..., None, :], -jnp.inf)
    g = b + jnp.moveaxis(m_st, 0, 2)[..., None]
    m_t = jnp.maximum(g, jnp.max(log_d, axis=-1))
    s = jnp.einsum('bhcld,bhcsd->bhcls', qc, kc) * jnp.exp(log_d - m_t[..., None])
    w_prev = jnp.exp(g - m_t)
    num = jnp.einsum('bhcls,bhcsv->bhclv', s, vc) + w_prev[..., None] * jnp.einsum('bhcld,cbhdv->bhclv', qc, c_st)
    den = jnp.sum(s, axis=-1) + w_prev * jnp.einsum('bhcld,cbhd->bhcl', qc, n_st)
    h = num / jnp.maximum(jnp.abs(den), jnp.exp(-m_t))[..., None]
    return h.reshape(q.shape[0], q.shape[1], -1, h.shape[-1])


def bidir_mlstm_states(k, v, ig, lf, init_f, init_b):
    st_f, fin_f = mlstm_states(k, v, ig[0], lf[0], init_f)
    st_b, fin_b = mlstm_states(_flip(k), _flip(v), _flip(ig[1]), _flip(lf[1]), init_b)
    return (st_f, st_b), (fin_f, fin_b)


def bidir_mlstm_out(q, k, v, ig, lf, starts):
    st_f, st_b = starts
    fwd = mlstm_out(q, k, v, ig[0], lf[0], st_f)
    bwd = _flip(mlstm_out(_flip(q), _flip(k), _flip(v), _flip(ig[1]), _flip(lf[1]), st_b))
    return fwd + bwd


def prepare_heads(p, rope, qn_w, kn_w, conv_w, gate_b):
    b, t, _ = p.shape
    offsets = [int(o) for o in np.cumsum(SPLIT_SIZES)[:-1]]
    aq, ak, av, rq, rk, rv, rg, mq, mk, mv, mo, mg = jnp.split(p, offsets, axis=-1)
    aq = rms_norm(to_heads(aq), qn_w)
    ak = rms_norm(to_heads(ak), kn_w)
    rq, rk = to_heads(rq), to_heads(rk) * (HEAD_DIM ** -0.5)
    if rope is not None:
        aq, ak, rq, rk = (apply_rope(z, rope[0], rope[1]) for z in (aq, ak, rq, rk))
    mqk = jax.nn.silu(conv_centered(jnp.concatenate([mq, mk], axis=-1), conv_w))
    gates = (mg.astype(F32).reshape(b, t, N_GATES, MLSTM_HEADS) + gate_b.astype(F32)).transpose(2, 0, 3, 1)
    return {'aq': aq, 'ak': ak, 'av': to_heads(av),
            'rq': rq, 'rk': rk, 'rv': to_heads(rv), 'rg': rg,
            'mq': to_heads(mqk[..., :MLSTM_W]), 'mk': to_heads(mqk[..., MLSTM_W:]) * (HEAD_DIM ** -0.5),
            'mv': to_heads(mv), 'mo': mo, 'ig': gates[:2], 'lf': jax.nn.log_sigmoid(gates[2:])}


def token_mixers(p_ctx, p_lat, rope, qn_w, kn_w, ret_decay, ret_norm_w, conv_w, gate_b, mlstm_norm_w, need_ctx_out):
    dt = p_lat.dtype
    b = p_lat.shape[0]
    cx = prepare_heads(p_ctx, None, qn_w, kn_w, conv_w, gate_b)
    lt = prepare_heads(p_lat, rope, qn_w, kn_w, conv_w, gate_b)
    log_gamma = jnp.log1p(-jnp.exp(ret_decay.astype(F32)))
    s0 = jnp.zeros((b, RET_HEADS, HEAD_DIM, HEAD_DIM), F32)
    m0 = (jnp.zeros((b, MLSTM_HEADS, HEAD_DIM, HEAD_DIM), F32), jnp.zeros((b, MLSTM_HEADS, HEAD_DIM), F32),
          jnp.zeros((b, MLSTM_HEADS), F32))
    ret_st_c, ret_fin_c = bidir_retention_states(cx['rk'], cx['rv'], log_gamma, s0, s0)
    ret_st_l, _ = bidir_retention_states(lt['rk'], lt['rv'], log_gamma, ret_fin_c[0], ret_fin_c[1])
    ml_st_c, ml_fin_c = bidir_mlstm_states(cx['mk'], cx['mv'], cx['ig'], cx['lf'], m0, m0)
    ml_st_l, _ = bidir_mlstm_states(lt['mk'], lt['mv'], lt['ig'], lt['lf'], ml_fin_c[0], ml_fin_c[1])

    def merge(d, att, ret_st, ml_st):
        ret = bidir_retention_out(d['rq'], d['rk'], d['rv'], log_gamma, ret_st)
        ret = from_heads(head_layer_norm(ret, ret_norm_w)).astype(dt) * jax.nn.silu(d['rg'])
        ml = bidir_mlstm_out(d['mq'], d['mk'], d['mv'], d['ig'], d['lf'], ml_st)
        ml = ml * jax.nn.sigmoid(to_heads(d['mo']).astype(F32))
        ml = from_heads(head_layer_norm(ml, mlstm_norm_w)).astype(dt)
        return jnp.concatenate([from_heads(att), ret, ml], axis=-1)

    att_l = latent_attention(lt['aq'], lt['ak'], lt['av'], cx['ak'], cx['av'])
    out_l = merge(lt, att_l, ret_st_l, ml_st_l)
    out_c = None
    if need_ctx_out:
        qc = cx['aq']
        att_c = attend(group_queries(qc), cx['ak'], cx['av']).reshape(qc.shape)
        out_c = merge(cx, att_c, ret_st_c, ml_st_c)
    return out_c, out_l


def swiglu(x, w_gate, w_up, w_down):
    return (jax.nn.silu(x @ w_gate) * (x @ w_up)) @ w_down


def moe_swiglu(x, router_w, router_b, w_gate, w_up, w_down):
    shp = x.shape
    xt = x.reshape(-1, shp[-1])
    n_tok = xt.shape[0]
    logits = (xt @ router_w + router_b).astype(F32)
    top_v, top_i = lax.top_k(logits, TOP_K)
    top_w = jax.nn.softmax(top_v, axis=-1)
    n_assign = n_tok * TOP_K
    flat_e = top_i.reshape(-1)
    order = jnp.argsort(flat_e)
    sorted_e = flat_e[order]
    sorted_tok = order // TOP_K
    sorted_w = top_w.reshape(-1)[order]
    counts = jnp.bincount(flat_e, length=N_EXPERTS)
    padded = (counts + MOE_BLOCK - 1) // MOE_BLOCK * MOE_BLOCK
    starts = jnp.cumsum(counts) - counts
    pad_ends = jnp.cumsum(padded)
    pad_starts = pad_ends - padded
    slot = pad_starts[sorted_e] + jnp.arange(n_assign) - starts[sorted_e]
    n_blocks = -(-n_assign // MOE_BLOCK) + N_EXPERTS
    n_slots = n_blocks * MOE_BLOCK
    slot_tok = jnp.zeros((n_slots,), jnp.int32).at[slot].set(sorted_tok.astype(jnp.int32))
    slot_w = jnp.zeros((n_slots,), F32).at[slot].set(sorted_w)
    block_e = jnp.minimum(jnp.searchsorted(pad_ends, jnp.arange(n_blocks) * MOE_BLOCK, side='right'), N_EXPERTS - 1)
    xb = xt[slot_tok].reshape(n_blocks, MOE_BLOCK, shp[-1])

    def expert_block(args):
        xi, e = args
        return swiglu(xi, w_gate[e], w_up[e], w_down[e])

    yb = lax.map(expert_block, (xb, block_e)).reshape(n_slots, shp[-1])
    y = jax.ops.segment_sum(yb * slot_w[:, None].astype(yb.dtype), slot_tok, num_segments=n_tok)
    return y.reshape(shp)


def setup_inputs(seed: int = 0) -> dict:
    key = jax.random.key(seed)
    ks = jax.random.split(key, 26)
    D = D_MODEL

    def nrm(k, shape, scale):
        return jax.random.normal(k, shape, F32) * scale

    ret_base = -(5.0 + jnp.arange(RET_HEADS, dtype=F32)) * math.log(2.0)
    fgate = jnp.linspace(3.0, 6.0, MLSTM_HEADS, dtype=F32)
    zero_h = jnp.zeros((MLSTM_HEADS,), F32)
    gate_base = jnp.stack([zero_h, zero_h, fgate, fgate])
    return {
        'x': nrm(ks[0], (BATCH, SEQ, D), 1.0),
        'c': nrm(ks[1], (BATCH, D), 1.0),
        'ctx': nrm(ks[2], (BATCH, CTX_LEN, D), 1.0),
        'c_ctx': nrm(ks[3], (D,), 1.0),
        'mod_w': nrm(ks[4], (DEPTH, D, 6 * D), 0.5 * D ** -0.5),
        'mod_b': nrm(ks[5], (DEPTH, 6 * D), 0.02),
        'norm1_w': 1.0 + nrm(ks[6], (DEPTH, D), 0.02),
        'norm2_w': 1.0 + nrm(ks[7], (DEPTH, D), 0.02),
        'w_in': nrm(ks[8], (DEPTH, D, N_IN), D ** -0.5),
        'attn_qn_w': 1.0 + nrm(ks[9], (DEPTH, HEAD_DIM), 0.02),
        'attn_kn_w': 1.0 + nrm(ks[10], (DEPTH, HEAD_DIM), 0.02),
        'ret_decay': ret_base + nrm(ks[11], (DEPTH, 2, RET_HEADS), 0.05),
        'ret_norm_w': 1.0 + nrm(ks[12], (DEPTH, RET_W), 0.02),
        'mlstm_conv_w': nrm(ks[13], (DEPTH, CONV_W, 2 * MLSTM_W), CONV_W ** -0.5),
        'mlstm_gate_b': gate_base + nrm(ks[14], (DEPTH, N_GATES, MLSTM_HEADS), 0.1),
        'mlstm_norm_w': 1.0 + nrm(ks[15], (DEPTH, MLSTM_W), 0.02),
        'w_out': nrm(ks[16], (DEPTH, MIX_W, D), MIX_W ** -0.5),
        'ffn_w_gate': nrm(ks[17], (N_DENSE, D, D_FF), D ** -0.5),
        'ffn_w_up': nrm(ks[18], (N_DENSE, D, D_FF), D ** -0.5),
        'ffn_w_down': nrm(ks[19], (N_DENSE, D_FF, D), D_FF ** -0.5),
        'router_w': nrm(ks[20], (N_MOE, D, N_EXPERTS), D ** -0.5),
        'router_b': nrm(ks[21], (N_MOE, N_EXPERTS), 0.01),
        'moe_w_gate': nrm(ks[22], (N_MOE, N_EXPERTS, D, D_FF_EXPERT), D ** -0.5),
        'moe_w_up': nrm(ks[23], (N_MOE, N_EXPERTS, D, D_FF_EXPERT), D ** -0.5),
        'moe_w_down': nrm(ks[24], (N_MOE, N_EXPERTS, D_FF_EXPERT, D), D_FF_EXPERT ** -0.5),
    }


def reference(x, c, ctx, c_ctx, mod_w, mod_b, norm1_w, norm2_w, w_in, attn_qn_w, attn_kn_w, ret_decay,
              ret_norm_w, mlstm_conv_w, mlstm_gate_b, mlstm_norm_w, w_out, ffn_w_gate, ffn_w_up, ffn_w_down,
              router_w, router_b, moe_w_gate, moe_w_up, moe_w_down):
    cos, sin = axial_rope_tables(x.shape[1])
    rope = (cos.astype(x.dtype), sin.astype(x.dtype))
    h_lat, h_ctx = x, ctx
    for layer in range(DEPTH):
        last = layer == DEPTH - 1
        mod_l = [m[:, None, :] for m in adaln_chunks(c, mod_w[layer], mod_b[layer])]
        mod_c = adaln_chunks(c_ctx, mod_w[layer], mod_b[layer])
        a_lat = modulate(rms_norm(h_lat, norm1_w[layer]), mod_l[0], mod_l[1])
        a_ctx = modulate(rms_norm(h_ctx, norm1_w[layer]), mod_c[0], mod_c[1])
        mix_c, mix_l = token_mixers(a_ctx @ w_in[layer], a_lat @ w_in[layer], rope, attn_qn_w[layer],
                                    attn_kn_w[layer], ret_decay[layer], ret_norm_w[layer], mlstm_conv_w[layer],
                                    mlstm_gate_b[layer], mlstm_norm_w[layer], not last)
        h_lat = h_lat + mod_l[2] * (mix_l @ w_out[layer])
        if not last:
            h_ctx = h_ctx + mod_c[2] * (mix_c @ w_out[layer])
        if layer % 2 == 0:
            ffn = functools.partial(swiglu, w_gate=ffn_w_gate[layer // 2], w_up=ffn_w_up[layer // 2],
                                    w_down=ffn_w_down[layer // 2])
        else:
            ffn = functools.partial(moe_swiglu, router_w=router_w[layer // 2], router_b=router_b[layer // 2],
                                    w_gate=moe_w_gate[layer // 2], w_up=moe_w_up[layer // 2],
                                    w_down=moe_w_down[layer // 2])
        f_lat = modulate(rms_norm(h_lat, norm2_w[layer]), mod_l[3], mod_l[4])
        h_lat = h_lat + mod_l[5] * ffn(f_lat)
        if not last:
            f_ctx = modulate(rms_norm(h_ctx, norm2_w[layer]), mod_c[3], mod_c[4])
            h_ctx = h_ctx + mod_c[5] * ffn(f_ctx)
    return h_lat
```

```python
import contextlib
import numpy as np
import concourse.bass as bass
import concourse.mybir as mybir
from concourse.bass_utils import run_bass_kernel_spmd

AF = mybir.ActivationFunctionType
ALU = mybir.AluOpType
AX = mybir.AxisListType
F32 = mybir.dt.float32
BF16 = mybir.dt.bfloat16

NCH = 34
NTOK = NCH * 128
D = 1024
NIN = 2832
EPS = 1e-6
DEPTH = 2
DENSE_MOE = False
FWD_ORDER = list(range(NCH))
BWD_ORDER = [1, 0] + list(range(NCH - 1, 1, -1))
PQ, PK, PV, PRV, PRQ, PRK, PRG, PMV, PMO, PMQ, PMK = 0, 512, 640, 768, 1024, 1280, 1536, 1792, 2048, 2304, 2560
PCOLS = 2816


class Res:
    __slots__ = ("name", "w", "r", "isem", "osem", "icnt", "ocnt", "persist")

    def __init__(self, name):
        self.name = name
        self.w = None
        self.r = {}
        self.isem = None
        self.osem = None
        self.icnt = 0
        self.ocnt = 0
        self.persist = False


class Tile:
    def __init__(self, h, name):
        self.h = h
        self.r = Res(name)

    def __getitem__(self, k):
        return self.h[k]


class FW:
    ROT = 30000

    def __init__(self, nc):
        self.nc = nc
        self.engs = {"pe": nc.tensor, "act": nc.scalar, "dve": nc.vector,
                     "pool": nc.gpsimd, "sp": nc.sync}
        self.csem = {}
        self.ccnt = {}
        self.known = {k: {} for k in self.engs}
        self.nsem = 0
        self.allsems = []
        self.sempool = []
        self.sempool_sw = []
        self.semq = {}
        self.dma_live = []
        self.old_counters = []
        for k in self.engs:
            self._newc(k)

    def sem(self, name):
        self.nsem += 1
        s = self.nc.alloc_semaphore(name=f"{name}_{self.nsem}")
        self.allsems.append(s)
        return s

    def _newc(self, k):
        if k in self.csem and self.ccnt[k] > 0:
            self.old_counters.append((self.csem[k], self.ccnt[k]))
        self.csem[k] = self.sem("c" + k)
        self.ccnt[k] = 0

    def _wait(self, eng, evs, noself=False):
        need = {}
        kn = self.known[eng]
        for ev in evs:
            if ev is None:
                continue
            s, v = ev
            if noself and s is self.csem[eng]:
                continue
            sid = id(s)
            if kn.get(sid, 0) >= v:
                continue
            if sid not in need or need[sid][1] < v:
                need[sid] = (s, v)
        for sid, (s, v) in need.items():
            self.engs[eng].wait_ge(s, v)
            kn[sid] = v

    @staticmethod
    def _deps(reads, writes, merge=False):
        evs = []
        for r in reads:
            evs.append(r.w)
        for w in writes:
            if not merge:
                evs.append(w.w)
            evs.extend(w.r.values())
        return evs

    @staticmethod
    def _commit(ev, reads, writes, merge=False):
        sid = id(ev[0])
        for r in reads:
            r.r[sid] = ev
        for w in writes:
            w.w = ev
            if not merge:
                w.r = {}

    def op(self, eng, fn, reads=(), writes=(), noself=None):
        reads = [x.r if isinstance(x, Tile) else x for x in reads]
        writes = [x.r if isinstance(x, Tile) else x for x in writes]
        if noself is None:
            noself = (eng == "pe")
        self._wait(eng, self._deps(reads, writes), noself=noself)
        ins = fn(self.engs[eng])
        self.ccnt[eng] += 1
        ins.then_inc(self.csem[eng], 1)
        ev = (self.csem[eng], self.ccnt[eng])
        self._commit(ev, reads, writes)
        if self.ccnt[eng] >= self.ROT:
            self._newc(eng)
        return ev

    def _getsem(self, q):
        pool_ = self.sempool_sw if q == "pool" else self.sempool
        if pool_:
            return pool_.pop()
        return (self.sem("dsw" if q == "pool" else "d"), 0)

    def dma(self, q, out, in_, reads=(), writes=(), holder=None, kind=None, merge=False, **kw):
        reads = [x.r if isinstance(x, Tile) else x for x in reads]
        writes = [x.r if isinstance(x, Tile) else x for x in writes]
        self._wait(q, self._deps(reads, writes, merge=merge))
        ins = self.engs[q].dma_start(out=out, in_=in_, **kw)
        if holder is None:
            holder, kind = (reads[0], "o") if kind == "o" else (writes[0], "i")
        elif isinstance(holder, Tile):
            holder = holder.r
        if kind == "i":
            if holder.isem is None:
                holder.isem, holder.icnt = self._getsem(q)
                self.semq[id(holder.isem)] = q == "pool"
                if not holder.persist:
                    self.dma_live.append(holder)
            assert self.semq[id(holder.isem)] == (q == "pool"), holder.name
            holder.icnt += 16
            ins.then_inc(holder.isem, 16)
            ev = (holder.isem, holder.icnt)
        else:
            if holder.osem is None:
                holder.osem, holder.ocnt = self._getsem(q)
                self.semq[id(holder.osem)] = q == "pool"
                self.dma_live.append(holder)
            assert self.semq[id(holder.osem)] == (q == "pool"), holder.name
            holder.ocnt += 16
            ins.then_inc(holder.osem, 16)
            ev = (holder.osem, holder.ocnt)
        self._commit(ev, reads, writes, merge=merge)
        return ev

    def idma(self, out, in_, idx_ap, scatter, bound, reads=(), writes=(), holder=None, kind="i", merge=False):
        q = "pool"
        reads = [x.r if isinstance(x, Tile) else x for x in reads]
        writes = [x.r if isinstance(x, Tile) else x for x in writes]
        self._wait(q, self._deps(reads, writes, merge=merge))
        off = bass.IndirectOffsetOnAxis(ap=idx_ap, axis=0)
        ins = self.nc.gpsimd.indirect_dma_start(out=out, out_offset=(off if scatter else None), in_=in_, in_offset=(None if scatter else off))
        if isinstance(holder, Tile):
            holder = holder.r
        if kind == "i":
            if holder.isem is None:
                holder.isem, holder.icnt = self._getsem(q)
                self.semq[id(holder.isem)] = True
                if not holder.persist:
                    self.dma_live.append(holder)
            holder.icnt += 16
            ins.then_inc(holder.isem, 16)
            ev = (holder.isem, holder.icnt)
        else:
            if holder.osem is None:
                holder.osem, holder.ocnt = self._getsem(q)
                self.semq[id(holder.osem)] = True
                self.dma_live.append(holder)
            holder.ocnt += 16
            ins.then_inc(holder.osem, 16)
            ev = (holder.osem, holder.ocnt)
        self._commit(ev, reads, writes, merge=merge)
        return ev

    def barrier(self, release=True):
        evs = [(self.csem[k], self.ccnt[k]) for k in self.engs if self.ccnt[k] > 0]
        evs += self.old_counters
        for h in self.dma_live:
            if h.isem is not None:
                evs.append((h.isem, h.icnt))
            if h.osem is not None:
                evs.append((h.osem, h.ocnt))
        for k in self.engs:
            self._wait(k, evs, noself=True)
        if release:
            for h in self.dma_live:
                if h.isem is not None:
                    (self.sempool_sw if self.semq[id(h.isem)] else self.sempool).append((h.isem, h.icnt))
                    h.isem = None
                if h.osem is not None:
                    (self.sempool_sw if self.semq[id(h.osem)] else self.sempool).append((h.osem, h.ocnt))
                    h.osem = None
            self.dma_live = []


class Ring:
    def __init__(self, tiles):
        self.tiles = tiles
        self.i = 0

    def next(self):
        t = self.tiles[self.i % len(self.tiles)]
        self.i += 1
        return t


class Phase:
    cnt = 0

    def __init__(self, nc, fw, name):
        self.nc, self.fw, self.name = nc, fw, name

    def __enter__(self):
        self.st = contextlib.ExitStack()
        return self

    def __exit__(self, *a):
        self.fw.barrier()
        self.st.close()
        return False

    def sb(self, name, shape, dt):
        Phase.cnt += 1
        nm = f"{self.name}_{name}_{Phase.cnt}"
        return Tile(self.st.enter_context(self.nc.sbuf_tensor(nm, shape, dt)), nm)

    def ps(self, name, shape, dt=F32):
        Phase.cnt += 1
        nm = f"{self.name}_{name}_{Phase.cnt}"
        return Tile(self.st.enter_context(self.nc.psum_tensor(nm, shape, dt)), nm)

    def ring(self, name, n, shape, dt, psum=False):
        f = self.ps if psum else self.sb
        return Ring([f(f"{name}{i}", shape, dt) for i in range(n)])


def bc(ap, shape):
    return ap.to_broadcast(list(shape))


def build_program(dbg=None, force_ne8=False):
    nc = bass.Bass("TRN2", target_bir_lowering=False)
    NE = 1 if (dbg and not dbg.endswith("1") and not force_ne8) else 8
    fw = FW(nc)

    def din(name, shape, dt=F32):
        return nc.dram_tensor(name, list(shape), dt, kind="ExternalInput").ap()

    def dscr(name, shape, dt=F32):
        return nc.dram_tensor(name, list(shape), dt, kind=("ExternalOutput" if (dbg and not name.startswith("W")) else "Internal")).ap()

    xin = din("xin", [NTOK, D])
    cT = din("cT", [128, 8, 2])
    mod_w = din("mod_w", [DEPTH, D, 6 * D])
    modbT = din("modbT", [DEPTH, 128, 48])
    n1T = din("n1T", [DEPTH, 128, 8])
    n2T = din("n2T", [DEPTH, 128, 8])
    w_in = din("w_in", [DEPTH, D, NIN])
    qn_w = din("qn_w", [DEPTH, 64])
    kn_w = din("kn_w", [DEPTH, 64])
    ret_decay = din("ret_decay", [DEPTH, 8])
    ret_norm_w = din("ret_norm_w", [DEPTH, 256])
    conv_w = din("conv_w", [DEPTH, 3, 512])
    gate_b = din("gate_b", [DEPTH, 16])
    mlstm_norm_w = din("mlstm_norm_w", [DEPTH, 256])
    w_out = din("w_out", [DEPTH, D, D])
    ffn_wg = din("ffn_wg", [1, D, 2816])
    ffn_wu = din("ffn_wu", [1, D, 2816])
    ffn_wd = din("ffn_wd", [1, 2816, D])
    router_w = din("router_w", [D, 8])
    router_b = din("router_b", [1, 8])
    moe_wg = din("moe_wg", [NE, D, 3584])
    moe_wu = din("moe_wu", [NE, D, 3584])
    moe_wd = din("moe_wd", [NE, 3584, D])
    ident_d = din("ident", [128, 128])
    maskF_d = din("maskF", [128, 128])
    maskB_d = din("maskB", [128, 128])
    cos_d = din("cos", [128, 32, 32])
    sin_d = din("sin", [128, 32, 32])
    pos_d = din("pos", [128, 2])
    n2row = din("n2row", [DEPTH, D])
    grid_d = din("grid512", [128, 24])
    out = nc.dram_tensor("out", [4096, D], F32, kind="ExternalOutput").ap()

    H = dscr("H", [NTOK, D])
    P = dscr("P", [NTOK, PCOLS], BF16)
    GT = dscr("GT", [16, NTOK])
    MIX = dscr("MIX", [NTOK, D], BF16)
    MD = dscr("MD", [2, 6 * D])
    NSLOT = 12288
    NTL = NSLOT // 512
    XS = dscr("XS", [NSLOT, D], BF16)
    YS = dscr("YS", [NSLOT, D], F32)
    XSres = Res("XS")
    YSres = Res("YS")
    I32 = mybir.dt.int32
    FF_CFG = [dict(nexp=1, nblk=11, nffc=2, wg=ffn_wg, wu=ffn_wu, wd=ffn_wd),
              dict(nexp=NE, nblk=7, nffc=4, wg=moe_wg, wu=moe_wu, wd=moe_wd)]
    for i, cfg in enumerate(FF_CFG):
        bw = cfg["nffc"] * 128
        cfg["bw"] = bw
        cfg["WG"] = dscr(f"WG{i}", [cfg["nexp"], cfg["nblk"], 128, 8, bw], BF16)
        cfg["WU"] = dscr(f"WU{i}", [cfg["nexp"], cfg["nblk"], 128, 8, bw], BF16)
        cfg["WD"] = dscr(f"WD{i}", [cfg["nexp"], cfg["nblk"], 128, cfg["nffc"], D], BF16)
        cfg["res"] = Res(f"wconv{i}")

    Hres = [Res(f"H{c}") for c in range(NCH)]
    Pres = [Res(f"P{c}") for c in range(NCH)]
    Pcv = [Res(f"Pcv{c}") for c in range(NCH)]
    GTres = [Res(f"GT{c}") for c in range(NCH)]
    MIXres = [Res(f"MIX{c}") for c in range(NCH)]
    MDres = Res("MD")
    OUTres = Res("OUT")

    gst = contextlib.ExitStack()
    with gst:
        G = Phase(nc, fw, "G")
        G.st = gst
        ident_f = G.sb("identf", [128, 128], F32)
        ident_b = G.sb("identb", [128, 128], BF16)
        maskF = G.sb("maskF", [128, 128], F32)
        maskB = G.sb("maskB", [128, 128], F32)
        cst = G.sb("cst", [128, 4], F32)
        modT = G.sb("modT", [128, 48, 2], F32)
        g1T = G.sb("g1T", [128, 8, 2], F32)
        g2T = G.sb("g2T", [128, 8, 2], F32)
        ones_f = G.sb("onesf", [128, 128], F32)

        fw.dma("sp", ident_f[:], ident_d, writes=[ident_f])
        fw.dma("sp", maskF[:], maskF_d, writes=[maskF])
        fw.dma("sp", maskB[:], maskB_d, writes=[maskB])
        fw.op("dve", lambda e: e.tensor_copy(out=ident_b[:], in_=ident_f[:]), reads=[ident_f], writes=[ident_b])
        fw.op("dve", lambda e: e.memset(cst[:, 0:1], EPS), writes=[cst])
        fw.op("dve", lambda e: e.memset(cst[:, 1:2], 1.0), writes=[cst])
        fw.op("dve", lambda e: e.memset(cst[:, 2:3], 0.0), writes=[cst])
        fw.op("dve", lambda e: e.memset(ones_f[:], 1.0), writes=[ones_f])

        wconv_list = []
        for cfg in FF_CFG:
            cfg["res"].persist = True
            cfg["first_idx"] = len(wconv_list)
            for e_ in range(cfg["nexp"]):
                for b_ in range(cfg["nblk"]):
                    n0 = b_ * cfg["bw"]
                    for (dst, src) in ((cfg["WG"], cfg["wg"]), (cfg["WU"], cfg["wu"])):
                        wconv_list.append((dst[e_, b_], src[e_].rearrange("(kc p) n -> p kc n", p=128)[:, :, n0:n0 + cfg["bw"]], cfg["res"]))
                    wconv_list.append((cfg["WD"][e_, b_], cfg["wd"][e_][n0:n0 + cfg["bw"], :].rearrange("(j p) n -> p j n", p=128), cfg["res"]))
            cfg["last_idx"] = len(wconv_list)
        wconv_pos = [0]

        def pump(n=1, upto=None):
            while (n > 0 or (upto is not None and wconv_pos[0] < upto)) and wconv_pos[0] < len(wconv_list):
                dst, src, r = wconv_list[wconv_pos[0]]
                wconv_pos[0] += 1
                n -= 1
                fw.dma("pool", dst, src, writes=[r], holder=r, kind="i", merge=True)

        for l in range(DEPTH):
            last = (l == DEPTH - 1)
            first_out_chunk = 2 if last else 0
            with Phase(nc, fw, f"S{l}") as ph:
                cTt = ph.sb("cT", [128, 8, 2], F32)
                sc = ph.sb("sc", [128, 8, 2], F32)
                mb = ph.sb("mb", [128, 48], F32)
                n1 = ph.sb("n1", [128, 8], F32)
                n2 = ph.sb("n2", [128, 8], F32)
                mwr = ph.ring("mw", 2, [128, 8, 512], F32)
                pm = ph.ps("pm", [128, 48, 2], F32)
                ptm = ph.ps("ptm", [48, 128], F32)
                mds = ph.sb("mds", [48, 128], F32)
                fw.dma("sp", cTt[:], cT, writes=[cTt])
                fw.dma("sp", mb[:], modbT[l], writes=[mb])
                fw.dma("sp", n1[:], n1T[l], writes=[n1])
                fw.dma("sp", n2[:], n2T[l], writes=[n2])
                fw.op("act", lambda e: e.activation(out=sc[:], in_=cTt[:], func=AF.Silu), reads=[cTt], writes=[sc])
                for piece in range(12):
                    mw = mwr.next()
                    fw.dma("sp", mw[:], mod_w[l].rearrange("(kc p) n -> p kc n", p=128)[:, :, piece * 512:(piece + 1) * 512],
                           writes=[mw])
                    for jj in range(4):
                        j = piece * 4 + jj
                        for kc in range(8):
                            fw.op("pe", lambda e, j=j, jj=jj, kc=kc, mw=mw: e.matmul(
                                pm[:, j, :], lhsT=mw[:, kc, jj * 128:(jj + 1) * 128], rhs=sc[:, kc, :],
                                start=(kc == 0), stop=(kc == 7)), reads=[mw, sc], writes=[pm])
                fw.op("dve", lambda e: e.tensor_tensor(out=modT[:], in0=pm[:], in1=bc(mb[:].unsqueeze(2), [128, 48, 2]), op=ALU.add),
                      reads=[pm, mb], writes=[modT])
                for (gT, lo, nn) in ((g1T, 8, n1), (g2T, 32, n2)):
                    fw.op("dve", lambda e, gT=gT, lo=lo: e.tensor_scalar(out=gT[:], in0=modT[:, lo:lo + 8, :], scalar1=1.0, scalar2=None, op0=ALU.add),
                          reads=[modT], writes=[gT])
                    fw.op("dve", lambda e, gT=gT, nn=nn: e.tensor_tensor(out=gT[:], in0=gT[:], in1=bc(nn[:].unsqueeze(2), [128, 8, 2]), op=ALU.mult),
                          reads=[gT, nn], writes=[gT])
                for t in range(2):
                    fw.op("pe", lambda e, t=t: e.transpose(out=ptm[:], in_=modT[:, :, t], identity=ident_f[:]),
                          reads=[modT, ident_f], writes=[ptm])
                    fw.op("dve", lambda e: e.tensor_copy(out=mds[:], in_=ptm[:]), reads=[ptm], writes=[mds])
                    fw.dma("sp", MD[t].rearrange("(j p) -> j p", p=128), mds[:], reads=[mds], writes=[MDres], kind="o")

            with Phase(nc, fw, f"A{l}") as ph:
                Win = ph.sb("Win", [128, 8, NIN], BF16)
                WC = ph.sb("WC", [128, 3, 8, 512], BF16)
                cwb = ph.sb("cwb", [128, 3, 512], F32)
                qnb = ph.sb("qnb", [128, 64], F32)
                knb = ph.sb("knb", [128, 64], F32)
                cos_t = ph.sb("cos", [128, 32, 32], F32)
                sin_t = ph.sb("sin", [128, 32, 32], F32)
                for kc in range(8):
                    fw.dma("pool", Win[:, kc, :], w_in[l][kc * 128:(kc + 1) * 128, :], writes=[Win], merge=True)
                fw.dma("sp", cwb[:], conv_w[l].partition_broadcast(128), writes=[cwb])
                fw.dma("sp", qnb[:], qn_w[l].partition_broadcast(128), writes=[qnb])
                fw.dma("sp", knb[:], kn_w[l].partition_broadcast(128), writes=[knb])
                fw.dma("sp", cos_t[:], cos_d, writes=[cos_t])
                fw.dma("sp", sin_t[:], sin_d, writes=[sin_t])
                for k in range(3):
                    fw.op("dve", lambda e, k=k: e.tensor_tensor(out=WC[:, k, :, :], in0=Win[:, :, 2304:2816],
                                                               in1=bc(cwb[:, k:k + 1, :], [128, 8, 512]), op=ALU.mult),
                          reads=[Win, cwb], writes=[WC])
                rings = dict(st=ph.ring("st", 4, [128, 4], F32), xn=ph.ring("xn", 2, [128, D], F32),
                             tp=ph.ring("tp", 2, [128, 4, 128], F32, psum=True))
                hcr = ph.ring("hc", 3, [128, D], F32)
                NLIN = 6
                LIN = [ph.sb(f"lin{i}", [128, 8, 130], BF16) for i in range(NLIN)]
                LINH = [Res(f"linh{i}") for i in range(NLIN)]
                pjr = ph.ring("pj", 5, [128, 512], F32, psum=True)
                pgr = ph.ring("pg", 1, [16, 128], F32, psum=True)
                Pcr = ph.ring("Pc", 3, [128, 2304], BF16)
                Pvr = ph.ring("Pv", 2, [128, 512], BF16)
                gsr = ph.ring("gs", 2, [16, 128], F32)
                tA = ph.ring("tA", 2, [128, 512], F32)
                tB = ph.ring("tB", 2, [128, 512], F32)
                tC = ph.ring("tC", 2, [128, 512], F32)
                tD = ph.ring("tD", 2, [128, 512], F32)
                s8r = ph.ring("s8", 4, [128, 16], F32)

                def rope(src, nh, dst_tile, dst_ap, lc):
                    sv = src[:, 0:nh * 64].rearrange("p (h d) -> p h d", d=64)
                    c_ = tC.next()
                    d_ = tD.next()
                    cv = c_[:, 0:nh * 64].rearrange("p (h d) -> p h d", d=64)
                    dv = d_[:, 0:nh * 64].rearrange("p (h d) -> p h d", d=64)
                    ov = dst_ap.rearrange("p (h d) -> p h d", d=64)
                    cb = bc(cos_t[:, lc:lc + 1, :], [128, nh, 32])
                    sb_ = bc(sin_t[:, lc:lc + 1, :], [128, nh, 32])
                    fw.op("pool", lambda e: e.tensor_tensor(out=cv[:, :, 0:32], in0=sv[:, :, 0:32], in1=cb, op=ALU.mult), reads=[src, cos_t], writes=[c_])
                    fw.op("pool", lambda e: e.tensor_tensor(out=cv[:, :, 32:64], in0=sv[:, :, 0:32], in1=sb_, op=ALU.mult), reads=[src, sin_t], writes=[c_])
                    fw.op("dve", lambda e: e.tensor_tensor(out=dv[:, :, 0:32], in0=sv[:, :, 32:64], in1=sb_, op=ALU.mult), reads=[src, sin_t], writes=[d_])
                    fw.op("dve", lambda e: e.tensor_tensor(out=dv[:, :, 32:64], in0=sv[:, :, 32:64], in1=cb, op=ALU.mult), reads=[src, cos_t], writes=[d_])
                    fw.op("pool", lambda e: e.tensor_tensor(out=ov[:, :, 0:32], in0=cv[:, :, 0:32], in1=dv[:, :, 0:32], op=ALU.subtract), reads=[c_, d_], writes=[dst_tile])
                    fw.op("dve", lambda e: e.tensor_tensor(out=ov[:, :, 32:64], in0=cv[:, :, 32:64], in1=dv[:, :, 32:64], op=ALU.add), reads=[c_, d_], writes=[dst_tile])

                def qknorm(pj, col0, nh, wb, dst_tile, dst_ap, c):
                    a_ = tA.next()
                    b_ = tB.next()
                    s8 = s8r.next()
                    n = nh * 64
                    pv = pj[:, col0:col0 + n]
                    fw.op("act", lambda e: e.activation(out=a_[:, 0:n], in_=pv, func=AF.Square), reads=[pj], writes=[a_])
                    fw.op("dve", lambda e: e.tensor_reduce(out=s8[:, 0:nh], in_=a_[:, 0:n].rearrange("p (h d) -> p h d", d=64), axis=AX.X, op=ALU.add),
                          reads=[a_], writes=[s8])
                    fw.op("act", lambda e: e.activation(out=s8[:, 8:8 + nh], in_=s8[:, 0:nh], func=AF.Sqrt, scale=1.0 / 64, bias=cst[:, 0:1]),
                          reads=[s8, cst], writes=[s8])
                    fw.op("dve", lambda e: e.reciprocal(out=s8[:, 0:nh], in_=s8[:, 8:8 + nh]), reads=[s8], writes=[s8])
                    fw.op("dve", lambda e: e.tensor_tensor(out=a_[:, 0:n].rearrange("p (h d) -> p h d", d=64), in0=pv.rearrange("p (h d) -> p h d", d=64),
                                                           in1=bc(s8[:, 0:nh].unsqueeze(2), [128, nh, 64]), op=ALU.mult), reads=[pj, s8], writes=[a_])
                    if c < 2:
                        fw.op("dve", lambda e: e.tensor_tensor(out=dst_ap.rearrange("p (h d) -> p h d", d=64), in0=a_[:, 0:n].rearrange("p (h d) -> p h d", d=64),
                                                               in1=bc(wb[:].unsqueeze(1), [128, nh, 64]), op=ALU.mult), reads=[a_, wb], writes=[dst_tile])
                    else:
                        fw.op("dve", lambda e: e.tensor_tensor(out=b_[:, 0:n].rearrange("p (h d) -> p h d", d=64), in0=a_[:, 0:n].rearrange("p (h d) -> p h d", d=64),
                                                               in1=bc(wb[:].unsqueeze(1), [128, nh, 64]), op=ALU.mult), reads=[a_, wb], writes=[b_])
                        rope(b_, nh, dst_tile, dst_ap, c - 2)

                def proj(lin, n0, n1_, pj, ncols):
                    for kc in range(8):
                        fw.op("pe", lambda e, kc=kc: e.matmul(pj[:, 0:ncols], lhsT=lin[:, kc, 1:129], rhs=Win[:, kc, n0:n1_],
                                                             start=(kc == 0), stop=(kc == 7)), reads=[lin, Win], writes=[pj])

                def front(c):
                    if True:
                        t = 1 if c < 2 else 0
                        lin = LIN[c % NLIN]
                        hc = hcr.next()
                        if l == 0:
                            fw.dma("sp", hc[:], xin[c * 128:(c + 1) * 128, :], writes=[hc])
                        else:
                            fw.dma("sp", hc[:], H[c * 128:(c + 1) * 128, :], reads=[Hres[c]], writes=[hc])
                        st4 = rings["st"].next()
                        xn = rings["xn"].next()
                        fw.op("act", lambda e: e.activation(out=xn[:], in_=hc[:], func=AF.Square, accum_out=st4[:, 0:1]), reads=[hc], writes=[xn, st4])
                        fw.op("act", lambda e: e.activation(out=st4[:, 1:2], in_=st4[:, 0:1], func=AF.Sqrt, scale=1.0 / D, bias=cst[:, 0:1]), reads=[st4, cst], writes=[st4])
                        fw.op("dve", lambda e: e.reciprocal(out=st4[:, 2:3], in_=st4[:, 1:2]), reads=[st4], writes=[st4])
                        fw.op("act", lambda e: e.activation(out=xn[:], in_=hc[:], func=AF.Copy, scale=st4[:, 2:3]), reads=[hc, st4], writes=[xn])
                        for half in range(2):
                            tp = rings["tp"].next()
                            for j in range(4):
                                kc = half * 4 + j
                                fw.op("pe", lambda e, kc=kc, j=j: e.transpose(out=tp[:, j, :], in_=xn[:, kc * 128:(kc + 1) * 128], identity=ident_f[:]),
                                      reads=[xn, ident_f], writes=[tp])
                            for j in range(4):
                                kc = half * 4 + j
                                fw.op("dve", lambda e, kc=kc, j=j: e.tensor_scalar(out=lin[:, kc, 1:129], in0=tp[:, j, :], scalar1=g1T[:, kc, t:t + 1],
                                                                                  scalar2=modT[:, kc, t:t + 1], op0=ALU.mult, op1=ALU.add),
                                      reads=[tp, g1T, modT], writes=[lin])
                        linh = LINH[c % NLIN]
                        if c in (0, 2):
                            fw.op("pool", lambda e: e.memset(lin[:, :, 0:1], 0.0), writes=[linh])
                        else:
                            prev = LIN[(c - 1) % NLIN]
                            fw.op("pool", lambda e: e.tensor_copy(out=lin[:, :, 0:1], in_=prev[:, :, 128:129]), reads=[prev], writes=[linh])
                            fw.op("pool", lambda e: e.tensor_copy(out=prev[:, :, 129:130], in_=lin[:, :, 1:2]), reads=[lin], writes=[LINH[(c - 1) % NLIN]])
                        if c in (1, NCH - 1):
                            fw.op("pool", lambda e: e.memset(lin[:, :, 129:130], 0.0), writes=[linh])
                def back(c):
                    front(c)
                    yield
                    if True:
                        lin = LIN[c % NLIN]
                        Pc = Pcr.next()
                        pj = pjr.next()
                        proj(lin, 0, 512, pj, 512)
                        qknorm(pj, 0, 8, qnb, Pc, Pc[:, PQ:PQ + 512], c)
                        pj = pjr.next()
                        proj(lin, 512, 1024, pj, 512)
                        qknorm(pj, 0, 2, knb, Pc, Pc[:, PK:PK + 128], c)
                        fw.op("act", lambda e, pj=pj: e.activation(out=Pc[:, PV:PV + 384], in_=pj[:, 128:512], func=AF.Copy), reads=[pj], writes=[Pc])
                        yield
                        pj = pjr.next()
                        proj(lin, 1024, 1536, pj, 512)
                        if c < 2:
                            fw.op("act", lambda e, pj=pj: e.activation(out=Pc[:, PRQ:PRQ + 512], in_=pj[:, 0:512], func=AF.Copy), reads=[pj], writes=[Pc])
                        else:
                            b_ = tB.next()
                            fw.op("act", lambda e, pj=pj, b_=b_: e.activation(out=b_[:], in_=pj[:, 0:512], func=AF.Copy), reads=[pj], writes=[b_])
                            rope(b_, 8, Pc, Pc[:, PRQ:PRQ + 512], c - 2)
                        pj = pjr.next()
                        proj(lin, 1536, 2048, pj, 512)
                        fw.op("act", lambda e, pj=pj: e.activation(out=Pc[:, PRG:PRG + 256], in_=pj[:, 0:256], func=AF.Silu), reads=[pj], writes=[Pc])
                        fw.op("act", lambda e, pj=pj: e.activation(out=Pc[:, PMV:PMV + 256], in_=pj[:, 256:512], func=AF.Copy), reads=[pj], writes=[Pc])
                        pj = pjr.next()
                        proj(lin, 2048, 2304, pj, 256)
                        fw.op("act", lambda e, pj=pj: e.activation(out=Pc[:, PMO:PMO + 256], in_=pj[:, 0:256], func=AF.Sigmoid), reads=[pj], writes=[Pc])
                        fw.dma("pool", P[c * 128:(c + 1) * 128, 0:2304], Pc[:], reads=[Pc], writes=[Pres[c]], kind="o")
                        pg = pgr.next()
                        for kc in range(8):
                            fw.op("pe", lambda e, kc=kc: e.matmul(pg[:], lhsT=Win[:, kc, 2816:2832], rhs=lin[:, kc, 1:129], start=(kc == 0), stop=(kc == 7)),
                                  reads=[lin, Win], writes=[pg])
                        gs = gsr.next()
                        fw.op("dve", lambda e: e.tensor_copy(out=gs[:], in_=pg[:]), reads=[pg], writes=[gs])
                        fw.dma("pool", GT[:, c * 128:(c + 1) * 128], gs[:], reads=[gs], writes=[GTres[c]], kind="o")
                    yield
                    cc = c
                    if True:
                        linp = LIN[cc % NLIN]
                        pj = pjr.next()
                        for k in range(3):
                            for kc in range(8):
                                fw.op("pe", lambda e, k=k, kc=kc: e.matmul(pj[:], lhsT=linp[:, kc, k:k + 128], rhs=WC[:, k, kc, :],
                                                                           start=(k == 0 and kc == 0), stop=(k == 2 and kc == 7)),
                                      reads=[linp, LINH[cc % NLIN], WC], writes=[pj])
                        Pv = Pvr.next()
                        fw.op("act", lambda e, pj=pj: e.activation(out=Pv[:], in_=pj[:], func=AF.Silu), reads=[pj], writes=[Pv])
                        fw.dma("pool", P[cc * 128:(cc + 1) * 128, 2304:2816], Pv[:], reads=[Pv], writes=[Pcv[cc]], kind="o")

                gens = []
                for c in list(range(NCH)) + [None]:
                    if c is not None:
                        pump(1)
                        gens.append(back(c))
                    for g_ in list(gens):
                        try:
                            next(g_)
                        except StopIteration:
                            gens.remove(g_)
                while gens:
                    for g_ in list(gens):
                        try:
                            next(g_)
                        except StopIteration:
                            gens.remove(g_)
            if dbg == f"A{l}":
                break

            with Phase(nc, fw, f"B{l}") as ph:
                kT = ph.sb("kT", [128, NTOK], BF16)
                Va = ph.sb("Va", [128, NCH, 2, 128], BF16)
                kvr = ph.ring("kv", 2, [128, 256], BF16)
                tqr = ph.ring("tq", 2, [128, 4, 128], BF16, psum=True)
                fw.op("pool", lambda e: e.memset(Va[:], 1.0), writes=[Va])
                for c in range(NCH):
                    kv = kvr.next()
                    fw.dma("sp", kv[:], P[c * 128:(c + 1) * 128, PK:PK + 256], reads=[Pres[c]], writes=[kv])
                    tk = tqr.next()
                    fw.op("pe", lambda e: e.transpose(out=tk[:, 0, :], in_=kv[:, 0:128], identity=ident_b[:]), reads=[kv, ident_b], writes=[tk])
                    fw.op("dve", lambda e, c=c: e.tensor_copy(out=kT[:, c * 128:(c + 1) * 128], in_=tk[:, 0, :]), reads=[tk], writes=[kT])
                    fw.op("pool", lambda e, c=c: e.tensor_copy(out=Va[:, c, :, 0:64], in_=kv[:, 128:256].rearrange("p (g d) -> p g d", d=64)),
                          reads=[kv], writes=[Va])
                qbr = ph.ring("qb", 2, [128, 512], BF16)
                qTr = ph.ring("qT", 2, [128, 2, 512], BF16)
                for qz in qTr.tiles:
                    fw.op("pool", lambda e, qz=qz: e.memset(qz[:], 0.0), writes=[qz])
                psr = ph.ring("pss", 3, [128, 512], F32, psum=True)
                PTr = ph.ring("PT", 5, [128, 512], BF16)
                oTr = ph.ring("oT", 2, [128, 512], F32, psum=True)
                otr = ph.ring("ot", 1, [128, 4, 128], F32, psum=True)
                oSr = ph.ring("oS", 2, [128, 512], F32)
                pending_epi = []
                recr = ph.ring("rec", 2, [128, 4], F32)
                attr = ph.ring("att", 3, [128, 512], BF16)
                for qb in range(first_out_chunk, NCH):
                    pump(1)
                    keys = [0, 1] if qb < 2 else list(range(NCH))
                    qt = qbr.next()
                    fw.dma("sp", qt[:], P[qb * 128:(qb + 1) * 128, PQ:PQ + 512], reads=[Pres[qb]], writes=[qt])
                    tq = tqr.next()
                    for i in range(4):
                        fw.op("pe", lambda e, i=i: e.transpose(out=tq[:, i, :], in_=qt[:, i * 128:(i + 1) * 128], identity=ident_b[:]),
                              reads=[qt, ident_b], writes=[tq])
                    qT = qTr.next()
                    fw.op("dve", lambda e: e.tensor_copy(out=qT[0:64, 0, :], in_=tq[0:64].rearrange("p a b -> p (a b)")), reads=[tq], writes=[qT])
                    fw.op("dve", lambda e: e.tensor_copy(out=qT[64:128, 1, :], in_=tq[64:128].rearrange("p a b -> p (a b)")), reads=[tq], writes=[qT])
                    att = attr.next()
                    for g in range(2):
                        oT = oTr.next()

                        def smm(kc, g=g):
                            pss = psr.next()
                            fw.op("pe", lambda e: e.matmul(pss[:], lhsT=kT[:, kc * 128:(kc + 1) * 128], rhs=qT[:, g, :], start=True, stop=True),
                                  reads=[kT, qT], writes=[pss])
                            return pss
                        pend = [smm(keys[0])]
                        if len(keys) > 1:
                            pend.append(smm(keys[1]))
                        for ki, kc in enumerate(keys):
                            pss = pend.pop(0)
                            if ki + 2 < len(keys):
                                pend.append(smm(keys[ki + 2]))
                            PT = PTr.next()
                            fw.op("act", lambda e, pss=pss, PT=PT: e.activation(out=PT[:], in_=pss[:], func=AF.Exp, scale=0.125), reads=[pss], writes=[PT])
                            fw.op("pe", lambda e, kc=kc, g=g, PT=PT, oT=oT, ki=ki: e.matmul(
                                oT[:, :], lhsT=Va[:, kc, g, :], rhs=PT[:, :], start=(ki == 0), stop=(ki == len(keys) - 1)), reads=[PT, Va], writes=[oT])
                            if ki == min(3, len(keys) - 1) and pending_epi:
                                pending_epi.pop(0)()

                        def epi(oT=oT, att=att, g=g, qb=qb, lastg=(g == 1)):
                            oS = oSr.next()
                            fw.op("dve", lambda e: e.tensor_copy(out=oS[:], in_=oT[:, :]), reads=[oT], writes=[oS])
                            ot = otr.next()
                            for i in range(4):
                                fw.op("pe", lambda e, i=i: e.transpose(out=ot[:, i, :], in_=oS[:, i * 128:(i + 1) * 128], identity=ident_f[:]),
                                      reads=[oS, ident_f], writes=[ot])
                            rec = recr.next()
                            fw.op("dve", lambda e: e.reciprocal(out=rec[:], in_=ot[:, :, 64]), reads=[ot], writes=[rec])
                            fw.op("dve", lambda e: e.tensor_tensor(
                                out=att[:, g * 256:(g + 1) * 256].rearrange("p (h d) -> p h d", d=64), in0=ot[:, :, 0:64],
                                in1=bc(rec[:].unsqueeze(2), [128, 4, 64]), op=ALU.mult), reads=[ot, rec], writes=[att])
                            if lastg:
                                fw.dma("pool", MIX[qb * 128:(qb + 1) * 128, 0:512], att[:], reads=[att], writes=[MIXres[qb]], kind="o")
                        pending_epi.append(epi)
                while pending_epi:
                    pending_epi.pop(0)()
            if dbg == f"B{l}":
                break

            for kind in ("ret", "ml"):
                if kind == "ret":
                    qcol, kcol, vcol, gcol, ocol = PRQ, PRK, PRV, PRG, 512
                    qres = kres = Pres
                else:
                    qcol, kcol, vcol, gcol, ocol = PMQ, PMK, PMV, PMO, 768
                    qres = kres = Pcv
                with Phase(nc, fw, f"{kind}{l}") as ph:
                    E = [ph.sb(f"E{d_}", [128, 4, NCH], F32) for d_ in range(2)]
                    Fm = [ph.sb(f"F{d_}", [128, 4, NCH], F32) for d_ in range(2)]
                    PRE = [ph.sb(f"PRE{d_}", [64, NCH, 4], F32) for d_ in range(2)]
                    POST = [ph.sb(f"POST{d_}", [64, 4], F32) for d_ in range(2)]
                    nwb = ph.sb("nwb", [128, 256], F32)
                    fw.dma("sp", nwb[:], (ret_norm_w if kind == "ret" else mlstm_norm_w)[l].partition_broadcast(128), writes=[nwb])
                    with Phase(nc, fw, f"{kind}{l}tab") as pt:
                        if kind == "ret":
                            rd = pt.sb("rd", [128, 8], F32)
                            lg = pt.sb("lg", [128, 8], F32)
                            pos = pt.sb("pos", [128, 2], F32)
                            tmp = pt.sb("tmp", [128, 8], F32)
                            tmp2 = pt.sb("tmp2", [128, 8], F32)
                            fw.dma("sp", rd[:], ret_decay[l].partition_broadcast(128), writes=[rd])
                            fw.dma("sp", pos[:], pos_d, writes=[pos])
                            fw.op("act", lambda e: e.activation(out=lg[:], in_=rd[:], func=AF.Exp), reads=[rd], writes=[lg])
                            fw.op("act", lambda e: e.activation(out=lg[:], in_=lg[:], func=AF.Ln, scale=-1.0, bias=cst[:, 1:2]), reads=[lg, cst], writes=[lg])
                            for d_ in range(2):
                                fw.op("dve", lambda e, d_=d_: e.tensor_scalar(out=tmp[:, d_ * 4:d_ * 4 + 4], in0=lg[:, d_ * 4:d_ * 4 + 4], scalar1=pos[:, d_:d_ + 1],
                                                                             scalar2=None, op0=ALU.mult), reads=[lg, pos], writes=[tmp])
                            fw.op("act", lambda e: e.activation(out=tmp2[:], in_=tmp[:], func=AF.Exp), reads=[tmp], writes=[tmp2])
                            for d_ in range(2):
                                fw.op("dve", lambda e, d_=d_: e.tensor_copy(out=Fm[d_][:], in_=bc(tmp2[:, d_ * 4:d_ * 4 + 4].unsqueeze(2), [128, 4, NCH])),
                                      reads=[tmp2], writes=[Fm[d_]])
                            fw.op("act", lambda e: e.activation(out=tmp2[:], in_=tmp[:], func=AF.Exp, scale=-1.0), reads=[tmp], writes=[tmp2])
                            fw.op("dve", lambda e: e.tensor_scalar(out=tmp2[:], in0=tmp2[:], scalar1=0.125, scalar2=None, op0=ALU.mult), reads=[tmp2], writes=[tmp2])
                            for d_ in range(2):
                                fw.op("dve", lambda e, d_=d_: e.tensor_copy(out=E[d_][:], in_=bc(tmp2[:, d_ * 4:d_ * 4 + 4].unsqueeze(2), [128, 4, NCH])),
                                      reads=[tmp2], writes=[E[d_]])
                            fw.op("act", lambda e: e.activation(out=tmp[:], in_=lg[:], func=AF.Exp, scale=128.0), reads=[lg], writes=[tmp])
                            for d_ in range(2):
                                fw.op("dve", lambda e, d_=d_: e.tensor_copy(out=POST[d_][:], in_=tmp[0:64, d_ * 4:d_ * 4 + 4]), reads=[tmp], writes=[POST[d_]])
                                fw.op("dve", lambda e, d_=d_: e.memset(PRE[d_][:], 1.0), writes=[PRE[d_]])
                        else:
                            Gt = pt.sb("Gt", [NCH, 16, 128], F32)
                            gb = pt.sb("gb", [NCH, 16], F32)
                            cs0 = pt.sb("cs0", [NCH, 8, 128], F32)
                            cs1 = pt.sb("cs1", [NCH, 8, 128], F32)
                            t2 = pt.sb("t2", [NCH, 8, 128], F32)
                            U = pt.sb("U", [NCH, 8, 128], F32)
                            NB = pt.sb("NB", [NCH, 8, 128], F32)
                            ub = pt.sb("ub", [NCH, 16], F32)
                            for c in range(NCH):
                                pass
                            fw.dma("sp", Gt[:], GT.rearrange("g (c t) -> c g t", t=128), reads=GTres, writes=[Gt])
                            fw.dma("sp", gb[:], gate_b[l].partition_broadcast(NCH), writes=[gb])
                            for d_ in range(2):
                                fw.op("dve", lambda e, d_=d_: e.memset(POST[d_][:], 1.0), writes=[POST[d_]])
                            fw.op("dve", lambda e: e.tensor_tensor(out=Gt[:], in0=Gt[:], in1=bc(gb[:].unsqueeze(2), [NCH, 16, 128]), op=ALU.add),
                                  reads=[Gt, gb], writes=[Gt])
                            fw.op("act", lambda e: e.activation(out=t2[:], in_=Gt[:, 8:16, :], func=AF.Exp, scale=-1.0), reads=[Gt], writes=[t2])
                            fw.op("act", lambda e: e.activation(out=t2[:], in_=t2[:], func=AF.Ln, scale=1.0, bias=cst[0:NCH, 1:2]), reads=[t2, cst], writes=[t2])
                            src, dst = t2, cs0
                            sh = 1
                            while sh < 128:
                                fw.op("dve", lambda e, src=src, dst=dst, sh=sh: e.tensor_tensor(out=dst[:, :, sh:128], in0=src[:, :, sh:128], in1=src[:, :, 0:128 - sh], op=ALU.add),
                                      reads=[src], writes=[dst])
                                fw.op("dve", lambda e, src=src, dst=dst, sh=sh: e.tensor_copy(out=dst[:, :, 0:sh], in_=src[:, :, 0:sh]), reads=[src], writes=[dst])
                                src = dst
                                dst = cs1 if dst is cs0 else cs0
                                if sh == 1:
                                    pass
                                sh *= 2
                            cs = src
                            other = dst
                            fw.op("dve", lambda e: e.tensor_copy(out=NB[:, 0:4, :], in_=cs[:, 0:4, :]), reads=[cs], writes=[NB])
                            fw.op("dve", lambda e: e.tensor_tensor(out=NB[:, 4:8, :], in0=t2[:, 4:8, :], in1=cs[:, 4:8, :], op=ALU.subtract), reads=[cs, t2], writes=[NB])
                            fw.op("dve", lambda e: e.tensor_tensor(out=NB[:, 4:8, :], in0=NB[:, 4:8, :], in1=bc(cs[:, 4:8, 127:128], [NCH, 4, 128]), op=ALU.add),
                                  reads=[cs, NB], writes=[NB])
                            fw.op("dve", lambda e: e.tensor_tensor(out=U[:], in0=Gt[:, 0:8, :], in1=NB[:], op=ALU.add), reads=[Gt, NB], writes=[U])
                            fw.op("dve", lambda e: e.tensor_reduce(out=ub[:, 0:8], in_=U[:], axis=AX.X, op=ALU.max), reads=[U], writes=[ub])
                            fw.op("dve", lambda e: e.tensor_scalar(out=ub[:, 8:16], in0=cs[:, :, 127], scalar1=-1.0, scalar2=None, op0=ALU.mult), reads=[cs], writes=[ub])
                            pq = pt.ps("pq", [4, 4, NCH], F32)
                            UB = pt.sb("UB", [4, 4, NCH], F32)
                            for qi in range(4):
                                fw.op("pe", lambda e, qi=qi: e.transpose(out=pq[:, qi, :], in_=ub[:, qi * 4:qi * 4 + 4], identity=ident_f[0:NCH, 0:NCH]),
                                      reads=[ub, ident_f], writes=[pq])
                            fw.op("dve", lambda e: e.tensor_copy(out=UB[:], in_=pq[:]), reads=[pq], writes=[UB])
                            mcur = pt.sb("mcur", [4, 2, NCH + 1], F32)
                            Mend = pt.sb("Mend", [4, 2, NCH], F32)
                            dd = pt.sb("dd", [4, 2, NCH], F32)
                            fw.op("dve", lambda e: e.memset(mcur[:], 0.0), writes=[mcur])
                            for d_, order in enumerate((FWD_ORDER, BWD_ORDER)):
                                for idx, c in enumerate(order):
                                    fw.op("dve", lambda e, d_=d_, idx=idx, c=c: e.tensor_tensor(out=Mend[:, d_, c:c + 1], in0=mcur[:, d_, idx:idx + 1],
                                                                                                in1=UB[:, d_, c:c + 1], op=ALU.max), reads=[mcur, UB], writes=[Mend])
                                    fw.op("dve", lambda e, d_=d_, idx=idx, c=c: e.tensor_tensor(out=mcur[:, d_, idx + 1:idx + 2], in0=Mend[:, d_, c:c + 1],
                                                                                                in1=UB[:, 2 + d_, c:c + 1], op=ALU.add), reads=[Mend, UB], writes=[mcur])
                                    fw.op("dve", lambda e, d_=d_, idx=idx, c=c: e.tensor_tensor(out=dd[:, d_, c:c + 1], in0=mcur[:, d_, idx:idx + 1],
                                                                                                in1=Mend[:, d_, c:c + 1], op=ALU.subtract), reads=[mcur, Mend], writes=[dd])
                            SC = pt.sb("SC", [4, 2, NCH], F32)
                            fw.op("act", lambda e: e.activation(out=SC[:], in_=dd[:], func=AF.Exp), reads=[dd], writes=[SC])
                            BD = pt.sb("BD", [4, NCH, 4], F32)
                            ppre = pt.ps("ppre", [64, NCH * 4], F32)
                            for d_ in range(2):
                                fw.op("dve", lambda e, d_=d_: e.tensor_tensor(out=BD[:], in0=bc(SC[:, d_, :].unsqueeze(2), [4, NCH, 4]),
                                                                             in1=bc(ident_f[0:4, 0:4].unsqueeze(1), [4, NCH, 4]), op=ALU.mult),
                                      reads=[SC, ident_f], writes=[BD])
                                fw.op("pe", lambda e: e.matmul(ppre[:], lhsT=ones_f[0:4, 0:64], rhs=BD[:].rearrange("p c h -> p (c h)"), start=True, stop=True),
                                      reads=[ones_f, BD], writes=[ppre])
                                fw.op("dve", lambda e, d_=d_: e.tensor_copy(out=PRE[d_][:].rearrange("p c h -> p (c h)"), in_=ppre[:]), reads=[ppre], writes=[PRE[d_]])
                            pm2 = pt.ps("pm2", [NCH, 8], F32)
                            MT = pt.sb("MT", [NCH, 8], F32)
                            for d_ in range(2):
                                fw.op("pe", lambda e, d_=d_: e.transpose(out=pm2[:, d_ * 4:d_ * 4 + 4], in_=Mend[:, d_, :], identity=ident_f[0:4, 0:4]),
                                      reads=[Mend, ident_f], writes=[pm2])
                            fw.op("dve", lambda e: e.tensor_copy(out=MT[:], in_=pm2[:]), reads=[pm2], writes=[MT])
                            fw.op("dve", lambda e: e.tensor_tensor(out=U[:], in0=U[:], in1=bc(MT[:].unsqueeze(2), [NCH, 8, 128]), op=ALU.subtract), reads=[U, MT], writes=[U])
                            fw.op("act", lambda e: e.activation(out=U[:], in_=U[:], func=AF.Exp), reads=[U], writes=[U])
                            fw.op("dve", lambda e: e.tensor_tensor(out=NB[:], in0=NB[:], in1=bc(MT[:].unsqueeze(2), [NCH, 8, 128]), op=ALU.subtract), reads=[NB, MT], writes=[NB])
                            fw.op("act", lambda e: e.activation(out=NB[:], in_=NB[:], func=AF.Exp), reads=[NB], writes=[NB])
                            ptab = pt.ps("ptab", [128, 4, NCH], F32)
                            for (srcT, dsts, scl) in ((U, E, 0.125), (NB, Fm, 1.0)):
                                for d_ in range(2):
                                    for h in range(4):
                                        fw.op("pe", lambda e, srcT=srcT, d_=d_, h=h: e.transpose(out=ptab[:, h, :], in_=srcT[:, d_ * 4 + h, :], identity=ident_f[0:NCH, 0:NCH]),
                                              reads=[srcT, ident_f], writes=[ptab])
                                    fw.op("dve", lambda e, dsts=dsts, d_=d_, scl=scl: e.tensor_scalar(out=dsts[d_][:], in0=ptab[:], scalar1=scl, scalar2=None, op0=ALU.mult),
                                          reads=[ptab], writes=[dsts[d_]])

                    SBs = ph.sb("SBs", [64, NCH, 4, 65], BF16)
                    ST = [ph.sb(f"ST{d_}", [64, 4, 65], F32) for d_ in range(2)]
                    SP = ph.ring("SP", 2, [64, 4, 65], F32)
                    SFb = ph.ring("SFb", 3, [64, 4, 65], BF16)
                    kvr = ph.ring("kv", 4, [128, 512], BF16)
                    Var = ph.ring("Va", 4, [128, 4, 65], BF16)
                    KEr = ph.ring("KE", 3, [128, 4, 64], BF16)
                    pinc = ph.ring("pinc", 2, [64, 4, 128], F32, psum=True)
                    for d_ in range(2):
                        fw.op("dve", lambda e, d_=d_: e.memset(ST[d_][:], 0.0), writes=[ST[d_]])
                    for va in Var.tiles:
                        fw.op("pool", lambda e, va=va: e.memset(va[:], 1.0), writes=[va])

                    def load_kv(c):
                        kv = kvr.next()
                        fw.dma("sp", kv[:, 0:256], P[c * 128:(c + 1) * 128, kcol:kcol + 256], reads=[kres[c]], writes=[kv], merge=True)
                        fw.dma("sp", kv[:, 256:512], P[c * 128:(c + 1) * 128, vcol:vcol + 256], reads=[Pres[c]], writes=[kv], merge=True)
                        va = Var.next()
                        fw.op("act", lambda e: e.activation(out=va[:, :, 0:64], in_=kv[:, 256:512].rearrange("p (h d) -> p h d", d=64), func=AF.Copy), reads=[kv], writes=[va])
                        return kv, va

                    def state_step(d_, c, kv, va, save_ap=None, save_tile=None):
                        sp = SP.next()
                        fw.op("dve", lambda e: e.tensor_tensor(out=sp[:], in0=ST[d_][:], in1=bc(PRE[d_][:, c, :].unsqueeze(2), [64, 4, 65]), op=ALU.mult),
                              reads=[ST[d_], PRE[d_]], writes=[sp])
                        fw.op("act", lambda e: e.activation(out=save_ap, in_=sp[:], func=AF.Copy), reads=[sp], writes=[save_tile])
                        ke = KEr.next()
                        fw.op("dve", lambda e: e.tensor_tensor(out=ke[:], in0=kv[:, 0:256].rearrange("p (h d) -> p h d", d=64),
                                                               in1=bc(E[d_][:, :, c:c + 1], [128, 4, 64]), op=ALU.mult), reads=[kv, E[d_]], writes=[ke])
                        pi = pinc.next()
                        for h in range(4):
                            fw.op("pe", lambda e, h=h: e.matmul(pi[:, h, 0:65], lhsT=ke[:, h, :], rhs=va[:, h, :], start=True, stop=True),
                                  reads=[ke, va], writes=[pi])
                        fw.op("dve", lambda e: e.tensor_tensor(out=ST[d_][:], in0=sp[:], in1=pi[:, :, 0:65], op=ALU.add), reads=[sp, pi], writes=[ST[d_]])
                        fw.op("dve", lambda e: e.tensor_tensor(out=ST[d_][:], in0=ST[d_][:], in1=bc(POST[d_][:].unsqueeze(2), [64, 4, 65]), op=ALU.mult),
                              reads=[ST[d_], POST[d_]], writes=[ST[d_]])

                    for c in BWD_ORDER:
                        pump(1)
                        kv, va = load_kv(c)
                        state_step(1, c, kv, va, save_ap=SBs[:, c, :, :], save_tile=SBs)

                    qgr = ph.ring("qg", 5, [128, 512], BF16)
                    ptq = ph.ring("ptq", 1, [64, 8, 128], BF16, psum=True)
                    qkT = ph.ring("qkT", 3, [64, 8, 128], BF16)
                    psc = ph.ring("psc", 1, [128, 4, 128], F32, psum=True)
                    tmpS = ph.ring("tmpS", 2, [128, 4, 128], F32)
                    SSr = [ph.ring(f"SS{d_}", 3, [128, 4, 128], BF16) for d_ in range(2)]
                    pR = [ph.ring(f"pR{d_}", 2, [128, 4, 128], F32, psum=True) for d_ in range(2)]
                    s4 = ph.ring("s4", 12, [128, 8], F32)
                    hh = ph.ring("hh", 16, [128, 4, 64], F32)
                    mo_r = ph.ring("mixo", 3, [128, 256], BF16)
                    def chunk_gen(c):
                        pump(1)
                        kv, va = load_kv(c)
                        sfb = SFb.next()
                        do_out = c >= first_out_chunk
                        if do_out:
                            qg = qgr.next()
                            fw.dma("sp", qg[:, 0:256], P[c * 128:(c + 1) * 128, qcol:qcol + 256], reads=[qres[c]], writes=[qg], merge=True)
                            fw.dma("sp", qg[:, 256:512], P[c * 128:(c + 1) * 128, gcol:gcol + 256], reads=[Pres[c]], writes=[qg], merge=True)
                        state_step(0, c, kv, va, save_ap=sfb[:], save_tile=sfb)
                        if not do_out:
                            return
                        tq = ptq.next()
                        for h in range(4):
                            fw.op("pe", lambda e, h=h: e.transpose(out=tq[:, h, :], in_=qg[:, h * 64:(h + 1) * 64], identity=ident_b[:]), reads=[qg, ident_b], writes=[tq])
                            fw.op("pe", lambda e, h=h: e.transpose(out=tq[:, 4 + h, :], in_=kv[:, h * 64:(h + 1) * 64], identity=ident_b[:]), reads=[kv, ident_b], writes=[tq])
                        qk = qkT.next()
                        fw.op("act", lambda e: e.activation(out=qk[:], in_=tq[:], func=AF.Copy), reads=[tq], writes=[qk])
                        ps_ = psc.next()
                        for h in range(4):
                            fw.op("pe", lambda e, h=h: e.matmul(ps_[:, h, :], lhsT=qk[:, 4 + h, :], rhs=qk[:, h, :], start=True, stop=True), reads=[qk], writes=[ps_])
                        sss = []
                        for d_ in range(2):
                            ts_ = tmpS.next()
                            ss = SSr[d_].next()
                            fw.op("dve", lambda e, d_=d_, ts_=ts_: e.tensor_tensor(out=ts_[:], in0=ps_[:], in1=bc(E[d_][:, :, c:c + 1], [128, 4, 128]), op=ALU.mult),
                                  reads=[ps_, E[d_]], writes=[ts_])
                            mk_ = maskF if d_ == 0 else maskB
                            fw.op("pool", lambda e, ts_=ts_, ss=ss, mk_=mk_: e.tensor_tensor(out=ss[:], in0=ts_[:], in1=bc(mk_[:].unsqueeze(1), [128, 4, 128]), op=ALU.mult),
                                  reads=[ts_, mk_], writes=[ss])
                            sss.append(ss)
                        yield
                        Rs = []
                        for d_ in range(2):
                            ss = sss[d_]
                            R = pR[d_].next()
                            for h in range(4):
                                fw.op("pe", lambda e, h=h, ss=ss, R=R: e.matmul(R[:, h, 0:65], lhsT=ss[:, h, :], rhs=va[:, h, :], start=True, stop=False),
                                      reads=[ss, va], writes=[R])
                                if d_ == 0:
                                    fw.op("pe", lambda e, h=h, R=R: e.matmul(R[:, h, 0:65], lhsT=qk[:, h, :], rhs=sfb[:, h, :], start=False, stop=True),
                                          reads=[qk, sfb], writes=[R])
                                else:
                                    fw.op("pe", lambda e, h=h, R=R: e.matmul(R[:, h, 0:65], lhsT=qk[:, h, :], rhs=SBs[:, c, h, :], start=False, stop=True),
                                          reads=[qk, SBs], writes=[R])
                            Rs.append(R)
                        yield
                        hsum = hh.next()
                        hd = []
                        for d_ in range(2):
                            R = Rs[d_]
                            hx = hh.next()
                            if kind == "ml":
                                den = s4.next()
                                fw.op("act", lambda e, R=R, den=den: e.activation(out=den[:, 0:4], in_=R[:, :, 64], func=AF.Abs), reads=[R], writes=[den])
                                fw.op("dve", lambda e, d_=d_, den=den: e.tensor_tensor(out=den[:, 0:4], in0=den[:, 0:4], in1=Fm[d_][:, :, c], op=ALU.max), reads=[den, Fm[d_]], writes=[den])
                                fw.op("dve", lambda e, den=den: e.reciprocal(out=den[:, 4:8], in_=den[:, 0:4]), reads=[den], writes=[den])
                                fw.op("dve", lambda e, R=R, den=den, hx=hx: e.tensor_tensor(out=hx[:], in0=R[:, :, 0:64], in1=bc(den[:, 4:8].unsqueeze(2), [128, 4, 64]), op=ALU.mult),
                                      reads=[R, den], writes=[hx])
                            else:
                                fw.op("dve", lambda e, R=R, d_=d_, hx=hx: e.tensor_tensor(out=hx[:], in0=R[:, :, 0:64], in1=bc(Fm[d_][:, :, c:c + 1], [128, 4, 64]), op=ALU.mult),
                                      reads=[R, Fm[d_]], writes=[hx])
                            hd.append(hx)
                        fw.op("pool", lambda e: e.tensor_tensor(out=hsum[:], in0=hd[0][:], in1=hd[1][:], op=ALU.add), reads=[hd[0], hd[1]], writes=[hsum])
                        gv = qg[:, 256:512].rearrange("p (h d) -> p h d", d=64)
                        if kind == "ml":
                            fw.op("pool", lambda e: e.tensor_tensor(out=hsum[:], in0=hsum[:], in1=gv, op=ALU.mult), reads=[hsum, qg], writes=[hsum])
                        yield
                        stt = s4.next()
                        xc = hh.next()
                        sq = hd[0]
                        fw.op("dve", lambda e: e.tensor_reduce(out=stt[:, 0:4], in_=hsum[:], axis=AX.X, op=ALU.add), reads=[hsum], writes=[stt])
                        fw.op("dve", lambda e: e.tensor_scalar(out=stt[:, 0:4], in0=stt[:, 0:4], scalar1=-1.0 / 64, scalar2=None, op0=ALU.mult), reads=[stt], writes=[stt])
                        fw.op("dve", lambda e: e.tensor_tensor(out=xc[:], in0=hsum[:], in1=bc(stt[:, 0:4].unsqueeze(2), [128, 4, 64]), op=ALU.add), reads=[hsum, stt], writes=[xc])
                        fw.op("act", lambda e: e.activation(out=sq[:], in_=xc[:], func=AF.Square), reads=[xc], writes=[sq])
                        fw.op("dve", lambda e: e.tensor_reduce(out=stt[:, 4:8], in_=sq[:], axis=AX.X, op=ALU.add), reads=[sq], writes=[stt])
                        fw.op("act", lambda e: e.activation(out=stt[:, 0:4], in_=stt[:, 4:8], func=AF.Sqrt, scale=1.0 / 64, bias=cst[:, 0:1]), reads=[stt, cst], writes=[stt])
                        fw.op("dve", lambda e: e.reciprocal(out=stt[:, 4:8], in_=stt[:, 0:4]), reads=[stt], writes=[stt])
                        fw.op("dve", lambda e: e.tensor_tensor(out=xc[:], in0=xc[:], in1=bc(stt[:, 4:8].unsqueeze(2), [128, 4, 64]), op=ALU.mult), reads=[xc, stt], writes=[xc])
                        mo_ = mo_r.next()
                        mv_ = mo_[:].rearrange("p (h d) -> p h d", d=64)
                        nv = nwb[:].rearrange("p (h d) -> p h d", d=64)
                        if kind == "ml":
                            fw.op("pool", lambda e: e.tensor_tensor(out=mv_, in0=xc[:], in1=nv, op=ALU.mult), reads=[xc, nwb], writes=[mo_])
                        else:
                            fw.op("pool", lambda e: e.tensor_tensor(out=xc[:], in0=xc[:], in1=nv, op=ALU.mult), reads=[xc, nwb], writes=[xc])
                            fw.op("pool", lambda e: e.tensor_tensor(out=mv_, in0=xc[:], in1=gv, op=ALU.mult), reads=[xc, qg], writes=[mo_])
                        fw.dma("pool", MIX[c * 128:(c + 1) * 128, ocol:ocol + 256], mo_[:], reads=[mo_], writes=[MIXres[c]], kind="o")

                    gens = []
                    for c in FWD_ORDER + [None]:
                        if c is not None:
                            gens.append(chunk_gen(c))
                        for g_ in list(gens):
                            try:
                                next(g_)
                            except StopIteration:
                                gens.remove(g_)
                    for g_ in gens:
                        for _ in g_:
                            pass
                if dbg == f"{kind}{l}":
                    break
            if dbg in (f"ret{l}", f"ml{l}"):
                break

            with Phase(nc, fw, f"E{l}") as ph:
                Wo = ph.sb("Wo", [128, 8, D], BF16)
                for kc in range(8):
                    fw.dma("pool", Wo[:, kc, :], w_out[l][kc * 128:(kc + 1) * 128, :], writes=[Wo], merge=True)
                gbc = [ph.sb(f"gbc{t}", [128, D], F32) for t in range(2)]
                for t in range(2):
                    fw.dma("sp", gbc[t][:], MD[t, 2 * D:3 * D].partition_broadcast(128), reads=[MDres], writes=[gbc[t]])
                mxr = ph.ring("mx", 2, [128, D], BF16)
                ptx = ph.ring("ptx", 2, [128, 8, 128], BF16, psum=True)
                mTr = ph.ring("mT", 2, [128, 8, 128], BF16)
                pyr = ph.ring("py", 4, [128, 512], F32, psum=True)
                hcr = ph.ring("hc", 3, [128, D], F32)
                tyr = ph.ring("ty", 2, [128, D], F32)
                for c in range(first_out_chunk, NCH):
                    pump(1)
                    t = 1 if c < 2 else 0
                    mx = mxr.next()
                    fw.dma("sp", mx[:], MIX[c * 128:(c + 1) * 128, :], reads=[MIXres[c]], writes=[mx])
                    hc = hcr.next()
                    if l == 0:
                        fw.dma("sp", hc[:], xin[c * 128:(c + 1) * 128, :], writes=[hc])
                    else:
                        fw.dma("sp", hc[:], H[c * 128:(c + 1) * 128, :], reads=[Hres[c]], writes=[hc])
                    tx = ptx.next()
                    for kc in range(8):
                        fw.op("pe", lambda e, kc=kc: e.transpose(out=tx[:, kc, :], in_=mx[:, kc * 128:(kc + 1) * 128], identity=ident_b[:]), reads=[mx, ident_b], writes=[tx])
                    mT = mTr.next()
                    fw.op("act", lambda e: e.activation(out=mT[:], in_=tx[:], func=AF.Copy), reads=[tx], writes=[mT])
                    ty = tyr.next()
                    for n in range(2):
                        py = pyr.next()
                        for kc in range(8):
                            fw.op("pe", lambda e, kc=kc, n=n, py=py: e.matmul(py[:], lhsT=mT[:, kc, :], rhs=Wo[:, kc, n * 512:(n + 1) * 512], start=(kc == 0), stop=(kc == 7)),
                                  reads=[mT, Wo], writes=[py])
                        fw.op("dve", lambda e, n=n, py=py: e.tensor_tensor(out=ty[:, n * 512:(n + 1) * 512], in0=py[:], in1=gbc[t][:, n * 512:(n + 1) * 512], op=ALU.mult),
                              reads=[py, gbc[t]], writes=[ty])
                    fw.op("pool", lambda e: e.tensor_tensor(out=hc[:], in0=hc[:], in1=ty[:], op=ALU.add), reads=[hc, ty], writes=[hc])
                    fw.dma("pool", H[c * 128:(c + 1) * 128, :], hc[:], reads=[hc], writes=[Hres[c]], kind="o")
            if dbg == f"E{l}":
                break

            cfg = FF_CFG[l % 2]
            pump(0, upto=cfg["last_idx"])
            moe = (l % 2 == 1)
            nffc, nblk, nexp = cfg["nffc"], cfg["nblk"], cfg["nexp"]
            if moe and not DENSE_MOE:
                with Phase(nc, fw, f"M{l}") as pm_:
                    SEL1 = pm_.sb("SEL1", [128, 32, 8], F32)
                    SEL2 = pm_.sb("SEL2", [128, 32, 8], F32)
                    GWS = pm_.sb("GWS", [128, 32, 8], F32)
                    RANK = pm_.sb("RANK", [128, 32, 8], F32)
                    BASE = pm_.sb("BASE", [128, 32, 8], F32)
                    SLOT_i = pm_.sb("SLOTi", [128, 2, 32], I32)
                    GWK = pm_.sb("GWK", [128, 2, 32], F32)
                    BE_i = pm_.sb("BEi", [128, NTL], I32)
                    IDXW = pm_.sb("IDXW", [128, NTL, 8], I32)
                    gbc = pm_.sb("gbc", [128, D], F32)
                    fw.dma("sp", gbc[:], MD[0, 5 * D:6 * D].partition_broadcast(128), reads=[MDres], writes=[gbc])
                    with Phase(nc, fw, f"MR{l}") as ph:
                        zt = ph.sb("zt", [128, 4, D], BF16)
                        fw.op("pool", lambda e: e.memset(zt[:], 0.0), writes=[zt])
                        for j in range(NTL):
                            fw.dma("sp", XS[j * 512:(j + 1) * 512, :].rearrange("(s p) d -> p s d", p=128), zt[:], reads=[zt], writes=[XSres], holder=zt, kind="o", merge=True)
                        ATS = ph.sb("ATS", [128, 32, D], BF16)
                        g2bc = ph.sb("g2bc", [128, D], F32)
                        sh2bc = ph.sb("sh2bc", [128, D], F32)
                        n2bc = ph.sb("n2bc", [128, D], F32)
                        Wr = ph.sb("Wr", [128, 8, 8], F32)
                        rbb = ph.sb("rbb", [128, 8], F32)
                        Ust = ph.sb("Ust", [128, 128], F32)
                        run = ph.sb("run", [128, 8], F32)
                        grid = ph.sb("grid", [128, 24], F32)
                        fw.dma("sp", g2bc[:], MD[0, 4 * D:5 * D].partition_broadcast(128), reads=[MDres], writes=[g2bc])
                        fw.dma("sp", sh2bc[:], MD[0, 3 * D:4 * D].partition_broadcast(128), reads=[MDres], writes=[sh2bc])
                        fw.dma("sp", n2bc[:], n2row[l].partition_broadcast(128), writes=[n2bc])
                        fw.dma("sp", Wr[:], router_w.rearrange("(kc p) e -> p kc e", p=128), writes=[Wr])
                        fw.dma("sp", rbb[:], router_b[0].partition_broadcast(128), writes=[rbb])
                        fw.dma("sp", grid[:], grid_d, writes=[grid])
                        fw.op("dve", lambda e: e.tensor_scalar(out=g2bc[:], in0=g2bc[:], scalar1=1.0, scalar2=None, op0=ALU.add), reads=[g2bc], writes=[g2bc])
                        fw.op("dve", lambda e: e.tensor_tensor(out=g2bc[:], in0=g2bc[:], in1=n2bc[:], op=ALU.mult), reads=[g2bc, n2bc], writes=[g2bc])
                        fw.op("dve", lambda e: e.tensor_tensor(out=Ust[:], in0=maskF[:], in1=ident_f[:], op=ALU.subtract), reads=[maskF, ident_f], writes=[Ust])
                        fw.op("dve", lambda e: e.memset(run[:], 0.0), writes=[run])
                        str_ = ph.ring("st", 4, [128, 4], F32)
                        xnr = ph.ring("xn", 2, [128, D], F32)
                        tpr = ph.ring("tp", 2, [128, 4, 128], F32, psum=True)
                        hcr = ph.ring("hc", 2, [128, D], F32)
                        tmr = ph.ring("tm", 2, [128, D], F32)
                        a32r = ph.ring("a32", 2, [128, 8, 128], F32)
                        LG = ph.sb("LG", [128, 32, 8], F32)
                        plr = ph.ring("pl", 2, [128, 64], F32, psum=True)
                        for s_ in range(32):
                            c = 2 + s_
                            hc = hcr.next()
                            fw.dma("sp", hc[:], H[c * 128:(c + 1) * 128, :], reads=[Hres[c]], writes=[hc])
                            st4 = str_.next()
                            xn = xnr.next()
                            fw.op("act", lambda e: e.activation(out=xn[:], in_=hc[:], func=AF.Square, accum_out=st4[:, 0:1]), reads=[hc], writes=[xn, st4])
                            fw.op("act", lambda e: e.activation(out=st4[:, 1:2], in_=st4[:, 0:1], func=AF.Sqrt, scale=1.0 / D, bias=cst[:, 0:1]), reads=[st4, cst], writes=[st4])
                            fw.op("dve", lambda e: e.reciprocal(out=st4[:, 2:3], in_=st4[:, 1:2]), reads=[st4], writes=[st4])
                            fw.op("act", lambda e: e.activation(out=xn[:], in_=hc[:], func=AF.Copy, scale=st4[:, 2:3]), reads=[hc, st4], writes=[xn])
                            tm = tmr.next()
                            fw.op("pool", lambda e: e.tensor_tensor(out=tm[:], in0=xn[:], in1=g2bc[:], op=ALU.mult), reads=[xn, g2bc], writes=[tm])
                            fw.op("pool", lambda e, s_=s_: e.tensor_tensor(out=ATS[:, s_, :], in0=tm[:], in1=sh2bc[:], op=ALU.add), reads=[tm, sh2bc], writes=[ATS])
                            a32 = a32r.next()
                            for half in range(2):
                                tp = tpr.next()
                                for j in range(4):
                                    kc = half * 4 + j
                                    fw.op("pe", lambda e, kc=kc, j=j: e.transpose(out=tp[:, j, :], in_=xn[:, kc * 128:(kc + 1) * 128], identity=ident_f[:]),
                                          reads=[xn, ident_f], writes=[tp])
                                for j in range(4):
                                    kc = half * 4 + j
                                    fw.op("dve", lambda e, kc=kc, j=j: e.tensor_scalar(out=a32[:, kc, :], in0=tp[:, j, :], scalar1=g2T[:, kc, 0:1],
                                                                                      scalar2=modT[:, 24 + kc, 0:1], op0=ALU.mult, op1=ALU.add),
                                          reads=[tp, g2T, modT], writes=[a32])
                            pl = plr.next()
                            for kc in range(8):
                                fw.op("pe", lambda e, kc=kc: e.matmul(pl[:, 0:8], lhsT=a32[:, kc, :], rhs=Wr[:, kc, :], start=(kc == 0), stop=(kc == 7)),
                                      reads=[a32, Wr], writes=[pl])
                            fw.op("dve", lambda e, s_=s_: e.tensor_tensor(out=LG[:, s_, :], in0=pl[:, 0:8], in1=rbb[:], op=ALU.add), reads=[pl, rbb], writes=[LG])
                        m12 = ph.sb("m12", [128, 4, 32], F32)
                        L2 = ph.sb("L2", [128, 32, 8], F32)
                        SEL = ph.sb("SEL", [128, 32, 8], F32)
                        EX = ph.sb("EX", [128, 32, 8], F32)
                        CN0 = ph.sb("CN0", [128, 32, 8], F32)
                        CN1 = ph.sb("CN1", [128, 32, 8], F32)
                        CN2 = ph.sb("CN2", [128, 32, 8], F32)
                        prk = ph.ps("prk", [128, 32, 8], F32)
                        pcn = ph.ps("pcn", [128, 32, 8], F32)
                        fw.op("dve", lambda e: e.tensor_reduce(out=m12[:, 0, :], in_=LG[:], axis=AX.X, op=ALU.max), reads=[LG], writes=[m12])
                        fw.op("dve", lambda e: e.tensor_tensor(out=SEL1[:], in0=LG[:], in1=bc(m12[:, 0, :].unsqueeze(2), [128, 32, 8]), op=ALU.is_ge), reads=[LG, m12], writes=[SEL1])
                        fw.op("dve", lambda e: e.scalar_tensor_tensor(out=L2[:], in0=SEL1[:], scalar=-1e30, in1=LG[:], op0=ALU.mult, op1=ALU.add), reads=[SEL1, LG], writes=[L2])
                        fw.op("dve", lambda e: e.tensor_reduce(out=m12[:, 1, :], in_=L2[:], axis=AX.X, op=ALU.max), reads=[L2], writes=[m12])
                        fw.op("dve", lambda e: e.tensor_tensor(out=SEL[:], in0=LG[:], in1=bc(m12[:, 1, :].unsqueeze(2), [128, 32, 8]), op=ALU.is_ge), reads=[LG, m12], writes=[SEL])
                        fw.op("dve", lambda e: e.tensor_tensor(out=SEL2[:], in0=SEL[:], in1=SEL1[:], op=ALU.subtract), reads=[SEL, SEL1], writes=[SEL2])
                        fw.op("dve", lambda e: e.tensor_tensor(out=EX[:], in0=LG[:], in1=bc(m12[:, 0, :].unsqueeze(2), [128, 32, 8]), op=ALU.subtract), reads=[LG, m12], writes=[EX])
                        fw.op("act", lambda e: e.activation(out=EX[:], in_=EX[:], func=AF.Exp), reads=[EX], writes=[EX])
                        fw.op("dve", lambda e: e.tensor_tensor(out=EX[:], in0=EX[:], in1=SEL[:], op=ALU.mult), reads=[EX, SEL], writes=[EX])
                        fw.op("dve", lambda e: e.tensor_reduce(out=m12[:, 2, :], in_=EX[:], axis=AX.X, op=ALU.add), reads=[EX], writes=[m12])
                        fw.op("dve", lambda e: e.reciprocal(out=m12[:, 3, :], in_=m12[:, 2, :]), reads=[m12], writes=[m12])
                        fw.op("dve", lambda e: e.tensor_tensor(out=GWS[:], in0=EX[:], in1=bc(m12[:, 3, :].unsqueeze(2), [128, 32, 8]), op=ALU.mult), reads=[EX, m12], writes=[GWS])
                        for s_ in range(32):
                            fw.op("pe", lambda e, s_=s_: e.matmul(prk[:, s_, :], lhsT=Ust[:], rhs=SEL[:, s_, :], start=True, stop=True), reads=[Ust, SEL], writes=[prk])
                            fw.op("pe", lambda e, s_=s_: e.matmul(pcn[:, s_, :], lhsT=ones_f[:], rhs=SEL[:, s_, :], start=True, stop=True), reads=[ones_f, SEL], writes=[pcn])
                        fw.op("dve", lambda e: e.tensor_copy(out=RANK[:], in_=prk[:]), reads=[prk], writes=[RANK])
                        fw.op("dve", lambda e: e.tensor_copy(out=CN0[:], in_=pcn[:]), reads=[pcn], writes=[CN0])
                        srcT, dstT = CN0, CN1
                        sh = 1
                        while sh < 32:
                            fw.op("dve", lambda e, srcT=srcT, dstT=dstT, sh=sh: e.tensor_tensor(out=dstT[:, sh:32, :], in0=srcT[:, sh:32, :], in1=srcT[:, 0:32 - sh, :], op=ALU.add), reads=[srcT], writes=[dstT])
                            fw.op("dve", lambda e, srcT=srcT, dstT=dstT, sh=sh: e.tensor_copy(out=dstT[:, 0:sh, :], in_=srcT[:, 0:sh, :]), reads=[srcT], writes=[dstT])
                            srcT = dstT
                            dstT = CN2 if dstT is CN1 else CN1
                            sh *= 2
                        fw.op("dve", lambda e: e.tensor_tensor(out=BASE[:], in0=srcT[:], in1=CN0[:], op=ALU.subtract), reads=[srcT, CN0], writes=[BASE])
                        fw.op("dve", lambda e: e.tensor_copy(out=run[:], in_=srcT[:, 31, :]), reads=[srcT], writes=[run])
                        w8 = ph.sb("w8", [128, 8, 8], F32)
                        w24 = ph.sb("w24", [128, NTL, 8], F32)
                        v8 = ph.sb("v8", [128, 6, 8], F32)
                        bef = ph.sb("bef", [128, NTL], F32)
                        SLF = ph.sb("SLF", [128, 32, 8], F32)
                        w32 = ph.sb("w32", [128, 32, 8], F32)
                        s2f = ph.sb("s2f", [128, 2, 32], F32)
                        fw.op("dve", lambda e: e.tensor_tensor(out=w8[:], in0=bc(run[:].unsqueeze(2), [128, 8, 8]), in1=bc(grid[:, 0:8].unsqueeze(1), [128, 8, 8]), op=ALU.is_gt),
                              reads=[run, grid], writes=[w8])
                        fw.op("dve", lambda e: e.tensor_reduce(out=v8[:, 0, :], in_=w8[:], axis=AX.X, op=ALU.add), reads=[w8], writes=[v8])
                        fw.op("dve", lambda e: e.tensor_scalar(out=v8[:, 0, :], in0=v8[:, 0, :], scalar1=512.0, scalar2=None, op0=ALU.mult), reads=[v8], writes=[v8])
                        srcI, dstI = 0, 1
                        for sh in (1, 2, 4):
                            fw.op("dve", lambda e, srcI=srcI, dstI=dstI, sh=sh: e.tensor_tensor(out=v8[:, dstI, sh:8], in0=v8[:, srcI, sh:8], in1=v8[:, srcI, 0:8 - sh], op=ALU.add), reads=[v8], writes=[v8])
                            fw.op("dve", lambda e, srcI=srcI, dstI=dstI, sh=sh: e.tensor_copy(out=v8[:, dstI, 0:sh], in_=v8[:, srcI, 0:sh]), reads=[v8], writes=[v8])
                            srcI = dstI
                            dstI = 2 if dstI == 1 else 1
                        PE_ = srcI
                        fw.op("dve", lambda e: e.tensor_tensor(out=v8[:, 4, :], in0=v8[:, PE_, :], in1=v8[:, 0, :], op=ALU.subtract), reads=[v8], writes=[v8])
                        fw.op("dve", lambda e: e.tensor_tensor(out=w24[:], in0=bc(v8[:, PE_:PE_ + 1, :], [128, NTL, 8]), in1=bc(grid[:].unsqueeze(2), [128, NTL, 8]), op=ALU.is_le),
                              reads=[v8, grid], writes=[w24])
                        fw.op("dve", lambda e: e.tensor_reduce(out=bef[:], in_=w24[:], axis=AX.X, op=ALU.add), reads=[w24], writes=[bef])
                        fw.op("dve", lambda e: e.tensor_scalar(out=bef[:], in0=bef[:], scalar1=7.0, scalar2=None, op0=ALU.min), reads=[bef], writes=[bef])
                        fw.op("dve", lambda e: e.tensor_copy(out=BE_i[:], in_=bef[:]), reads=[bef], writes=[BE_i])
                        posw = ph.sb("posw", [128, 2], F32)
                        idf = ph.sb("idf", [128, NTL, 8], F32)
                        fw.dma("sp", posw[:], pos_d, writes=[posw])
                        fw.op("dve", lambda e: e.tensor_scalar(out=posw[:, 1:2], in0=posw[:, 0:1], scalar1=-1.0, scalar2=None, op0=ALU.add), reads=[posw], writes=[posw])
                        fw.op("dve", lambda e: e.tensor_scalar(out=bef[:], in0=bef[:], scalar1=896.0, scalar2=posw[:, 1:2], op0=ALU.mult, op1=ALU.add), reads=[bef, posw], writes=[bef])
                        for b_ in range(7):
                            fw.op("dve", lambda e, b_=b_: e.tensor_scalar(out=idf[:, :, b_], in0=bef[:], scalar1=float(128 * b_), scalar2=None, op0=ALU.add), reads=[bef], writes=[idf])
                        fw.op("dve", lambda e: e.tensor_copy(out=IDXW[:, :, 0:7], in_=idf[:, :, 0:7]), reads=[idf], writes=[IDXW])
                        fw.op("dve", lambda e: e.tensor_tensor(out=SLF[:], in0=RANK[:], in1=BASE[:], op=ALU.add), reads=[RANK, BASE], writes=[SLF])
                        fw.op("dve", lambda e: e.tensor_tensor(out=SLF[:], in0=SLF[:], in1=bc(v8[:, 4:5, :], [128, 32, 8]), op=ALU.add), reads=[SLF, v8], writes=[SLF])
                        for k, SELk in enumerate((SEL1, SEL2)):
                            fw.op("dve", lambda e, SELk=SELk: e.tensor_tensor(out=w32[:], in0=SLF[:], in1=SELk[:], op=ALU.mult), reads=[SLF, SELk], writes=[w32])
                            fw.op("dve", lambda e, k=k: e.tensor_reduce(out=s2f[:, k, :], in_=w32[:], axis=AX.X, op=ALU.add), reads=[w32], writes=[s2f])
                            fw.op("dve", lambda e, SELk=SELk: e.tensor_tensor(out=w32[:], in0=GWS[:], in1=SELk[:], op=ALU.mult), reads=[GWS, SELk], writes=[w32])
                            fw.op("dve", lambda e, k=k: e.tensor_reduce(out=GWK[:, k, :], in_=w32[:], axis=AX.X, op=ALU.add), reads=[w32], writes=[GWK])
                        fw.op("dve", lambda e: e.tensor_scalar(out=s2f[:], in0=s2f[:], scalar1=float(NSLOT - 1), scalar2=0.0, op0=ALU.min, op1=ALU.max), reads=[s2f], writes=[s2f])
                        fw.op("dve", lambda e: e.tensor_copy(out=SLOT_i[:], in_=s2f[:]), reads=[s2f], writes=[SLOT_i])
                        for s_ in range(32):
                            for k in range(2):
                                fw.idma(XS[:, :], ATS[:, s_, :], SLOT_i[:, k, s_:s_ + 1], True, NSLOT - 1, reads=[ATS, SLOT_i], writes=[XSres], holder=ATS, kind="o", merge=True)

                    with Phase(nc, fw, f"MC{l}") as ph:
                        xsr = ph.ring("xs", 2, [128, 4, D], BF16)
                        fTr = ph.ring("fT", 2, [128, 8, 512], BF16)
                        tpb = ph.ring("tpb", 2, [128, 8, 128], BF16, psum=True)
                        acc = ph.sb("acc", [128, 4, D], F32)
                        WGr = ph.ring("WG", 3, [128, 8, 512], BF16)
                        WUr = ph.ring("WU", 3, [128, 8, 512], BF16)
                        WDr = ph.ring("WD", 3, [128, 4, D], BF16)
                        pgu = ph.ring("pgu", 4, [128, 512], F32, psum=True)
                        pyr = ph.ring("py", 2, [128, 512], F32, psum=True)
                        sgr = ph.ring("sg", 2, [128, 512], F32)
                        gTr = ph.ring("gT", 2, [128, 4, 512], BF16)

                        def mprologue(j):
                            xs = xsr.next()
                            fw.dma("sp", xs[:], XS[j * 512:(j + 1) * 512, :].rearrange("(s p) d -> p s d", p=128), reads=[XSres], writes=[xs])
                            fT = fTr.next()
                            for sub in range(4):
                                tp = tpb.next()
                                for kc in range(8):
                                    fw.op("pe", lambda e, kc=kc, sub=sub: e.transpose(out=tp[:, kc, :], in_=xs[:, sub, kc * 128:(kc + 1) * 128], identity=ident_b[:]),
                                          reads=[xs, ident_b], writes=[tp])
                                eng = "act" if sub % 2 == 0 else "dve"
                                if eng == "act":
                                    fw.op("act", lambda e, sub=sub, tp=tp: e.activation(out=fT[:, :, sub * 128:(sub + 1) * 128], in_=tp[:], func=AF.Copy), reads=[tp], writes=[fT])
                                else:
                                    fw.op("dve", lambda e, sub=sub, tp=tp: e.tensor_copy(out=fT[:, :, sub * 128:(sub + 1) * 128], in_=tp[:]), reads=[tp], writes=[fT])
                            return fT, None

                        cfgm = FF_CFG[1]
                        nxt_pro = mprologue(0)
                        for j in range(NTL):
                            fT, ev = nxt_pro
                            T = 512

                            def gate_up(b):
                                WG, WU, WDt = WGr.next(), WUr.next(), WDr.next()
                                for (Wt_, key_) in ((WG, "WG"), (WU, "WU"), (WDt, "WD")):
                                    fw.idma(Wt_[:].rearrange("p k n -> p (k n)"), cfgm[key_].rearrange("e b p k n -> (e b p) (k n)"), IDXW[:, j, b:b + 1], False, None,
                                            reads=[cfgm["res"], IDXW], writes=[Wt_], holder=Wt_, kind="i")
                                gT = gTr.next()
                                for jj in range(4):
                                    pg_, pu_ = pgu.next(), pgu.next()
                                    for (pp, W_) in ((pg_, WG), (pu_, WU)):
                                        for kc in range(8):
                                            fw.op("pe", lambda e, kc=kc, jj=jj, pp=pp, W_=W_: e.matmul(pp[:, 0:T], lhsT=W_[:, kc, jj * 128:(jj + 1) * 128], rhs=fT[:, kc, 0:T],
                                                                                                     start=(kc == 0), stop=(kc == 7)), reads=[W_, fT], writes=[pp])
                                    sg = sgr.next()
                                    fw.op("act", lambda e, pg_=pg_, sg=sg: e.activation(out=sg[:, 0:T], in_=pg_[:, 0:T], func=AF.Silu), reads=[pg_], writes=[sg])
                                    fw.op("dve", lambda e, pu_=pu_, sg=sg, jj=jj: e.tensor_tensor(out=gT[:, jj, 0:T], in0=pu_[:, 0:T], in1=sg[:, 0:T], op=ALU.mult), reads=[pu_, sg], writes=[gT])
                                return gT, WDt

                            def down(gT, WDt, first):
                                for sub in range(4):
                                    for n in range(2):
                                        py = pyr.next()
                                        for jj in range(4):
                                            fw.op("pe", lambda e, jj=jj, sub=sub, n=n, py=py: e.matmul(py[:], lhsT=gT[:, jj, sub * 128:(sub + 1) * 128], rhs=WDt[:, jj, n * 512:(n + 1) * 512],
                                                                                                     start=(jj == 0), stop=(jj == 3)), reads=[gT, WDt], writes=[py])
                                        av = acc[:, sub, n * 512:(n + 1) * 512]
                                        if first:
                                            fw.op("dve", lambda e, py=py, av=av: e.tensor_copy(out=av, in_=py[:]), reads=[py], writes=[acc])
                                        else:
                                            fw.op("dve", lambda e, py=py, av=av: e.tensor_tensor(out=av, in0=py[:], in1=av, op=ALU.add), reads=[py, acc], writes=[acc])

                            prev = None
                            for b in range(7):
                                gT, WDt = gate_up(b)
                                if prev is not None:
                                    down(prev[0], prev[1], prev[2])
                                prev = (gT, WDt, b == 0)
                                if b == 2 and j + 1 < NTL:
                                    nxt_pro = mprologue(j + 1)
                            down(prev[0], prev[1], prev[2])
                            fw.dma("pool", YS[j * 512:(j + 1) * 512, :].rearrange("(s p) d -> p s d", p=128), acc[:], reads=[acc], writes=[YSres], holder=acc, kind="o", merge=True)

                    with Phase(nc, fw, f"MO{l}") as ph:
                        y1r = ph.ring("y1", 4, [128, D], F32)
                        y2r = ph.ring("y2", 4, [128, D], F32)
                        hcr = ph.ring("hc", 4, [128, D], F32)
                        loaded = {}

                        def mo_load(s_):
                            c = 2 + s_
                            y1, y2, hc = y1r.next(), y2r.next(), hcr.next()
                            fw.dma("sp", hc[:], H[c * 128:(c + 1) * 128, :], reads=[Hres[c]], writes=[hc])
                            fw.idma(y1[:, :], YS[:, :], SLOT_i[:, 0, s_:s_ + 1], False, NSLOT - 1, reads=[YSres, SLOT_i], writes=[y1], holder=y1, kind="i")
                            fw.idma(y2[:, :], YS[:, :], SLOT_i[:, 1, s_:s_ + 1], False, NSLOT - 1, reads=[YSres, SLOT_i], writes=[y2], holder=y2, kind="i")
                            loaded[s_] = (y1, y2, hc)

                        def mo_comp(s_):
                            y1, y2, hc = loaded.pop(s_)
                            fw.op("dve", lambda e: e.tensor_scalar(out=y1[:], in0=y1[:], scalar1=GWK[:, 0, s_:s_ + 1], scalar2=None, op0=ALU.mult), reads=[y1, GWK], writes=[y1])
                            fw.op("dve", lambda e: e.scalar_tensor_tensor(out=y1[:], in0=y2[:], scalar=GWK[:, 1, s_:s_ + 1], in1=y1[:], op0=ALU.mult, op1=ALU.add),
                                  reads=[y1, y2, GWK], writes=[y1])
                            fw.op("dve", lambda e: e.tensor_tensor(out=y1[:], in0=y1[:], in1=gbc[:], op=ALU.mult), reads=[y1, gbc], writes=[y1])
                            fw.op("dve", lambda e: e.tensor_tensor(out=y1[:], in0=y1[:], in1=hc[:], op=ALU.add), reads=[y1, hc], writes=[y1])
                            fw.dma("sp", out[s_ * 128:(s_ + 1) * 128, :], y1[:], reads=[y1], writes=[OUTres], holder=y1, kind="o", merge=True)
                        LOOK = 2
                        for s_ in range(32 + LOOK):
                            if s_ < 32:
                                mo_load(s_)
                            if s_ >= LOOK:
                                mo_comp(s_ - LOOK)
                if dbg == f"F{l}":
                    break
                continue
            with Phase(nc, fw, f"F{l}") as ph:
                gbc = [ph.sb(f"gbc{t}", [128, D], F32) for t in range(2)]
                for t in range(2):
                    fw.dma("sp", gbc[t][:], MD[t, 5 * D:6 * D].partition_broadcast(128), reads=[MDres], writes=[gbc[t]])
                if moe:
                    Wr = ph.sb("Wr", [128, 8, 8], F32)
                    rbb = ph.sb("rbb", [128, 8], F32)
                    fw.dma("sp", Wr[:], router_w.rearrange("(kc p) e -> p kc e", p=128), writes=[Wr])
                    fw.dma("sp", rbb[:], router_b[0].partition_broadcast(128), writes=[rbb])
                rings = dict(st=ph.ring("st", 4, [128, 4], F32), xn=ph.ring("xn", 2, [128, D], F32),
                             tp=ph.ring("tp", 2, [128, 4, 128], F32, psum=True))
                hTr = ph.ring("hT", 2, [128, 4, D], F32)
                acc = ph.sb("acc", [128, 4, D], F32)
                fTr = ph.ring("fT", 2, [128, 8, 512], BF16)
                a32 = ph.sb("a32", [128, 8, 128], F32)
                gwr = ph.ring("gw", 2, [128, 4, 8], F32)
                lgt = ph.ring("lgt", 2, [128, 32], F32)
                WGr = ph.ring("WG", 3, [128, 8, cfg["bw"]], BF16)
                WUr = ph.ring("WU", 3, [128, 8, cfg["bw"]], BF16)
                WDr = ph.ring("WD", 3, [128, nffc, D], BF16)
                pgu = ph.ring("pgu", 4, [128, 512], F32, psum=True)
                pyr = ph.ring("py", 2, [128, 512], F32, psum=True)
                sgr = ph.ring("sg", 2, [128, 512], F32)
                gTr = ph.ring("gT", 2, [128, nffc, 512], BF16)
                tiles = ([] if last else [(0, 2)]) + [(2 + 4 * i, 4) for i in range(8)]

                def prologue(c0, nsub):
                    t = 1 if c0 < 2 else 0
                    hT, fT, gw = hTr.next(), fTr.next(), gwr.next()
                    for s in range(nsub):
                        c = c0 + s
                        fw.dma("sp", hT[:, s, :], H[c * 128:(c + 1) * 128, :], reads=[Hres[c]], writes=[hT], merge=(s > 0))
                    for s in range(nsub):
                        st4 = rings["st"].next()
                        xn = rings["xn"].next()
                        fw.op("act", lambda e, s=s: e.activation(out=xn[:], in_=hT[:, s, :], func=AF.Square, accum_out=st4[:, 0:1]), reads=[hT], writes=[xn, st4])
                        fw.op("act", lambda e: e.activation(out=st4[:, 1:2], in_=st4[:, 0:1], func=AF.Sqrt, scale=1.0 / D, bias=cst[:, 0:1]), reads=[st4, cst], writes=[st4])
                        fw.op("dve", lambda e: e.reciprocal(out=st4[:, 2:3], in_=st4[:, 1:2]), reads=[st4], writes=[st4])
                        fw.op("act", lambda e, s=s: e.activation(out=xn[:], in_=hT[:, s, :], func=AF.Copy, scale=st4[:, 2:3]), reads=[hT, st4], writes=[xn])
                        for half in range(2):
                            tp = rings["tp"].next()
                            for j in range(4):
                                kc = half * 4 + j
                                fw.op("pe", lambda e, kc=kc, j=j: e.transpose(out=tp[:, j, :], in_=xn[:, kc * 128:(kc + 1) * 128], identity=ident_f[:]),
                                      reads=[xn, ident_f], writes=[tp])
                            for j in range(4):
                                kc = half * 4 + j
                                fw.op("dve", lambda e, kc=kc, j=j, s=s: e.tensor_scalar(out=fT[:, kc, s * 128:(s + 1) * 128], in0=tp[:, j, :], scalar1=g2T[:, kc, t:t + 1],
                                                                                       scalar2=modT[:, 24 + kc, t:t + 1], op0=ALU.mult, op1=ALU.add),
                                      reads=[tp, g2T, modT], writes=[fT])
                                if moe:
                                    fw.op("dve", lambda e, kc=kc, j=j: e.tensor_scalar(out=a32[:, kc, :], in0=tp[:, j, :], scalar1=g2T[:, kc, t:t + 1],
                                                                                      scalar2=modT[:, 24 + kc, t:t + 1], op0=ALU.mult, op1=ALU.add),
                                          reads=[tp, g2T, modT], writes=[a32])
                        if moe:
                            pl = rings["tp"].next()
                            plv = pl[:].rearrange("p a b -> p (a b)")
                            for kc in range(8):
                                fw.op("pe", lambda e, kc=kc: e.matmul(plv[:, 0:8], lhsT=a32[:, kc, :], rhs=Wr[:, kc, :], start=(kc == 0), stop=(kc == 7)),
                                      reads=[a32, Wr], writes=[pl])
                            lg_ = lgt.next()
                            L = lg_[:, 0:8]
                            fw.op("dve", lambda e: e.tensor_tensor(out=L, in0=plv[:, 0:8], in1=rbb[:], op=ALU.add), reads=[pl, rbb], writes=[lg_])
                            fw.op("dve", lambda e: e.tensor_reduce(out=lg_[:, 24:25], in_=L, axis=AX.X, op=ALU.max), reads=[lg_], writes=[lg_])
                            fw.op("dve", lambda e: e.tensor_scalar(out=lg_[:, 8:16], in0=L, scalar1=lg_[:, 24:25], scalar2=-1e30, op0=ALU.is_ge, op1=ALU.mult), reads=[lg_], writes=[lg_])
                            fw.op("dve", lambda e: e.tensor_tensor(out=lg_[:, 8:16], in0=lg_[:, 8:16], in1=L, op=ALU.add), reads=[lg_], writes=[lg_])
                            fw.op("dve", lambda e: e.tensor_reduce(out=lg_[:, 25:26], in_=lg_[:, 8:16], axis=AX.X, op=ALU.max), reads=[lg_], writes=[lg_])
                            fw.op("dve", lambda e: e.tensor_scalar(out=lg_[:, 8:16], in0=L, scalar1=lg_[:, 25:26], scalar2=None, op0=ALU.is_ge), reads=[lg_], writes=[lg_])
                            fw.op("dve", lambda e: e.tensor_scalar(out=lg_[:, 16:24], in0=L, scalar1=lg_[:, 24:25], scalar2=None, op0=ALU.subtract), reads=[lg_], writes=[lg_])
                            fw.op("act", lambda e: e.activation(out=lg_[:, 16:24], in_=lg_[:, 16:24], func=AF.Exp), reads=[lg_], writes=[lg_])
                            fw.op("dve", lambda e: e.tensor_tensor(out=lg_[:, 16:24], in0=lg_[:, 16:24], in1=lg_[:, 8:16], op=ALU.mult), reads=[lg_], writes=[lg_])
                            fw.op("dve", lambda e: e.tensor_reduce(out=lg_[:, 26:27], in_=lg_[:, 16:24], axis=AX.X, op=ALU.add), reads=[lg_], writes=[lg_])
                            fw.op("dve", lambda e: e.reciprocal(out=lg_[:, 27:28], in_=lg_[:, 26:27]), reads=[lg_], writes=[lg_])
                            fw.op("dve", lambda e, s=s: e.tensor_scalar(out=gw[:, s, :], in0=lg_[:, 16:24], scalar1=lg_[:, 27:28], scalar2=None, op0=ALU.mult), reads=[lg_], writes=[gw])
                    return hT, fT, gw

                blocks = [(ex, b) for ex in range(nexp) for b in range(nblk)]
                nxt_pro = prologue(*tiles[0])
                for ti, (c0, nsub) in enumerate(tiles):
                    t = 1 if c0 < 2 else 0
                    T = nsub * 128
                    hT, fT, gw = nxt_pro

                    def gate_up(ex, b):
                        WG, WU, WDt = WGr.next(), WUr.next(), WDr.next()
                        fw.dma("sp", WG[:], cfg["WG"][ex, b], reads=[cfg["res"]], writes=[WG])
                        fw.dma("sp", WU[:], cfg["WU"][ex, b], reads=[cfg["res"]], writes=[WU])
                        fw.dma("sp", WDt[:], cfg["WD"][ex, b], reads=[cfg["res"]], writes=[WDt])
                        gT = gTr.next()
                        for j in range(nffc):
                            pg_, pu_ = pgu.next(), pgu.next()
                            for (pp, W_) in ((pg_, WG), (pu_, WU)):
                                for kc in range(8):
                                    fw.op("pe", lambda e, kc=kc, j=j, pp=pp, W_=W_: e.matmul(pp[:, 0:T], lhsT=W_[:, kc, j * 128:(j + 1) * 128], rhs=fT[:, kc, 0:T],
                                                                                           start=(kc == 0), stop=(kc == 7)), reads=[W_, fT], writes=[pp])
                            sg = sgr.next()
                            fw.op("act", lambda e, pg_=pg_, sg=sg: e.activation(out=sg[:, 0:T], in_=pg_[:, 0:T], func=AF.Silu), reads=[pg_], writes=[sg])
                            fw.op("dve", lambda e, pu_=pu_, sg=sg, j=j: e.tensor_tensor(out=gT[:, j, 0:T], in0=pu_[:, 0:T], in1=sg[:, 0:T], op=ALU.mult), reads=[pu_, sg], writes=[gT])
                        return gT, WDt

                    def down(ex, gT, WDt, first):
                        for s in range(nsub):
                            for n in range(2):
                                py = pyr.next()
                                for j in range(nffc):
                                    fw.op("pe", lambda e, j=j, s=s, n=n, py=py: e.matmul(py[:], lhsT=gT[:, j, s * 128:(s + 1) * 128], rhs=WDt[:, j, n * 512:(n + 1) * 512],
                                                                                       start=(j == 0), stop=(j == nffc - 1)), reads=[gT, WDt], writes=[py])
                                av = acc[:, s, n * 512:(n + 1) * 512]
                                if moe:
                                    if first:
                                        fw.op("dve", lambda e, py=py, av=av, s=s: e.tensor_scalar(out=av, in0=py[:], scalar1=gw[:, s, ex:ex + 1], scalar2=None, op0=ALU.mult),
                                              reads=[py, gw], writes=[acc])
                                    else:
                                        fw.op("dve", lambda e, py=py, av=av, s=s: e.scalar_tensor_tensor(out=av, in0=py[:], scalar=gw[:, s, ex:ex + 1], in1=av,
                                                                                                       op0=ALU.mult, op1=ALU.add), reads=[py, gw, acc], writes=[acc])
                                else:
                                    if first:
                                        fw.op("dve", lambda e, py=py, av=av: e.tensor_copy(out=av, in_=py[:]), reads=[py], writes=[acc])
                                    else:
                                        fw.op("dve", lambda e, py=py, av=av: e.tensor_tensor(out=av, in0=py[:], in1=av, op=ALU.add), reads=[py, acc], writes=[acc])

                    prev = None
                    for bi, (ex, b) in enumerate(blocks):
                        gT, WDt = gate_up(ex, b)
                        if prev is not None:
                            down(prev[0], prev[1], prev[2], prev[3])
                        prev = (ex, gT, WDt, bi == 0)
                        if bi == min(2, len(blocks) - 1) and ti + 1 < len(tiles):
                            nxt_pro = prologue(*tiles[ti + 1])
                    down(prev[0], prev[1], prev[2], prev[3])
                    for s in range(nsub):
                        c = c0 + s
                        fw.op("pool", lambda e, s=s: e.tensor_tensor(out=acc[:, s, :], in0=acc[:, s, :], in1=gbc[t][:], op=ALU.mult), reads=[acc, gbc[t]], writes=[acc])
                        fw.op("pool", lambda e, s=s: e.tensor_tensor(out=acc[:, s, :], in0=acc[:, s, :], in1=hT[:, s, :], op=ALU.add), reads=[acc, hT], writes=[acc])
                        if last:
                            fw.dma("pool", out[(c - 2) * 128:(c - 1) * 128, :], acc[:, s, :], reads=[acc], writes=[OUTres], holder=acc, kind="o", merge=True)
                        else:
                            fw.dma("pool", H[c * 128:(c + 1) * 128, :], acc[:, s, :], reads=[acc], writes=[Hres[c]], holder=acc, kind="o")
            if dbg == f"F{l}":
                break
        for cfg in FF_CFG:
            r_ = cfg["res"]
            if r_.isem is not None and r_.icnt:
                for k_ in fw.engs:
                    fw._wait(k_, [(r_.isem, r_.icnt)])
        fw.barrier(release=False)
    return nc


_PERM = None


def _w_in_perm():
    q = []
    for i in range(4):
        q += list(range(i * 64, (i + 1) * 64)) + list(range((4 + i) * 64, (5 + i) * 64))
    ak, av, rq, rk, rv, rg, mq, mk, mv, mo, mg = 512, 640, 768, 1024, 1280, 1536, 1792, 2048, 2304, 2560, 2816
    r = lambda a, n: list(range(a, a + n))
    perm = q + r(ak, 128) + r(av, 128) + r(rv, 256) + r(rq, 256) + r(rk, 256) + r(rg, 256) + r(mv, 256) + r(mo, 256) + r(mq, 256) + r(mk, 256) + r(mg, 16)
    assert len(perm) == NIN and len(set(perm)) == NIN
    return np.array(perm)


def _consts():
    ident = np.eye(128, dtype=np.float32)
    s = np.arange(128)[:, None]
    l_ = np.arange(128)[None, :]
    maskF = (s <= l_).astype(np.float32)
    maskB = (s >= l_).astype(np.float32)
    n_freq = 16
    inv_freq = (10000.0 ** (-np.arange(n_freq, dtype=np.float32) / n_freq)).astype(np.float32)
    tok = np.arange(4096)
    row = (tok // 64).astype(np.float32)
    col = (tok % 64).astype(np.float32)
    ang = np.concatenate([row[:, None] * inv_freq, col[:, None] * inv_freq], -1).astype(np.float32)
    cos = np.cos(ang).astype(np.float32).reshape(32, 128, 32).transpose(1, 0, 2)
    sin = np.sin(ang).astype(np.float32).reshape(32, 128, 32).transpose(1, 0, 2)
    pos = np.stack([np.arange(128) + 1.0, 128.0 - np.arange(128)], 1).astype(np.float32)
    grid = np.broadcast_to((np.arange(24, dtype=np.float32) * 512.0)[None, :], (128, 24)).copy()
    return dict(grid512=grid, ident=ident, maskF=maskF, maskB=maskB, cos=np.ascontiguousarray(cos), sin=np.ascontiguousarray(sin), pos=pos)


def make_in_maps(x, c, ctx, c_ctx, mod_w, mod_b, norm1_w, norm2_w, w_in, attn_qn_w, attn_kn_w, ret_decay,
                 ret_norm_w, mlstm_conv_w, mlstm_gate_b, mlstm_norm_w, w_out, ffn_w_gate, ffn_w_up, ffn_w_down,
                 router_w, router_b, moe_w_gate, moe_w_up, moe_w_down, moe_nexp=8):
    f = lambda a: np.ascontiguousarray(np.asarray(a, dtype=np.float32))
    perm = _w_in_perm()
    shared = dict(
        mod_w=f(mod_w),
        modbT=f(np.asarray(mod_b).reshape(DEPTH, 48, 128).transpose(0, 2, 1)),
        n1T=f(np.asarray(norm1_w).reshape(DEPTH, 8, 128).transpose(0, 2, 1)),
        n2T=f(np.asarray(norm2_w).reshape(DEPTH, 8, 128).transpose(0, 2, 1)),
        n2row=f(norm2_w),
        w_in=f(np.asarray(w_in)[:, :, perm]),
        qn_w=f(attn_qn_w), kn_w=f(attn_kn_w),
        ret_decay=f(np.asarray(ret_decay).reshape(DEPTH, 8)),
        ret_norm_w=f(ret_norm_w), conv_w=f(mlstm_conv_w),
        gate_b=f(np.asarray(mlstm_gate_b).reshape(DEPTH, 16)),
        mlstm_norm_w=f(mlstm_norm_w), w_out=f(w_out),
        ffn_wg=f(ffn_w_gate), ffn_wu=f(ffn_w_up), ffn_wd=f(ffn_w_down),
        router_w=f(np.asarray(router_w)[0]), router_b=f(np.asarray(router_b)[0:1]),
        moe_wg=f(np.asarray(moe_w_gate)[0][:moe_nexp]), moe_wu=f(np.asarray(moe_w_up)[0][:moe_nexp]), moe_wd=f(np.asarray(moe_w_down)[0][:moe_nexp]),
    )
    shared.update(_consts())
    x = np.asarray(x); ctx = np.asarray(ctx); c = np.asarray(c); c_ctx = np.asarray(c_ctx)
    maps = []
    for b in range(8):
        m = dict(shared)
        m["xin"] = f(np.concatenate([ctx[b], x[b]], axis=0))
        m["cT"] = f(np.stack([c[b].reshape(8, 128).T, c_ctx.reshape(8, 128).T], axis=-1))
        maps.append(m)
    return maps


def kernel(**inputs):
    nc = build_program()
    maps = make_in_maps(**inputs)
    res = run_bass_kernel_spmd(nc, maps, core_ids=list(range(8)))
    return np.stack([np.asarray(r["out"], dtype=np.float32) for r in res.results], axis=0)
```

```python
import contextlib
import numpy as np
import concourse.bass as bass
import concourse.mybir as mybir
from concourse.bass_utils import run_bass_kernel_spmd

AF = mybir.ActivationFunctionType
ALU = mybir.AluOpType
AX = mybir.AxisListType
F32 = mybir.dt.float32
BF16 = mybir.dt.bfloat16

NCH = 34
NTOK = NCH * 128
D = 1024
NIN = 2832
EPS = 1e-6
DEPTH = 2
DENSE_MOE = False
FWD_ORDER = list(range(NCH))
BWD_ORDER = [1, 0] + list(range(NCH - 1, 1, -1))
PQ, PK, PV, PRV, PRQ, PRK, PRG, PMV, PMO, PMQ, PMK = 0, 512, 640, 768, 1024, 1280, 1536, 1792, 2048, 2304, 2560
PCOLS = 2816


class Res:
    __slots__ = ("name", "w", "r", "isem", "osem", "icnt", "ocnt", "persist")

    def __init__(self, name):
        self.name = name
        self.w = None
        self.r = {}
        self.isem = None
        self.osem = None
        self.icnt = 0
        self.ocnt = 0
        self.persist = False


class Tile:
    def __init__(self, h, name):
        self.h = h
        self.r = Res(name)

    def __getitem__(self, k):
        return self.h[k]


class FW:
    ROT = 30000

    def __init__(self, nc):
        self.nc = nc
        self.engs = {"pe": nc.tensor, "act": nc.scalar, "dve": nc.vector,
                     "pool": nc.gpsimd, "sp": nc.sync}
        self.csem = {}
        self.ccnt = {}
        self.known = {k: {} for k in self.engs}
        self.nsem = 0
        self.allsems = []
        self.sempool = []
        self.sempool_sw = []
        self.semq = {}
        self.dma_live = []
        self.old_counters = []
        for k in self.engs:
            self._newc(k)

    def sem(self, name):
        self.nsem += 1
        s = self.nc.alloc_semaphore(name=f"{name}_{self.nsem}")
        self.allsems.append(s)
        return s

    def _newc(self, k):
        if k in self.csem and self.ccnt[k] > 0:
            self.old_counters.append((self.csem[k], self.ccnt[k]))
        self.csem[k] = self.sem("c" + k)
        self.ccnt[k] = 0

    def _wait(self, eng, evs, noself=False):
        need = {}
        kn = self.known[eng]
        for ev in evs:
            if ev is None:
                continue
            s, v = ev
            if noself and s is self.csem[eng]:
                continue
            sid = id(s)
            if kn.get(sid, 0) >= v:
                continue
            if sid not in need or need[sid][1] < v:
                need[sid] = (s, v)
        for sid, (s, v) in need.items():
            self.engs[eng].wait_ge(s, v)
            kn[sid] = v

    @staticmethod
    def _deps(reads, writes, merge=False):
        evs = []
        for r in reads:
            evs.append(r.w)
        for w in writes:
            if not merge:
                evs.append(w.w)
            evs.extend(w.r.values())
        return evs

    @staticmethod
    def _commit(ev, reads, writes, merge=False):
        sid = id(ev[0])
        for r in reads:
            r.r[sid] = ev
        for w in writes:
            w.w = ev
            if not merge:
                w.r = {}

    def op(self, eng, fn, reads=(), writes=(), noself=None):
        reads = [x.r if isinstance(x, Tile) else x for x in reads]
        writes = [x.r if isinstance(x, Tile) else x for x in writes]
        if noself is None:
            noself = (eng == "pe")
        self._wait(eng, self._deps(reads, writes), noself=noself)
        ins = fn(self.engs[eng])
        self.ccnt[eng] += 1
        ins.then_inc(self.csem[eng], 1)
        ev = (self.csem[eng], self.ccnt[eng])
        self._commit(ev, reads, writes)
        if self.ccnt[eng] >= self.ROT:
            self._newc(eng)
        return ev

    def _getsem(self, q):
        pool_ = self.sempool_sw if q == "pool" else self.sempool
        if pool_:
            return pool_.pop()
        return (self.sem("dsw" if q == "pool" else "d"), 0)

    def dma(self, q, out, in_, reads=(), writes=(), holder=None, kind=None, merge=False, **kw):
        reads = [x.r if isinstance(x, Tile) else x for x in reads]
        writes = [x.r if isinstance(x, Tile) else x for x in writes]
        self._wait(q, self._deps(reads, writes, merge=merge))
        ins = self.engs[q].dma_start(out=out, in_=in_, **kw)
        if holder is None:
            holder, kind = (reads[0], "o") if kind == "o" else (writes[0], "i")
        elif isinstance(holder, Tile):
            holder = holder.r
        if kind == "i":
            if holder.isem is None:
                holder.isem, holder.icnt = self._getsem(q)
                self.semq[id(holder.isem)] = q == "pool"
                if not holder.persist:
                    self.dma_live.append(holder)
            assert self.semq[id(holder.isem)] == (q == "pool"), holder.name
            holder.icnt += 16
            ins.then_inc(holder.isem, 16)
            ev = (holder.isem, holder.icnt)
        else:
            if holder.osem is None:
                holder.osem, holder.ocnt = self._getsem(q)
                self.semq[id(holder.osem)] = q == "pool"
                self.dma_live.append(holder)
            assert self.semq[id(holder.osem)] == (q == "pool"), holder.name
            holder.ocnt += 16
            ins.then_inc(holder.osem, 16)
            ev = (holder.osem, holder.ocnt)
        self._commit(ev, reads, writes, merge=merge)
        return ev

    def idma(self, out, in_, idx_ap, scatter, bound, reads=(), writes=(), holder=None, kind="i", merge=False):
        q = "pool"
        reads = [x.r if isinstance(x, Tile) else x for x in reads]
        writes = [x.r if isinstance(x, Tile) else x for x in writes]
        self._wait(q, self._deps(reads, writes, merge=merge))
        off = bass.IndirectOffsetOnAxis(ap=idx_ap, axis=0)
        ins = self.nc.gpsimd.indirect_dma_start(out=out, out_offset=(off if scatter else None), in_=in_, in_offset=(None if scatter else off))
        if isinstance(holder, Tile):
            holder = holder.r
        if kind == "i":
            if holder.isem is None:
                holder.isem, holder.icnt = self._getsem(q)
                self.semq[id(holder.isem)] = True
                if not holder.persist:
                    self.dma_live.append(holder)
            holder.icnt += 16
            ins.then_inc(holder.isem, 16)
            ev = (holder.isem, holder.icnt)
        else:
            if holder.osem is None:
                holder.osem, holder.ocnt = self._getsem(q)
                self.semq[id(holder.osem)] = True
                self.dma_live.append(holder)
            holder.ocnt += 16
            ins.then_inc(holder.osem, 16)
            ev = (holder.osem, holder.ocnt)
        self._commit(ev, reads, writes, merge=merge)
        return ev

    def barrier(self, release=True):
        evs = [(self.csem[k], self.ccnt[k]) for k in self.engs if self.ccnt[k] > 0]
        evs += self.old_counters
        for h in self.dma_live:
            if h.isem is not None:
                evs.append((h.isem, h.icnt))
            if h.osem is not None:
                evs.append((h.osem, h.ocnt))
        for k in self.engs:
            self._wait(k, evs, noself=True)
        if release:
            for h in self.dma_live:
                if h.isem is not None:
                    (self.sempool_sw if self.semq[id(h.isem)] else self.sempool).append((h.isem, h.icnt))
                    h.isem = None
                if h.osem is not None:
                    (self.sempool_sw if self.semq[id(h.osem)] else self.sempool).append((h.osem, h.ocnt))
                    h.osem = None
            self.dma_live = []


class Ring:
    def __init__(self, tiles):
        self.tiles = tiles
        self.i = 0

    def next(self):
        t = self.tiles[self.i % len(self.tiles)]
        self.i += 1
        return t


class Phase:
    cnt = 0

    def __init__(self, nc, fw, name):
        self.nc, self.fw, self.name = nc, fw, name

    def __enter__(self):
        self.st = contextlib.ExitStack()
        return self

    def __exit__(self, *a):
        self.fw.barrier()
        self.st.close()
        return False

    def sb(self, name, shape, dt):
        Phase.cnt += 1
        nm = f"{self.name}_{name}_{Phase.cnt}"
        return Tile(self.st.enter_context(self.nc.sbuf_tensor(nm, shape, dt)), nm)

    def ps(self, name, shape, dt=F32):
        Phase.cnt += 1
        nm = f"{self.name}_{name}_{Phase.cnt}"
        return Tile(self.st.enter_context(self.nc.psum_tensor(nm, shape, dt)), nm)

    def ring(self, name, n, shape, dt, psum=False):
        f = self.ps if psum else self.sb
        return Ring([f(f"{name}{i}", shape, dt) for i in range(n)])


def bc(ap, shape):
    return ap.to_broadcast(list(shape))


def build_program(dbg=None, force_ne8=False):
    nc = bass.Bass("TRN2", target_bir_lowering=False)
    NE = 1 if (dbg and not dbg.endswith("1") and not force_ne8) else 8
    fw = FW(nc)

    def din(name, shape, dt=F32):
        return nc.dram_tensor(name, list(shape), dt, kind="ExternalInput").ap()

    def dscr(name, shape, dt=F32):
        return nc.dram_tensor(name, list(shape), dt, kind=("ExternalOutput" if (dbg and not name.startswith("W")) else "Internal")).ap()

    xin = din("xin", [NTOK, D])
    cT = din("cT", [128, 8, 2])
    mod_w = din("mod_w", [DEPTH, D, 6 * D])
    modbT = din("modbT", [DEPTH, 128, 48])
    n1T = din("n1T", [DEPTH, 128, 8])
    n2T = din("n2T", [DEPTH, 128, 8])
    w_in = din("w_in", [DEPTH, D, NIN])
    qn_w = din("qn_w", [DEPTH, 64])
    kn_w = din("kn_w", [DEPTH, 64])
    ret_decay = din("ret_decay", [DEPTH, 8])
    ret_norm_w = din("ret_norm_w", [DEPTH, 256])
    conv_w = din("conv_w", [DEPTH, 3, 512])
    gate_b = din("gate_b", [DEPTH, 16])
    mlstm_norm_w = din("mlstm_norm_w", [DEPTH, 256])
    w_out = din("w_out", [DEPTH, D, D])
    ffn_wg = din("ffn_wg", [1, D, 2816])
    ffn_wu = din("ffn_wu", [1, D, 2816])
    ffn_wd = din("ffn_wd", [1, 2816, D])
    router_w = din("router_w", [D, 8])
    router_b = din("router_b", [1, 8])
    moe_wg = din("moe_wg", [NE, D, 3584])
    moe_wu = din("moe_wu", [NE, D, 3584])
    moe_wd = din("moe_wd", [NE, 3584, D])
    ident_d = din("ident", [128, 128])
    maskF_d = din("maskF", [128, 128])
    maskB_d = din("maskB", [128, 128])
    cos_d = din("cos", [128, 32, 32])
    sin_d = din("sin", [128, 32, 32])
    pos_d = din("pos", [128, 2])
    n2row = din("n2row", [DEPTH, D])
    grid_d = din("grid512", [128, 24])
    out = nc.dram_tensor("out", [4096, D], F32, kind="ExternalOutput").ap()

    H = dscr("H", [NTOK, D])
    P = dscr("P", [NTOK, PCOLS], BF16)
    GT = dscr("GT", [16, NTOK])
    MIX = dscr("MIX", [NTOK, D], BF16)
    MD = dscr("MD", [2, 6 * D])
    NSLOT = 12288
    NTL = NSLOT // 512
    XS = dscr("XS", [NSLOT, D], BF16)
    YS = dscr("YS", [NSLOT, D], F32)
    XSres = Res("XS")
    YSres = Res("YS")
    I32 = mybir.dt.int32
    FF_CFG = [dict(nexp=1, nblk=11, nffc=2, wg=ffn_wg, wu=ffn_wu, wd=ffn_wd),
              dict(nexp=NE, nblk=7, nffc=4, wg=moe_wg, wu=moe_wu, wd=moe_wd)]
    for i, cfg in enumerate(FF_CFG):
        bw = cfg["nffc"] * 128
        cfg["bw"] = bw
        cfg["WG"] = dscr(f"WG{i}", [cfg["nexp"], cfg["nblk"], 128, 8, bw], BF16)
        cfg["WU"] = dscr(f"WU{i}", [cfg["nexp"], cfg["nblk"], 128, 8, bw], BF16)
        cfg["WD"] = dscr(f"WD{i}", [cfg["nexp"], cfg["nblk"], 128, cfg["nffc"], D], BF16)
        cfg["res"] = Res(f"wconv{i}")

    Hres = [Res(f"H{c}") for c in range(NCH)]
    Pres = [Res(f"P{c}") for c in range(NCH)]
    Pcv = [Res(f"Pcv{c}") for c in range(NCH)]
    GTres = [Res(f"GT{c}") for c in range(NCH)]
    MIXres = [Res(f"MIX{c}") for c in range(NCH)]
    MDres = Res("MD")
    OUTres = Res("OUT")

    gst = contextlib.ExitStack()
    with gst:
        G = Phase(nc, fw, "G")
        G.st = gst
        ident_f = G.sb("identf", [128, 128], F32)
        ident_b = G.sb("identb", [128, 128], BF16)
        maskF = G.sb("maskF", [128, 128], F32)
        maskB = G.sb("maskB", [128, 128], F32)
        cst = G.sb("cst", [128, 4], F32)
        modT = G.sb("modT", [128, 48, 2], F32)
        g1T = G.sb("g1T", [128, 8, 2], F32)
        g2T = G.sb("g2T", [128, 8, 2], F32)
        ones_f = G.sb("onesf", [128, 128], F32)

        fw.dma("sp", ident_f[:], ident_d, writes=[ident_f])
        fw.dma("sp", maskF[:], maskF_d, writes=[maskF])
        fw.dma("sp", maskB[:], maskB_d, writes=[maskB])
        fw.op("dve", lambda e: e.tensor_copy(out=ident_b[:], in_=ident_f[:]), reads=[ident_f], writes=[ident_b])
        fw.op("dve", lambda e: e.memset(cst[:, 0:1], EPS), writes=[cst])
        fw.op("dve", lambda e: e.memset(cst[:, 1:2], 1.0), writes=[cst])
        fw.op("dve", lambda e: e.memset(cst[:, 2:3], 0.0), writes=[cst])
        fw.op("dve", lambda e: e.memset(ones_f[:], 1.0), writes=[ones_f])

        wconv_list = []
        for cfg in FF_CFG:
            cfg["res"].persist = True
            cfg["first_idx"] = len(wconv_list)
            for e_ in range(cfg["nexp"]):
                for b_ in range(cfg["nblk"]):
                    n0 = b_ * cfg["bw"]
                    for (dst, src) in ((cfg["WG"], cfg["wg"]), (cfg["WU"], cfg["wu"])):
                        wconv_list.append((dst[e_, b_], src[e_].rearrange("(kc p) n -> p kc n", p=128)[:, :, n0:n0 + cfg["bw"]], cfg["res"]))
                    wconv_list.append((cfg["WD"][e_, b_], cfg["wd"][e_][n0:n0 + cfg["bw"], :].rearrange("(j p) n -> p j n", p=128), cfg["res"]))
            cfg["last_idx"] = len(wconv_list)
        wconv_pos = [0]

        def pump(n=1, upto=None):
            while (n > 0 or (upto is not None and wconv_pos[0] < upto)) and wconv_pos[0] < len(wconv_list):
                dst, src, r = wconv_list[wconv_pos[0]]
                wconv_pos[0] += 1
                n -= 1
                fw.dma("pool", dst, src, writes=[r], holder=r, kind="i", merge=True)

        for l in range(DEPTH):
            last = (l == DEPTH - 1)
            first_out_chunk = 2 if last else 0
            with Phase(nc, fw, f"S{l}") as ph:
                cTt = ph.sb("cT", [128, 8, 2], F32)
                sc = ph.sb("sc", [128, 8, 2], F32)
                mb = ph.sb("mb", [128, 48], F32)
                n1 = ph.sb("n1", [128, 8], F32)
                n2 = ph.sb("n2", [128, 8], F32)
                mwr = ph.ring("mw", 2, [128, 8, 512], F32)
                pm = ph.ps("pm", [128, 48, 2], F32)
                ptm = ph.ps("ptm", [48, 128], F32)
                mds = ph.sb("mds", [48, 128], F32)
                fw.dma("sp", cTt[:], cT, writes=[cTt])
                fw.dma("sp", mb[:], modbT[l], writes=[mb])
                fw.dma("sp", n1[:], n1T[l], writes=[n1])
                fw.dma("sp", n2[:], n2T[l], writes=[n2])
                fw.op("act", lambda e: e.activation(out=sc[:], in_=cTt[:], func=AF.Silu), reads=[cTt], writes=[sc])
                for piece in range(12):
                    mw = mwr.next()
                    fw.dma("sp", mw[:], mod_w[l].rearrange("(kc p) n -> p kc n", p=128)[:, :, piece * 512:(piece + 1) * 512],
                           writes=[mw])
                    for jj in range(4):
                        j = piece * 4 + jj
                        for kc in range(8):
                            fw.op("pe", lambda e, j=j, jj=jj, kc=kc, mw=mw: e.matmul(
                                pm[:, j, :], lhsT=mw[:, kc, jj * 128:(jj + 1) * 128], rhs=sc[:, kc, :],
                                start=(kc == 0), stop=(kc == 7)), reads=[mw, sc], writes=[pm])
                fw.op("dve", lambda e: e.tensor_tensor(out=modT[:], in0=pm[:], in1=bc(mb[:].unsqueeze(2), [128, 48, 2]), op=ALU.add),
                      reads=[pm, mb], writes=[modT])
                for (gT, lo, nn) in ((g1T, 8, n1), (g2T, 32, n2)):
                    fw.op("dve", lambda e, gT=gT, lo=lo: e.tensor_scalar(out=gT[:], in0=modT[:, lo:lo + 8, :], scalar1=1.0, scalar2=None, op0=ALU.add),
                          reads=[modT], writes=[gT])
                    fw.op("dve", lambda e, gT=gT, nn=nn: e.tensor_tensor(out=gT[:], in0=gT[:], in1=bc(nn[:].unsqueeze(2), [128, 8, 2]), op=ALU.mult),
                          reads=[gT, nn], writes=[gT])
                for t in range(2):
                    fw.op("pe", lambda e, t=t: e.transpose(out=ptm[:], in_=modT[:, :, t], identity=ident_f[:]),
                          reads=[modT, ident_f], writes=[ptm])
                    fw.op("dve", lambda e: e.tensor_copy(out=mds[:], in_=ptm[:]), reads=[ptm], writes=[mds])
                    fw.dma("sp", MD[t].rearrange("(j p) -> j p", p=128), mds[:], reads=[mds], writes=[MDres], kind="o")

            with Phase(nc, fw, f"A{l}") as ph:
                Win = ph.sb("Win", [128, 8, NIN], BF16)
                WC = ph.sb("WC", [128, 3, 8, 512], BF16)
                cwb = ph.sb("cwb", [128, 3, 512], F32)
                qnb = ph.sb("qnb", [128, 64], F32)
                knb = ph.sb("knb", [128, 64], F32)
                cos_t = ph.sb("cos", [128, 32, 32], F32)
                sin_t = ph.sb("sin", [128, 32, 32], F32)
                for kc in range(8):
                    fw.dma("pool", Win[:, kc, :], w_in[l][kc * 128:(kc + 1) * 128, :], writes=[Win], merge=True)
                fw.dma("sp", cwb[:], conv_w[l].partition_broadcast(128), writes=[cwb])
                fw.dma("sp", qnb[:], qn_w[l].partition_broadcast(128), writes=[qnb])
                fw.dma("sp", knb[:], kn_w[l].partition_broadcast(128), writes=[knb])
                fw.dma("sp", cos_t[:], cos_d, writes=[cos_t])
                fw.dma("sp", sin_t[:], sin_d, writes=[sin_t])
                for k in range(3):
                    fw.op("dve", lambda e, k=k: e.tensor_tensor(out=WC[:, k, :, :], in0=Win[:, :, 2304:2816],
                                                               in1=bc(cwb[:, k:k + 1, :], [128, 8, 512]), op=ALU.mult),
                          reads=[Win, cwb], writes=[WC])
                rings = dict(st=ph.ring("st", 4, [128, 4], F32), xn=ph.ring("xn", 2, [128, D], F32),
                             tp=ph.ring("tp", 2, [128, 4, 128], F32, psum=True))
                hcr = ph.ring("hc", 3, [128, D], F32)
                NLIN = 6
                LIN = [ph.sb(f"lin{i}", [128, 8, 130], BF16) for i in range(NLIN)]
                LINH = [Res(f"linh{i}") for i in range(NLIN)]
                pjr = ph.ring("pj", 5, [128, 512], F32, psum=True)
                pgr = ph.ring("pg", 1, [16, 128], F32, psum=True)
                Pcr = ph.ring("Pc", 3, [128, 2304], BF16)
                Pvr = ph.ring("Pv", 2, [128, 512], BF16)
                gsr = ph.ring("gs", 2, [16, 128], F32)
                tA = ph.ring("tA", 2, [128, 512], F32)
                tB = ph.ring("tB", 2, [128, 512], F32)
                tC = ph.ring("tC", 2, [128, 512], F32)
                tD = ph.ring("tD", 2, [128, 512], F32)
                s8r = ph.ring("s8", 4, [128, 16], F32)

                def rope(src, nh, dst_tile, dst_ap, lc):
                    sv = src[:, 0:nh * 64].rearrange("p (h d) -> p h d", d=64)
                    c_ = tC.next()
                    d_ = tD.next()
                    cv = c_[:, 0:nh * 64].rearrange("p (h d) -> p h d", d=64)
                    dv = d_[:, 0:nh * 64].rearrange("p (h d) -> p h d", d=64)
                    ov = dst_ap.rearrange("p (h d) -> p h d", d=64)
                    cb = bc(cos_t[:, lc:lc + 1, :], [128, nh, 32])
                    sb_ = bc(sin_t[:, lc:lc + 1, :], [128, nh, 32])
                    fw.op("pool", lambda e: e.tensor_tensor(out=cv[:, :, 0:32], in0=sv[:, :, 0:32], in1=cb, op=ALU.mult), reads=[src, cos_t], writes=[c_])
                    fw.op("pool", lambda e: e.tensor_tensor(out=cv[:, :, 32:64], in0=sv[:, :, 0:32], in1=sb_, op=ALU.mult), reads=[src, sin_t], writes=[c_])
                    fw.op("dve", lambda e: e.tensor_tensor(out=dv[:, :, 0:32], in0=sv[:, :, 32:64], in1=sb_, op=ALU.mult), reads=[src, sin_t], writes=[d_])
                    fw.op("dve", lambda e: e.tensor_tensor(out=dv[:, :, 32:64], in0=sv[:, :, 32:64], in1=cb, op=ALU.mult), reads=[src, cos_t], writes=[d_])
                    fw.op("pool", lambda e: e.tensor_tensor(out=ov[:, :, 0:32], in0=cv[:, :, 0:32], in1=dv[:, :, 0:32], op=ALU.subtract), reads=[c_, d_], writes=[dst_tile])
                    fw.op("dve", lambda e: e.tensor_tensor(out=ov[:, :, 32:64], in0=cv[:, :, 32:64], in1=dv[:, :, 32:64], op=ALU.add), reads=[c_, d_], writes=[dst_tile])

                def qknorm(pj, col0, nh, wb, dst_tile, dst_ap, c):
                    a_ = tA.next()
                    b_ = tB.next()
                    s8 = s8r.next()
                    n = nh * 64
                    pv = pj[:, col0:col0 + n]
                    fw.op("act", lambda e: e.activation(out=a_[:, 0:n], in_=pv, func=AF.Square), reads=[pj], writes=[a_])
                    fw.op("dve", lambda e: e.tensor_reduce(out=s8[:, 0:nh], in_=a_[:, 0:n].rearrange("p (h d) -> p h d", d=64), axis=AX.X, op=ALU.add),
                          reads=[a_], writes=[s8])
                    fw.op("act", lambda e: e.activation(out=s8[:, 8:8 + nh], in_=s8[:, 0:nh], func=AF.Sqrt, scale=1.0 / 64, bias=cst[:, 0:1]),
                          reads=[s8, cst], writes=[s8])
                    fw.op("dve", lambda e: e.reciprocal(out=s8[:, 0:nh], in_=s8[:, 8:8 + nh]), reads=[s8], writes=[s8])
                    fw.op("dve", lambda e: e.tensor_tensor(out=a_[:, 0:n].rearrange("p (h d) -> p h d", d=64), in0=pv.rearrange("p (h d) -> p h d", d=64),
                                                           in1=bc(s8[:, 0:nh].unsqueeze(2), [128, nh, 64]), op=ALU.mult), reads=[pj, s8], writes=[a_])
                    if c < 2:
                        fw.op("dve", lambda e: e.tensor_tensor(out=dst_ap.rearrange("p (h d) -> p h d", d=64), in0=a_[:, 0:n].rearrange("p (h d) -> p h d", d=64),
                                                               in1=bc(wb[:].unsqueeze(1), [128, nh, 64]), op=ALU.mult), reads=[a_, wb], writes=[dst_tile])
                    else:
                        fw.op("dve", lambda e: e.tensor_tensor(out=b_[:, 0:n].rearrange("p (h d) -> p h d", d=64), in0=a_[:, 0:n].rearrange("p (h d) -> p h d", d=64),
                                                               in1=bc(wb[:].unsqueeze(1), [128, nh, 64]), op=ALU.mult), reads=[a_, wb], writes=[b_])
                        rope(b_, nh, dst_tile, dst_ap, c - 2)

                def proj(lin, n0, n1_, pj, ncols):
                    for kc in range(8):
                        fw.op("pe", lambda e, kc=kc: e.matmul(pj[:, 0:ncols], lhsT=lin[:, kc, 1:129], rhs=Win[:, kc, n0:n1_],
                                                             start=(kc == 0), stop=(kc == 7)), reads=[lin, Win], writes=[pj])

                def front(c):
                    if True:
                        t = 1 if c < 2 else 0
                        lin = LIN[c % NLIN]
                        hc = hcr.next()
                        if l == 0:
                            fw.dma("sp", hc[:], xin[c * 128:(c + 1) * 128, :], writes=[hc])
                        else:
                            fw.dma("sp", hc[:], H[c * 128:(c + 1) * 128, :], reads=[Hres[c]], writes=[hc])
                        st4 = rings["st"].next()
                        xn = rings["xn"].next()
                        fw.op("act", lambda e: e.activation(out=xn[:], in_=hc[:], func=AF.Square, accum_out=st4[:, 0:1]), reads=[hc], writes=[xn, st4])
                        fw.op("act", lambda e: e.activation(out=st4[:, 1:2], in_=st4[:, 0:1], func=AF.Sqrt, scale=1.0 / D, bias=cst[:, 0:1]), reads=[st4, cst], writes=[st4])
                        fw.op("dve", lambda e: e.reciprocal(out=st4[:, 2:3], in_=st4[:, 1:2]), reads=[st4], writes=[st4])
                        fw.op("act", lambda e: e.activation(out=xn[:], in_=hc[:], func=AF.Copy, scale=st4[:, 2:3]), reads=[hc, st4], writes=[xn])
                        for half in range(2):
                            tp = rings["tp"].next()
                            for j in range(4):
                                kc = half * 4 + j
                                fw.op("pe", lambda e, kc=kc, j=j: e.transpose(out=tp[:, j, :], in_=xn[:, kc * 128:(kc + 1) * 128], identity=ident_f[:]),
                                      reads=[xn, ident_f], writes=[tp])
                            for j in range(4):
                                kc = half * 4 + j
                                fw.op("dve", lambda e, kc=kc, j=j: e.tensor_scalar(out=lin[:, kc, 1:129], in0=tp[:, j, :], scalar1=g1T[:, kc, t:t + 1],
                                                                                  scalar2=modT[:, kc, t:t + 1], op0=ALU.mult, op1=ALU.add),
                                      reads=[tp, g1T, modT], writes=[lin])
                        linh = LINH[c % NLIN]
                        if c in (0, 2):
                            fw.op("pool", lambda e: e.memset(lin[:, :, 0:1], 0.0), writes=[linh])
                        else:
                            prev = LIN[(c - 1) % NLIN]
                            fw.op("pool", lambda e: e.tensor_copy(out=lin[:, :, 0:1], in_=prev[:, :, 128:129]), reads=[prev], writes=[linh])
                            fw.op("pool", lambda e: e.tensor_copy(out=prev[:, :, 129:130], in_=lin[:, :, 1:2]), reads=[lin], writes=[LINH[(c - 1) % NLIN]])
                        if c in (1, NCH - 1):
                            fw.op("pool", lambda e: e.memset(lin[:, :, 129:130], 0.0), writes=[linh])
                def back(c):
                    front(c)
                    yield
                    if True:
                        lin = LIN[c % NLIN]
                        Pc = Pcr.next()
                        pj = pjr.next()
                        proj(lin, 0, 512, pj, 512)
                        qknorm(pj, 0, 8, qnb, Pc, Pc[:, PQ:PQ + 512], c)
                        pj = pjr.next()
                        proj(lin, 512, 1024, pj, 512)
                        qknorm(pj, 0, 2, knb, Pc, Pc[:, PK:PK + 128], c)
                        fw.op("act", lambda e, pj=pj: e.activation(out=Pc[:, PV:PV + 384], in_=pj[:, 128:512], func=AF.Copy), reads=[pj], writes=[Pc])
                        yield
                        pj = pjr.next()
                        proj(lin, 1024, 1536, pj, 512)
                        if c < 2:
                            fw.op("act", lambda e, pj=pj: e.activation(out=Pc[:, PRQ:PRQ + 512], in_=pj[:, 0:512], func=AF.Copy), reads=[pj], writes=[Pc])
                        else:
                            b_ = tB.next()
                            fw.op("act", lambda e, pj=pj, b_=b_: e.activation(out=b_[:], in_=pj[:, 0:512], func=AF.Copy), reads=[pj], writes=[b_])
                            rope(b_, 8, Pc, Pc[:, PRQ:PRQ + 512], c - 2)
                        pj = pjr.next()
                        proj(lin, 1536, 2048, pj, 512)
                        fw.op("act", lambda e, pj=pj: e.activation(out=Pc[:, PRG:PRG + 256], in_=pj[:, 0:256], func=AF.Silu), reads=[pj], writes=[Pc])
                        fw.op("act", lambda e, pj=pj: e.activation(out=Pc[:, PMV:PMV + 256], in_=pj[:, 256:512], func=AF.Copy), reads=[pj], writes=[Pc])
                        pj = pjr.next()
                        proj(lin, 2048, 2304, pj, 256)
                        fw.op("act", lambda e, pj=pj: e.activation(out=Pc[:, PMO:PMO + 256], in_=pj[:, 0:256], func=AF.Sigmoid), reads=[pj], writes=[Pc])
                        fw.dma("pool", P[c * 128:(c + 1) * 128, 0:2304], Pc[:], reads=[Pc], writes=[Pres[c]], kind="o")
                        pg = pgr.next()
                        for kc in range(8):
                            fw.op("pe", lambda e, kc=kc: e.matmul(pg[:], lhsT=Win[:, kc, 2816:2832], rhs=lin[:, kc, 1:129], start=(kc == 0), stop=(kc == 7)),
                                  reads=[lin, Win], writes=[pg])
                        gs = gsr.next()
                        fw.op("dve", lambda e: e.tensor_copy(out=gs[:], in_=pg[:]), reads=[pg], writes=[gs])
                        fw.dma("pool", GT[:, c * 128:(c + 1) * 128], gs[:], reads=[gs], writes=[GTres[c]], kind="o")
                    yield
                    cc = c
                    if True:
                        linp = LIN[cc % NLIN]
                        pj = pjr.next()
                        for k in range(3):
                            for kc in range(8):
                                fw.op("pe", lambda e, k=k, kc=kc: e.matmul(pj[:], lhsT=linp[:, kc, k:k + 128], rhs=WC[:, k, kc, :],
                                                                           start=(k == 0 and kc == 0), stop=(k == 2 and kc == 7)),
                                      reads=[linp, LINH[cc % NLIN], WC], writes=[pj])
                        Pv = Pvr.next()
                        fw.op("act", lambda e, pj=pj: e.activation(out=Pv[:], in_=pj[:], func=AF.Silu), reads=[pj], writes=[Pv])
                        fw.dma("pool", P[cc * 128:(cc + 1) * 128, 2304:2816], Pv[:], reads=[Pv], writes=[Pcv[cc]], kind="o")

                gens = []
                for c in list(range(NCH)) + [None]:
                    if c is not None:
                        pump(1)
                        gens.append(back(c))
                    for g_ in list(gens):
                        try:
                            next(g_)
                        except StopIteration:
                            gens.remove(g_)
                while gens:
                    for g_ in list(gens):
                        try:
                            next(g_)
                        except StopIteration:
                            gens.remove(g_)
            if dbg == f"A{l}":
                break

            with Phase(nc, fw, f"B{l}") as ph:
                kT = ph.sb("kT", [128, NTOK], BF16)
                Va = ph.sb("Va", [128, NCH, 2, 128], BF16)
                kvr = ph.ring("kv", 2, [128, 256], BF16)
                tqr = ph.ring("tq", 2, [128, 4, 128], BF16, psum=True)
                fw.op("pool", lambda e: e.memset(Va[:], 1.0), writes=[Va])
                for c in range(NCH):
                    kv = kvr.next()
                    fw.dma("sp", kv[:], P[c * 128:(c + 1) * 128, PK:PK + 256], reads=[Pres[c]], writes=[kv])
                    tk = tqr.next()
                    fw.op("pe", lambda e: e.transpose(out=tk[:, 0, :], in_=kv[:, 0:128], identity=ident_b[:]), reads=[kv, ident_b], writes=[tk])
                    fw.op("dve", lambda e, c=c: e.tensor_copy(out=kT[:, c * 128:(c + 1) * 128], in_=tk[:, 0, :]), reads=[tk], writes=[kT])
                    fw.op("pool", lambda e, c=c: e.tensor_copy(out=Va[:, c, :, 0:64], in_=kv[:, 128:256].rearrange("p (g d) -> p g d", d=64)),
                          reads=[kv], writes=[Va])
                qbr = ph.ring("qb", 2, [128, 512], BF16)
                qTr = ph.ring("qT", 2, [128, 2, 512], BF16)
                for qz in qTr.tiles:
                    fw.op("pool", lambda e, qz=qz: e.memset(qz[:], 0.0), writes=[qz])
                psr = ph.ring("pss", 3, [128, 512], F32, psum=True)
                PTr = ph.ring("PT", 5, [128, 512], BF16)
                oTr = ph.ring("oT", 2, [128, 512], F32, psum=True)
                otr = ph.ring("ot", 1, [128, 4, 128], F32, psum=True)
                oSr = ph.ring("oS", 2, [128, 512], F32)
                pending_epi = []
                recr = ph.ring("rec", 2, [128, 4], F32)
                attr = ph.ring("att", 3, [128, 512], BF16)
                for qb in range(first_out_chunk, NCH):
                    pump(1)
                    keys = [0, 1] if qb < 2 else list(range(NCH))
                    qt = qbr.next()
                    fw.dma("sp", qt[:], P[qb * 128:(qb + 1) * 128, PQ:PQ + 512], reads=[Pres[qb]], writes=[qt])
                    tq = tqr.next()
                    for i in range(4):
                        fw.op("pe", lambda e, i=i: e.transpose(out=tq[:, i, :], in_=qt[:, i * 128:(i + 1) * 128], identity=ident_b[:]),
                              reads=[qt, ident_b], writes=[tq])
                    qT = qTr.next()
                    fw.op("dve", lambda e: e.tensor_copy(out=qT[0:64, 0, :], in_=tq[0:64].rearrange("p a b -> p (a b)")), reads=[tq], writes=[qT])
                    fw.op("dve", lambda e: e.tensor_copy(out=qT[64:128, 1, :], in_=tq[64:128].rearrange("p a b -> p (a b)")), reads=[tq], writes=[qT])
                    att = attr.next()
                    for g in range(2):
                        oT = oTr.next()

                        def smm(kc, g=g):
                            pss = psr.next()
                            fw.op("pe", lambda e: e.matmul(pss[:], lhsT=kT[:, kc * 128:(kc + 1) * 128], rhs=qT[:, g, :], start=True, stop=True),
                                  reads=[kT, qT], writes=[pss])
                            return pss
                        pend = [smm(keys[0])]
                        if len(keys) > 1:
                            pend.append(smm(keys[1]))
                        for ki, kc in enumerate(keys):
                            pss = pend.pop(0)
                            if ki + 2 < len(keys):
                                pend.append(smm(keys[ki + 2]))
                            PT = PTr.next()
                            fw.op("act", lambda e, pss=pss, PT=PT: e.activation(out=PT[:], in_=pss[:], func=AF.Exp, scale=0.125), reads=[pss], writes=[PT])
                            fw.op("pe", lambda e, kc=kc, g=g, PT=PT, oT=oT, ki=ki: e.matmul(
                                oT[:, :], lhsT=Va[:, kc, g, :], rhs=PT[:, :], start=(ki == 0), stop=(ki == len(keys) - 1)), reads=[PT, Va], writes=[oT])
                            if ki == min(3, len(keys) - 1) and pending_epi:
                                pending_epi.pop(0)()

                        def epi(oT=oT, att=att, g=g, qb=qb, lastg=(g == 1)):
                            oS = oSr.next()
                            fw.op("dve", lambda e: e.tensor_copy(out=oS[:], in_=oT[:, :]), reads=[oT], writes=[oS])
                            ot = otr.next()
                            for i in range(4):
                                fw.op("pe", lambda e, i=i: e.transpose(out=ot[:, i, :], in_=oS[:, i * 128:(i + 1) * 128], identity=ident_f[:]),
                                      reads=[oS, ident_f], writes=[ot])
                            rec = recr.next()
                            fw.op("dve", lambda e: e.reciprocal(out=rec[:], in_=ot[:, :, 64]), reads=[ot], writes=[rec])
                            fw.op("dve", lambda e: e.tensor_tensor(
                                out=att[:, g * 256:(g + 1) * 256].rearrange("p (h d) -> p h d", d=64), in0=ot[:, :, 0:64],
                                in1=bc(rec[:].unsqueeze(2), [128, 4, 64]), op=ALU.mult), reads=[ot, rec], writes=[att])
                            if lastg:
                                fw.dma("pool", MIX[qb * 128:(qb + 1) * 128, 0:512], att[:], reads=[att], writes=[MIXres[qb]], kind="o")
                        pending_epi.append(epi)
                while pending_epi:
                    pending_epi.pop(0)()
            if dbg == f"B{l}":
                break

            for kind in ("ret", "ml"):
                if kind == "ret":
                    qcol, kcol, vcol, gcol, ocol = PRQ, PRK, PRV, PRG, 512
                    qres = kres = Pres
                else:
                    qcol, kcol, vcol, gcol, ocol = PMQ, PMK, PMV, PMO, 768
                    qres = kres = Pcv
                with Phase(nc, fw, f"{kind}{l}") as ph:
                    E = [ph.sb(f"E{d_}", [128, 4, NCH], F32) for d_ in range(2)]
                    Fm = [ph.sb(f"F{d_}", [128, 4, NCH], F32) for d_ in range(2)]
                    PRE = [ph.sb(f"PRE{d_}", [64, NCH, 4], F32) for d_ in range(2)]
                    POST = [ph.sb(f"POST{d_}", [64, 4], F32) for d_ in range(2)]
                    nwb = ph.sb("nwb", [128, 256], F32)
                    fw.dma("sp", nwb[:], (ret_norm_w if kind == "ret" else mlstm_norm_w)[l].partition_broadcast(128), writes=[nwb])
                    with Phase(nc, fw, f"{kind}{l}tab") as pt:
                        if kind == "ret":
                            rd = pt.sb("rd", [128, 8], F32)
                            lg = pt.sb("lg", [128, 8], F32)
                            pos = pt.sb("pos", [128, 2], F32)
                            tmp = pt.sb("tmp", [128, 8], F32)
                            tmp2 = pt.sb("tmp2", [128, 8], F32)
                            fw.dma("sp", rd[:], ret_decay[l].partition_broadcast(128), writes=[rd])
                            fw.dma("sp", pos[:], pos_d, writes=[pos])
                            fw.op("act", lambda e: e.activation(out=lg[:], in_=rd[:], func=AF.Exp), reads=[rd], writes=[lg])
                            fw.op("act", lambda e: e.activation(out=lg[:], in_=lg[:], func=AF.Ln, scale=-1.0, bias=cst[:, 1:2]), reads=[lg, cst], writes=[lg])
                            for d_ in range(2):
                                fw.op("dve", lambda e, d_=d_: e.tensor_scalar(out=tmp[:, d_ * 4:d_ * 4 + 4], in0=lg[:, d_ * 4:d_ * 4 + 4], scalar1=pos[:, d_:d_ + 1],
                                                                             scalar2=None, op0=ALU.mult), reads=[lg, pos], writes=[tmp])
                            fw.op("act", lambda e: e.activation(out=tmp2[:], in_=tmp[:], func=AF.Exp), reads=[tmp], writes=[tmp2])
                            for d_ in range(2):
                                fw.op("dve", lambda e, d_=d_: e.tensor_copy(out=Fm[d_][:], in_=bc(tmp2[:, d_ * 4:d_ * 4 + 4].unsqueeze(2), [128, 4, NCH])),
                                      reads=[tmp2], writes=[Fm[d_]])
                            fw.op("act", lambda e: e.activation(out=tmp2[:], in_=tmp[:], func=AF.Exp, scale=-1.0), reads=[tmp], writes=[tmp2])
                            fw.op("dve", lambda e: e.tensor_scalar(out=tmp2[:], in0=tmp2[:], scalar1=0.125, scalar2=None, op0=ALU.mult), reads=[tmp2], writes=[tmp2])
                            for d_ in range(2):
                                fw.op("dve", lambda e, d_=d_: e.tensor_copy(out=E[d_][:], in_=bc(tmp2[:, d_ * 4:d_ * 4 + 4].unsqueeze(2), [128, 4, NCH])),
                                      reads=[tmp2], writes=[E[d_]])
                            fw.op("act", lambda e: e.activation(out=tmp[:], in_=lg[:], func=AF.Exp, scale=128.0), reads=[lg], writes=[tmp])
                            for d_ in range(2):
                                fw.op("dve", lambda e, d_=d_: e.tensor_copy(out=POST[d_][:], in_=tmp[0:64, d_ * 4:d_ * 4 + 4]), reads=[tmp], writes=[POST[d_]])
                                fw.op("dve", lambda e, d_=d_: e.memset(PRE[d_][:], 1.0), writes=[PRE[d_]])
                        else:
                            Gt = pt.sb("Gt", [NCH, 16, 128], F32)
                            gb = pt.sb("gb", [NCH, 16], F32)
                            cs0 = pt.sb("cs0", [NCH, 8, 128], F32)
                            cs1 = pt.sb("cs1", [NCH, 8, 128], F32)
                            t2 = pt.sb("t2", [NCH, 8, 128], F32)
                            U = pt.sb("U", [NCH, 8, 128], F32)
                            NB = pt.sb("NB", [NCH, 8, 128], F32)
                            ub = pt.sb("ub", [NCH, 16], F32)
                            for c in range(NCH):
                                pass
                            fw.dma("sp", Gt[:], GT.rearrange("g (c t) -> c g t", t=128), reads=GTres, writes=[Gt])
                            fw.dma("sp", gb[:], gate_b[l].partition_broadcast(NCH), writes=[gb])
                            for d_ in range(2):
                                fw.op("dve", lambda e, d_=d_: e.memset(POST[d_][:], 1.0), writes=[POST[d_]])
                            fw.op("dve", lambda e: e.tensor_tensor(out=Gt[:], in0=Gt[:], in1=bc(gb[:].unsqueeze(2), [NCH, 16, 128]), op=ALU.add),
                                  reads=[Gt, gb], writes=[Gt])
                            fw.op("act", lambda e: e.activation(out=t2[:], in_=Gt[:, 8:16, :], func=AF.Exp, scale=-1.0), reads=[Gt], writes=[t2])
                            fw.op("act", lambda e: e.activation(out=t2[:], in_=t2[:], func=AF.Ln, scale=1.0, bias=cst[0:NCH, 1:2]), reads=[t2, cst], writes=[t2])
                            src, dst = t2, cs0
                            sh = 1
                            while sh < 128:
                                fw.op("dve", lambda e, src=src, dst=dst, sh=sh: e.tensor_tensor(out=dst[:, :, sh:128], in0=src[:, :, sh:128], in1=src[:, :, 0:128 - sh], op=ALU.add),
                                      reads=[src], writes=[dst])
                                fw.op("dve", lambda e, src=src, dst=dst, sh=sh: e.tensor_copy(out=dst[:, :, 0:sh], in_=src[:, :, 0:sh]), reads=[src], writes=[dst])
                                src = dst
                                dst = cs1 if dst is cs0 else cs0
                                if sh == 1:
                                    pass
                                sh *= 2
                            cs = src
                            other = dst
                            fw.op("dve", lambda e: e.tensor_copy(out=NB[:, 0:4, :], in_=cs[:, 0:4, :]), reads=[cs], writes=[NB])
                            fw.op("dve", lambda e: e.tensor_tensor(out=NB[:, 4:8, :], in0=t2[:, 4:8, :], in1=cs[:, 4:8, :], op=ALU.subtract), reads=[cs, t2], writes=[NB])
                            fw.op("dve", lambda e: e.tensor_tensor(out=NB[:, 4:8, :], in0=NB[:, 4:8, :], in1=bc(cs[:, 4:8, 127:128], [NCH, 4, 128]), op=ALU.add),
                                  reads=[cs, NB], writes=[NB])
                            fw.op("dve", lambda e: e.tensor_tensor(out=U[:], in0=Gt[:, 0:8, :], in1=NB[:], op=ALU.add), reads=[Gt, NB], writes=[U])
                            fw.op("dve", lambda e: e.tensor_reduce(out=ub[:, 0:8], in_=U[:], axis=AX.X, op=ALU.max), reads=[U], writes=[ub])
                            fw.op("dve", lambda e: e.tensor_scalar(out=ub[:, 8:16], in0=cs[:, :, 127], scalar1=-1.0, scalar2=None, op0=ALU.mult), reads=[cs], writes=[ub])
                            pq = pt.ps("pq", [4, 4, NCH], F32)
                            UB = pt.sb("UB", [4, 4, NCH], F32)
                            for qi in range(4):
                                fw.op("pe", lambda e, qi=qi: e.transpose(out=pq[:, qi, :], in_=ub[:, qi * 4:qi * 4 + 4], identity=ident_f[0:NCH, 0:NCH]),
                                      reads=[ub, ident_f], writes=[pq])
                            fw.op("dve", lambda e: e.tensor_copy(out=UB[:], in_=pq[:]), reads=[pq], writes=[UB])
                            mcur = pt.sb("mcur", [4, 2, NCH + 1], F32)
                            Mend = pt.sb("Mend", [4, 2, NCH], F32)
                            dd = pt.sb("dd", [4, 2, NCH], F32)
                            fw.op("dve", lambda e: e.memset(mcur[:], 0.0), writes=[mcur])
                            for d_, order in enumerate((FWD_ORDER, BWD_ORDER)):
                                for idx, c in enumerate(order):
                                    fw.op("dve", lambda e, d_=d_, idx=idx, c=c: e.tensor_tensor(out=Mend[:, d_, c:c + 1], in0=mcur[:, d_, idx:idx + 1],
                                                                                                in1=UB[:, d_, c:c + 1], op=ALU.max), reads=[mcur, UB], writes=[Mend])
                                    fw.op("dve", lambda e, d_=d_, idx=idx, c=c: e.tensor_tensor(out=mcur[:, d_, idx + 1:idx + 2], in0=Mend[:, d_, c:c + 1],
                                                                                                in1=UB[:, 2 + d_, c:c + 1], op=ALU.add), reads=[Mend, UB], writes=[mcur])
                                    fw.op("dve", lambda e, d_=d_, idx=idx, c=c: e.tensor_tensor(out=dd[:, d_, c:c + 1], in0=mcur[:, d_, idx:idx + 1],
                                                                                                in1=Mend[:, d_, c:c + 1], op=ALU.subtract), reads=[mcur, Mend], writes=[dd])
                            SC = pt.sb("SC", [4, 2, NCH], F32)
                            fw.op("act", lambda e: e.activation(out=SC[:], in_=dd[:], func=AF.Exp), reads=[dd], writes=[SC])
                            BD = pt.sb("BD", [4, NCH, 4], F32)
                            ppre = pt.ps("ppre", [64, NCH * 4], F32)
                            for d_ in range(2):
                                fw.op("dve", lambda e, d_=d_: e.tensor_tensor(out=BD[:], in0=bc(SC[:, d_, :].unsqueeze(2), [4, NCH, 4]),
                                                                             in1=bc(ident_f[0:4, 0:4].unsqueeze(1), [4, NCH, 4]), op=ALU.mult),
                                      reads=[SC, ident_f], writes=[BD])
                                fw.op("pe", lambda e: e.matmul(ppre[:], lhsT=ones_f[0:4, 0:64], rhs=BD[:].rearrange("p c h -> p (c h)"), start=True, stop=True),
                                      reads=[ones_f, BD], writes=[ppre])
                                fw.op("dve", lambda e, d_=d_: e.tensor_copy(out=PRE[d_][:].rearrange("p c h -> p (c h)"), in_=ppre[:]), reads=[ppre], writes=[PRE[d_]])
                            pm2 = pt.ps("pm2", [NCH, 8], F32)
                            MT = pt.sb("MT", [NCH, 8], F32)
                            for d_ in range(2):
                                fw.op("pe", lambda e, d_=d_: e.transpose(out=pm2[:, d_ * 4:d_ * 4 + 4], in_=Mend[:, d_, :], identity=ident_f[0:4, 0:4]),
                                      reads=[Mend, ident_f], writes=[pm2])
                            fw.op("dve", lambda e: e.tensor_copy(out=MT[:], in_=pm2[:]), reads=[pm2], writes=[MT])
                            fw.op("dve", lambda e: e.tensor_tensor(out=U[:], in0=U[:], in1=bc(MT[:].unsqueeze(2), [NCH, 8, 128]), op=ALU.subtract), reads=[U, MT], writes=[U])
                            fw.op("act", lambda e: e.activation(out=U[:], in_=U[:], func=AF.Exp), reads=[U], writes=[U])
                            fw.op("dve", lambda e: e.tensor_tensor(out=NB[:], in0=NB[:], in1=bc(MT[:].unsqueeze(2), [NCH, 8, 128]), op=ALU.subtract), reads=[NB, MT], writes=[NB])
                            fw.op("act", lambda e: e.activation(out=NB[:], in_=NB[:], func=AF.Exp), reads=[NB], writes=[NB])
                            ptab = pt.ps("ptab", [128, 4, NCH], F32)
                            for (srcT, dsts, scl) in ((U, E, 0.125), (NB, Fm, 1.0)):
                                for d_ in range(2):
                                    for h in range(4):
                                        fw.op("pe", lambda e, srcT=srcT, d_=d_, h=h: e.transpose(out=ptab[:, h, :], in_=srcT[:, d_ * 4 + h, :], identity=ident_f[0:NCH, 0:NCH]),
                                              reads=[srcT, ident_f], writes=[ptab])
                                    fw.op("dve", lambda e, dsts=dsts, d_=d_, scl=scl: e.tensor_scalar(out=dsts[d_][:], in0=ptab[:], scalar1=scl, scalar2=None, op0=ALU.mult),
                                          reads=[ptab], writes=[dsts[d_]])

                    SBs = ph.sb("SBs", [64, NCH, 4, 65], BF16)
                    ST = [ph.sb(f"ST{d_}", [64, 4, 65], F32) for d_ in range(2)]
                    SP = ph.ring("SP", 2, [64, 4, 65], F32)
                    SFb = ph.ring("SFb", 3, [64, 4, 65], BF16)
                    kvr = ph.ring("kv", 4, [128, 512], BF16)
                    Var = ph.ring("Va", 4, [128, 4, 65], BF16)
                    KEr = ph.ring("KE", 3, [128, 4, 64], BF16)
                    pinc = ph.ring("pinc", 2, [64, 4, 128], F32, psum=True)
                    for d_ in range(2):
                        fw.op("dve", lambda e, d_=d_: e.memset(ST[d_][:], 0.0), writes=[ST[d_]])
                    for va in Var.tiles:
                        fw.op("pool", lambda e, va=va: e.memset(va[:], 1.0), writes=[va])

                    def load_kv(c):
                        kv = kvr.next()
                        fw.dma("sp", kv[:, 0:256], P[c * 128:(c + 1) * 128, kcol:kcol + 256], reads=[kres[c]], writes=[kv], merge=True)
                        fw.dma("sp", kv[:, 256:512], P[c * 128:(c + 1) * 128, vcol:vcol + 256], reads=[Pres[c]], writes=[kv], merge=True)
                        va = Var.next()
                        fw.op("act", lambda e: e.activation(out=va[:, :, 0:64], in_=kv[:, 256:512].rearrange("p (h d) -> p h d", d=64), func=AF.Copy), reads=[kv], writes=[va])
                        return kv, va

                    def state_step(d_, c, kv, va, save_ap=None, save_tile=None):
                        sp = SP.next()
                        fw.op("dve", lambda e: e.tensor_tensor(out=sp[:], in0=ST[d_][:], in1=bc(PRE[d_][:, c, :].unsqueeze(2), [64, 4, 65]), op=ALU.mult),
                              reads=[ST[d_], PRE[d_]], writes=[sp])
                        fw.op("act", lambda e: e.activation(out=save_ap, in_=sp[:], func=AF.Copy), reads=[sp], writes=[save_tile])
                        ke = KEr.next()
                        fw.op("dve", lambda e: e.tensor_tensor(out=ke[:], in0=kv[:, 0:256].rearrange("p (h d) -> p h d", d=64),
                                                               in1=bc(E[d_][:, :, c:c + 1], [128, 4, 64]), op=ALU.mult), reads=[kv, E[d_]], writes=[ke])
                        pi = pinc.next()
                        for h in range(4):
                            fw.op("pe", lambda e, h=h: e.matmul(pi[:, h, 0:65], lhsT=ke[:, h, :], rhs=va[:, h, :], start=True, stop=True),
                                  reads=[ke, va], writes=[pi])
                        fw.op("dve", lambda e: e.tensor_tensor(out=ST[d_][:], in0=sp[:], in1=pi[:, :, 0:65], op=ALU.add), reads=[sp, pi], writes=[ST[d_]])
                        fw.op("dve", lambda e: e.tensor_tensor(out=ST[d_][:], in0=ST[d_][:], in1=bc(POST[d_][:].unsqueeze(2), [64, 4, 65]), op=ALU.mult),
                              reads=[ST[d_], POST[d_]], writes=[ST[d_]])

                    for c in BWD_ORDER:
                        pump(1)
                        kv, va = load_kv(c)
                        state_step(1, c, kv, va, save_ap=SBs[:, c, :, :], save_tile=SBs)

                    qgr = ph.ring("qg", 5, [128, 512], BF16)
                    ptq = ph.ring("ptq", 1, [64, 8, 128], BF16, psum=True)
                    qkT = ph.ring("qkT", 3, [64, 8, 128], BF16)
                    psc = ph.ring("psc", 1, [128, 4, 128], F32, psum=True)
                    tmpS = ph.ring("tmpS", 2, [128, 4, 128], F32)
                    SSr = [ph.ring(f"SS{d_}", 3, [128, 4, 128], BF16) for d_ in range(2)]
                    pR = [ph.ring(f"pR{d_}", 2, [128, 4, 128], F32, psum=True) for d_ in range(2)]
                    s4 = ph.ring("s4", 12, [128, 8], F32)
                    hh = ph.ring("hh", 16, [128, 4, 64], F32)
                    mo_r = ph.ring("mixo", 3, [128, 256], BF16)
                    def chunk_gen(c):
                        pump(1)
                        kv, va = load_kv(c)
                        sfb = SFb.next()
                        do_out = c >= first_out_chunk
                        if do_out:
                            qg = qgr.next()
                            fw.dma("sp", qg[:, 0:256], P[c * 128:(c + 1) * 128, qcol:qcol + 256], reads=[qres[c]], writes=[qg], merge=True)
                            fw.dma("sp", qg[:, 256:512], P[c * 128:(c + 1) * 128, gcol:gcol + 256], reads=[Pres[c]], writes=[qg], merge=True)
                        state_step(0, c, kv, va, save_ap=sfb[:], save_tile=sfb)
                        if not do_out:
                            return
                        tq = ptq.next()
                        for h in range(4):
                            fw.op("pe", lambda e, h=h: e.transpose(out=tq[:, h, :], in_=qg[:, h * 64:(h + 1) * 64], identity=ident_b[:]), reads=[qg, ident_b], writes=[tq])
                            fw.op("pe", lambda e, h=h: e.transpose(out=tq[:, 4 + h, :], in_=kv[:, h * 64:(h + 1) * 64], identity=ident_b[:]), reads=[kv, ident_b], writes=[tq])
                        qk = qkT.next()
                        fw.op("act", lambda e: e.activation(out=qk[:], in_=tq[:], func=AF.Copy), reads=[tq], writes=[qk])
                        ps_ = psc.next()
                        for h in range(4):
                            fw.op("pe", lambda e, h=h: e.matmul(ps_[:, h, :], lhsT=qk[:, 4 + h, :], rhs=qk[:, h, :], start=True, stop=True), reads=[qk], writes=[ps_])
                        sss = []
                        for d_ in range(2):
                            ts_ = tmpS.next()
                            ss = SSr[d_].next()
                            fw.op("dve", lambda e, d_=d_, ts_=ts_: e.tensor_tensor(out=ts_[:], in0=ps_[:], in1=bc(E[d_][:, :, c:c + 1], [128, 4, 128]), op=ALU.mult),
                                  reads=[ps_, E[d_]], writes=[ts_])
                            mk_ = maskF if d_ == 0 else maskB
                            fw.op("pool", lambda e, ts_=ts_, ss=ss, mk_=mk_: e.tensor_tensor(out=ss[:], in0=ts_[:], in1=bc(mk_[:].unsqueeze(1), [128, 4, 128]), op=ALU.mult),
                                  reads=[ts_, mk_], writes=[ss])
                            sss.append(ss)
                        yield
                        Rs = []
                        for d_ in range(2):
                            ss = sss[d_]
                            R = pR[d_].next()
                            for h in range(4):
                                fw.op("pe", lambda e, h=h, ss=ss, R=R: e.matmul(R[:, h, 0:65], lhsT=ss[:, h, :], rhs=va[:, h, :], start=True, stop=False),
                                      reads=[ss, va], writes=[R])
                                if d_ == 0:
                                    fw.op("pe", lambda e, h=h, R=R: e.matmul(R[:, h, 0:65], lhsT=qk[:, h, :], rhs=sfb[:, h, :], start=False, stop=True),
                                          reads=[qk, sfb], writes=[R])
                                else:
                                    fw.op("pe", lambda e, h=h, R=R: e.matmul(R[:, h, 0:65], lhsT=qk[:, h, :], rhs=SBs[:, c, h, :], start=False, stop=True),
                                          reads=[qk, SBs], writes=[R])
                            Rs.append(R)
                        yield
                        hsum = hh.next()
                        hd = []
                        for d_ in range(2):
                            R = Rs[d_]
                            hx = hh.next()
                            if kind == "ml":
                                den = s4.next()
                                fw.op("act", lambda e, R=R, den=den: e.activation(out=den[:, 0:4], in_=R[:, :, 64], func=AF.Abs), reads=[R], writes=[den])
                                fw.op("dve", lambda e, d_=d_, den=den: e.tensor_tensor(out=den[:, 0:4], in0=den[:, 0:4], in1=Fm[d_][:, :, c], op=ALU.max), reads=[den, Fm[d_]], writes=[den])
                                fw.op("dve", lambda e, den=den: e.reciprocal(out=den[:, 4:8], in_=den[:, 0:4]), reads=[den], writes=[den])
                                fw.op("dve", lambda e, R=R, den=den, hx=hx: e.tensor_tensor(out=hx[:], in0=R[:, :, 0:64], in1=bc(den[:, 4:8].unsqueeze(2), [128, 4, 64]), op=ALU.mult),
                                      reads=[R, den], writes=[hx])
                            else:
                                fw.op("dve", lambda e, R=R, d_=d_, hx=hx: e.tensor_tensor(out=hx[:], in0=R[:, :, 0:64], in1=bc(Fm[d_][:, :, c:c + 1], [128, 4, 64]), op=ALU.mult),
                                      reads=[R, Fm[d_]], writes=[hx])
                            hd.append(hx)
                        fw.op("pool", lambda e: e.tensor_tensor(out=hsum[:], in0=hd[0][:], in1=hd[1][:], op=ALU.add), reads=[hd[0], hd[1]], writes=[hsum])
                        gv = qg[:, 256:512].rearrange("p (h d) -> p h d", d=64)
                        if kind == "ml":
                            fw.op("pool", lambda e: e.tensor_tensor(out=hsum[:], in0=hsum[:], in1=gv, op=ALU.mult), reads=[hsum, qg], writes=[hsum])
                        yield
                        stt = s4.next()
                        xc = hh.next()
                        sq = hd[0]
                        fw.op("dve", lambda e: e.tensor_reduce(out=stt[:, 0:4], in_=hsum[:], axis=AX.X, op=ALU.add), reads=[hsum], writes=[stt])
                        fw.op("dve", lambda e: e.tensor_scalar(out=stt[:, 0:4], in0=stt[:, 0:4], scalar1=-1.0 / 64, scalar2=None, op0=ALU.mult), reads=[stt], writes=[stt])
                        fw.op("dve", lambda e: e.tensor_tensor(out=xc[:], in0=hsum[:], in1=bc(stt[:, 0:4].unsqueeze(2), [128, 4, 64]), op=ALU.add), reads=[hsum, stt], writes=[xc])
                        fw.op("act", lambda e: e.activation(out=sq[:], in_=xc[:], func=AF.Square), reads=[xc], writes=[sq])
                        fw.op("dve", lambda e: e.tensor_reduce(out=stt[:, 4:8], in_=sq[:], axis=AX.X, op=ALU.add), reads=[sq], writes=[stt])
                        fw.op("act", lambda e: e.activation(out=stt[:, 0:4], in_=stt[:, 4:8], func=AF.Sqrt, scale=1.0 / 64, bias=cst[:, 0:1]), reads=[stt, cst], writes=[stt])
                        fw.op("dve", lambda e: e.reciprocal(out=stt[:, 4:8], in_=stt[:, 0:4]), reads=[stt], writes=[stt])
                        fw.op("dve", lambda e: e.tensor_tensor(out=xc[:], in0=xc[:], in1=bc(stt[:, 4:8].unsqueeze(2), [128, 4, 64]), op=ALU.mult), reads=[xc, stt], writes=[xc])
                        mo_ = mo_r.next()
                        mv_ = mo_[:].rearrange("p (h d) -> p h d", d=64)
                        nv = nwb[:].rearrange("p (h d) -> p h d", d=64)
                        if kind == "ml":
                            fw.op("pool", lambda e: e.tensor_tensor(out=mv_, in0=xc[:], in1=nv, op=ALU.mult), reads=[xc, nwb], writes=[mo_])
                        else:
                            fw.op("pool", lambda e: e.tensor_tensor(out=xc[:], in0=xc[:], in1=nv, op=ALU.mult), reads=[xc, nwb], writes=[xc])
                            fw.op("pool", lambda e: e.tensor_tensor(out=mv_, in0=xc[:], in1=gv, op=ALU.mult), reads=[xc, qg], writes=[mo_])
                        fw.dma("pool", MIX[c * 128:(c + 1) * 128, ocol:ocol + 256], mo_[:], reads=[mo_], writes=[MIXres[c]], kind="o")

                    gens = []
                    for c in FWD_ORDER + [None]:
                        if c is not None:
                            gens.append(chunk_gen(c))
                        for g_ in list(gens):
                            try:
                                next(g_)
                            except StopIteration:
                                gens.remove(g_)
                    for g_ in gens:
                        for _ in g_:
                            pass
                if dbg == f"{kind}{l}":
                    break
            if dbg in (f"ret{l}", f"ml{l}"):
                break

            with Phase(nc, fw, f"E{l}") as ph:
                Wo = ph.sb("Wo", [128, 8, D], BF16)
                for kc in range(8):
                    fw.dma("pool", Wo[:, kc, :], w_out[l][kc * 128:(kc + 1) * 128, :], writes=[Wo], merge=True)
                gbc = [ph.sb(f"gbc{t}", [128, D], F32) for t in range(2)]
                for t in range(2):
                    fw.dma("sp", gbc[t][:], MD[t, 2 * D:3 * D].partition_broadcast(128), reads=[MDres], writes=[gbc[t]])
                mxr = ph.ring("mx", 2, [128, D], BF16)
                ptx = ph.ring("ptx", 2, [128, 8, 128], BF16, psum=True)
                mTr = ph.ring("mT", 2, [128, 8, 128], BF16)
                pyr = ph.ring("py", 4, [128, 512], F32, psum=True)
                hcr = ph.ring("hc", 3, [128, D], F32)
                tyr = ph.ring("ty", 2, [128, D], F32)
                for c in range(first_out_chunk, NCH):
                    pump(1)
                    t = 1 if c < 2 else 0
                    mx = mxr.next()
                    fw.dma("sp", mx[:], MIX[c * 128:(c + 1) * 128, :], reads=[MIXres[c]], writes=[mx])
                    hc = hcr.next()
                    if l == 0:
                        fw.dma("sp", hc[:], xin[c * 128:(c + 1) * 128, :], writes=[hc])
                    else:
                        fw.dma("sp", hc[:], H[c * 128:(c + 1) * 128, :], reads=[Hres[c]], writes=[hc])
                    tx = ptx.next()
                    for kc in range(8):
                        fw.op("pe", lambda e, kc=kc: e.transpose(out=tx[:, kc, :], in_=mx[:, kc * 128:(kc + 1) * 128], identity=ident_b[:]), reads=[mx, ident_b], writes=[tx])
                    mT = mTr.next()
                    fw.op("act", lambda e: e.activation(out=mT[:], in_=tx[:], func=AF.Copy), reads=[tx], writes=[mT])
                    ty = tyr.next()
                    for n in range(2):
                        py = pyr.next()
                        for kc in range(8):
                            fw.op("pe", lambda e, kc=kc, n=n, py=py: e.matmul(py[:], lhsT=mT[:, kc, :], rhs=Wo[:, kc, n * 512:(n + 1) * 512], start=(kc == 0), stop=(kc == 7)),
                                  reads=[mT, Wo], writes=[py])
                        fw.op("dve", lambda e, n=n, py=py: e.tensor_tensor(out=ty[:, n * 512:(n + 1) * 512], in0=py[:], in1=gbc[t][:, n * 512:(n + 1) * 512], op=ALU.mult),
                              reads=[py, gbc[t]], writes=[ty])
                    fw.op("pool", lambda e: e.tensor_tensor(out=hc[:], in0=hc[:], in1=ty[:], op=ALU.add), reads=[hc, ty], writes=[hc])
                    fw.dma("pool", H[c * 128:(c + 1) * 128, :], hc[:], reads=[hc], writes=[Hres[c]], kind="o")
            if dbg == f"E{l}":
                break

            cfg = FF_CFG[l % 2]
            pump(0, upto=cfg["last_idx"])
            moe = (l % 2 == 1)
            nffc, nblk, nexp = cfg["nffc"], cfg["nblk"], cfg["nexp"]
            if moe and not DENSE_MOE:
                with Phase(nc, fw, f"M{l}") as pm_:
                    SEL1 = pm_.sb("SEL1", [128, 32, 8], F32)
                    SEL2 = pm_.sb("SEL2", [128, 32, 8], F32)
                    GWS = pm_.sb("GWS", [128, 32, 8], F32)
                    RANK = pm_.sb("RANK", [128, 32, 8], F32)
                    BASE = pm_.sb("BASE", [128, 32, 8], F32)
                    SLOT_i = pm_.sb("SLOTi", [128, 2, 32], I32)
                    GWK = pm_.sb("GWK", [128, 2, 32], F32)
                    BE_i = pm_.sb("BEi", [128, NTL], I32)
                    IDXW = pm_.sb("IDXW", [128, NTL, 8], I32)
                    gbc = pm_.sb("gbc", [128, D], F32)
                    fw.dma("sp", gbc[:], MD[0, 5 * D:6 * D].partition_broadcast(128), reads=[MDres], writes=[gbc])
                    with Phase(nc, fw, f"MR{l}") as ph:
                        zt = ph.sb("zt", [128, 4, D], BF16)
                        fw.op("pool", lambda e: e.memset(zt[:], 0.0), writes=[zt])
                        for j in range(NTL):
                            fw.dma("sp", XS[j * 512:(j + 1) * 512, :].rearrange("(s p) d -> p s d", p=128), zt[:], reads=[zt], writes=[XSres], holder=zt, kind="o", merge=True)
                        ATS = ph.sb("ATS", [128, 32, D], BF16)
                        g2bc = ph.sb("g2bc", [128, D], F32)
                        sh2bc = ph.sb("sh2bc", [128, D], F32)
                        n2bc = ph.sb("n2bc", [128, D], F32)
                        Wr = ph.sb("Wr", [128, 8, 8], F32)
                        rbb = ph.sb("rbb", [128, 8], F32)
                        Ust = ph.sb("Ust", [128, 128], F32)
                        run = ph.sb("run", [128, 8], F32)
                        grid = ph.sb("grid", [128, 24], F32)
                        fw.dma("sp", g2bc[:], MD[0, 4 * D:5 * D].partition_broadcast(128), reads=[MDres], writes=[g2bc])
                        fw.dma("sp", sh2bc[:], MD[0, 3 * D:4 * D].partition_broadcast(128), reads=[MDres], writes=[sh2bc])
                        fw.dma("sp", n2bc[:], n2row[l].partition_broadcast(128), writes=[n2bc])
                        fw.dma("sp", Wr[:], router_w.rearrange("(kc p) e -> p kc e", p=128), writes=[Wr])
                        fw.dma("sp", rbb[:], router_b[0].partition_broadcast(128), writes=[rbb])
                        fw.dma("sp", grid[:], grid_d, writes=[grid])
                        fw.op("dve", lambda e: e.tensor_scalar(out=g2bc[:], in0=g2bc[:], scalar1=1.0, scalar2=None, op0=ALU.add), reads=[g2bc], writes=[g2bc])
                        fw.op("dve", lambda e: e.tensor_tensor(out=g2bc[:], in0=g2bc[:], in1=n2bc[:], op=ALU.mult), reads=[g2bc, n2bc], writes=[g2bc])
                        fw.op("dve", lambda e: e.tensor_tensor(out=Ust[:], in0=maskF[:], in1=ident_f[:], op=ALU.subtract), reads=[maskF, ident_f], writes=[Ust])
                        fw.op("dve", lambda e: e.memset(run[:], 0.0), writes=[run])
                        str_ = ph.ring("st", 4, [128, 4], F32)
                        xnr = ph.ring("xn", 2, [128, D], F32)
                        tpr = ph.ring("tp", 2, [128, 4, 128], F32, psum=True)
                        hcr = ph.ring("hc", 2, [128, D], F32)
                        tmr = ph.ring("tm", 2, [128, D], F32)
                        a32r = ph.ring("a32", 2, [128, 8, 128], F32)
                        LG = ph.sb("LG", [128, 32, 8], F32)
                        plr = ph.ring("pl", 2, [128, 64], F32, psum=True)
                        for s_ in range(32):
                            c = 2 + s_
                            hc = hcr.next()
                            fw.dma("sp", hc[:], H[c * 128:(c + 1) * 128, :], reads=[Hres[c]], writes=[hc])
                            st4 = str_.next()
                            xn = xnr.next()
                            fw.op("act", lambda e: e.activation(out=xn[:], in_=hc[:], func=AF.Square, accum_out=st4[:, 0:1]), reads=[hc], writes=[xn, st4])
                            fw.op("act", lambda e: e.activation(out=st4[:, 1:2], in_=st4[:, 0:1], func=AF.Sqrt, scale=1.0 / D, bias=cst[:, 0:1]), reads=[st4, cst], writes=[st4])
                            fw.op("dve", lambda e: e.reciprocal(out=st4[:, 2:3], in_=st4[:, 1:2]), reads=[st4], writes=[st4])
                            fw.op("act", lambda e: e.activation(out=xn[:], in_=hc[:], func=AF.Copy, scale=st4[:, 2:3]), reads=[hc, st4], writes=[xn])
                            tm = tmr.next()
                            fw.op("pool", lambda e: e.tensor_tensor(out=tm[:], in0=xn[:], in1=g2bc[:], op=ALU.mult), reads=[xn, g2bc], writes=[tm])
                            fw.op("pool", lambda e, s_=s_: e.tensor_tensor(out=ATS[:, s_, :], in0=tm[:], in1=sh2bc[:], op=ALU.add), reads=[tm, sh2bc], writes=[ATS])
                            a32 = a32r.next()
                            for half in range(2):
                                tp = tpr.next()
                                for j in range(4):
                                    kc = half * 4 + j
                                    fw.op("pe", lambda e, kc=kc, j=j: e.transpose(out=tp[:, j, :], in_=xn[:, kc * 128:(kc + 1) * 128], identity=ident_f[:]),
                                          reads=[xn, ident_f], writes=[tp])
                                for j in range(4):
                                    kc = half * 4 + j
                                    fw.op("dve", lambda e, kc=kc, j=j: e.tensor_scalar(out=a32[:, kc, :], in0=tp[:, j, :], scalar1=g2T[:, kc, 0:1],
                                                                                      scalar2=modT[:, 24 + kc, 0:1], op0=ALU.mult, op1=ALU.add),
                                          reads=[tp, g2T, modT], writes=[a32])
                            pl = plr.next()
                            for kc in range(8):
                                fw.op("pe", lambda e, kc=kc: e.matmul(pl[:, 0:8], lhsT=a32[:, kc, :], rhs=Wr[:, kc, :], start=(kc == 0), stop=(kc == 7)),
                                      reads=[a32, Wr], writes=[pl])
                            fw.op("dve", lambda e, s_=s_: e.tensor_tensor(out=LG[:, s_, :], in0=pl[:, 0:8], in1=rbb[:], op=ALU.add), reads=[pl, rbb], writes=[LG])
                        m12 = ph.sb("m12", [128, 4, 32], F32)
                        L2 = ph.sb("L2", [128, 32, 8], F32)
                        SEL = ph.sb("SEL", [128, 32, 8], F32)
                        EX = ph.sb("EX", [128, 32, 8], F32)
                        CN0 = ph.sb("CN0", [128, 32, 8], F32)
                        CN1 = ph.sb("CN1", [128, 32, 8], F32)
                        CN2 = ph.sb("CN2", [128, 32, 8], F32)
                        prk = ph.ps("prk", [128, 32, 8], F32)
                        pcn = ph.ps("pcn", [128, 32, 8], F32)
                        fw.op("dve", lambda e: e.tensor_reduce(out=m12[:, 0, :], in_=LG[:], axis=AX.X, op=ALU.max), reads=[LG], writes=[m12])
                        fw.op("dve", lambda e: e.tensor_tensor(out=SEL1[:], in0=LG[:], in1=bc(m12[:, 0, :].unsqueeze(2), [128, 32, 8]), op=ALU.is_ge), reads=[LG, m12], writes=[SEL1])
                        fw.op("dve", lambda e: e.scalar_tensor_tensor(out=L2[:], in0=SEL1[:], scalar=-1e30, in1=LG[:], op0=ALU.mult, op1=ALU.add), reads=[SEL1, LG], writes=[L2])
                        fw.op("dve", lambda e: e.tensor_reduce(out=m12[:, 1, :], in_=L2[:], axis=AX.X, op=ALU.max), reads=[L2], writes=[m12])
                        fw.op("dve", lambda e: e.tensor_tensor(out=SEL[:], in0=LG[:], in1=bc(m12[:, 1, :].unsqueeze(2), [128, 32, 8]), op=ALU.is_ge), reads=[LG, m12], writes=[SEL])
                        fw.op("dve", lambda e: e.tensor_tensor(out=SEL2[:], in0=SEL[:], in1=SEL1[:], op=ALU.subtract), reads=[SEL, SEL1], writes=[SEL2])
                        fw.op("dve", lambda e: e.tensor_tensor(out=EX[:], in0=LG[:], in1=bc(m12[:, 0, :].unsqueeze(2), [128, 32, 8]), op=ALU.subtract), reads=[LG, m12], writes=[EX])
                        fw.op("act", lambda e: e.activation(out=EX[:], in_=EX[:], func=AF.Exp), reads=[EX], writes=[EX])
                        fw.op("dve", lambda e: e.tensor_tensor(out=EX[:], in0=EX[:], in1=SEL[:], op=ALU.mult), reads=[EX, SEL], writes=[EX])
                        fw.op("dve", lambda e: e.tensor_reduce(out=m12[:, 2, :], in_=EX[:], axis=AX.X, op=ALU.add), reads=[EX], writes=[m12])
                        fw.op("dve", lambda e: e.reciprocal(out=m12[:, 3, :], in_=m12[:, 2, :]), reads=[m12], writes=[m12])
                        fw.op("dve", lambda e: e.tensor_tensor(out=GWS[:], in0=EX[:], in1=bc(m12[:, 3, :].unsqueeze(2), [128, 32, 8]), op=ALU.mult), reads=[EX, m12], writes=[GWS])
                        for s_ in range(32):
                            fw.op("pe", lambda e, s_=s_: e.matmul(prk[:, s_, :], lhsT=Ust[:], rhs=SEL[:, s_, :], start=True, stop=True), reads=[Ust, SEL], writes=[prk])
                            fw.op("pe", lambda e, s_=s_: e.matmul(pcn[:, s_, :], lhsT=ones_f[:], rhs=SEL[:, s_, :], start=True, stop=True), reads=[ones_f, SEL], writes=[pcn])
                        fw.op("dve", lambda e: e.tensor_copy(out=RANK[:], in_=prk[:]), reads=[prk], writes=[RANK])
                        fw.op("dve", lambda e: e.tensor_copy(out=CN0[:], in_=pcn[:]), reads=[pcn], writes=[CN0])
                        srcT, dstT = CN0, CN1
                        sh = 1
                        while sh < 32:
                            fw.op("dve", lambda e, srcT=srcT, dstT=dstT, sh=sh: e.tensor_tensor(out=dstT[:, sh:32, :], in0=srcT[:, sh:32, :], in1=srcT[:, 0:32 - sh, :], op=ALU.add), reads=[srcT], writes=[dstT])
                            fw.op("dve", lambda e, srcT=srcT, dstT=dstT, sh=sh: e.tensor_copy(out=dstT[:, 0:sh, :], in_=srcT[:, 0:sh, :]), reads=[srcT], writes=[dstT])
                            srcT = dstT
                            dstT = CN2 if dstT is CN1 else CN1
                            sh *= 2
                        fw.op("dve", lambda e: e.tensor_tensor(out=BASE[:], in0=srcT[:], in1=CN0[:], op=ALU.subtract), reads=[srcT, CN0], writes=[BASE])
                        fw.op("dve", lambda e: e.tensor_copy(out=run[:], in_=srcT[:, 31, :]), reads=[srcT], writes=[run])
                        w8 = ph.sb("w8", [128, 8, 8], F32)
                        w24 = ph.sb("w24", [128, NTL, 8], F32)
                        v8 = ph.sb("v8", [128, 6, 8], F32)
                        bef = ph.sb("bef", [128, NTL], F32)
                        SLF = ph.sb("SLF", [128, 32, 8], F32)
                        w32 = ph.sb("w32", [128, 32, 8], F32)
                        s2f = ph.sb("s2f", [128, 2, 32], F32)
                        fw.op("dve", lambda e: e.tensor_tensor(out=w8[:], in0=bc(run[:].unsqueeze(2), [128, 8, 8]), in1=bc(grid[:, 0:8].unsqueeze(1), [128, 8, 8]), op=ALU.is_gt),
                              reads=[run, grid], writes=[w8])
                        fw.op("dve", lambda e: e.tensor_reduce(out=v8[:, 0, :], in_=w8[:], axis=AX.X, op=ALU.add), reads=[w8], writes=[v8])
                        fw.op("dve", lambda e: e.tensor_scalar(out=v8[:, 0, :], in0=v8[:, 0, :], scalar1=512.0, scalar2=None, op0=ALU.mult), reads=[v8], writes=[v8])
                        srcI, dstI = 0, 1
                        for sh in (1, 2, 4):
                            fw.op("dve", lambda e, srcI=srcI, dstI=dstI, sh=sh: e.tensor_tensor(out=v8[:, dstI, sh:8], in0=v8[:, srcI, sh:8], in1=v8[:, srcI, 0:8 - sh], op=ALU.add), reads=[v8], writes=[v8])
                            fw.op("dve", lambda e, srcI=srcI, dstI=dstI, sh=sh: e.tensor_copy(out=v8[:, dstI, 0:sh], in_=v8[:, srcI, 0:sh]), reads=[v8], writes=[v8])
                            srcI = dstI
                            dstI = 2 if dstI == 1 else 1
                        PE_ = srcI
                        fw.op("dve", lambda e: e.tensor_tensor(out=v8[:, 4, :], in0=v8[:, PE_, :], in1=v8[:, 0, :], op=ALU.subtract), reads=[v8], writes=[v8])
                        fw.op("dve", lambda e: e.tensor_tensor(out=w24[:], in0=bc(v8[:, PE_:PE_ + 1, :], [128, NTL, 8]), in1=bc(grid[:].unsqueeze(2), [128, NTL, 8]), op=ALU.is_le),
                              reads=[v8, grid], writes=[w24])
                        fw.op("dve", lambda e: e.tensor_reduce(out=bef[:], in_=w24[:], axis=AX.X, op=ALU.add), reads=[w24], writes=[bef])
                        fw.op("dve", lambda e: e.tensor_scalar(out=bef[:], in0=bef[:], scalar1=7.0, scalar2=None, op0=ALU.min), reads=[bef], writes=[bef])
                        fw.op("dve", lambda e: e.tensor_copy(out=BE_i[:], in_=bef[:]), reads=[bef], writes=[BE_i])
                        posw = ph.sb("posw", [128, 2], F32)
                        idf = ph.sb("idf", [128, NTL, 8], F32)
                        fw.dma("sp", posw[:], pos_d, writes=[posw])
                        fw.op("dve", lambda e: e.tensor_scalar(out=posw[:, 1:2], in0=posw[:, 0:1], scalar1=-1.0, scalar2=None, op0=ALU.add), reads=[posw], writes=[posw])
                        fw.op("dve", lambda e: e.tensor_scalar(out=bef[:], in0=bef[:], scalar1=896.0, scalar2=posw[:, 1:2], op0=ALU.mult, op1=ALU.add), reads=[bef, posw], writes=[bef])
                        for b_ in range(7):
                            fw.op("dve", lambda e, b_=b_: e.tensor_scalar(out=idf[:, :, b_], in0=bef[:], scalar1=float(128 * b_), scalar2=None, op0=ALU.add), reads=[bef], writes=[idf])
                        fw.op("dve", lambda e: e.tensor_copy(out=IDXW[:, :, 0:7], in_=idf[:, :, 0:7]), reads=[idf], writes=[IDXW])
                        fw.op("dve", lambda e: e.tensor_tensor(out=SLF[:], in0=RANK[:], in1=BASE[:], op=ALU.add), reads=[RANK, BASE], writes=[SLF])
                        fw.op("dve", lambda e: e.tensor_tensor(out=SLF[:], in0=SLF[:], in1=bc(v8[:, 4:5, :], [128, 32, 8]), op=ALU.add), reads=[SLF, v8], writes=[SLF])
                        for k, SELk in enumerate((SEL1, SEL2)):
                            fw.op("dve", lambda e, SELk=SELk: e.tensor_tensor(out=w32[:], in0=SLF[:], in1=SELk[:], op=ALU.mult), reads=[SLF, SELk], writes=[w32])
                            fw.op("dve", lambda e, k=k: e.tensor_reduce(out=s2f[:, k, :], in_=w32[:], axis=AX.X, op=ALU.add), reads=[w32], writes=[s2f])
                            fw.op("dve", lambda e, SELk=SELk: e.tensor_tensor(out=w32[:], in0=GWS[:], in1=SELk[:], op=ALU.mult), reads=[GWS, SELk], writes=[w32])
                            fw.op("dve", lambda e, k=k: e.tensor_reduce(out=GWK[:, k, :], in_=w32[:], axis=AX.X, op=ALU.add), reads=[w32], writes=[GWK])
                        fw.op("dve", lambda e: e.tensor_scalar(out=s2f[:], in0=s2f[:], scalar1=float(NSLOT - 1), scalar2=0.0, op0=ALU.min, op1=ALU.max), reads=[s2f], writes=[s2f])
                        fw.op("dve", lambda e: e.tensor_copy(out=SLOT_i[:], in_=s2f[:]), reads=[s2f], writes=[SLOT_i])
                        for s_ in range(32):
                            for k in range(2):
                                fw.idma(XS[:, :], ATS[:, s_, :], SLOT_i[:, k, s_:s_ + 1], True, NSLOT - 1, reads=[ATS, SLOT_i], writes=[XSres], holder=ATS, kind="o",
                                        merge=not (s_ == 0 and k == 0))

                    with Phase(nc, fw, f"MC{l}") as ph:
                        xsr = ph.ring("xs", 2, [128, 4, D], BF16)
                        fTr = ph.ring("fT", 2, [128, 8, 512], BF16)
                        tpb = ph.ring("tpb", 2, [128, 8, 128], BF16, psum=True)
                        acc = ph.sb("acc", [128, 4, D], F32)
                        WGr = ph.ring("WG", 3, [128, 8, 512], BF16)
                        WUr = ph.ring("WU", 3, [128, 8, 512], BF16)
                        WDr = ph.ring("WD", 3, [128, 4, D], BF16)
                        pgu = ph.ring("pgu", 4, [128, 512], F32, psum=True)
                        pyr = ph.ring("py", 2, [128, 512], F32, psum=True)
                        sgr = ph.ring("sg", 2, [128, 512], F32)
                        gTr = ph.ring("gT", 2, [128, 4, 512], BF16)

                        def mprologue(j):
                            xs = xsr.next()
                            fw.dma("sp", xs[:], XS[j * 512:(j + 1) * 512, :].rearrange("(s p) d -> p s d", p=128), reads=[XSres], writes=[xs])
                            fT = fTr.next()
                            for sub in range(4):
                                tp = tpb.next()
                                for kc in range(8):
                                    fw.op("pe", lambda e, kc=kc, sub=sub: e.transpose(out=tp[:, kc, :], in_=xs[:, sub, kc * 128:(kc + 1) * 128], identity=ident_b[:]),
                                          reads=[xs, ident_b], writes=[tp])
                                eng = "act" if sub % 2 == 0 else "dve"
                                if eng == "act":
                                    fw.op("act", lambda e, sub=sub, tp=tp: e.activation(out=fT[:, :, sub * 128:(sub + 1) * 128], in_=tp[:], func=AF.Copy), reads=[tp], writes=[fT])
                                else:
                                    fw.op("dve", lambda e, sub=sub, tp=tp: e.tensor_copy(out=fT[:, :, sub * 128:(sub + 1) * 128], in_=tp[:]), reads=[tp], writes=[fT])
                            return fT, None

                        cfgm = FF_CFG[1]
                        nxt_pro = mprologue(0)
                        for j in range(NTL):
                            fT, ev = nxt_pro
                            T = 512

                            def gate_up(b):
                                WG, WU, WDt = WGr.next(), WUr.next(), WDr.next()
                                for (Wt_, key_) in ((WG, "WG"), (WU, "WU"), (WDt, "WD")):
                                    fw.idma(Wt_[:].rearrange("p k n -> p (k n)"), cfgm[key_].rearrange("e b p k n -> (e b p) (k n)"), IDXW[:, j, b:b + 1], False, None,
                                            reads=[cfgm["res"], IDXW], writes=[Wt_], holder=Wt_, kind="i")
                                gT = gTr.next()
                                for jj in range(4):
                                    pg_, pu_ = pgu.next(), pgu.next()
                                    for (pp, W_) in ((pg_, WG), (pu_, WU)):
                                        for kc in range(8):
                                            fw.op("pe", lambda e, kc=kc, jj=jj, pp=pp, W_=W_: e.matmul(pp[:, 0:T], lhsT=W_[:, kc, jj * 128:(jj + 1) * 128], rhs=fT[:, kc, 0:T],
                                                                                                     start=(kc == 0), stop=(kc == 7)), reads=[W_, fT], writes=[pp])
                                    sg = sgr.next()
                                    fw.op("act", lambda e, pg_=pg_, sg=sg: e.activation(out=sg[:, 0:T], in_=pg_[:, 0:T], func=AF.Silu), reads=[pg_], writes=[sg])
                                    fw.op("dve", lambda e, pu_=pu_, sg=sg, jj=jj: e.tensor_tensor(out=gT[:, jj, 0:T], in0=pu_[:, 0:T], in1=sg[:, 0:T], op=ALU.mult), reads=[pu_, sg], writes=[gT])
                                return gT, WDt

                            def down(gT, WDt, first):
                                for sub in range(4):
                                    for n in range(2):
                                        py = pyr.next()
                                        for jj in range(4):
                                            fw.op("pe", lambda e, jj=jj, sub=sub, n=n, py=py: e.matmul(py[:], lhsT=gT[:, jj, sub * 128:(sub + 1) * 128], rhs=WDt[:, jj, n * 512:(n + 1) * 512],
                                                                                                     start=(jj == 0), stop=(jj == 3)), reads=[gT, WDt], writes=[py])
                                        av = acc[:, sub, n * 512:(n + 1) * 512]
                                        if first:
                                            fw.op("dve", lambda e, py=py, av=av: e.tensor_copy(out=av, in_=py[:]), reads=[py], writes=[acc])
                                        else:
                                            fw.op("dve", lambda e, py=py, av=av: e.tensor_tensor(out=av, in0=py[:], in1=av, op=ALU.add), reads=[py, acc], writes=[acc])

                            prev = None
                            for b in range(7):
                                gT, WDt = gate_up(b)
                                if prev is not None:
                                    down(prev[0], prev[1], prev[2])
                                prev = (gT, WDt, b == 0)
                                if b == 2 and j + 1 < NTL:
                                    nxt_pro = mprologue(j + 1)
                            down(prev[0], prev[1], prev[2])
                            fw.dma("pool", YS[j * 512:(j + 1) * 512, :].rearrange("(s p) d -> p s d", p=128), acc[:], reads=[acc], writes=[YSres], holder=acc, kind="o", merge=True)

                    with Phase(nc, fw, f"MO{l}") as ph:
                        y1r = ph.ring("y1", 4, [128, D], F32)
                        y2r = ph.ring("y2", 4, [128, D], F32)
                        hcr = ph.ring("hc", 4, [128, D], F32)
                        loaded = {}

                        def mo_load(s_):
                            c = 2 + s_
                            y1, y2, hc = y1r.next(), y2r.next(), hcr.next()
                            fw.dma("sp", hc[:], H[c * 128:(c + 1) * 128, :], reads=[Hres[c]], writes=[hc])
                            fw.idma(y1[:, :], YS[:, :], SLOT_i[:, 0, s_:s_ + 1], False, NSLOT - 1, reads=[YSres, SLOT_i], writes=[y1], holder=y1, kind="i")
                            fw.idma(y2[:, :], YS[:, :], SLOT_i[:, 1, s_:s_ + 1], False, NSLOT - 1, reads=[YSres, SLOT_i], writes=[y2], holder=y2, kind="i")
                            loaded[s_] = (y1, y2, hc)

                        def mo_comp(s_):
                            y1, y2, hc = loaded.pop(s_)
                            fw.op("dve", lambda e: e.tensor_scalar(out=y1[:], in0=y1[:], scalar1=GWK[:, 0, s_:s_ + 1], scalar2=None, op0=ALU.mult), reads=[y1, GWK], writes=[y1])
                            fw.op("dve", lambda e: e.scalar_tensor_tensor(out=y1[:], in0=y2[:], scalar=GWK[:, 1, s_:s_ + 1], in1=y1[:], op0=ALU.mult, op1=ALU.add),
                                  reads=[y1, y2, GWK], writes=[y1])
                            fw.op("dve", lambda e: e.tensor_tensor(out=y1[:], in0=y1[:], in1=gbc[:], op=ALU.mult), reads=[y1, gbc], writes=[y1])
                            fw.op("dve", lambda e: e.tensor_tensor(out=y1[:], in0=y1[:], in1=hc[:], op=ALU.add), reads=[y1, hc], writes=[y1])
                            fw.dma("sp", out[s_ * 128:(s_ + 1) * 128, :], y1[:], reads=[y1], writes=[OUTres], holder=y1, kind="o", merge=True)
                        LOOK = 2
                        for s_ in range(32 + LOOK):
                            if s_ < 32:
                                mo_load(s_)
                            if s_ >= LOOK:
                                mo_comp(s_ - LOOK)
                if dbg == f"F{l}":
                    break
                continue
            with Phase(nc, fw, f"F{l}") as ph:
                gbc = [ph.sb(f"gbc{t}", [128, D], F32) for t in range(2)]
                for t in range(2):
                    fw.dma("sp", gbc[t][:], MD[t, 5 * D:6 * D].partition_broadcast(128), reads=[MDres], writes=[gbc[t]])
                if moe:
                    Wr = ph.sb("Wr", [128, 8, 8], F32)
                    rbb = ph.sb("rbb", [128, 8], F32)
                    fw.dma("sp", Wr[:], router_w.rearrange("(kc p) e -> p kc e", p=128), writes=[Wr])
                    fw.dma("sp", rbb[:], router_b[0].partition_broadcast(128), writes=[rbb])
                rings = dict(st=ph.ring("st", 4, [128, 4], F32), xn=ph.ring("xn", 2, [128, D], F32),
                             tp=ph.ring("tp", 2, [128, 4, 128], F32, psum=True))
                hTr = ph.ring("hT", 2, [128, 4, D], F32)
                acc = ph.sb("acc", [128, 4, D], F32)
                fTr = ph.ring("fT", 2, [128, 8, 512], BF16)
                a32 = ph.sb("a32", [128, 8, 128], F32)
                gwr = ph.ring("gw", 2, [128, 4, 8], F32)
                lgt = ph.ring("lgt", 2, [128, 32], F32)
                WGr = ph.ring("WG", 3, [128, 8, cfg["bw"]], BF16)
                WUr = ph.ring("WU", 3, [128, 8, cfg["bw"]], BF16)
                WDr = ph.ring("WD", 3, [128, nffc, D], BF16)
                pgu = ph.ring("pgu", 4, [128, 512], F32, psum=True)
                pyr = ph.ring("py", 2, [128, 512], F32, psum=True)
                sgr = ph.ring("sg", 2, [128, 512], F32)
                gTr = ph.ring("gT", 2, [128, nffc, 512], BF16)
                tiles = ([] if last else [(0, 2)]) + [(2 + 4 * i, 4) for i in range(8)]

                def prologue(c0, nsub):
                    t = 1 if c0 < 2 else 0
                    hT, fT, gw = hTr.next(), fTr.next(), gwr.next()
                    for s in range(nsub):
                        c = c0 + s
                        fw.dma("sp", hT[:, s, :], H[c * 128:(c + 1) * 128, :], reads=[Hres[c]], writes=[hT], merge=(s > 0))
                    for s in range(nsub):
                        st4 = rings["st"].next()
                        xn = rings["xn"].next()
                        fw.op("act", lambda e, s=s: e.activation(out=xn[:], in_=hT[:, s, :], func=AF.Square, accum_out=st4[:, 0:1]), reads=[hT], writes=[xn, st4])
                        fw.op("act", lambda e: e.activation(out=st4[:, 1:2], in_=st4[:, 0:1], func=AF.Sqrt, scale=1.0 / D, bias=cst[:, 0:1]), reads=[st4, cst], writes=[st4])
                        fw.op("dve", lambda e: e.reciprocal(out=st4[:, 2:3], in_=st4[:, 1:2]), reads=[st4], writes=[st4])
                        fw.op("act", lambda e, s=s: e.activation(out=xn[:], in_=hT[:, s, :], func=AF.Copy, scale=st4[:, 2:3]), reads=[hT, st4], writes=[xn])
                        for half in range(2):
                            tp = rings["tp"].next()
                            for j in range(4):
                                kc = half * 4 + j
                                fw.op("pe", lambda e, kc=kc, j=j: e.transpose(out=tp[:, j, :], in_=xn[:, kc * 128:(kc + 1) * 128], identity=ident_f[:]),
                                      reads=[xn, ident_f], writes=[tp])
                            for j in range(4):
                                kc = half * 4 + j
                                fw.op("dve", lambda e, kc=kc, j=j, s=s: e.tensor_scalar(out=fT[:, kc, s * 128:(s + 1) * 128], in0=tp[:, j, :], scalar1=g2T[:, kc, t:t + 1],
                                                                                       scalar2=modT[:, 24 + kc, t:t + 1], op0=ALU.mult, op1=ALU.add),
                                      reads=[tp, g2T, modT], writes=[fT])
                                if moe:
                                    fw.op("dve", lambda e, kc=kc, j=j: e.tensor_scalar(out=a32[:, kc, :], in0=tp[:, j, :], scalar1=g2T[:, kc, t:t + 1],
                                                                                      scalar2=modT[:, 24 + kc, t:t + 1], op0=ALU.mult, op1=ALU.add),
                                          reads=[tp, g2T, modT], writes=[a32])
                        if moe:
                            pl = rings["tp"].next()
                            plv = pl[:].rearrange("p a b -> p (a b)")
                            for kc in range(8):
                                fw.op("pe", lambda e, kc=kc: e.matmul(plv[:, 0:8], lhsT=a32[:, kc, :], rhs=Wr[:, kc, :], start=(kc == 0), stop=(kc == 7)),
                                      reads=[a32, Wr], writes=[pl])
                            lg_ = lgt.next()
                            L = lg_[:, 0:8]
                            fw.op("dve", lambda e: e.tensor_tensor(out=L, in0=plv[:, 0:8], in1=rbb[:], op=ALU.add), reads=[pl, rbb], writes=[lg_])
                            fw.op("dve", lambda e: e.tensor_reduce(out=lg_[:, 24:25], in_=L, axis=AX.X, op=ALU.max), reads=[lg_], writes=[lg_])
                            fw.op("dve", lambda e: e.tensor_scalar(out=lg_[:, 8:16], in0=L, scalar1=lg_[:, 24:25], scalar2=-1e30, op0=ALU.is_ge, op1=ALU.mult), reads=[lg_], writes=[lg_])
                            fw.op("dve", lambda e: e.tensor_tensor(out=lg_[:, 8:16], in0=lg_[:, 8:16], in1=L, op=ALU.add), reads=[lg_], writes=[lg_])
                            fw.op("dve", lambda e: e.tensor_reduce(out=lg_[:, 25:26], in_=lg_[:, 8:16], axis=AX.X, op=ALU.max), reads=[lg_], writes=[lg_])
                            fw.op("dve", lambda e: e.tensor_scalar(out=lg_[:, 8:16], in0=L, scalar1=lg_[:, 25:26], scalar2=None, op0=ALU.is_ge), reads=[lg_], writes=[lg_])
                            fw.op("dve", lambda e: e.tensor_scalar(out=lg_[:, 16:24], in0=L, scalar1=lg_[:, 24:25], scalar2=None, op0=ALU.subtract), reads=[lg_], writes=[lg_])
                            fw.op("act", lambda e: e.activation(out=lg_[:, 16:24], in_=lg_[:, 16:24], func=AF.Exp), reads=[lg_], writes=[lg_])
                            fw.op("dve", lambda e: e.tensor_tensor(out=lg_[:, 16:24], in0=lg_[:, 16:24], in1=lg_[:, 8:16], op=ALU.mult), reads=[lg_], writes=[lg_])
                            fw.op("dve", lambda e: e.tensor_reduce(out=lg_[:, 26:27], in_=lg_[:, 16:24], axis=AX.X, op=ALU.add), reads=[lg_], writes=[lg_])
                            fw.op("dve", lambda e: e.reciprocal(out=lg_[:, 27:28], in_=lg_[:, 26:27]), reads=[lg_], writes=[lg_])
                            fw.op("dve", lambda e, s=s: e.tensor_scalar(out=gw[:, s, :], in0=lg_[:, 16:24], scalar1=lg_[:, 27:28], scalar2=None, op0=ALU.mult), reads=[lg_], writes=[gw])
                    return hT, fT, gw

                blocks = [(ex, b) for ex in range(nexp) for b in range(nblk)]
                nxt_pro = prologue(*tiles[0])
                for ti, (c0, nsub) in enumerate(tiles):
                    t = 1 if c0 < 2 else 0
                    T = nsub * 128
                    hT, fT, gw = nxt_pro

                    def gate_up(ex, b):
                        WG, WU, WDt = WGr.next(), WUr.next(), WDr.next()
                        fw.dma("sp", WG[:], cfg["WG"][ex, b], reads=[cfg["res"]], writes=[WG])
                        fw.dma("sp", WU[:], cfg["WU"][ex, b], reads=[cfg["res"]], writes=[WU])
                        fw.dma("sp", WDt[:], cfg["WD"][ex, b], reads=[cfg["res"]], writes=[WDt])
                        gT = gTr.next()
                        for j in range(nffc):
                            pg_, pu_ = pgu.next(), pgu.next()
                            for (pp, W_) in ((pg_, WG), (pu_, WU)):
                                for kc in range(8):
                                    fw.op("pe", lambda e, kc=kc, j=j, pp=pp, W_=W_: e.matmul(pp[:, 0:T], lhsT=W_[:, kc, j * 128:(j + 1) * 128], rhs=fT[:, kc, 0:T],
                                                                                           start=(kc == 0), stop=(kc == 7)), reads=[W_, fT], writes=[pp])
                            sg = sgr.next()
                            fw.op("act", lambda e, pg_=pg_, sg=sg: e.activation(out=sg[:, 0:T], in_=pg_[:, 0:T], func=AF.Silu), reads=[pg_], writes=[sg])
                            fw.op("dve", lambda e, pu_=pu_, sg=sg, j=j: e.tensor_tensor(out=gT[:, j, 0:T], in0=pu_[:, 0:T], in1=sg[:, 0:T], op=ALU.mult), reads=[pu_, sg], writes=[gT])
                        return gT, WDt

                    def down(ex, gT, WDt, first):
                        for s in range(nsub):
                            for n in range(2):
                                py = pyr.next()
                                for j in range(nffc):
                                    fw.op("pe", lambda e, j=j, s=s, n=n, py=py: e.matmul(py[:], lhsT=gT[:, j, s * 128:(s + 1) * 128], rhs=WDt[:, j, n * 512:(n + 1) * 512],
                                                                                       start=(j == 0), stop=(j == nffc - 1)), reads=[gT, WDt], writes=[py])
                                av = acc[:, s, n * 512:(n + 1) * 512]
                                if moe:
                                    if first:
                                        fw.op("dve", lambda e, py=py, av=av, s=s: e.tensor_scalar(out=av, in0=py[:], scalar1=gw[:, s, ex:ex + 1], scalar2=None, op0=ALU.mult),
                                              reads=[py, gw], writes=[acc])
                                    else:
                                        fw.op("dve", lambda e, py=py, av=av, s=s: e.scalar_tensor_tensor(out=av, in0=py[:], scalar=gw[:, s, ex:ex + 1], in1=av,
                                                                                                       op0=ALU.mult, op1=ALU.add), reads=[py, gw, acc], writes=[acc])
                                else:
                                    if first:
                                        fw.op("dve", lambda e, py=py, av=av: e.tensor_copy(out=av, in_=py[:]), reads=[py], writes=[acc])
                                    else:
                                        fw.op("dve", lambda e, py=py, av=av: e.tensor_tensor(out=av, in0=py[:], in1=av, op=ALU.add), reads=[py, acc], writes=[acc])

                    prev = None
                    for bi, (ex, b) in enumerate(blocks):
                        gT, WDt = gate_up(ex, b)
                        if prev is not None:
                            down(prev[0], prev[1], prev[2], prev[3])
                        prev = (ex, gT, WDt, bi == 0)
                        if bi == min(2, len(blocks) - 1) and ti + 1 < len(tiles):
                            nxt_pro = prologue(*tiles[ti + 1])
                    down(prev[0], prev[1], prev[2], prev[3])
                    for s in range(nsub):
                        c = c0 + s
                        fw.op("pool", lambda e, s=s: e.tensor_tensor(out=acc[:, s, :], in0=acc[:, s, :], in1=gbc[t][:], op=ALU.mult), reads=[acc, gbc[t]], writes=[acc])
                        fw.op("pool", lambda e, s=s: e.tensor_tensor(out=acc[:, s, :], in0=acc[:, s, :], in1=hT[:, s, :], op=ALU.add), reads=[acc, hT], writes=[acc])
                        if last:
                            fw.dma("pool", out[(c - 2) * 128:(c - 1) * 128, :], acc[:, s, :], reads=[acc], writes=[OUTres], holder=acc, kind="o", merge=True)
                        else:
                            fw.dma("pool", H[c * 128:(c + 1) * 128, :], acc[:, s, :], reads=[acc], writes=[Hres[c]], holder=acc, kind="o")
            if dbg == f"F{l}":
                break
        for cfg in FF_CFG:
            r_ = cfg["res"]
            if r_.isem is not None and r_.icnt:
                for k_ in fw.engs:
                    fw._wait(k_, [(r_.isem, r_.icnt)])
        fw.barrier(release=False)
    return nc


_PERM = None


def _w_in_perm():
    q = []
    for i in range(4):
        q += list(range(i * 64, (i + 1) * 64)) + list(range((4 + i) * 64, (5 + i) * 64))
    ak, av, rq, rk, rv, rg, mq, mk, mv, mo, mg = 512, 640, 768, 1024, 1280, 1536, 1792, 2048, 2304, 2560, 2816
    r = lambda a, n: list(range(a, a + n))
    perm = q + r(ak, 128) + r(av, 128) + r(rv, 256) + r(rq, 256) + r(rk, 256) + r(rg, 256) + r(mv, 256) + r(mo, 256) + r(mq, 256) + r(mk, 256) + r(mg, 16)
    assert len(perm) == NIN and len(set(perm)) == NIN
    return np.array(perm)


def _consts():
    ident = np.eye(128, dtype=np.float32)
    s = np.arange(128)[:, None]
    l_ = np.arange(128)[None, :]
    maskF = (s <= l_).astype(np.float32)
    maskB = (s >= l_).astype(np.float32)
    n_freq = 16
    inv_freq = (10000.0 ** (-np.arange(n_freq, dtype=np.float32) / n_freq)).astype(np.float32)
    tok = np.arange(4096)
    row = (tok // 64).astype(np.float32)
    col = (tok % 64).astype(np.float32)
    ang = np.concatenate([row[:, None] * inv_freq, col[:, None] * inv_freq], -1).astype(np.float32)
    cos = np.cos(ang).astype(np.float32).reshape(32, 128, 32).transpose(1, 0, 2)
    sin = np.sin(ang).astype(np.float32).reshape(32, 128, 32).transpose(1, 0, 2)
    pos = np.stack([np.arange(128) + 1.0, 128.0 - np.arange(128)], 1).astype(np.float32)
    grid = np.broadcast_to((np.arange(24, dtype=np.float32) * 512.0)[None, :], (128, 24)).copy()
    return dict(grid512=grid, ident=ident, maskF=maskF, maskB=maskB, cos=np.ascontiguousarray(cos), sin=np.ascontiguousarray(sin), pos=pos)


def make_in_maps(x, c, ctx, c_ctx, mod_w, mod_b, norm1_w, norm2_w, w_in, attn_qn_w, attn_kn_w, ret_decay,
                 ret_norm_w, mlstm_conv_w, mlstm_gate_b, mlstm_norm_w, w_out, ffn_w_gate, ffn_w_up, ffn_w_down,
                 router_w, router_b, moe_w_gate, moe_w_up, moe_w_down, moe_nexp=8):
    f = lambda a: np.ascontiguousarray(np.asarray(a, dtype=np.float32))
    perm = _w_in_perm()
    shared = dict(
        mod_w=f(mod_w),
        modbT=f(np.asarray(mod_b).reshape(DEPTH, 48, 128).transpose(0, 2, 1)),
        n1T=f(np.asarray(norm1_w).reshape(DEPTH, 8, 128).transpose(0, 2, 1)),
        n2T=f(np.asarray(norm2_w).reshape(DEPTH, 8, 128).transpose(0, 2, 1)),
        n2row=f(norm2_w),
        w_in=f(np.asarray(w_in)[:, :, perm]),
        qn_w=f(attn_qn_w), kn_w=f(attn_kn_w),
        ret_decay=f(np.asarray(ret_decay).reshape(DEPTH, 8)),
        ret_norm_w=f(ret_norm_w), conv_w=f(mlstm_conv_w),
        gate_b=f(np.asarray(mlstm_gate_b).reshape(DEPTH, 16)),
        mlstm_norm_w=f(mlstm_norm_w), w_out=f(w_out),
        ffn_wg=f(ffn_w_gate), ffn_wu=f(ffn_w_up), ffn_wd=f(ffn_w_down),
        router_w=f(np.asarray(router_w)[0]), router_b=f(np.asarray(router_b)[0:1]),
        moe_wg=f(np.asarray(moe_w_gate)[0][:moe_nexp]), moe_wu=f(np.asarray(moe_w_up)[0][:moe_nexp]), moe_wd=f(np.asarray(moe_w_down)[0][:moe_nexp]),
    )
    shared.update(_consts())
    x = np.asarray(x); ctx = np.asarray(ctx); c = np.asarray(c); c_ctx = np.asarray(c_ctx)
    maps = []
    for b in range(8):
        m = dict(shared)
        m["xin"] = f(np.concatenate([ctx[b], x[b]], axis=0))
        m["cT"] = f(np.stack([c[b].reshape(8, 128).T, c_ctx.reshape(8, 128).T], axis=-1))
        maps.append(m)
    return maps


def kernel(**inputs):
    nc = build_program()
    maps = make_in_maps(**inputs)
    res = run_bass_kernel_spmd(nc, maps, core_ids=list(range(8)))
    return np.stack([np.asarray(r["out"], dtype=np.float32) for r in res.results], axis=0)
```

```python
import contextlib
import numpy as np
import concourse.bass as bass
import concourse.mybir as mybir
from concourse.bass_utils import run_bass_kernel_spmd

AF = mybir.ActivationFunctionType
ALU = mybir.AluOpType
AX = mybir.AxisListType
F32 = mybir.dt.float32
BF16 = mybir.dt.bfloat16

NCH = 34
NTOK = NCH * 128
D = 1024
NIN = 2832
EPS = 1e-6
DEPTH = 2
DENSE_MOE = False
FWD_ORDER = list(range(NCH))
BWD_ORDER = [1, 0] + list(range(NCH - 1, 1, -1))
PQ, PK, PV, PRV, PRQ, PRK, PRG, PMV, PMO, PMQ, PMK = 0, 512, 640, 768, 1024, 1280, 1536, 1792, 2048, 2304, 2560
PCOLS = 2816


class Res:
    __slots__ = ("name", "w", "r", "isem", "osem", "icnt", "ocnt", "persist")

    def __init__(self, name):
        self.name = name
        self.w = None
        self.r = {}
        self.isem = None
        self.osem = None
        self.icnt = 0
        self.ocnt = 0
        self.persist = False


class Tile:
    def __init__(self, h, name):
        self.h = h
        self.r = Res(name)

    def __getitem__(self, k):
        return self.h[k]


class FW:
    ROT = 30000

    def __init__(self, nc):
        self.nc = nc
        self.engs = {"pe": nc.tensor, "act": nc.scalar, "dve": nc.vector,
                     "pool": nc.gpsimd, "sp": nc.sync}
        self.csem = {}
        self.ccnt = {}
        self.known = {k: {} for k in self.engs}
        self.nsem = 0
        self.allsems = []
        self.sempool = []
        self.sempool_sw = []
        self.semq = {}
        self.dma_live = []
        self.old_counters = []
        for k in self.engs:
            self._newc(k)

    def sem(self, name):
        self.nsem += 1
        s = self.nc.alloc_semaphore(name=f"{name}_{self.nsem}")
        self.allsems.append(s)
        return s

    def _newc(self, k):
        if k in self.csem and self.ccnt[k] > 0:
            self.old_counters.append((self.csem[k], self.ccnt[k]))
        self.csem[k] = self.sem("c" + k)
        self.ccnt[k] = 0

    def _wait(self, eng, evs, noself=False):
        need = {}
        kn = self.known[eng]
        for ev in evs:
            if ev is None:
                continue
            s, v = ev
            if noself and s is self.csem[eng]:
                continue
            sid = id(s)
            if kn.get(sid, 0) >= v:
                continue
            if sid not in need or need[sid][1] < v:
                need[sid] = (s, v)
        for sid, (s, v) in need.items():
            self.engs[eng].wait_ge(s, v)
            kn[sid] = v

    @staticmethod
    def _deps(reads, writes, merge=False):
        evs = []
        for r in reads:
            evs.append(r.w)
        for w in writes:
            if not merge:
                evs.append(w.w)
            evs.extend(w.r.values())
        return evs

    @staticmethod
    def _commit(ev, reads, writes, merge=False):
        sid = id(ev[0])
        for r in reads:
            r.r[sid] = ev
        for w in writes:
            w.w = ev
            if not merge:
                w.r = {}

    def op(self, eng, fn, reads=(), writes=(), noself=None):
        reads = [x.r if isinstance(x, Tile) else x for x in reads]
        writes = [x.r if isinstance(x, Tile) else x for x in writes]
        if noself is None:
            noself = (eng == "pe")
        self._wait(eng, self._deps(reads, writes), noself=noself)
        ins = fn(self.engs[eng])
        self.ccnt[eng] += 1
        ins.then_inc(self.csem[eng], 1)
        ev = (self.csem[eng], self.ccnt[eng])
        self._commit(ev, reads, writes)
        if self.ccnt[eng] >= self.ROT:
            self._newc(eng)
        return ev

    def _getsem(self, q):
        pool_ = self.sempool_sw if q == "pool" else self.sempool
        if pool_:
            return pool_.pop()
        return (self.sem("dsw" if q == "pool" else "d"), 0)

    def dma(self, q, out, in_, reads=(), writes=(), holder=None, kind=None, merge=False, **kw):
        reads = [x.r if isinstance(x, Tile) else x for x in reads]
        writes = [x.r if isinstance(x, Tile) else x for x in writes]
        self._wait(q, self._deps(reads, writes, merge=merge))
        ins = self.engs[q].dma_start(out=out, in_=in_, **kw)
        if holder is None:
            holder, kind = (reads[0], "o") if kind == "o" else (writes[0], "i")
        elif isinstance(holder, Tile):
            holder = holder.r
        if kind == "i":
            if holder.isem is None:
                holder.isem, holder.icnt = self._getsem(q)
                self.semq[id(holder.isem)] = q == "pool"
                if not holder.persist:
                    self.dma_live.append(holder)
            assert self.semq[id(holder.isem)] == (q == "pool"), holder.name
            holder.icnt += 16
            ins.then_inc(holder.isem, 16)
            ev = (holder.isem, holder.icnt)
        else:
            if holder.osem is None:
                holder.osem, holder.ocnt = self._getsem(q)
                self.semq[id(holder.osem)] = q == "pool"
                self.dma_live.append(holder)
            assert self.semq[id(holder.osem)] == (q == "pool"), holder.name
            holder.ocnt += 16
            ins.then_inc(holder.osem, 16)
            ev = (holder.osem, holder.ocnt)
        self._commit(ev, reads, writes, merge=merge)
        return ev

    def idma(self, out, in_, idx_ap, scatter, bound, reads=(), writes=(), holder=None, kind="i", merge=False):
        q = "pool"
        reads = [x.r if isinstance(x, Tile) else x for x in reads]
        writes = [x.r if isinstance(x, Tile) else x for x in writes]
        self._wait(q, self._deps(reads, writes, merge=merge))
        off = bass.IndirectOffsetOnAxis(ap=idx_ap, axis=0)
        ins = self.nc.gpsimd.indirect_dma_start(out=out, out_offset=(off if scatter else None), in_=in_, in_offset=(None if scatter else off))
        if isinstance(holder, Tile):
            holder = holder.r
        if kind == "i":
            if holder.isem is None:
                holder.isem, holder.icnt = self._getsem(q)
                self.semq[id(holder.isem)] = True
                if not holder.persist:
                    self.dma_live.append(holder)
            holder.icnt += 16
            ins.then_inc(holder.isem, 16)
            ev = (holder.isem, holder.icnt)
        else:
            if holder.osem is None:
                holder.osem, holder.ocnt = self._getsem(q)
                self.semq[id(holder.osem)] = True
                self.dma_live.append(holder)
            holder.ocnt += 16
            ins.then_inc(holder.osem, 16)
            ev = (holder.osem, holder.ocnt)
        self._commit(ev, reads, writes, merge=merge)
        return ev

    def barrier(self, release=True):
        evs = [(self.csem[k], self.ccnt[k]) for k in self.engs if self.ccnt[k] > 0]
        evs += self.old_counters
        for h in self.dma_live:
            if h.isem is not None:
                evs.append((h.isem, h.icnt))
            if h.osem is not None:
                evs.append((h.osem, h.ocnt))
        for k in self.engs:
            self._wait(k, evs, noself=True)
        if release:
            for h in self.dma_live:
                if h.isem is not None:
                    (self.sempool_sw if self.semq[id(h.isem)] else self.sempool).append((h.isem, h.icnt))
                    h.isem = None
                if h.osem is not None:
                    (self.sempool_sw if self.semq[id(h.osem)] else self.sempool).append((h.osem, h.ocnt))
                    h.osem = None
            self.dma_live = []


class Ring:
    def __init__(self, tiles):
        self.tiles = tiles
        self.i = 0

    def next(self):
        t = self.tiles[self.i % len(self.tiles)]
        self.i += 1
        return t


class Phase:
    cnt = 0

    def __init__(self, nc, fw, name):
        self.nc, self.fw, self.name = nc, fw, name

    def __enter__(self):
        self.st = contextlib.ExitStack()
        return self

    def __exit__(self, *a):
        self.fw.barrier()
        self.st.close()
        return False

    def sb(self, name, shape, dt):
        Phase.cnt += 1
        nm = f"{self.name}_{name}_{Phase.cnt}"
        return Tile(self.st.enter_context(self.nc.sbuf_tensor(nm, shape, dt)), nm)

    def ps(self, name, shape, dt=F32):
        Phase.cnt += 1
        nm = f"{self.name}_{name}_{Phase.cnt}"
        return Tile(self.st.enter_context(self.nc.psum_tensor(nm, shape, dt)), nm)

    def ring(self, name, n, shape, dt, psum=False):
        f = self.ps if psum else self.sb
        return Ring([f(f"{name}{i}", shape, dt) for i in range(n)])


def bc(ap, shape):
    return ap.to_broadcast(list(shape))


def build_program(dbg=None, force_ne8=False):
    nc = bass.Bass("TRN2", target_bir_lowering=False)
    NE = 1 if (dbg and not dbg.endswith("1") and not force_ne8) else 8
    fw = FW(nc)

    def din(name, shape, dt=F32):
        return nc.dram_tensor(name, list(shape), dt, kind="ExternalInput").ap()

    def dscr(name, shape, dt=F32):
        return nc.dram_tensor(name, list(shape), dt, kind=("ExternalOutput" if (dbg and not name.startswith("W")) else "Internal")).ap()

    xin = din("xin", [NTOK, D])
    cT = din("cT", [128, 8, 2])
    mod_w = din("mod_w", [DEPTH, D, 6 * D])
    modbT = din("modbT", [DEPTH, 128, 48])
    n1T = din("n1T", [DEPTH, 128, 8])
    n2T = din("n2T", [DEPTH, 128, 8])
    w_in = din("w_in", [DEPTH, D, NIN])
    qn_w = din("qn_w", [DEPTH, 64])
    kn_w = din("kn_w", [DEPTH, 64])
    ret_decay = din("ret_decay", [DEPTH, 8])
    ret_norm_w = din("ret_norm_w", [DEPTH, 256])
    conv_w = din("conv_w", [DEPTH, 3, 512])
    gate_b = din("gate_b", [DEPTH, 16])
    mlstm_norm_w = din("mlstm_norm_w", [DEPTH, 256])
    w_out = din("w_out", [DEPTH, D, D])
    ffn_wg = din("ffn_wg", [1, D, 2816])
    ffn_wu = din("ffn_wu", [1, D, 2816])
    ffn_wd = din("ffn_wd", [1, 2816, D])
    router_w = din("router_w", [D, 8])
    router_b = din("router_b", [1, 8])
    moe_wg = din("moe_wg", [NE, D, 3584])
    moe_wu = din("moe_wu", [NE, D, 3584])
    moe_wd = din("moe_wd", [NE, 3584, D])
    ident_d = din("ident", [128, 128])
    maskF_d = din("maskF", [128, 128])
    maskB_d = din("maskB", [128, 128])
    cos_d = din("cos", [128, 32, 32])
    sin_d = din("sin", [128, 32, 32])
    pos_d = din("pos", [128, 2])
    n2row = din("n2row", [DEPTH, D])
    grid_d = din("grid512", [128, 24])
    out = nc.dram_tensor("out", [4096, D], F32, kind="ExternalOutput").ap()

    H = dscr("H", [NTOK, D])
    P = dscr("P", [NTOK, PCOLS], BF16)
    GT = dscr("GT", [16, NTOK])
    MIX = dscr("MIX", [NTOK, D], BF16)
    MD = dscr("MD", [2, 6 * D])
    NSLOT = 12288
    NTL = NSLOT // 512
    XS = dscr("XS", [NSLOT, D], BF16)
    YS = dscr("YS", [NSLOT, D], F32)
    XSres = Res("XS")
    YSres = Res("YS")
    I32 = mybir.dt.int32
    FF_CFG = [dict(nexp=1, nblk=11, nffc=2, wg=ffn_wg, wu=ffn_wu, wd=ffn_wd),
              dict(nexp=NE, nblk=7, nffc=4, wg=moe_wg, wu=moe_wu, wd=moe_wd)]
    for i, cfg in enumerate(FF_CFG):
        bw = cfg["nffc"] * 128
        cfg["bw"] = bw
        cfg["WG"] = dscr(f"WG{i}", [cfg["nexp"], cfg["nblk"], 128, 8, bw], BF16)
        cfg["WU"] = dscr(f"WU{i}", [cfg["nexp"], cfg["nblk"], 128, 8, bw], BF16)
        cfg["WD"] = dscr(f"WD{i}", [cfg["nexp"], cfg["nblk"], 128, cfg["nffc"], D], BF16)
        cfg["res"] = Res(f"wconv{i}")

    Hres = [Res(f"H{c}") for c in range(NCH)]
    Pres = [Res(f"P{c}") for c in range(NCH)]
    Pcv = [Res(f"Pcv{c}") for c in range(NCH)]
    GTres = [Res(f"GT{c}") for c in range(NCH)]
    MIXres = [Res(f"MIX{c}") for c in range(NCH)]
    MDres = Res("MD")
    OUTres = Res("OUT")

    gst = contextlib.ExitStack()
    with gst:
        G = Phase(nc, fw, "G")
        G.st = gst
        ident_f = G.sb("identf", [128, 128], F32)
        ident_b = G.sb("identb", [128, 128], BF16)
        maskF = G.sb("maskF", [128, 128], F32)
        maskB = G.sb("maskB", [128, 128], F32)
        cst = G.sb("cst", [128, 4], F32)
        modT = G.sb("modT", [128, 48, 2], F32)
        g1T = G.sb("g1T", [128, 8, 2], F32)
        g2T = G.sb("g2T", [128, 8, 2], F32)
        ones_f = G.sb("onesf", [128, 128], F32)

        fw.dma("sp", ident_f[:], ident_d, writes=[ident_f])
        fw.dma("sp", maskF[:], maskF_d, writes=[maskF])
        fw.dma("sp", maskB[:], maskB_d, writes=[maskB])
        fw.op("dve", lambda e: e.tensor_copy(out=ident_b[:], in_=ident_f[:]), reads=[ident_f], writes=[ident_b])
        fw.op("dve", lambda e: e.memset(cst[:, 0:1], EPS), writes=[cst])
        fw.op("dve", lambda e: e.memset(cst[:, 1:2], 1.0), writes=[cst])
        fw.op("dve", lambda e: e.memset(cst[:, 2:3], 0.0), writes=[cst])
        fw.op("dve", lambda e: e.memset(ones_f[:], 1.0), writes=[ones_f])

        wconv_list = []
        for cfg in FF_CFG:
            cfg["res"].persist = True
            cfg["first_idx"] = len(wconv_list)
            for e_ in range(cfg["nexp"]):
                for b_ in range(cfg["nblk"]):
                    n0 = b_ * cfg["bw"]
                    for (dst, src) in ((cfg["WG"], cfg["wg"]), (cfg["WU"], cfg["wu"])):
                        wconv_list.append((dst[e_, b_], src[e_].rearrange("(kc p) n -> p kc n", p=128)[:, :, n0:n0 + cfg["bw"]], cfg["res"]))
                    wconv_list.append((cfg["WD"][e_, b_], cfg["wd"][e_][n0:n0 + cfg["bw"], :].rearrange("(j p) n -> p j n", p=128), cfg["res"]))
            cfg["last_idx"] = len(wconv_list)
        wconv_pos = [0]

        def pump(n=1, upto=None):
            while (n > 0 or (upto is not None and wconv_pos[0] < upto)) and wconv_pos[0] < len(wconv_list):
                dst, src, r = wconv_list[wconv_pos[0]]
                wconv_pos[0] += 1
                n -= 1
                fw.dma("pool", dst, src, writes=[r], holder=r, kind="i", merge=True)

        for l in range(DEPTH):
            last = (l == DEPTH - 1)
            first_out_chunk = 2 if last else 0
            with Phase(nc, fw, f"S{l}") as ph:
                cTt = ph.sb("cT", [128, 8, 2], F32)
                sc = ph.sb("sc", [128, 8, 2], F32)
                mb = ph.sb("mb", [128, 48], F32)
                n1 = ph.sb("n1", [128, 8], F32)
                n2 = ph.sb("n2", [128, 8], F32)
                mwr = ph.ring("mw", 2, [128, 8, 512], F32)
                pm = ph.ps("pm", [128, 48, 2], F32)
                ptm = ph.ps("ptm", [48, 128], F32)
                mds = ph.sb("mds", [48, 128], F32)
                fw.dma("sp", cTt[:], cT, writes=[cTt])
                fw.dma("sp", mb[:], modbT[l], writes=[mb])
                fw.dma("sp", n1[:], n1T[l], writes=[n1])
                fw.dma("sp", n2[:], n2T[l], writes=[n2])
                fw.op("act", lambda e: e.activation(out=sc[:], in_=cTt[:], func=AF.Silu), reads=[cTt], writes=[sc])
                for piece in range(12):
                    mw = mwr.next()
                    fw.dma("sp", mw[:], mod_w[l].rearrange("(kc p) n -> p kc n", p=128)[:, :, piece * 512:(piece + 1) * 512],
                           writes=[mw])
                    for jj in range(4):
                        j = piece * 4 + jj
                        for kc in range(8):
                            fw.op("pe", lambda e, j=j, jj=jj, kc=kc, mw=mw: e.matmul(
                                pm[:, j, :], lhsT=mw[:, kc, jj * 128:(jj + 1) * 128], rhs=sc[:, kc, :],
                                start=(kc == 0), stop=(kc == 7)), reads=[mw, sc], writes=[pm])
                fw.op("dve", lambda e: e.tensor_tensor(out=modT[:], in0=pm[:], in1=bc(mb[:].unsqueeze(2), [128, 48, 2]), op=ALU.add),
                      reads=[pm, mb], writes=[modT])
                for (gT, lo, nn) in ((g1T, 8, n1), (g2T, 32, n2)):
                    fw.op("dve", lambda e, gT=gT, lo=lo: e.tensor_scalar(out=gT[:], in0=modT[:, lo:lo + 8, :], scalar1=1.0, scalar2=None, op0=ALU.add),
                          reads=[modT], writes=[gT])
                    fw.op("dve", lambda e, gT=gT, nn=nn: e.tensor_tensor(out=gT[:], in0=gT[:], in1=bc(nn[:].unsqueeze(2), [128, 8, 2]), op=ALU.mult),
                          reads=[gT, nn], writes=[gT])
                for t in range(2):
                    fw.op("pe", lambda e, t=t: e.transpose(out=ptm[:], in_=modT[:, :, t], identity=ident_f[:]),
                          reads=[modT, ident_f], writes=[ptm])
                    fw.op("dve", lambda e: e.tensor_copy(out=mds[:], in_=ptm[:]), reads=[ptm], writes=[mds])
                    fw.dma("sp", MD[t].rearrange("(j p) -> j p", p=128), mds[:], reads=[mds], writes=[MDres], kind="o")

            with Phase(nc, fw, f"A{l}") as ph:
                Win = ph.sb("Win", [128, 8, NIN], BF16)
                WC = ph.sb("WC", [128, 3, 8, 512], BF16)
                cwb = ph.sb("cwb", [128, 3, 512], F32)
                qnb = ph.sb("qnb", [128, 64], F32)
                knb = ph.sb("knb", [128, 64], F32)
                cos_t = ph.sb("cos", [128, 32, 32], F32)
                sin_t = ph.sb("sin", [128, 32, 32], F32)
                for kc in range(8):
                    fw.dma("pool", Win[:, kc, :], w_in[l][kc * 128:(kc + 1) * 128, :], writes=[Win], merge=True)
                fw.dma("sp", cwb[:], conv_w[l].partition_broadcast(128), writes=[cwb])
                fw.dma("sp", qnb[:], qn_w[l].partition_broadcast(128), writes=[qnb])
                fw.dma("sp", knb[:], kn_w[l].partition_broadcast(128), writes=[knb])
                fw.dma("sp", cos_t[:], cos_d, writes=[cos_t])
                fw.dma("sp", sin_t[:], sin_d, writes=[sin_t])
                for k in range(3):
                    fw.op("dve", lambda e, k=k: e.tensor_tensor(out=WC[:, k, :, :], in0=Win[:, :, 2304:2816],
                                                               in1=bc(cwb[:, k:k + 1, :], [128, 8, 512]), op=ALU.mult),
                          reads=[Win, cwb], writes=[WC])
                rings = dict(st=ph.ring("st", 4, [128, 4], F32), xn=ph.ring("xn", 2, [128, D], F32),
                             tp=ph.ring("tp", 2, [128, 4, 128], F32, psum=True))
                hcr = ph.ring("hc", 3, [128, D], F32)
                NLIN = 6
                LIN = [ph.sb(f"lin{i}", [128, 8, 130], BF16) for i in range(NLIN)]
                LINH = [Res(f"linh{i}") for i in range(NLIN)]
                pjr = ph.ring("pj", 5, [128, 512], F32, psum=True)
                pgr = ph.ring("pg", 1, [16, 128], F32, psum=True)
                Pcr = ph.ring("Pc", 3, [128, 2304], BF16)
                Pvr = ph.ring("Pv", 2, [128, 512], BF16)
                gsr = ph.ring("gs", 2, [16, 128], F32)
                tA = ph.ring("tA", 2, [128, 512], F32)
                tB = ph.ring("tB", 2, [128, 512], F32)
                tC = ph.ring("tC", 2, [128, 512], F32)
                tD = ph.ring("tD", 2, [128, 512], F32)
                s8r = ph.ring("s8", 4, [128, 16], F32)

                def rope(src, nh, dst_tile, dst_ap, lc):
                    sv = src[:, 0:nh * 64].rearrange("p (h d) -> p h d", d=64)
                    c_ = tC.next()
                    d_ = tD.next()
                    cv = c_[:, 0:nh * 64].rearrange("p (h d) -> p h d", d=64)
                    dv = d_[:, 0:nh * 64].rearrange("p (h d) -> p h d", d=64)
                    ov = dst_ap.rearrange("p (h d) -> p h d", d=64)
                    cb = bc(cos_t[:, lc:lc + 1, :], [128, nh, 32])
                    sb_ = bc(sin_t[:, lc:lc + 1, :], [128, nh, 32])
                    fw.op("pool", lambda e: e.tensor_tensor(out=cv[:, :, 0:32], in0=sv[:, :, 0:32], in1=cb, op=ALU.mult), reads=[src, cos_t], writes=[c_])
                    fw.op("pool", lambda e: e.tensor_tensor(out=cv[:, :, 32:64], in0=sv[:, :, 0:32], in1=sb_, op=ALU.mult), reads=[src, sin_t], writes=[c_])
                    fw.op("dve", lambda e: e.tensor_tensor(out=dv[:, :, 0:32], in0=sv[:, :, 32:64], in1=sb_, op=ALU.mult), reads=[src, sin_t], writes=[d_])
                    fw.op("dve", lambda e: e.tensor_tensor(out=dv[:, :, 32:64], in0=sv[:, :, 32:64], in1=cb, op=ALU.mult), reads=[src, cos_t], writes=[d_])
                    fw.op("pool", lambda e: e.tensor_tensor(out=ov[:, :, 0:32], in0=cv[:, :, 0:32], in1=dv[:, :, 0:32], op=ALU.subtract), reads=[c_, d_], writes=[dst_tile])
                    fw.op("dve", lambda e: e.tensor_tensor(out=ov[:, :, 32:64], in0=cv[:, :, 32:64], in1=dv[:, :, 32:64], op=ALU.add), reads=[c_, d_], writes=[dst_tile])

                def qknorm(pj, col0, nh, wb, dst_tile, dst_ap, c):
                    a_ = tA.next()
                    b_ = tB.next()
                    s8 = s8r.next()
                    n = nh * 64
                    pv = pj[:, col0:col0 + n]
                    fw.op("act", lambda e: e.activation(out=a_[:, 0:n], in_=pv, func=AF.Square), reads=[pj], writes=[a_])
                    fw.op("dve", lambda e: e.tensor_reduce(out=s8[:, 0:nh], in_=a_[:, 0:n].rearrange("p (h d) -> p h d", d=64), axis=AX.X, op=ALU.add),
                          reads=[a_], writes=[s8])
                    fw.op("act", lambda e: e.activation(out=s8[:, 8:8 + nh], in_=s8[:, 0:nh], func=AF.Sqrt, scale=1.0 / 64, bias=cst[:, 0:1]),
                          reads=[s8, cst], writes=[s8])
                    fw.op("dve", lambda e: e.reciprocal(out=s8[:, 0:nh], in_=s8[:, 8:8 + nh]), reads=[s8], writes=[s8])
                    fw.op("dve", lambda e: e.tensor_tensor(out=a_[:, 0:n].rearrange("p (h d) -> p h d", d=64), in0=pv.rearrange("p (h d) -> p h d", d=64),
                                                           in1=bc(s8[:, 0:nh].unsqueeze(2), [128, nh, 64]), op=ALU.mult), reads=[pj, s8], writes=[a_])
                    if c < 2:
                        fw.op("dve", lambda e: e.tensor_tensor(out=dst_ap.rearrange("p (h d) -> p h d", d=64), in0=a_[:, 0:n].rearrange("p (h d) -> p h d", d=64),
                                                               in1=bc(wb[:].unsqueeze(1), [128, nh, 64]), op=ALU.mult), reads=[a_, wb], writes=[dst_tile])
                    else:
                        fw.op("dve", lambda e: e.tensor_tensor(out=b_[:, 0:n].rearrange("p (h d) -> p h d", d=64), in0=a_[:, 0:n].rearrange("p (h d) -> p h d", d=64),
                                                               in1=bc(wb[:].unsqueeze(1), [128, nh, 64]), op=ALU.mult), reads=[a_, wb], writes=[b_])
                        rope(b_, nh, dst_tile, dst_ap, c - 2)

                def proj(lin, n0, n1_, pj, ncols):
                    for kc in range(8):
                        fw.op("pe", lambda e, kc=kc: e.matmul(pj[:, 0:ncols], lhsT=lin[:, kc, 1:129], rhs=Win[:, kc, n0:n1_],
                                                             start=(kc == 0), stop=(kc == 7)), reads=[lin, Win], writes=[pj])

                def front(c):
                    if True:
                        t = 1 if c < 2 else 0
                        lin = LIN[c % NLIN]
                        hc = hcr.next()
                        if l == 0:
                            fw.dma("sp", hc[:], xin[c * 128:(c + 1) * 128, :], writes=[hc])
                        else:
                            fw.dma("sp", hc[:], H[c * 128:(c + 1) * 128, :], reads=[Hres[c]], writes=[hc])
                        st4 = rings["st"].next()
                        xn = rings["xn"].next()
                        fw.op("act", lambda e: e.activation(out=xn[:], in_=hc[:], func=AF.Square, accum_out=st4[:, 0:1]), reads=[hc], writes=[xn, st4])
                        fw.op("act", lambda e: e.activation(out=st4[:, 1:2], in_=st4[:, 0:1], func=AF.Sqrt, scale=1.0 / D, bias=cst[:, 0:1]), reads=[st4, cst], writes=[st4])
                        fw.op("dve", lambda e: e.reciprocal(out=st4[:, 2:3], in_=st4[:, 1:2]), reads=[st4], writes=[st4])
                        fw.op("act", lambda e: e.activation(out=xn[:], in_=hc[:], func=AF.Copy, scale=st4[:, 2:3]), reads=[hc, st4], writes=[xn])
                        for half in range(2):
                            tp = rings["tp"].next()
                            for j in range(4):
                                kc = half * 4 + j
                                fw.op("pe", lambda e, kc=kc, j=j: e.transpose(out=tp[:, j, :], in_=xn[:, kc * 128:(kc + 1) * 128], identity=ident_f[:]),
                                      reads=[xn, ident_f], writes=[tp])
                            for j in range(4):
                                kc = half * 4 + j
                                fw.op("dve", lambda e, kc=kc, j=j: e.tensor_scalar(out=lin[:, kc, 1:129], in0=tp[:, j, :], scalar1=g1T[:, kc, t:t + 1],
                                                                                  scalar2=modT[:, kc, t:t + 1], op0=ALU.mult, op1=ALU.add),
                                      reads=[tp, g1T, modT], writes=[lin])
                        linh = LINH[c % NLIN]
                        if c in (0, 2):
                            fw.op("pool", lambda e: e.memset(lin[:, :, 0:1], 0.0), writes=[linh])
                        else:
                            prev = LIN[(c - 1) % NLIN]
                            fw.op("pool", lambda e: e.tensor_copy(out=lin[:, :, 0:1], in_=prev[:, :, 128:129]), reads=[prev], writes=[linh])
                            fw.op("pool", lambda e: e.tensor_copy(out=prev[:, :, 129:130], in_=lin[:, :, 1:2]), reads=[lin], writes=[LINH[(c - 1) % NLIN]])
                        if c in (1, NCH - 1):
                            fw.op("pool", lambda e: e.memset(lin[:, :, 129:130], 0.0), writes=[linh])
                def back(c):
                    front(c)
                    yield
                    if True:
                        lin = LIN[c % NLIN]
                        Pc = Pcr.next()
                        pj = pjr.next()
                        proj(lin, 0, 512, pj, 512)
                        qknorm(pj, 0, 8, qnb, Pc, Pc[:, PQ:PQ + 512], c)
                        pj = pjr.next()
                        proj(lin, 512, 1024, pj, 512)
                        qknorm(pj, 0, 2, knb, Pc, Pc[:, PK:PK + 128], c)
                        fw.op("act", lambda e, pj=pj: e.activation(out=Pc[:, PV:PV + 384], in_=pj[:, 128:512], func=AF.Copy), reads=[pj], writes=[Pc])
                        yield
                        pj = pjr.next()
                        proj(lin, 1024, 1536, pj, 512)
                        if c < 2:
                            fw.op("act", lambda e, pj=pj: e.activation(out=Pc[:, PRQ:PRQ + 512], in_=pj[:, 0:512], func=AF.Copy), reads=[pj], writes=[Pc])
                        else:
                            b_ = tB.next()
                            fw.op("act", lambda e, pj=pj, b_=b_: e.activation(out=b_[:], in_=pj[:, 0:512], func=AF.Copy), reads=[pj], writes=[b_])
                            rope(b_, 8, Pc, Pc[:, PRQ:PRQ + 512], c - 2)
                        pj = pjr.next()
                        proj(lin, 1536, 2048, pj, 512)
                        fw.op("act", lambda e, pj=pj: e.activation(out=Pc[:, PRG:PRG + 256], in_=pj[:, 0:256], func=AF.Silu), reads=[pj], writes=[Pc])
                        fw.op("act", lambda e, pj=pj: e.activation(out=Pc[:, PMV:PMV + 256], in_=pj[:, 256:512], func=AF.Copy), reads=[pj], writes=[Pc])
                        pj = pjr.next()
                        proj(lin, 2048, 2304, pj, 256)
                        fw.op("act", lambda e, pj=pj: e.activation(out=Pc[:, PMO:PMO + 256], in_=pj[:, 0:256], func=AF.Sigmoid), reads=[pj], writes=[Pc])
                        fw.dma("pool", P[c * 128:(c + 1) * 128, 0:2304], Pc[:], reads=[Pc], writes=[Pres[c]], kind="o")
                        pg = pgr.next()
                        for kc in range(8):
                            fw.op("pe", lambda e, kc=kc: e.matmul(pg[:], lhsT=Win[:, kc, 2816:2832], rhs=lin[:, kc, 1:129], start=(kc == 0), stop=(kc == 7)),
                                  reads=[lin, Win], writes=[pg])
                        gs = gsr.next()
                        fw.op("dve", lambda e: e.tensor_copy(out=gs[:], in_=pg[:]), reads=[pg], writes=[gs])
                        fw.dma("pool", GT[:, c * 128:(c + 1) * 128], gs[:], reads=[gs], writes=[GTres[c]], kind="o")
                    yield
                    cc = c
                    if True:
                        linp = LIN[cc % NLIN]
                        pj = pjr.next()
                        for k in range(3):
                            for kc in range(8):
                                fw.op("pe", lambda e, k=k, kc=kc: e.matmul(pj[:], lhsT=linp[:, kc, k:k + 128], rhs=WC[:, k, kc, :],
                                                                           start=(k == 0 and kc == 0), stop=(k == 2 and kc == 7)),
                                      reads=[linp, LINH[cc % NLIN], WC], writes=[pj])
                        Pv = Pvr.next()
                        fw.op("act", lambda e, pj=pj: e.activation(out=Pv[:], in_=pj[:], func=AF.Silu), reads=[pj], writes=[Pv])
                        fw.dma("pool", P[cc * 128:(cc + 1) * 128, 2304:2816], Pv[:], reads=[Pv], writes=[Pcv[cc]], kind="o")

                gens = []
                for c in list(range(NCH)) + [None]:
                    if c is not None:
                        pump(1)
                        gens.append(back(c))
                    for g_ in list(gens):
                        try:
                            next(g_)
                        except StopIteration:
                            gens.remove(g_)
                while gens:
                    for g_ in list(gens):
                        try:
                            next(g_)
                        except StopIteration:
                            gens.remove(g_)
            if dbg == f"A{l}":
                break

            with Phase(nc, fw, f"B{l}") as ph:
                kT = ph.sb("kT", [128, NTOK], BF16)
                Va = ph.sb("Va", [128, NCH, 2, 128], BF16)
                kvr = ph.ring("kv", 2, [128, 256], BF16)
                tqr = ph.ring("tq", 2, [128, 4, 128], BF16, psum=True)
                fw.op("pool", lambda e: e.memset(Va[:], 1.0), writes=[Va])
                for c in range(NCH):
                    kv = kvr.next()
                    fw.dma("sp", kv[:], P[c * 128:(c + 1) * 128, PK:PK + 256], reads=[Pres[c]], writes=[kv])
                    tk = tqr.next()
                    fw.op("pe", lambda e: e.transpose(out=tk[:, 0, :], in_=kv[:, 0:128], identity=ident_b[:]), reads=[kv, ident_b], writes=[tk])
                    fw.op("dve", lambda e, c=c: e.tensor_copy(out=kT[:, c * 128:(c + 1) * 128], in_=tk[:, 0, :]), reads=[tk], writes=[kT])
                    fw.op("pool", lambda e, c=c: e.tensor_copy(out=Va[:, c, :, 0:64], in_=kv[:, 128:256].rearrange("p (g d) -> p g d", d=64)),
                          reads=[kv], writes=[Va])
                qbr = ph.ring("qb", 2, [128, 512], BF16)
                qTr = ph.ring("qT", 2, [128, 2, 512], BF16)
                for qz in qTr.tiles:
                    fw.op("pool", lambda e, qz=qz: e.memset(qz[:], 0.0), writes=[qz])
                psr = ph.ring("pss", 3, [128, 512], F32, psum=True)
                PTr = ph.ring("PT", 5, [128, 512], BF16)
                oTr = ph.ring("oT", 2, [128, 512], F32, psum=True)
                otr = ph.ring("ot", 1, [128, 4, 128], F32, psum=True)
                oSr = ph.ring("oS", 2, [128, 512], F32)
                pending_epi = []
                recr = ph.ring("rec", 2, [128, 4], F32)
                attr = ph.ring("att", 3, [128, 512], BF16)
                for qb in range(first_out_chunk, NCH):
                    pump(1)
                    keys = [0, 1] if qb < 2 else list(range(NCH))
                    qt = qbr.next()
                    fw.dma("sp", qt[:], P[qb * 128:(qb + 1) * 128, PQ:PQ + 512], reads=[Pres[qb]], writes=[qt])
                    tq = tqr.next()
                    for i in range(4):
                        fw.op("pe", lambda e, i=i: e.transpose(out=tq[:, i, :], in_=qt[:, i * 128:(i + 1) * 128], identity=ident_b[:]),
                              reads=[qt, ident_b], writes=[tq])
                    qT = qTr.next()
                    fw.op("dve", lambda e: e.tensor_copy(out=qT[0:64, 0, :], in_=tq[0:64].rearrange("p a b -> p (a b)")), reads=[tq], writes=[qT])
                    fw.op("dve", lambda e: e.tensor_copy(out=qT[64:128, 1, :], in_=tq[64:128].rearrange("p a b -> p (a b)")), reads=[tq], writes=[qT])
                    att = attr.next()
                    for g in range(2):
                        oT = oTr.next()

                        def smm(kc, g=g):
                            pss = psr.next()
                            fw.op("pe", lambda e: e.matmul(pss[:], lhsT=kT[:, kc * 128:(kc + 1) * 128], rhs=qT[:, g, :], start=True, stop=True),
                                  reads=[kT, qT], writes=[pss])
                            return pss
                        pend = [smm(keys[0])]
                        if len(keys) > 1:
                            pend.append(smm(keys[1]))
                        for ki, kc in enumerate(keys):
                            pss = pend.pop(0)
                            if ki + 2 < len(keys):
                                pend.append(smm(keys[ki + 2]))
                            PT = PTr.next()
                            fw.op("act", lambda e, pss=pss, PT=PT: e.activation(out=PT[:], in_=pss[:], func=AF.Exp, scale=0.125), reads=[pss], writes=[PT])
                            fw.op("pe", lambda e, kc=kc, g=g, PT=PT, oT=oT, ki=ki: e.matmul(
                                oT[:, :], lhsT=Va[:, kc, g, :], rhs=PT[:, :], start=(ki == 0), stop=(ki == len(keys) - 1)), reads=[PT, Va], writes=[oT])
                            if ki == min(3, len(keys) - 1) and pending_epi:
                                pending_epi.pop(0)()

                        def epi(oT=oT, att=att, g=g, qb=qb, lastg=(g == 1)):
                            oS = oSr.next()
                            fw.op("dve", lambda e: e.tensor_copy(out=oS[:], in_=oT[:, :]), reads=[oT], writes=[oS])
                            ot = otr.next()
                            for i in range(4):
                                fw.op("pe", lambda e, i=i: e.transpose(out=ot[:, i, :], in_=oS[:, i * 128:(i + 1) * 128], identity=ident_f[:]),
                                      reads=[oS, ident_f], writes=[ot])
                            rec = recr.next()
                            fw.op("dve", lambda e: e.reciprocal(out=rec[:], in_=ot[:, :, 64]), reads=[ot], writes=[rec])
                            fw.op("dve", lambda e: e.tensor_tensor(
                                out=att[:, g * 256:(g + 1) * 256].rearrange("p (h d) -> p h d", d=64), in0=ot[:, :, 0:64],
                                in1=bc(rec[:].unsqueeze(2), [128, 4, 64]), op=ALU.mult), reads=[ot, rec], writes=[att])
                            if lastg:
                                fw.dma("pool", MIX[qb * 128:(qb + 1) * 128, 0:512], att[:], reads=[att], writes=[MIXres[qb]], kind="o")
                        pending_epi.append(epi)
                while pending_epi:
                    pending_epi.pop(0)()
            if dbg == f"B{l}":
                break

            for kind in ("ret", "ml"):
                if kind == "ret":
                    qcol, kcol, vcol, gcol, ocol = PRQ, PRK, PRV, PRG, 512
                    qres = kres = Pres
                else:
                    qcol, kcol, vcol, gcol, ocol = PMQ, PMK, PMV, PMO, 768
                    qres = kres = Pcv
                with Phase(nc, fw, f"{kind}{l}") as ph:
                    E = [ph.sb(f"E{d_}", [128, 4, NCH], F32) for d_ in range(2)]
                    Fm = [ph.sb(f"F{d_}", [128, 4, NCH], F32) for d_ in range(2)]
                    PRE = [ph.sb(f"PRE{d_}", [64, NCH, 4], F32) for d_ in range(2)]
                    POST = [ph.sb(f"POST{d_}", [64, 4], F32) for d_ in range(2)]
                    nwb = ph.sb("nwb", [128, 256], F32)
                    fw.dma("sp", nwb[:], (ret_norm_w if kind == "ret" else mlstm_norm_w)[l].partition_broadcast(128), writes=[nwb])
                    with Phase(nc, fw, f"{kind}{l}tab") as pt:
                        if kind == "ret":
                            rd = pt.sb("rd", [128, 8], F32)
                            lg = pt.sb("lg", [128, 8], F32)
                            pos = pt.sb("pos", [128, 2], F32)
                            tmp = pt.sb("tmp", [128, 8], F32)
                            tmp2 = pt.sb("tmp2", [128, 8], F32)
                            fw.dma("sp", rd[:], ret_decay[l].partition_broadcast(128), writes=[rd])
                            fw.dma("sp", pos[:], pos_d, writes=[pos])
                            fw.op("act", lambda e: e.activation(out=lg[:], in_=rd[:], func=AF.Exp), reads=[rd], writes=[lg])
                            fw.op("act", lambda e: e.activation(out=lg[:], in_=lg[:], func=AF.Ln, scale=-1.0, bias=cst[:, 1:2]), reads=[lg, cst], writes=[lg])
                            for d_ in range(2):
                                fw.op("dve", lambda e, d_=d_: e.tensor_scalar(out=tmp[:, d_ * 4:d_ * 4 + 4], in0=lg[:, d_ * 4:d_ * 4 + 4], scalar1=pos[:, d_:d_ + 1],
                                                                             scalar2=None, op0=ALU.mult), reads=[lg, pos], writes=[tmp])
                            fw.op("act", lambda e: e.activation(out=tmp2[:], in_=tmp[:], func=AF.Exp), reads=[tmp], writes=[tmp2])
                            for d_ in range(2):
                                fw.op("dve", lambda e, d_=d_: e.tensor_copy(out=Fm[d_][:], in_=bc(tmp2[:, d_ * 4:d_ * 4 + 4].unsqueeze(2), [128, 4, NCH])),
                                      reads=[tmp2], writes=[Fm[d_]])
                            fw.op("act", lambda e: e.activation(out=tmp2[:], in_=tmp[:], func=AF.Exp, scale=-1.0), reads=[tmp], writes=[tmp2])
                            fw.op("dve", lambda e: e.tensor_scalar(out=tmp2[:], in0=tmp2[:], scalar1=0.125, scalar2=None, op0=ALU.mult), reads=[tmp2], writes=[tmp2])
                            for d_ in range(2):
                                fw.op("dve", lambda e, d_=d_: e.tensor_copy(out=E[d_][:], in_=bc(tmp2[:, d_ * 4:d_ * 4 + 4].unsqueeze(2), [128, 4, NCH])),
                                      reads=[tmp2], writes=[E[d_]])
                            fw.op("act", lambda e: e.activation(out=tmp[:], in_=lg[:], func=AF.Exp, scale=128.0), reads=[lg], writes=[tmp])
                            for d_ in range(2):
                                fw.op("dve", lambda e, d_=d_: e.tensor_copy(out=POST[d_][:], in_=tmp[0:64, d_ * 4:d_ * 4 + 4]), reads=[tmp], writes=[POST[d_]])
                                fw.op("dve", lambda e, d_=d_: e.memset(PRE[d_][:], 1.0), writes=[PRE[d_]])
                        else:
                            Gt = pt.sb("Gt", [NCH, 16, 128], F32)
                            gb = pt.sb("gb", [NCH, 16], F32)
                            cs0 = pt.sb("cs0", [NCH, 8, 128], F32)
                            cs1 = pt.sb("cs1", [NCH, 8, 128], F32)
                            t2 = pt.sb("t2", [NCH, 8, 128], F32)
                            U = pt.sb("U", [NCH, 8, 128], F32)
                            NB = pt.sb("NB", [NCH, 8, 128], F32)
                            ub = pt.sb("ub", [NCH, 16], F32)
                            for c in range(NCH):
                                pass
                            fw.dma("sp", Gt[:], GT.rearrange("g (c t) -> c g t", t=128), reads=GTres, writes=[Gt])
                            fw.dma("sp", gb[:], gate_b[l].partition_broadcast(NCH), writes=[gb])
                            for d_ in range(2):
                                fw.op("dve", lambda e, d_=d_: e.memset(POST[d_][:], 1.0), writes=[POST[d_]])
                            fw.op("dve", lambda e: e.tensor_tensor(out=Gt[:], in0=Gt[:], in1=bc(gb[:].unsqueeze(2), [NCH, 16, 128]), op=ALU.add),
                                  reads=[Gt, gb], writes=[Gt])
                            fw.op("act", lambda e: e.activation(out=t2[:], in_=Gt[:, 8:16, :], func=AF.Exp, scale=-1.0), reads=[Gt], writes=[t2])
                            fw.op("act", lambda e: e.activation(out=t2[:], in_=t2[:], func=AF.Ln, scale=1.0, bias=cst[0:NCH, 1:2]), reads=[t2, cst], writes=[t2])
                            src, dst = t2, cs0
                            sh = 1
                            while sh < 128:
                                fw.op("dve", lambda e, src=src, dst=dst, sh=sh: e.tensor_tensor(out=dst[:, :, sh:128], in0=src[:, :, sh:128], in1=src[:, :, 0:128 - sh], op=ALU.add),
                                      reads=[src], writes=[dst])
                                fw.op("dve", lambda e, src=src, dst=dst, sh=sh: e.tensor_copy(out=dst[:, :, 0:sh], in_=src[:, :, 0:sh]), reads=[src], writes=[dst])
                                src = dst
                                dst = cs1 if dst is cs0 else cs0
                                if sh == 1:
                                    pass
                                sh *= 2
                            cs = src
                            other = dst
                            fw.op("dve", lambda e: e.tensor_copy(out=NB[:, 0:4, :], in_=cs[:, 0:4, :]), reads=[cs], writes=[NB])
                            fw.op("dve", lambda e: e.tensor_tensor(out=NB[:, 4:8, :], in0=t2[:, 4:8, :], in1=cs[:, 4:8, :], op=ALU.subtract), reads=[cs, t2], writes=[NB])
                            fw.op("dve", lambda e: e.tensor_tensor(out=NB[:, 4:8, :], in0=NB[:, 4:8, :], in1=bc(cs[:, 4:8, 127:128], [NCH, 4, 128]), op=ALU.add),
                                  reads=[cs, NB], writes=[NB])
                            fw.op("dve", lambda e: e.tensor_tensor(out=U[:], in0=Gt[:, 0:8, :], in1=NB[:], op=ALU.add), reads=[Gt, NB], writes=[U])
                            fw.op("dve", lambda e: e.tensor_reduce(out=ub[:, 0:8], in_=U[:], axis=AX.X, op=ALU.max), reads=[U], writes=[ub])
                            fw.op("dve", lambda e: e.tensor_scalar(out=ub[:, 8:16], in0=cs[:, :, 127], scalar1=-1.0, scalar2=None, op0=ALU.mult), reads=[cs], writes=[ub])
                            pq = pt.ps("pq", [4, 4, NCH], F32)
                            UB = pt.sb("UB", [4, 4, NCH], F32)
                            for qi in range(4):
                                fw.op("pe", lambda e, qi=qi: e.transpose(out=pq[:, qi, :], in_=ub[:, qi * 4:qi * 4 + 4], identity=ident_f[0:NCH, 0:NCH]),
                                      reads=[ub, ident_f], writes=[pq])
                            fw.op("dve", lambda e: e.tensor_copy(out=UB[:], in_=pq[:]), reads=[pq], writes=[UB])
                            mcur = pt.sb("mcur", [4, 2, NCH + 1], F32)
                            Mend = pt.sb("Mend", [4, 2, NCH], F32)
                            dd = pt.sb("dd", [4, 2, NCH], F32)
                            fw.op("dve", lambda e: e.memset(mcur[:], 0.0), writes=[mcur])
                            for d_, order in enumerate((FWD_ORDER, BWD_ORDER)):
                                for idx, c in enumerate(order):
                                    fw.op("dve", lambda e, d_=d_, idx=idx, c=c: e.tensor_tensor(out=Mend[:, d_, c:c + 1], in0=mcur[:, d_, idx:idx + 1],
                                                                                                in1=UB[:, d_, c:c + 1], op=ALU.max), reads=[mcur, UB], writes=[Mend])
                                    fw.op("dve", lambda e, d_=d_, idx=idx, c=c: e.tensor_tensor(out=mcur[:, d_, idx + 1:idx + 2], in0=Mend[:, d_, c:c + 1],
                                                                                                in1=UB[:, 2 + d_, c:c + 1], op=ALU.add), reads=[Mend, UB], writes=[mcur])
                                    fw.op("dve", lambda e, d_=d_, idx=idx, c=c: e.tensor_tensor(out=dd[:, d_, c:c + 1], in0=mcur[:, d_, idx:idx + 1],
                                                                                                in1=Mend[:, d_, c:c + 1], op=ALU.subtract), reads=[mcur, Mend], writes=[dd])
                            SC = pt.sb("SC", [4, 2, NCH], F32)
                            fw.op("act", lambda e: e.activation(out=SC[:], in_=dd[:], func=AF.Exp), reads=[dd], writes=[SC])
                            BD = pt.sb("BD", [4, NCH, 4], F32)
                            ppre = pt.ps("ppre", [64, NCH * 4], F32)
                            for d_ in range(2):
                                fw.op("dve", lambda e, d_=d_: e.tensor_tensor(out=BD[:], in0=bc(SC[:, d_, :].unsqueeze(2), [4, NCH, 4]),
                                                                             in1=bc(ident_f[0:4, 0:4].unsqueeze(1), [4, NCH, 4]), op=ALU.mult),
                                      reads=[SC, ident_f], writes=[BD])
                                fw.op("pe", lambda e: e.matmul(ppre[:], lhsT=ones_f[0:4, 0:64], rhs=BD[:].rearrange("p c h -> p (c h)"), start=True, stop=True),
                                      reads=[ones_f, BD], writes=[ppre])
                                fw.op("dve", lambda e, d_=d_: e.tensor_copy(out=PRE[d_][:].rearrange("p c h -> p (c h)"), in_=ppre[:]), reads=[ppre], writes=[PRE[d_]])
                            pm2 = pt.ps("pm2", [NCH, 8], F32)
                            MT = pt.sb("MT", [NCH, 8], F32)
                            for d_ in range(2):
                                fw.op("pe", lambda e, d_=d_: e.transpose(out=pm2[:, d_ * 4:d_ * 4 + 4], in_=Mend[:, d_, :], identity=ident_f[0:4, 0:4]),
                                      reads=[Mend, ident_f], writes=[pm2])
                            fw.op("dve", lambda e: e.tensor_copy(out=MT[:], in_=pm2[:]), reads=[pm2], writes=[MT])
                            fw.op("dve", lambda e: e.tensor_tensor(out=U[:], in0=U[:], in1=bc(MT[:].unsqueeze(2), [NCH, 8, 128]), op=ALU.subtract), reads=[U, MT], writes=[U])
                            fw.op("act", lambda e: e.activation(out=U[:], in_=U[:], func=AF.Exp), reads=[U], writes=[U])
                            fw.op("dve", lambda e: e.tensor_tensor(out=NB[:], in0=NB[:], in1=bc(MT[:].unsqueeze(2), [NCH, 8, 128]), op=ALU.subtract), reads=[NB, MT], writes=[NB])
                            fw.op("act", lambda e: e.activation(out=NB[:], in_=NB[:], func=AF.Exp), reads=[NB], writes=[NB])
                            ptab = pt.ps("ptab", [128, 4, NCH], F32)
                            for (srcT, dsts, scl) in ((U, E, 0.125), (NB, Fm, 1.0)):
                                for d_ in range(2):
                                    for h in range(4):
                                        fw.op("pe", lambda e, srcT=srcT, d_=d_, h=h: e.transpose(out=ptab[:, h, :], in_=srcT[:, d_ * 4 + h, :], identity=ident_f[0:NCH, 0:NCH]),
                                              reads=[srcT, ident_f], writes=[ptab])
                                    fw.op("dve", lambda e, dsts=dsts, d_=d_, scl=scl: e.tensor_scalar(out=dsts[d_][:], in0=ptab[:], scalar1=scl, scalar2=None, op0=ALU.mult),
                                          reads=[ptab], writes=[dsts[d_]])

                    SBs = ph.sb("SBs", [64, NCH, 4, 65], BF16)
                    ST = [ph.sb(f"ST{d_}", [64, 4, 65], F32) for d_ in range(2)]
                    SP = ph.ring("SP", 2, [64, 4, 65], F32)
                    SFb = ph.ring("SFb", 3, [64, 4, 65], BF16)
                    kvr = ph.ring("kv", 4, [128, 512], BF16)
                    Var = ph.ring("Va", 4, [128, 4, 65], BF16)
                    KEr = ph.ring("KE", 3, [128, 4, 64], BF16)
                    pinc = ph.ring("pinc", 2, [64, 4, 128], F32, psum=True)
                    for d_ in range(2):
                        fw.op("dve", lambda e, d_=d_: e.memset(ST[d_][:], 0.0), writes=[ST[d_]])
                    for va in Var.tiles:
                        fw.op("pool", lambda e, va=va: e.memset(va[:], 1.0), writes=[va])

                    def load_kv(c):
                        kv = kvr.next()
                        fw.dma("sp", kv[:, 0:256], P[c * 128:(c + 1) * 128, kcol:kcol + 256], reads=[kres[c]], writes=[kv], merge=True)
                        fw.dma("sp", kv[:, 256:512], P[c * 128:(c + 1) * 128, vcol:vcol + 256], reads=[Pres[c]], writes=[kv], merge=True)
                        va = Var.next()
                        fw.op("act", lambda e: e.activation(out=va[:, :, 0:64], in_=kv[:, 256:512].rearrange("p (h d) -> p h d", d=64), func=AF.Copy), reads=[kv], writes=[va])
                        return kv, va

                    def state_step(d_, c, kv, va, save_ap=None, save_tile=None):
                        sp = SP.next()
                        fw.op("dve", lambda e: e.tensor_tensor(out=sp[:], in0=ST[d_][:], in1=bc(PRE[d_][:, c, :].unsqueeze(2), [64, 4, 65]), op=ALU.mult),
                              reads=[ST[d_], PRE[d_]], writes=[sp])
                        fw.op("act", lambda e: e.activation(out=save_ap, in_=sp[:], func=AF.Copy), reads=[sp], writes=[save_tile])
                        ke = KEr.next()
                        fw.op("dve", lambda e: e.tensor_tensor(out=ke[:], in0=kv[:, 0:256].rearrange("p (h d) -> p h d", d=64),
                                                               in1=bc(E[d_][:, :, c:c + 1], [128, 4, 64]), op=ALU.mult), reads=[kv, E[d_]], writes=[ke])
                        pi = pinc.next()
                        for h in range(4):
                            fw.op("pe", lambda e, h=h: e.matmul(pi[:, h, 0:65], lhsT=ke[:, h, :], rhs=va[:, h, :], start=True, stop=True),
                                  reads=[ke, va], writes=[pi])
                        fw.op("dve", lambda e: e.tensor_tensor(out=ST[d_][:], in0=sp[:], in1=pi[:, :, 0:65], op=ALU.add), reads=[sp, pi], writes=[ST[d_]])
                        fw.op("dve", lambda e: e.tensor_tensor(out=ST[d_][:], in0=ST[d_][:], in1=bc(POST[d_][:].unsqueeze(2), [64, 4, 65]), op=ALU.mult),
                              reads=[ST[d_], POST[d_]], writes=[ST[d_]])

                    for c in BWD_ORDER:
                        pump(1)
                        kv, va = load_kv(c)
                        state_step(1, c, kv, va, save_ap=SBs[:, c, :, :], save_tile=SBs)

                    qgr = ph.ring("qg", 5, [128, 512], BF16)
                    ptq = ph.ring("ptq", 1, [64, 8, 128], BF16, psum=True)
                    qkT = ph.ring("qkT", 3, [64, 8, 128], BF16)
                    psc = ph.ring("psc", 1, [128, 4, 128], F32, psum=True)
                    tmpS = ph.ring("tmpS", 2, [128, 4, 128], F32)
                    SSr = [ph.ring(f"SS{d_}", 3, [128, 4, 128], BF16) for d_ in range(2)]
                    pR = [ph.ring(f"pR{d_}", 2, [128, 4, 128], F32, psum=True) for d_ in range(2)]
                    s4 = ph.ring("s4", 12, [128, 8], F32)
                    hh = ph.ring("hh", 16, [128, 4, 64], F32)
                    mo_r = ph.ring("mixo", 3, [128, 256], BF16)
                    def chunk_gen(c):
                        pump(1)
                        kv, va = load_kv(c)
                        sfb = SFb.next()
                        do_out = c >= first_out_chunk
                        if do_out:
                            qg = qgr.next()
                            fw.dma("sp", qg[:, 0:256], P[c * 128:(c + 1) * 128, qcol:qcol + 256], reads=[qres[c]], writes=[qg], merge=True)
                            fw.dma("sp", qg[:, 256:512], P[c * 128:(c + 1) * 128, gcol:gcol + 256], reads=[Pres[c]], writes=[qg], merge=True)
                        state_step(0, c, kv, va, save_ap=sfb[:], save_tile=sfb)
                        if not do_out:
                            return
                        tq = ptq.next()
                        for h in range(4):
                            fw.op("pe", lambda e, h=h: e.transpose(out=tq[:, h, :], in_=qg[:, h * 64:(h + 1) * 64], identity=ident_b[:]), reads=[qg, ident_b], writes=[tq])
                            fw.op("pe", lambda e, h=h: e.transpose(out=tq[:, 4 + h, :], in_=kv[:, h * 64:(h + 1) * 64], identity=ident_b[:]), reads=[kv, ident_b], writes=[tq])
                        qk = qkT.next()
                        fw.op("act", lambda e: e.activation(out=qk[:], in_=tq[:], func=AF.Copy), reads=[tq], writes=[qk])
                        ps_ = psc.next()
                        for h in range(4):
                            fw.op("pe", lambda e, h=h: e.matmul(ps_[:, h, :], lhsT=qk[:, 4 + h, :], rhs=qk[:, h, :], start=True, stop=True), reads=[qk], writes=[ps_])
                        sss = []
                        for d_ in range(2):
                            ts_ = tmpS.next()
                            ss = SSr[d_].next()
                            fw.op("dve", lambda e, d_=d_, ts_=ts_: e.tensor_tensor(out=ts_[:], in0=ps_[:], in1=bc(E[d_][:, :, c:c + 1], [128, 4, 128]), op=ALU.mult),
                                  reads=[ps_, E[d_]], writes=[ts_])
                            mk_ = maskF if d_ == 0 else maskB
                            fw.op("pool", lambda e, ts_=ts_, ss=ss, mk_=mk_: e.tensor_tensor(out=ss[:], in0=ts_[:], in1=bc(mk_[:].unsqueeze(1), [128, 4, 128]), op=ALU.mult),
                                  reads=[ts_, mk_], writes=[ss])
                            sss.append(ss)
                        yield
                        Rs = []
                        for d_ in range(2):
                            ss = sss[d_]
                            R = pR[d_].next()
                            for h in range(4):
                                fw.op("pe", lambda e, h=h, ss=ss, R=R: e.matmul(R[:, h, 0:65], lhsT=ss[:, h, :], rhs=va[:, h, :], start=True, stop=False),
                                      reads=[ss, va], writes=[R])
                                if d_ == 0:
                                    fw.op("pe", lambda e, h=h, R=R: e.matmul(R[:, h, 0:65], lhsT=qk[:, h, :], rhs=sfb[:, h, :], start=False, stop=True),
                                          reads=[qk, sfb], writes=[R])
                                else:
                                    fw.op("pe", lambda e, h=h, R=R: e.matmul(R[:, h, 0:65], lhsT=qk[:, h, :], rhs=SBs[:, c, h, :], start=False, stop=True),
                                          reads=[qk, SBs], writes=[R])
                            Rs.append(R)
                        yield
                        hsum = hh.next()
                        hd = []
                        for d_ in range(2):
                            R = Rs[d_]
                            hx = hh.next()
                            if kind == "ml":
                                den = s4.next()
                                fw.op("act", lambda e, R=R, den=den: e.activation(out=den[:, 0:4], in_=R[:, :, 64], func=AF.Abs), reads=[R], writes=[den])
                                fw.op("dve", lambda e, d_=d_, den=den: e.tensor_tensor(out=den[:, 0:4], in0=den[:, 0:4], in1=Fm[d_][:, :, c], op=ALU.max), reads=[den, Fm[d_]], writes=[den])
                                fw.op("dve", lambda e, den=den: e.reciprocal(out=den[:, 4:8], in_=den[:, 0:4]), reads=[den], writes=[den])
                                fw.op("dve", lambda e, R=R, den=den, hx=hx: e.tensor_tensor(out=hx[:], in0=R[:, :, 0:64], in1=bc(den[:, 4:8].unsqueeze(2), [128, 4, 64]), op=ALU.mult),
                                      reads=[R, den], writes=[hx])
                            else:
                                fw.op("dve", lambda e, R=R, d_=d_, hx=hx: e.tensor_tensor(out=hx[:], in0=R[:, :, 0:64], in1=bc(Fm[d_][:, :, c:c + 1], [128, 4, 64]), op=ALU.mult),
                                      reads=[R, Fm[d_]], writes=[hx])
                            hd.append(hx)
                        fw.op("pool", lambda e: e.tensor_tensor(out=hsum[:], in0=hd[0][:], in1=hd[1][:], op=ALU.add), reads=[hd[0], hd[1]], writes=[hsum])
                        gv = qg[:, 256:512].rearrange("p (h d) -> p h d", d=64)
                        if kind == "ml":
                            fw.op("pool", lambda e: e.tensor_tensor(out=hsum[:], in0=hsum[:], in1=gv, op=ALU.mult), reads=[hsum, qg], writes=[hsum])
                        yield
                        stt = s4.next()
                        xc = hh.next()
                        sq = hd[0]
                        fw.op("dve", lambda e: e.tensor_reduce(out=stt[:, 0:4], in_=hsum[:], axis=AX.X, op=ALU.add), reads=[hsum], writes=[stt])
                        fw.op("dve", lambda e: e.tensor_scalar(out=stt[:, 0:4], in0=stt[:, 0:4], scalar1=-1.0 / 64, scalar2=None, op0=ALU.mult), reads=[stt], writes=[stt])
                        fw.op("dve", lambda e: e.tensor_tensor(out=xc[:], in0=hsum[:], in1=bc(stt[:, 0:4].unsqueeze(2), [128, 4, 64]), op=ALU.add), reads=[hsum, stt], writes=[xc])
                        fw.op("act", lambda e: e.activation(out=sq[:], in_=xc[:], func=AF.Square), reads=[xc], writes=[sq])
                        fw.op("dve", lambda e: e.tensor_reduce(out=stt[:, 4:8], in_=sq[:], axis=AX.X, op=ALU.add), reads=[sq], writes=[stt])
                        fw.op("act", lambda e: e.activation(out=stt[:, 0:4], in_=stt[:, 4:8], func=AF.Sqrt, scale=1.0 / 64, bias=cst[:, 0:1]), reads=[stt, cst], writes=[stt])
                        fw.op("dve", lambda e: e.reciprocal(out=stt[:, 4:8], in_=stt[:, 0:4]), reads=[stt], writes=[stt])
                        fw.op("dve", lambda e: e.tensor_tensor(out=xc[:], in0=xc[:], in1=bc(stt[:, 4:8].unsqueeze(2), [128, 4, 64]), op=ALU.mult), reads=[xc, stt], writes=[xc])
                        mo_ = mo_r.next()
                        mv_ = mo_[:].rearrange("p (h d) -> p h d", d=64)
                        nv = nwb[:].rearrange("p (h d) -> p h d", d=64)
                        if kind == "ml":
                            fw.op("pool", lambda e: e.tensor_tensor(out=mv_, in0=xc[:], in1=nv, op=ALU.mult), reads=[xc, nwb], writes=[mo_])
                        else:
                            fw.op("pool", lambda e: e.tensor_tensor(out=xc[:], in0=xc[:], in1=nv, op=ALU.mult), reads=[xc, nwb], writes=[xc])
                            fw.op("pool", lambda e: e.tensor_tensor(out=mv_, in0=xc[:], in1=gv, op=ALU.mult), reads=[xc, qg], writes=[mo_])
                        fw.dma("pool", MIX[c * 128:(c + 1) * 128, ocol:ocol + 256], mo_[:], reads=[mo_], writes=[MIXres[c]], kind="o")

                    gens = []
                    for c in FWD_ORDER + [None]:
                        if c is not None:
                            gens.append(chunk_gen(c))
                        for g_ in list(gens):
                            try:
                                next(g_)
                            except StopIteration:
                                gens.remove(g_)
                    for g_ in gens:
                        for _ in g_:
                            pass
                if dbg == f"{kind}{l}":
                    break
            if dbg in (f"ret{l}", f"ml{l}"):
                break

            with Phase(nc, fw, f"E{l}") as ph:
                Wo = ph.sb("Wo", [128, 8, D], BF16)
                for kc in range(8):
                    fw.dma("pool", Wo[:, kc, :], w_out[l][kc * 128:(kc + 1) * 128, :], writes=[Wo], merge=True)
                gbc = [ph.sb(f"gbc{t}", [128, D], F32) for t in range(2)]
                for t in range(2):
                    fw.dma("sp", gbc[t][:], MD[t, 2 * D:3 * D].partition_broadcast(128), reads=[MDres], writes=[gbc[t]])
                mxr = ph.ring("mx", 2, [128, D], BF16)
                ptx = ph.ring("ptx", 2, [128, 8, 128], BF16, psum=True)
                mTr = ph.ring("mT", 2, [128, 8, 128], BF16)
                pyr = ph.ring("py", 4, [128, 512], F32, psum=True)
                hcr = ph.ring("hc", 3, [128, D], F32)
                tyr = ph.ring("ty", 2, [128, D], F32)
                for c in range(first_out_chunk, NCH):
                    pump(1)
                    t = 1 if c < 2 else 0
                    mx = mxr.next()
                    fw.dma("sp", mx[:], MIX[c * 128:(c + 1) * 128, :], reads=[MIXres[c]], writes=[mx])
                    hc = hcr.next()
                    if l == 0:
                        fw.dma("sp", hc[:], xin[c * 128:(c + 1) * 128, :], writes=[hc])
                    else:
                        fw.dma("sp", hc[:], H[c * 128:(c + 1) * 128, :], reads=[Hres[c]], writes=[hc])
                    tx = ptx.next()
                    for kc in range(8):
                        fw.op("pe", lambda e, kc=kc: e.transpose(out=tx[:, kc, :], in_=mx[:, kc * 128:(kc + 1) * 128], identity=ident_b[:]), reads=[mx, ident_b], writes=[tx])
                    mT = mTr.next()
                    fw.op("act", lambda e: e.activation(out=mT[:], in_=tx[:], func=AF.Copy), reads=[tx], writes=[mT])
                    ty = tyr.next()
                    for n in range(2):
                        py = pyr.next()
                        for kc in range(8):
                            fw.op("pe", lambda e, kc=kc, n=n, py=py: e.matmul(py[:], lhsT=mT[:, kc, :], rhs=Wo[:, kc, n * 512:(n + 1) * 512], start=(kc == 0), stop=(kc == 7)),
                                  reads=[mT, Wo], writes=[py])
                        fw.op("dve", lambda e, n=n, py=py: e.tensor_tensor(out=ty[:, n * 512:(n + 1) * 512], in0=py[:], in1=gbc[t][:, n * 512:(n + 1) * 512], op=ALU.mult),
                              reads=[py, gbc[t]], writes=[ty])
                    fw.op("pool", lambda e: e.tensor_tensor(out=hc[:], in0=hc[:], in1=ty[:], op=ALU.add), reads=[hc, ty], writes=[hc])
                    fw.dma("pool", H[c * 128:(c + 1) * 128, :], hc[:], reads=[hc], writes=[Hres[c]], kind="o")
            if dbg == f"E{l}":
                break

            cfg = FF_CFG[l % 2]
            pump(0, upto=cfg["last_idx"])
            moe = (l % 2 == 1)
            nffc, nblk, nexp = cfg["nffc"], cfg["nblk"], cfg["nexp"]
            if moe and not DENSE_MOE:
                with Phase(nc, fw, f"M{l}") as pm_:
                    SEL1 = pm_.sb("SEL1", [128, 32, 8], F32)
                    SEL2 = pm_.sb("SEL2", [128, 32, 8], F32)
                    GWS = pm_.sb("GWS", [128, 32, 8], F32)
                    RANK = pm_.sb("RANK", [128, 32, 8], F32)
                    BASE = pm_.sb("BASE", [128, 32, 8], F32)
                    SLOT_i = pm_.sb("SLOTi", [128, 2, 32], I32)
                    GWK = pm_.sb("GWK", [128, 2, 32], F32)
                    BE_i = pm_.sb("BEi", [128, NTL], I32)
                    IDXW = pm_.sb("IDXW", [128, NTL, 8], I32)
                    gbc = pm_.sb("gbc", [128, D], F32)
                    fw.dma("sp", gbc[:], MD[0, 5 * D:6 * D].partition_broadcast(128), reads=[MDres], writes=[gbc])
                    with Phase(nc, fw, f"MR{l}") as ph:
                        zt = ph.sb("zt", [128, 4, D], BF16)
                        fw.op("pool", lambda e: e.memset(zt[:], 0.0), writes=[zt])
                        for j in range(NTL):
                            fw.dma("sp", XS[j * 512:(j + 1) * 512, :].rearrange("(s p) d -> p s d", p=128), zt[:], reads=[zt], writes=[XSres], holder=zt, kind="o", merge=True)
                        ATS = ph.sb("ATS", [128, 32, D], BF16)
                        g2bc = ph.sb("g2bc", [128, D], F32)
                        sh2bc = ph.sb("sh2bc", [128, D], F32)
                        n2bc = ph.sb("n2bc", [128, D], F32)
                        Wr = ph.sb("Wr", [128, 8, 8], F32)
                        rbb = ph.sb("rbb", [128, 8], F32)
                        Ust = ph.sb("Ust", [128, 128], F32)
                        run = ph.sb("run", [128, 8], F32)
                        grid = ph.sb("grid", [128, 24], F32)
                        fw.dma("sp", g2bc[:], MD[0, 4 * D:5 * D].partition_broadcast(128), reads=[MDres], writes=[g2bc])
                        fw.dma("sp", sh2bc[:], MD[0, 3 * D:4 * D].partition_broadcast(128), reads=[MDres], writes=[sh2bc])
                        fw.dma("sp", n2bc[:], n2row[l].partition_broadcast(128), writes=[n2bc])
                        fw.dma("sp", Wr[:], router_w.rearrange("(kc p) e -> p kc e", p=128), writes=[Wr])
                        fw.dma("sp", rbb[:], router_b[0].partition_broadcast(128), writes=[rbb])
                        fw.dma("sp", grid[:], grid_d, writes=[grid])
                        fw.op("dve", lambda e: e.tensor_scalar(out=g2bc[:], in0=g2bc[:], scalar1=1.0, scalar2=None, op0=ALU.add), reads=[g2bc], writes=[g2bc])
                        fw.op("dve", lambda e: e.tensor_tensor(out=g2bc[:], in0=g2bc[:], in1=n2bc[:], op=ALU.mult), reads=[g2bc, n2bc], writes=[g2bc])
                        fw.op("dve", lambda e: e.tensor_tensor(out=Ust[:], in0=maskF[:], in1=ident_f[:], op=ALU.subtract), reads=[maskF, ident_f], writes=[Ust])
                        fw.op("dve", lambda e: e.memset(run[:], 0.0), writes=[run])
                        str_ = ph.ring("st", 4, [128, 4], F32)
                        xnr = ph.ring("xn", 2, [128, D], F32)
                        tpr = ph.ring("tp", 2, [128, 4, 128], F32, psum=True)
                        hcr = ph.ring("hc", 2, [128, D], F32)
                        tmr = ph.ring("tm", 2, [128, D], F32)
                        a32r = ph.ring("a32", 2, [128, 8, 128], F32)
                        LG = ph.sb("LG", [128, 32, 8], F32)
                        plr = ph.ring("pl", 2, [128, 64], F32, psum=True)
                        for s_ in range(32):
                            c = 2 + s_
                            hc = hcr.next()
                            fw.dma("sp", hc[:], H[c * 128:(c + 1) * 128, :], reads=[Hres[c]], writes=[hc])
                            st4 = str_.next()
                            xn = xnr.next()
                            fw.op("act", lambda e: e.activation(out=xn[:], in_=hc[:], func=AF.Square, accum_out=st4[:, 0:1]), reads=[hc], writes=[xn, st4])
                            fw.op("act", lambda e: e.activation(out=st4[:, 1:2], in_=st4[:, 0:1], func=AF.Sqrt, scale=1.0 / D, bias=cst[:, 0:1]), reads=[st4, cst], writes=[st4])
                            fw.op("dve", lambda e: e.reciprocal(out=st4[:, 2:3], in_=st4[:, 1:2]), reads=[st4], writes=[st4])
                            fw.op("act", lambda e: e.activation(out=xn[:], in_=hc[:], func=AF.Copy, scale=st4[:, 2:3]), reads=[hc, st4], writes=[xn])
                            tm = tmr.next()
                            fw.op("pool", lambda e: e.tensor_tensor(out=tm[:], in0=xn[:], in1=g2bc[:], op=ALU.mult), reads=[xn, g2bc], writes=[tm])
                            fw.op("pool", lambda e, s_=s_: e.tensor_tensor(out=ATS[:, s_, :], in0=tm[:], in1=sh2bc[:], op=ALU.add), reads=[tm, sh2bc], writes=[ATS])
                            a32 = a32r.next()
                            for half in range(2):
                                tp = tpr.next()
                                for j in range(4):
                                    kc = half * 4 + j
                                    fw.op("pe", lambda e, kc=kc, j=j: e.transpose(out=tp[:, j, :], in_=xn[:, kc * 128:(kc + 1) * 128], identity=ident_f[:]),
                                          reads=[xn, ident_f], writes=[tp])
                                for j in range(4):
                                    kc = half * 4 + j
                                    fw.op("dve", lambda e, kc=kc, j=j: e.tensor_scalar(out=a32[:, kc, :], in0=tp[:, j, :], scalar1=g2T[:, kc, 0:1],
                                                                                      scalar2=modT[:, 24 + kc, 0:1], op0=ALU.mult, op1=ALU.add),
                                          reads=[tp, g2T, modT], writes=[a32])
                            pl = plr.next()
                            for kc in range(8):
                                fw.op("pe", lambda e, kc=kc: e.matmul(pl[:, 0:8], lhsT=a32[:, kc, :], rhs=Wr[:, kc, :], start=(kc == 0), stop=(kc == 7)),
                                      reads=[a32, Wr], writes=[pl])
                            fw.op("dve", lambda e, s_=s_: e.tensor_tensor(out=LG[:, s_, :], in0=pl[:, 0:8], in1=rbb[:], op=ALU.add), reads=[pl, rbb], writes=[LG])
                        m12 = ph.sb("m12", [128, 4, 32], F32)
                        L2 = ph.sb("L2", [128, 32, 8], F32)
                        SEL = ph.sb("SEL", [128, 32, 8], F32)
                        EX = ph.sb("EX", [128, 32, 8], F32)
                        CN0 = ph.sb("CN0", [128, 32, 8], F32)
                        CN1 = ph.sb("CN1", [128, 32, 8], F32)
                        CN2 = ph.sb("CN2", [128, 32, 8], F32)
                        prk = ph.ps("prk", [128, 32, 8], F32)
                        pcn = ph.ps("pcn", [128, 32, 8], F32)
                        fw.op("dve", lambda e: e.tensor_reduce(out=m12[:, 0, :], in_=LG[:], axis=AX.X, op=ALU.max), reads=[LG], writes=[m12])
                        fw.op("dve", lambda e: e.tensor_tensor(out=SEL1[:], in0=LG[:], in1=bc(m12[:, 0, :].unsqueeze(2), [128, 32, 8]), op=ALU.is_ge), reads=[LG, m12], writes=[SEL1])
                        fw.op("dve", lambda e: e.scalar_tensor_tensor(out=L2[:], in0=SEL1[:], scalar=-1e30, in1=LG[:], op0=ALU.mult, op1=ALU.add), reads=[SEL1, LG], writes=[L2])
                        fw.op("dve", lambda e: e.tensor_reduce(out=m12[:, 1, :], in_=L2[:], axis=AX.X, op=ALU.max), reads=[L2], writes=[m12])
                        fw.op("dve", lambda e: e.tensor_tensor(out=SEL[:], in0=LG[:], in1=bc(m12[:, 1, :].unsqueeze(2), [128, 32, 8]), op=ALU.is_ge), reads=[LG, m12], writes=[SEL])
                        fw.op("dve", lambda e: e.tensor_tensor(out=SEL2[:], in0=SEL[:], in1=SEL1[:], op=ALU.subtract), reads=[SEL, SEL1], writes=[SEL2])
                        fw.op("dve", lambda e: e.tensor_tensor(out=EX[:], in0=LG[:], in1=bc(m12[:, 0, :].unsqueeze(2), [128, 32, 8]), op=ALU.subtract), reads=[LG, m12], writes=[EX])
                        fw.op("act", lambda e: e.activation(out=EX[:], in_=EX[:], func=AF.Exp), reads=[EX], writes=[EX])
                        fw.op("dve", lambda e: e.tensor_tensor(out=EX[:], in0=EX[:], in1=SEL[:], op=ALU.mult), reads=[EX, SEL], writes=[EX])
                        fw.op("dve", lambda e: e.tensor_reduce(out=m12[:, 2, :], in_=EX[:], axis=AX.X, op=ALU.add), reads=[EX], writes=[m12])
                        fw.op("dve", lambda e: e.reciprocal(out=m12[:, 3, :], in_=m12[:, 2, :]), reads=[m12], writes=[m12])
                        fw.op("dve", lambda e: e.tensor_tensor(out=GWS[:], in0=EX[:], in1=bc(m12[:, 3, :].unsqueeze(2), [128, 32, 8]), op=ALU.mult), reads=[EX, m12], writes=[GWS])
                        for s_ in range(32):
                            fw.op("pe", lambda e, s_=s_: e.matmul(prk[:, s_, :], lhsT=Ust[:], rhs=SEL[:, s_, :], start=True, stop=True), reads=[Ust, SEL], writes=[prk])
                            fw.op("pe", lambda e, s_=s_: e.matmul(pcn[:, s_, :], lhsT=ones_f[:], rhs=SEL[:, s_, :], start=True, stop=True), reads=[ones_f, SEL], writes=[pcn])
                        fw.op("dve", lambda e: e.tensor_copy(out=RANK[:], in_=prk[:]), reads=[prk], writes=[RANK])
                        fw.op("dve", lambda e: e.tensor_copy(out=CN0[:], in_=pcn[:]), reads=[pcn], writes=[CN0])
                        srcT, dstT = CN0, CN1
                        sh = 1
                        while sh < 32:
                            fw.op("dve", lambda e, srcT=srcT, dstT=dstT, sh=sh: e.tensor_tensor(out=dstT[:, sh:32, :], in0=srcT[:, sh:32, :], in1=srcT[:, 0:32 - sh, :], op=ALU.add), reads=[srcT], writes=[dstT])
                            fw.op("dve", lambda e, srcT=srcT, dstT=dstT, sh=sh: e.tensor_copy(out=dstT[:, 0:sh, :], in_=srcT[:, 0:sh, :]), reads=[srcT], writes=[dstT])
                            srcT = dstT
                            dstT = CN2 if dstT is CN1 else CN1
                            sh *= 2
                        fw.op("dve", lambda e: e.tensor_tensor(out=BASE[:], in0=srcT[:], in1=CN0[:], op=ALU.subtract), reads=[srcT, CN0], writes=[BASE])
                        fw.op("dve", lambda e: e.tensor_copy(out=run[:], in_=srcT[:, 31, :]), reads=[srcT], writes=[run])
                        w8 = ph.sb("w8", [128, 8, 8], F32)
                        w24 = ph.sb("w24", [128, NTL, 8], F32)
                        v8 = ph.sb("v8", [128, 6, 8], F32)
                        bef = ph.sb("bef", [128, NTL], F32)
                        SLF = ph.sb("SLF", [128, 32, 8], F32)
                        w32 = ph.sb("w32", [128, 32, 8], F32)
                        s2f = ph.sb("s2f", [128, 2, 32], F32)
                        fw.op("dve", lambda e: e.tensor_tensor(out=w8[:], in0=bc(run[:].unsqueeze(2), [128, 8, 8]), in1=bc(grid[:, 0:8].unsqueeze(1), [128, 8, 8]), op=ALU.is_gt),
                              reads=[run, grid], writes=[w8])
                        fw.op("dve", lambda e: e.tensor_reduce(out=v8[:, 0, :], in_=w8[:], axis=AX.X, op=ALU.add), reads=[w8], writes=[v8])
                        fw.op("dve", lambda e: e.tensor_scalar(out=v8[:, 0, :], in0=v8[:, 0, :], scalar1=512.0, scalar2=None, op0=ALU.mult), reads=[v8], writes=[v8])
                        srcI, dstI = 0, 1
                        for sh in (1, 2, 4):
                            fw.op("dve", lambda e, srcI=srcI, dstI=dstI, sh=sh: e.tensor_tensor(out=v8[:, dstI, sh:8], in0=v8[:, srcI, sh:8], in1=v8[:, srcI, 0:8 - sh], op=ALU.add), reads=[v8], writes=[v8])
                            fw.op("dve", lambda e, srcI=srcI, dstI=dstI, sh=sh: e.tensor_copy(out=v8[:, dstI, 0:sh], in_=v8[:, srcI, 0:sh]), reads=[v8], writes=[v8])
                            srcI = dstI
                            dstI = 2 if dstI == 1 else 1
                        PE_ = srcI
                        fw.op("dve", lambda e: e.tensor_tensor(out=v8[:, 4, :], in0=v8[:, PE_, :], in1=v8[:, 0, :], op=ALU.subtract), reads=[v8], writes=[v8])
                        fw.op("dve", lambda e: e.tensor_tensor(out=w24[:], in0=bc(v8[:, PE_:PE_ + 1, :], [128, NTL, 8]), in1=bc(grid[:].unsqueeze(2), [128, NTL, 8]), op=ALU.is_le),
                              reads=[v8, grid], writes=[w24])
                        fw.op("dve", lambda e: e.tensor_reduce(out=bef[:], in_=w24[:], axis=AX.X, op=ALU.add), reads=[w24], writes=[bef])
                        fw.op("dve", lambda e: e.tensor_scalar(out=bef[:], in0=bef[:], scalar1=7.0, scalar2=None, op0=ALU.min), reads=[bef], writes=[bef])
                        fw.op("dve", lambda e: e.tensor_copy(out=BE_i[:], in_=bef[:]), reads=[bef], writes=[BE_i])
                        posw = ph.sb("posw", [128, 2], F32)
                        idf = ph.sb("idf", [128, NTL, 8], F32)
                        fw.dma("sp", posw[:], pos_d, writes=[posw])
                        fw.op("dve", lambda e: e.tensor_scalar(out=posw[:, 1:2], in0=posw[:, 0:1], scalar1=-1.0, scalar2=None, op0=ALU.add), reads=[posw], writes=[posw])
                        fw.op("dve", lambda e: e.tensor_scalar(out=bef[:], in0=bef[:], scalar1=896.0, scalar2=posw[:, 1:2], op0=ALU.mult, op1=ALU.add), reads=[bef, posw], writes=[bef])
                        for b_ in range(7):
                            fw.op("dve", lambda e, b_=b_: e.tensor_scalar(out=idf[:, :, b_], in0=bef[:], scalar1=float(128 * b_), scalar2=None, op0=ALU.add), reads=[bef], writes=[idf])
                        fw.op("dve", lambda e: e.tensor_copy(out=IDXW[:, :, 0:7], in_=idf[:, :, 0:7]), reads=[idf], writes=[IDXW])
                        fw.op("dve", lambda e: e.tensor_tensor(out=SLF[:], in0=RANK[:], in1=BASE[:], op=ALU.add), reads=[RANK, BASE], writes=[SLF])
                        fw.op("dve", lambda e: e.tensor_tensor(out=SLF[:], in0=SLF[:], in1=bc(v8[:, 4:5, :], [128, 32, 8]), op=ALU.add), reads=[SLF, v8], writes=[SLF])
                        for k, SELk in enumerate((SEL1, SEL2)):
                            fw.op("dve", lambda e, SELk=SELk: e.tensor_tensor(out=w32[:], in0=SLF[:], in1=SELk[:], op=ALU.mult), reads=[SLF, SELk], writes=[w32])
                            fw.op("dve", lambda e, k=k: e.tensor_reduce(out=s2f[:, k, :], in_=w32[:], axis=AX.X, op=ALU.add), reads=[w32], writes=[s2f])
                            fw.op("dve", lambda e, SELk=SELk: e.tensor_tensor(out=w32[:], in0=GWS[:], in1=SELk[:], op=ALU.mult), reads=[GWS, SELk], writes=[w32])
                            fw.op("dve", lambda e, k=k: e.tensor_reduce(out=GWK[:, k, :], in_=w32[:], axis=AX.X, op=ALU.add), reads=[w32], writes=[GWK])
                        fw.op("dve", lambda e: e.tensor_scalar(out=s2f[:], in0=s2f[:], scalar1=float(NSLOT - 1), scalar2=0.0, op0=ALU.min, op1=ALU.max), reads=[s2f], writes=[s2f])
                        fw.op("dve", lambda e: e.tensor_copy(out=SLOT_i[:], in_=s2f[:]), reads=[s2f], writes=[SLOT_i])
                        for s_ in range(32):
                            for k in range(2):
                                fw.idma(XS[:, :], ATS[:, s_, :], SLOT_i[:, k, s_:s_ + 1], True, NSLOT - 1, reads=[ATS, SLOT_i], writes=[XSres], holder=ATS, kind="o",
                                        merge=not (s_ == 0 and k == 0))

                    with Phase(nc, fw, f"MC{l}") as ph:
                        xsr = ph.ring("xs", 2, [128, 4, D], BF16)
                        fTr = ph.ring("fT", 2, [128, 8, 512], BF16)
                        tpb = ph.ring("tpb", 2, [128, 8, 128], BF16, psum=True)
                        acc = ph.sb("acc", [128, 4, D], F32)
                        WGr = ph.ring("WG", 3, [128, 8, 512], BF16)
                        WUr = ph.ring("WU", 3, [128, 8, 512], BF16)
                        WDr = ph.ring("WD", 3, [128, 4, D], BF16)
                        pgu = ph.ring("pgu", 4, [128, 512], F32, psum=True)
                        pyr = ph.ring("py", 2, [128, 512], F32, psum=True)
                        sgr = ph.ring("sg", 2, [128, 512], F32)
                        gTr = ph.ring("gT", 2, [128, 4, 512], BF16)

                        def mprologue(j):
                            xs = xsr.next()
                            fw.dma("sp", xs[:], XS[j * 512:(j + 1) * 512, :].rearrange("(s p) d -> p s d", p=128), reads=[XSres], writes=[xs])
                            fT = fTr.next()
                            for sub in range(4):
                                tp = tpb.next()
                                for kc in range(8):
                                    fw.op("pe", lambda e, kc=kc, sub=sub: e.transpose(out=tp[:, kc, :], in_=xs[:, sub, kc * 128:(kc + 1) * 128], identity=ident_b[:]),
                                          reads=[xs, ident_b], writes=[tp])
                                eng = "act" if sub % 2 == 0 else "dve"
                                if eng == "act":
                                    fw.op("act", lambda e, sub=sub, tp=tp: e.activation(out=fT[:, :, sub * 128:(sub + 1) * 128], in_=tp[:], func=AF.Copy), reads=[tp], writes=[fT])
                                else:
                                    fw.op("dve", lambda e, sub=sub, tp=tp: e.tensor_copy(out=fT[:, :, sub * 128:(sub + 1) * 128], in_=tp[:]), reads=[tp], writes=[fT])
                            return fT, None

                        cfgm = FF_CFG[1]
                        nxt_pro = mprologue(0)
                        for j in range(NTL):
                            fT, ev = nxt_pro
                            T = 512

                            def gate_up(b):
                                WG, WU, WDt = WGr.next(), WUr.next(), WDr.next()
                                for (Wt_, key_) in ((WG, "WG"), (WU, "WU"), (WDt, "WD")):
                                    fw.idma(Wt_[:].rearrange("p k n -> p (k n)"), cfgm[key_].rearrange("e b p k n -> (e b p) (k n)"), IDXW[:, j, b:b + 1], False, None,
                                            reads=[cfgm["res"], IDXW], writes=[Wt_], holder=Wt_, kind="i")
                                gT = gTr.next()
                                for jj in range(4):
                                    pg_, pu_ = pgu.next(), pgu.next()
                                    for (pp, W_) in ((pg_, WG), (pu_, WU)):
                                        for kc in range(8):
                                            fw.op("pe", lambda e, kc=kc, jj=jj, pp=pp, W_=W_: e.matmul(pp[:, 0:T], lhsT=W_[:, kc, jj * 128:(jj + 1) * 128], rhs=fT[:, kc, 0:T],
                                                                                                     start=(kc == 0), stop=(kc == 7)), reads=[W_, fT], writes=[pp])
                                    sg = sgr.next()
                                    fw.op("act", lambda e, pg_=pg_, sg=sg: e.activation(out=sg[:, 0:T], in_=pg_[:, 0:T], func=AF.Silu), reads=[pg_], writes=[sg])
                                    fw.op("dve", lambda e, pu_=pu_, sg=sg, jj=jj: e.tensor_tensor(out=gT[:, jj, 0:T], in0=pu_[:, 0:T], in1=sg[:, 0:T], op=ALU.mult), reads=[pu_, sg], writes=[gT])
                                return gT, WDt

                            def down(gT, WDt, first):
                                for sub in range(4):
                                    for n in range(2):
                                        py = pyr.next()
                                        for jj in range(4):
                                            fw.op("pe", lambda e, jj=jj, sub=sub, n=n, py=py: e.matmul(py[:], lhsT=gT[:, jj, sub * 128:(sub + 1) * 128], rhs=WDt[:, jj, n * 512:(n + 1) * 512],
                                                                                                     start=(jj == 0), stop=(jj == 3)), reads=[gT, WDt], writes=[py])
                                        av = acc[:, sub, n * 512:(n + 1) * 512]
                                        if first:
                                            fw.op("dve", lambda e, py=py, av=av: e.tensor_copy(out=av, in_=py[:]), reads=[py], writes=[acc])
                                        else:
                                            fw.op("dve", lambda e, py=py, av=av: e.tensor_tensor(out=av, in0=py[:], in1=av, op=ALU.add), reads=[py, acc], writes=[acc])

                            prev = None
                            for b in range(7):
                                gT, WDt = gate_up(b)
                                if prev is not None:
                                    down(prev[0], prev[1], prev[2])
                                prev = (gT, WDt, b == 0)
                                if b == 2 and j + 1 < NTL:
                                    nxt_pro = mprologue(j + 1)
                            down(prev[0], prev[1], prev[2])
                            fw.dma("pool", YS[j * 512:(j + 1) * 512, :].rearrange("(s p) d -> p s d", p=128), acc[:], reads=[acc], writes=[YSres], holder=acc, kind="o", merge=True)

                    with Phase(nc, fw, f"MO{l}") as ph:
                        y1r = ph.ring("y1", 4, [128, D], F32)
                        y2r = ph.ring("y2", 4, [128, D], F32)
                        hcr = ph.ring("hc", 4, [128, D], F32)
                        loaded = {}

                        def mo_load(s_):
                            c = 2 + s_
                            y1, y2, hc = y1r.next(), y2r.next(), hcr.next()
                            fw.dma("sp", hc[:], H[c * 128:(c + 1) * 128, :], reads=[Hres[c]], writes=[hc])
                            fw.idma(y1[:, :], YS[:, :], SLOT_i[:, 0, s_:s_ + 1], False, NSLOT - 1, reads=[YSres, SLOT_i], writes=[y1], holder=y1, kind="i")
                            fw.idma(y2[:, :], YS[:, :], SLOT_i[:, 1, s_:s_ + 1], False, NSLOT - 1, reads=[YSres, SLOT_i], writes=[y2], holder=y2, kind="i")
                            loaded[s_] = (y1, y2, hc)

                        def mo_comp(s_):
                            y1, y2, hc = loaded.pop(s_)
                            fw.op("dve", lambda e: e.tensor_scalar(out=y1[:], in0=y1[:], scalar1=GWK[:, 0, s_:s_ + 1], scalar2=None, op0=ALU.mult), reads=[y1, GWK], writes=[y1])
                            fw.op("dve", lambda e: e.scalar_tensor_tensor(out=y1[:], in0=y2[:], scalar=GWK[:, 1, s_:s_ + 1], in1=y1[:], op0=ALU.mult, op1=ALU.add),
                                  reads=[y1, y2, GWK], writes=[y1])
                            fw.op("dve", lambda e: e.tensor_tensor(out=y1[:], in0=y1[:], in1=gbc[:], op=ALU.mult), reads=[y1, gbc], writes=[y1])
                            fw.op("dve", lambda e: e.tensor_tensor(out=y1[:], in0=y1[:], in1=hc[:], op=ALU.add), reads=[y1, hc], writes=[y1])
                            fw.dma("sp", out[s_ * 128:(s_ + 1) * 128, :], y1[:], reads=[y1], writes=[OUTres], holder=y1, kind="o", merge=True)
                        LOOK = 2
                        for s_ in range(32 + LOOK):
                            if s_ < 32:
                                mo_load(s_)
                            if s_ >= LOOK:
                                mo_comp(s_ - LOOK)
                if dbg == f"F{l}":
                    break
                continue
            with Phase(nc, fw, f"F{l}") as ph:
                gbc = [ph.sb(f"gbc{t}", [128, D], F32) for t in range(2)]
                for t in range(2):
                    fw.dma("sp", gbc[t][:], MD[t, 5 * D:6 * D].partition_broadcast(128), reads=[MDres], writes=[gbc[t]])
                if moe:
                    Wr = ph.sb("Wr", [128, 8, 8], F32)
                    rbb = ph.sb("rbb", [128, 8], F32)
                    fw.dma("sp", Wr[:], router_w.rearrange("(kc p) e -> p kc e", p=128), writes=[Wr])
                    fw.dma("sp", rbb[:], router_b[0].partition_broadcast(128), writes=[rbb])
                rings = dict(st=ph.ring("st", 4, [128, 4], F32), xn=ph.ring("xn", 2, [128, D], F32),
                             tp=ph.ring("tp", 2, [128, 4, 128], F32, psum=True))
                hTr = ph.ring("hT", 2, [128, 4, D], F32)
                acc = ph.sb("acc", [128, 4, D], F32)
                fTr = ph.ring("fT", 2, [128, 8, 512], BF16)
                a32 = ph.sb("a32", [128, 8, 128], F32)
                gwr = ph.ring("gw", 2, [128, 4, 8], F32)
                lgt = ph.ring("lgt", 2, [128, 32], F32)
                WGr = ph.ring("WG", 3, [128, 8, cfg["bw"]], BF16)
                WUr = ph.ring("WU", 3, [128, 8, cfg["bw"]], BF16)
                WDr = ph.ring("WD", 5, [128, nffc, D], BF16)
                pgu = ph.ring("pgu", 4, [128, 512], F32, psum=True)
                pyr = ph.ring("py", 2, [128, 512], F32, psum=True)
                sgr = ph.ring("sg", 2, [128, 512], F32)
                gTr = ph.ring("gT", 5, [128, nffc, 512], BF16)
                tiles = ([] if last else [(0, 2)]) + [(2 + 4 * i, 4) for i in range(8)]

                def prologue(c0, nsub):
                    t = 1 if c0 < 2 else 0
                    hT, fT, gw = hTr.next(), fTr.next(), gwr.next()
                    for s in range(nsub):
                        c = c0 + s
                        fw.dma("sp", hT[:, s, :], H[c * 128:(c + 1) * 128, :], reads=[Hres[c]], writes=[hT], merge=(s > 0))
                    for s in range(nsub):
                        st4 = rings["st"].next()
                        xn = rings["xn"].next()
                        fw.op("act", lambda e, s=s: e.activation(out=xn[:], in_=hT[:, s, :], func=AF.Square, accum_out=st4[:, 0:1]), reads=[hT], writes=[xn, st4])
                        fw.op("act", lambda e: e.activation(out=st4[:, 1:2], in_=st4[:, 0:1], func=AF.Sqrt, scale=1.0 / D, bias=cst[:, 0:1]), reads=[st4, cst], writes=[st4])
                        fw.op("dve", lambda e: e.reciprocal(out=st4[:, 2:3], in_=st4[:, 1:2]), reads=[st4], writes=[st4])
                        fw.op("act", lambda e, s=s: e.activation(out=xn[:], in_=hT[:, s, :], func=AF.Copy, scale=st4[:, 2:3]), reads=[hT, st4], writes=[xn])
                        for half in range(2):
                            tp = rings["tp"].next()
                            for j in range(4):
                                kc = half * 4 + j
                                fw.op("pe", lambda e, kc=kc, j=j: e.transpose(out=tp[:, j, :], in_=xn[:, kc * 128:(kc + 1) * 128], identity=ident_f[:]),
                                      reads=[xn, ident_f], writes=[tp])
                            for j in range(4):
                                kc = half * 4 + j
                                fw.op("dve", lambda e, kc=kc, j=j, s=s: e.tensor_scalar(out=fT[:, kc, s * 128:(s + 1) * 128], in0=tp[:, j, :], scalar1=g2T[:, kc, t:t + 1],
                                                                                       scalar2=modT[:, 24 + kc, t:t + 1], op0=ALU.mult, op1=ALU.add),
                                      reads=[tp, g2T, modT], writes=[fT])
                                if moe:
                                    fw.op("dve", lambda e, kc=kc, j=j: e.tensor_scalar(out=a32[:, kc, :], in0=tp[:, j, :], scalar1=g2T[:, kc, t:t + 1],
                                                                                      scalar2=modT[:, 24 + kc, t:t + 1], op0=ALU.mult, op1=ALU.add),
                                          reads=[tp, g2T, modT], writes=[a32])
                        if moe:
                            pl = rings["tp"].next()
                            plv = pl[:].rearrange("p a b -> p (a b)")
                            for kc in range(8):
                                fw.op("pe", lambda e, kc=kc: e.matmul(plv[:, 0:8], lhsT=a32[:, kc, :], rhs=Wr[:, kc, :], start=(kc == 0), stop=(kc == 7)),
                                      reads=[a32, Wr], writes=[pl])
                            lg_ = lgt.next()
                            L = lg_[:, 0:8]
                            fw.op("dve", lambda e: e.tensor_tensor(out=L, in0=plv[:, 0:8], in1=rbb[:], op=ALU.add), reads=[pl, rbb], writes=[lg_])
                            fw.op("dve", lambda e: e.tensor_reduce(out=lg_[:, 24:25], in_=L, axis=AX.X, op=ALU.max), reads=[lg_], writes=[lg_])
                            fw.op("dve", lambda e: e.tensor_scalar(out=lg_[:, 8:16], in0=L, scalar1=lg_[:, 24:25], scalar2=-1e30, op0=ALU.is_ge, op1=ALU.mult), reads=[lg_], writes=[lg_])
                            fw.op("dve", lambda e: e.tensor_tensor(out=lg_[:, 8:16], in0=lg_[:, 8:16], in1=L, op=ALU.add), reads=[lg_], writes=[lg_])
                            fw.op("dve", lambda e: e.tensor_reduce(out=lg_[:, 25:26], in_=lg_[:, 8:16], axis=AX.X, op=ALU.max), reads=[lg_], writes=[lg_])
                            fw.op("dve", lambda e: e.tensor_scalar(out=lg_[:, 8:16], in0=L, scalar1=lg_[:, 25:26], scalar2=None, op0=ALU.is_ge), reads=[lg_], writes=[lg_])
                            fw.op("dve", lambda e: e.tensor_scalar(out=lg_[:, 16:24], in0=L, scalar1=lg_[:, 24:25], scalar2=None, op0=ALU.subtract), reads=[lg_], writes=[lg_])
                            fw.op("act", lambda e: e.activation(out=lg_[:, 16:24], in_=lg_[:, 16:24], func=AF.Exp), reads=[lg_], writes=[lg_])
                            fw.op("dve", lambda e: e.tensor_tensor(out=lg_[:, 16:24], in0=lg_[:, 16:24], in1=lg_[:, 8:16], op=ALU.mult), reads=[lg_], writes=[lg_])
                            fw.op("dve", lambda e: e.tensor_reduce(out=lg_[:, 26:27], in_=lg_[:, 16:24], axis=AX.X, op=ALU.add), reads=[lg_], writes=[lg_])
                            fw.op("dve", lambda e: e.reciprocal(out=lg_[:, 27:28], in_=lg_[:, 26:27]), reads=[lg_], writes=[lg_])
                            fw.op("dve", lambda e, s=s: e.tensor_scalar(out=gw[:, s, :], in0=lg_[:, 16:24], scalar1=lg_[:, 27:28], scalar2=None, op0=ALU.mult), reads=[lg_], writes=[gw])
                    return hT, fT, gw

                blocks = [(ex, b) for ex in range(nexp) for b in range(nblk)]
                nxt_pro = prologue(*tiles[0])
                for ti, (c0, nsub) in enumerate(tiles):
                    t = 1 if c0 < 2 else 0
                    T = nsub * 128
                    hT, fT, gw = nxt_pro

                    def gate_up(ex, b):
                        WG, WU, WDt = WGr.next(), WUr.next(), WDr.next()
                        fw.dma("sp", WG[:], cfg["WG"][ex, b], reads=[cfg["res"]], writes=[WG])
                        fw.dma("sp", WU[:], cfg["WU"][ex, b], reads=[cfg["res"]], writes=[WU])
                        fw.dma("sp", WDt[:], cfg["WD"][ex, b], reads=[cfg["res"]], writes=[WDt])
                        gT = gTr.next()
                        for j in range(nffc):
                            pg_, pu_ = pgu.next(), pgu.next()
                            for (pp, W_) in ((pg_, WG), (pu_, WU)):
                                for kc in range(8):
                                    fw.op("pe", lambda e, kc=kc, j=j, pp=pp, W_=W_: e.matmul(pp[:, 0:T], lhsT=W_[:, kc, j * 128:(j + 1) * 128], rhs=fT[:, kc, 0:T],
                                                                                           start=(kc == 0), stop=(kc == 7)), reads=[W_, fT], writes=[pp])
                            sg = sgr.next()
                            fw.op("act", lambda e, pg_=pg_, sg=sg: e.activation(out=sg[:, 0:T], in_=pg_[:, 0:T], func=AF.Silu), reads=[pg_], writes=[sg])
                            fw.op("dve", lambda e, pu_=pu_, sg=sg, j=j: e.tensor_tensor(out=gT[:, j, 0:T], in0=pu_[:, 0:T], in1=sg[:, 0:T], op=ALU.mult), reads=[pu_, sg], writes=[gT])
                        return gT, WDt

                    def down(ex, items, first):
                        for s in range(nsub):
                            for n in range(2):
                                py = pyr.next()
                                nmm = len(items) * nffc
                                q_ = 0
                                for (gT, WDt) in items:
                                    for j in range(nffc):
                                        fw.op("pe", lambda e, j=j, s=s, n=n, py=py, gT=gT, WDt=WDt, q_=q_: e.matmul(
                                            py[:], lhsT=gT[:, j, s * 128:(s + 1) * 128], rhs=WDt[:, j, n * 512:(n + 1) * 512],
                                            start=(q_ == 0), stop=(q_ == nmm - 1)), reads=[gT, WDt], writes=[py])
                                        q_ += 1
                                av = acc[:, s, n * 512:(n + 1) * 512]
                                if moe:
                                    if first:
                                        fw.op("dve", lambda e, py=py, av=av, s=s: e.tensor_scalar(out=av, in0=py[:], scalar1=gw[:, s, ex:ex + 1], scalar2=None, op0=ALU.mult),
                                              reads=[py, gw], writes=[acc])
                                    else:
                                        fw.op("dve", lambda e, py=py, av=av, s=s: e.scalar_tensor_tensor(out=av, in0=py[:], scalar=gw[:, s, ex:ex + 1], in1=av,
                                                                                                       op0=ALU.mult, op1=ALU.add), reads=[py, gw, acc], writes=[acc])
                                else:
                                    if first:
                                        fw.op("dve", lambda e, py=py, av=av: e.tensor_copy(out=av, in_=py[:]), reads=[py], writes=[acc])
                                    else:
                                        fw.op("dve", lambda e, py=py, av=av: e.tensor_tensor(out=av, in0=py[:], in1=av, op=ALU.add), reads=[py, acc], writes=[acc])

                    GRP = 1 if moe else 2
                    pendl = []
                    isfirst = True
                    for bi, (ex, b) in enumerate(blocks):
                        gT, WDt = gate_up(ex, b)
                        pendl.append((gT, WDt))
                        if len(pendl) == GRP + 1:
                            down(ex if GRP == 1 else 0, pendl[:GRP], isfirst) if GRP > 1 else down(blocks[bi - 1][0], pendl[:1], isfirst)
                            pendl = pendl[GRP:]
                            isfirst = False
                        if bi == min(2, len(blocks) - 1) and ti + 1 < len(tiles):
                            nxt_pro = prologue(*tiles[ti + 1])
                    while pendl:
                        down(blocks[-1][0], pendl[:GRP], isfirst)
                        pendl = pendl[GRP:]
                        isfirst = False
                    for s in range(nsub):
                        c = c0 + s
                        fw.op("pool", lambda e, s=s: e.tensor_tensor(out=acc[:, s, :], in0=acc[:, s, :], in1=gbc[t][:], op=ALU.mult), reads=[acc, gbc[t]], writes=[acc])
                        fw.op("pool", lambda e, s=s: e.tensor_tensor(out=acc[:, s, :], in0=acc[:, s, :], in1=hT[:, s, :], op=ALU.add), reads=[acc, hT], writes=[acc])
                        if last:
                            fw.dma("pool", out[(c - 2) * 128:(c - 1) * 128, :], acc[:, s, :], reads=[acc], writes=[OUTres], holder=acc, kind="o", merge=True)
                        else:
                            fw.dma("pool", H[c * 128:(c + 1) * 128, :], acc[:, s, :], reads=[acc], writes=[Hres[c]], holder=acc, kind="o")
            if dbg == f"F{l}":
                break
        for cfg in FF_CFG:
            r_ = cfg["res"]
            if r_.isem is not None and r_.icnt:
                for k_ in fw.engs:
                    fw._wait(k_, [(r_.isem, r_.icnt)])
        fw.barrier(release=False)
    return nc


_PERM = None


def _w_in_perm():
    q = []
    for i in range(4):
        q += list(range(i * 64, (i + 1) * 64)) + list(range((4 + i) * 64, (5 + i) * 64))
    ak, av, rq, rk, rv, rg, mq, mk, mv, mo, mg = 512, 640, 768, 1024, 1280, 1536, 1792, 2048, 2304, 2560, 2816
    r = lambda a, n: list(range(a, a + n))
    perm = q + r(ak, 128) + r(av, 128) + r(rv, 256) + r(rq, 256) + r(rk, 256) + r(rg, 256) + r(mv, 256) + r(mo, 256) + r(mq, 256) + r(mk, 256) + r(mg, 16)
    assert len(perm) == NIN and len(set(perm)) == NIN
    return np.array(perm)


def _consts():
    ident = np.eye(128, dtype=np.float32)
    s = np.arange(128)[:, None]
    l_ = np.arange(128)[None, :]
    maskF = (s <= l_).astype(np.float32)
    maskB = (s >= l_).astype(np.float32)
    n_freq = 16
    inv_freq = (10000.0 ** (-np.arange(n_freq, dtype=np.float32) / n_freq)).astype(np.float32)
    tok = np.arange(4096)
    row = (tok // 64).astype(np.float32)
    col = (tok % 64).astype(np.float32)
    ang = np.concatenate([row[:, None] * inv_freq, col[:, None] * inv_freq], -1).astype(np.float32)
    cos = np.cos(ang).astype(np.float32).reshape(32, 128, 32).transpose(1, 0, 2)
    sin = np.sin(ang).astype(np.float32).reshape(32, 128, 32).transpose(1, 0, 2)
    pos = np.stack([np.arange(128) + 1.0, 128.0 - np.arange(128)], 1).astype(np.float32)
    grid = np.broadcast_to((np.arange(24, dtype=np.float32) * 512.0)[None, :], (128, 24)).copy()
    return dict(grid512=grid, ident=ident, maskF=maskF, maskB=maskB, cos=np.ascontiguousarray(cos), sin=np.ascontiguousarray(sin), pos=pos)


def make_in_maps(x, c, ctx, c_ctx, mod_w, mod_b, norm1_w, norm2_w, w_in, attn_qn_w, attn_kn_w, ret_decay,
                 ret_norm_w, mlstm_conv_w, mlstm_gate_b, mlstm_norm_w, w_out, ffn_w_gate, ffn_w_up, ffn_w_down,
                 router_w, router_b, moe_w_gate, moe_w_up, moe_w_down, moe_nexp=8):
    f = lambda a: np.ascontiguousarray(np.asarray(a, dtype=np.float32))
    perm = _w_in_perm()
    shared = dict(
        mod_w=f(mod_w),
        modbT=f(np.asarray(mod_b).reshape(DEPTH, 48, 128).transpose(0, 2, 1)),
        n1T=f(np.asarray(norm1_w).reshape(DEPTH, 8, 128).transpose(0, 2, 1)),
        n2T=f(np.asarray(norm2_w).reshape(DEPTH, 8, 128).transpose(0, 2, 1)),
        n2row=f(norm2_w),
        w_in=f(np.asarray(w_in)[:, :, perm]),
        qn_w=f(attn_qn_w), kn_w=f(attn_kn_w),
        ret_decay=f(np.asarray(ret_decay).reshape(DEPTH, 8)),
        ret_norm_w=f(ret_norm_w), conv_w=f(mlstm_conv_w),
        gate_b=f(np.asarray(mlstm_gate_b).reshape(DEPTH, 16)),
        mlstm_norm_w=f(mlstm_norm_w), w_out=f(w_out),
        ffn_wg=f(ffn_w_gate), ffn_wu=f(ffn_w_up), ffn_wd=f(ffn_w_down),
        router_w=f(np.asarray(router_w)[0]), router_b=f(np.asarray(router_b)[0:1]),
        moe_wg=f(np.asarray(moe_w_gate)[0][:moe_nexp]), moe_wu=f(np.asarray(moe_w_up)[0][:moe_nexp]), moe_wd=f(np.asarray(moe_w_down)[0][:moe_nexp]),
    )
    shared.update(_consts())
    x = np.asarray(x); ctx = np.asarray(ctx); c = np.asarray(c); c_ctx = np.asarray(c_ctx)
    maps = []
    for b in range(8):
        m = dict(shared)
        m["xin"] = f(np.concatenate([ctx[b], x[b]], axis=0))
        m["cT"] = f(np.stack([c[b].reshape(8, 128).T, c_ctx.reshape(8, 128).T], axis=-1))
        maps.append(m)
    return maps


def kernel(**inputs):
    nc = build_program()
    maps = make_in_maps(**inputs)
    res = run_bass_kernel_spmd(nc, maps, core_ids=list(range(8)))
    return np.stack([np.asarray(r["out"], dtype=np.float32) for r in res.results], axis=0)
```

```python
import contextlib
import numpy as np
import concourse.bass as bass
import concourse.mybir as mybir
from concourse.bass_utils import run_bass_kernel_spmd

AF = mybir.ActivationFunctionType
ALU = mybir.AluOpType
AX = mybir.AxisListType
F32 = mybir.dt.float32
BF16 = mybir.dt.bfloat16

NCH = 34
NTOK = NCH * 128
D = 1024
NIN = 2832
EPS = 1e-6
DEPTH = 2
DENSE_MOE = False
FWD_ORDER = list(range(NCH))
BWD_ORDER = [1, 0] + list(range(NCH - 1, 1, -1))
PQ, PK, PV, PRV, PRQ, PRK, PRG, PMV, PMO, PMQ, PMK = 0, 512, 640, 768, 1024, 1280, 1536, 1792, 2048, 2304, 2560
PCOLS = 2816


class Res:
    __slots__ = ("name", "w", "r", "isem", "osem", "icnt", "ocnt", "persist")

    def __init__(self, name):
        self.name = name
        self.w = None
        self.r = {}
        self.isem = None
        self.osem = None
        self.icnt = 0
        self.ocnt = 0
        self.persist = False


class Tile:
    def __init__(self, h, name):
        self.h = h
        self.r = Res(name)

    def __getitem__(self, k):
        return self.h[k]


class FW:
    ROT = 30000

    def __init__(self, nc):
        self.nc = nc
        self.engs = {"pe": nc.tensor, "act": nc.scalar, "dve": nc.vector,
                     "pool": nc.gpsimd, "sp": nc.sync}
        self.csem = {}
        self.ccnt = {}
        self.known = {k: {} for k in self.engs}
        self.nsem = 0
        self.allsems = []
        self.sempool = []
        self.sempool_sw = []
        self.semq = {}
        self.dma_live = []
        self.old_counters = []
        for k in self.engs:
            self._newc(k)

    def sem(self, name):
        self.nsem += 1
        s = self.nc.alloc_semaphore(name=f"{name}_{self.nsem}")
        self.allsems.append(s)
        return s

    def _newc(self, k):
        if k in self.csem and self.ccnt[k] > 0:
            self.old_counters.append((self.csem[k], self.ccnt[k]))
        self.csem[k] = self.sem("c" + k)
        self.ccnt[k] = 0

    def _wait(self, eng, evs, noself=False):
        need = {}
        kn = self.known[eng]
        for ev in evs:
            if ev is None:
                continue
            s, v = ev
            if noself and s is self.csem[eng]:
                continue
            sid = id(s)
            if kn.get(sid, 0) >= v:
                continue
            if sid not in need or need[sid][1] < v:
                need[sid] = (s, v)
        for sid, (s, v) in need.items():
            self.engs[eng].wait_ge(s, v)
            kn[sid] = v

    @staticmethod
    def _deps(reads, writes, merge=False):
        evs = []
        for r in reads:
            evs.append(r.w)
        for w in writes:
            if not merge:
                evs.append(w.w)
            evs.extend(w.r.values())
        return evs

    @staticmethod
    def _commit(ev, reads, writes, merge=False):
        sid = id(ev[0])
        for r in reads:
            r.r[sid] = ev
        for w in writes:
            w.w = ev
            if not merge:
                w.r = {}

    def op(self, eng, fn, reads=(), writes=(), noself=None):
        reads = [x.r if isinstance(x, Tile) else x for x in reads]
        writes = [x.r if isinstance(x, Tile) else x for x in writes]
        if noself is None:
            noself = (eng == "pe")
        self._wait(eng, self._deps(reads, writes), noself=noself)
        ins = fn(self.engs[eng])
        self.ccnt[eng] += 1
        ins.then_inc(self.csem[eng], 1)
        ev = (self.csem[eng], self.ccnt[eng])
        self._commit(ev, reads, writes)
        if self.ccnt[eng] >= self.ROT:
            self._newc(eng)
        return ev

    def _getsem(self, q):
        pool_ = self.sempool_sw if q == "pool" else self.sempool
        if pool_:
            return pool_.pop()
        return (self.sem("dsw" if q == "pool" else "d"), 0)

    def dma(self, q, out, in_, reads=(), writes=(), holder=None, kind=None, merge=False, **kw):
        reads = [x.r if isinstance(x, Tile) else x for x in reads]
        writes = [x.r if isinstance(x, Tile) else x for x in writes]
        self._wait(q, self._deps(reads, writes, merge=merge))
        ins = self.engs[q].dma_start(out=out, in_=in_, **kw)
        if holder is None:
            holder, kind = (reads[0], "o") if kind == "o" else (writes[0], "i")
        elif isinstance(holder, Tile):
            holder = holder.r
        if kind == "i":
            if holder.isem is None:
                holder.isem, holder.icnt = self._getsem(q)
                self.semq[id(holder.isem)] = q == "pool"
                if not holder.persist:
                    self.dma_live.append(holder)
            assert self.semq[id(holder.isem)] == (q == "pool"), holder.name
            holder.icnt += 16
            ins.then_inc(holder.isem, 16)
            ev = (holder.isem, holder.icnt)
        else:
            if holder.osem is None:
                holder.osem, holder.ocnt = self._getsem(q)
                self.semq[id(holder.osem)] = q == "pool"
                self.dma_live.append(holder)
            assert self.semq[id(holder.osem)] == (q == "pool"), holder.name
            holder.ocnt += 16
            ins.then_inc(holder.osem, 16)
            ev = (holder.osem, holder.ocnt)
        self._commit(ev, reads, writes, merge=merge)
        return ev

    def idma(self, out, in_, idx_ap, scatter, bound, reads=(), writes=(), holder=None, kind="i", merge=False):
        q = "pool"
        reads = [x.r if isinstance(x, Tile) else x for x in reads]
        writes = [x.r if isinstance(x, Tile) else x for x in writes]
        self._wait(q, self._deps(reads, writes, merge=merge))
        off = bass.IndirectOffsetOnAxis(ap=idx_ap, axis=0)
        ins = self.nc.gpsimd.indirect_dma_start(out=out, out_offset=(off if scatter else None), in_=in_, in_offset=(None if scatter else off))
        if isinstance(holder, Tile):
            holder = holder.r
        if kind == "i":
            if holder.isem is None:
                holder.isem, holder.icnt = self._getsem(q)
                self.semq[id(holder.isem)] = True
                if not holder.persist:
                    self.dma_live.append(holder)
            holder.icnt += 16
            ins.then_inc(holder.isem, 16)
            ev = (holder.isem, holder.icnt)
        else:
            if holder.osem is None:
                holder.osem, holder.ocnt = self._getsem(q)
                self.semq[id(holder.osem)] = True
                self.dma_live.append(holder)
            holder.ocnt += 16
            ins.then_inc(holder.osem, 16)
            ev = (holder.osem, holder.ocnt)
        self._commit(ev, reads, writes, merge=merge)
        return ev

    def barrier(self, release=True):
        evs = [(self.csem[k], self.ccnt[k]) for k in self.engs if self.ccnt[k] > 0]
        evs += self.old_counters
        for h in self.dma_live:
            if h.isem is not None:
                evs.append((h.isem, h.icnt))
            if h.osem is not None:
                evs.append((h.osem, h.ocnt))
        for k in self.engs:
            self._wait(k, evs, noself=True)
        if release:
            for h in self.dma_live:
                if h.isem is not None:
                    (self.sempool_sw if self.semq[id(h.isem)] else self.sempool).append((h.isem, h.icnt))
                    h.isem = None
                if h.osem is not None:
                    (self.sempool_sw if self.semq[id(h.osem)] else self.sempool).append((h.osem, h.ocnt))
                    h.osem = None
            self.dma_live = []


class Ring:
    def __init__(self, tiles):
        self.tiles = tiles
        self.i = 0

    def next(self):
        t = self.tiles[self.i % len(self.tiles)]
        self.i += 1
        return t


class Phase:
    cnt = 0

    def __init__(self, nc, fw, name):
        self.nc, self.fw, self.name = nc, fw, name

    def __enter__(self):
        self.st = contextlib.ExitStack()
        return self

    def __exit__(self, *a):
        self.fw.barrier()
        self.st.close()
        return False

    def sb(self, name, shape, dt):
        Phase.cnt += 1
        nm = f"{self.name}_{name}_{Phase.cnt}"
        return Tile(self.st.enter_context(self.nc.sbuf_tensor(nm, shape, dt)), nm)

    def ps(self, name, shape, dt=F32):
        Phase.cnt += 1
        nm = f"{self.name}_{name}_{Phase.cnt}"
        return Tile(self.st.enter_context(self.nc.psum_tensor(nm, shape, dt)), nm)

    def ring(self, name, n, shape, dt, psum=False):
        f = self.ps if psum else self.sb
        return Ring([f(f"{name}{i}", shape, dt) for i in range(n)])


def bc(ap, shape):
    return ap.to_broadcast(list(shape))


def build_program(dbg=None, force_ne8=False):
    nc = bass.Bass("TRN2", target_bir_lowering=False)
    NE = 1 if (dbg and not dbg.endswith("1") and not force_ne8) else 8
    fw = FW(nc)

    def din(name, shape, dt=F32):
        return nc.dram_tensor(name, list(shape), dt, kind="ExternalInput").ap()

    def dscr(name, shape, dt=F32):
        return nc.dram_tensor(name, list(shape), dt, kind=("ExternalOutput" if (dbg and not name.startswith("W")) else "Internal")).ap()

    xin = din("xin", [NTOK, D])
    cT = din("cT", [128, 8, 2])
    mod_w = din("mod_w", [DEPTH, D, 6 * D])
    modbT = din("modbT", [DEPTH, 128, 48])
    n1T = din("n1T", [DEPTH, 128, 8])
    n2T = din("n2T", [DEPTH, 128, 8])
    w_in = din("w_in", [DEPTH, D, NIN])
    qn_w = din("qn_w", [DEPTH, 64])
    kn_w = din("kn_w", [DEPTH, 64])
    ret_decay = din("ret_decay", [DEPTH, 8])
    ret_norm_w = din("ret_norm_w", [DEPTH, 256])
    conv_w = din("conv_w", [DEPTH, 3, 512])
    gate_b = din("gate_b", [DEPTH, 16])
    mlstm_norm_w = din("mlstm_norm_w", [DEPTH, 256])
    w_out = din("w_out", [DEPTH, D, D])
    ffn_wg = din("ffn_wg", [1, D, 2816])
    ffn_wu = din("ffn_wu", [1, D, 2816])
    ffn_wd = din("ffn_wd", [1, 2816, D])
    router_w = din("router_w", [D, 8])
    router_b = din("router_b", [1, 8])
    moe_wg = din("moe_wg", [NE, D, 3584])
    moe_wu = din("moe_wu", [NE, D, 3584])
    moe_wd = din("moe_wd", [NE, 3584, D])
    ident_d = din("ident", [128, 128])
    maskF_d = din("maskF", [128, 128])
    maskB_d = din("maskB", [128, 128])
    cos_d = din("cos", [128, 32, 32])
    sin_d = din("sin", [128, 32, 32])
    pos_d = din("pos", [128, 2])
    n2row = din("n2row", [DEPTH, D])
    grid_d = din("grid512", [128, 24])
    out = nc.dram_tensor("out", [4096, D], F32, kind="ExternalOutput").ap()

    H = dscr("H", [NTOK, D])
    P = dscr("P", [NTOK, PCOLS], BF16)
    GT = dscr("GT", [16, NTOK])
    MIX = dscr("MIX", [NTOK, D], BF16)
    MD = dscr("MD", [2, 6 * D])
    NSLOT = 12288
    NTL = NSLOT // 512
    XS = dscr("XS", [NSLOT, D], BF16)
    YS = dscr("YS", [NSLOT, D], F32)
    XSres = Res("XS")
    YSres = Res("YS")
    I32 = mybir.dt.int32
    FF_CFG = [dict(nexp=1, nblk=11, nffc=2, wg=ffn_wg, wu=ffn_wu, wd=ffn_wd),
              dict(nexp=NE, nblk=7, nffc=4, wg=moe_wg, wu=moe_wu, wd=moe_wd)]
    for i, cfg in enumerate(FF_CFG):
        bw = cfg["nffc"] * 128
        cfg["bw"] = bw
        cfg["WG"] = dscr(f"WG{i}", [cfg["nexp"], cfg["nblk"], 128, 8, bw], BF16)
        cfg["WU"] = dscr(f"WU{i}", [cfg["nexp"], cfg["nblk"], 128, 8, bw], BF16)
        cfg["WD"] = dscr(f"WD{i}", [cfg["nexp"], cfg["nblk"], 128, cfg["nffc"], D], BF16)
        cfg["res"] = Res(f"wconv{i}")

    Hres = [Res(f"H{c}") for c in range(NCH)]
    Pres = [Res(f"P{c}") for c in range(NCH)]
    Pcv = [Res(f"Pcv{c}") for c in range(NCH)]
    GTres = [Res(f"GT{c}") for c in range(NCH)]
    MIXres = [Res(f"MIX{c}") for c in range(NCH)]
    MDres = Res("MD")
    OUTres = Res("OUT")

    gst = contextlib.ExitStack()
    with gst:
        G = Phase(nc, fw, "G")
        G.st = gst
        ident_f = G.sb("identf", [128, 128], F32)
        ident_b = G.sb("identb", [128, 128], BF16)
        maskF = G.sb("maskF", [128, 128], F32)
        maskB = G.sb("maskB", [128, 128], F32)
        cst = G.sb("cst", [128, 4], F32)
        modT = G.sb("modT", [128, 48, 2], F32)
        g1T = G.sb("g1T", [128, 8, 2], F32)
        g2T = G.sb("g2T", [128, 8, 2], F32)
        ones_f = G.sb("onesf", [128, 128], F32)

        fw.dma("sp", ident_f[:], ident_d, writes=[ident_f])
        fw.dma("sp", maskF[:], maskF_d, writes=[maskF])
        fw.dma("sp", maskB[:], maskB_d, writes=[maskB])
        fw.op("dve", lambda e: e.tensor_copy(out=ident_b[:], in_=ident_f[:]), reads=[ident_f], writes=[ident_b])
        fw.op("dve", lambda e: e.memset(cst[:, 0:1], EPS), writes=[cst])
        fw.op("dve", lambda e: e.memset(cst[:, 1:2], 1.0), writes=[cst])
        fw.op("dve", lambda e: e.memset(cst[:, 2:3], 0.0), writes=[cst])
        fw.op("dve", lambda e: e.memset(ones_f[:], 1.0), writes=[ones_f])

        wconv_list = []
        for cfg in FF_CFG:
            cfg["res"].persist = True
            cfg["first_idx"] = len(wconv_list)
            for e_ in range(cfg["nexp"]):
                for b_ in range(cfg["nblk"]):
                    n0 = b_ * cfg["bw"]
                    for (dst, src) in ((cfg["WG"], cfg["wg"]), (cfg["WU"], cfg["wu"])):
                        wconv_list.append((dst[e_, b_], src[e_].rearrange("(kc p) n -> p kc n", p=128)[:, :, n0:n0 + cfg["bw"]], cfg["res"]))
                    wconv_list.append((cfg["WD"][e_, b_], cfg["wd"][e_][n0:n0 + cfg["bw"], :].rearrange("(j p) n -> p j n", p=128), cfg["res"]))
            cfg["last_idx"] = len(wconv_list)
        wconv_pos = [0]

        def pump(n=1, upto=None):
            while (n > 0 or (upto is not None and wconv_pos[0] < upto)) and wconv_pos[0] < len(wconv_list):
                dst, src, r = wconv_list[wconv_pos[0]]
                wconv_pos[0] += 1
                n -= 1
                fw.dma("pool", dst, src, writes=[r], holder=r, kind="i", merge=True)

        for l in range(DEPTH):
            last = (l == DEPTH - 1)
            first_out_chunk = 2 if last else 0
            with Phase(nc, fw, f"S{l}") as ph:
                cTt = ph.sb("cT", [128, 8, 2], F32)
                sc = ph.sb("sc", [128, 8, 2], F32)
                mb = ph.sb("mb", [128, 48], F32)
                n1 = ph.sb("n1", [128, 8], F32)
                n2 = ph.sb("n2", [128, 8], F32)
                mwr = ph.ring("mw", 2, [128, 8, 512], F32)
                pm = ph.ps("pm", [128, 48, 2], F32)
                ptm = ph.ps("ptm", [48, 128], F32)
                mds = ph.sb("mds", [48, 128], F32)
                fw.dma("sp", cTt[:], cT, writes=[cTt])
                fw.dma("sp", mb[:], modbT[l], writes=[mb])
                fw.dma("sp", n1[:], n1T[l], writes=[n1])
                fw.dma("sp", n2[:], n2T[l], writes=[n2])
                fw.op("act", lambda e: e.activation(out=sc[:], in_=cTt[:], func=AF.Silu), reads=[cTt], writes=[sc])
                for piece in range(12):
                    mw = mwr.next()
                    fw.dma("sp", mw[:], mod_w[l].rearrange("(kc p) n -> p kc n", p=128)[:, :, piece * 512:(piece + 1) * 512],
                           writes=[mw])
                    for jj in range(4):
                        j = piece * 4 + jj
                        for kc in range(8):
                            fw.op("pe", lambda e, j=j, jj=jj, kc=kc, mw=mw: e.matmul(
                                pm[:, j, :], lhsT=mw[:, kc, jj * 128:(jj + 1) * 128], rhs=sc[:, kc, :],
                                start=(kc == 0), stop=(kc == 7)), reads=[mw, sc], writes=[pm])
                fw.op("dve", lambda e: e.tensor_tensor(out=modT[:], in0=pm[:], in1=bc(mb[:].unsqueeze(2), [128, 48, 2]), op=ALU.add),
                      reads=[pm, mb], writes=[modT])
                for (gT, lo, nn) in ((g1T, 8, n1), (g2T, 32, n2)):
                    fw.op("dve", lambda e, gT=gT, lo=lo: e.tensor_scalar(out=gT[:], in0=modT[:, lo:lo + 8, :], scalar1=1.0, scalar2=None, op0=ALU.add),
                          reads=[modT], writes=[gT])
                    fw.op("dve", lambda e, gT=gT, nn=nn: e.tensor_tensor(out=gT[:], in0=gT[:], in1=bc(nn[:].unsqueeze(2), [128, 8, 2]), op=ALU.mult),
                          reads=[gT, nn], writes=[gT])
                for t in range(2):
                    fw.op("pe", lambda e, t=t: e.transpose(out=ptm[:], in_=modT[:, :, t], identity=ident_f[:]),
                          reads=[modT, ident_f], writes=[ptm])
                    fw.op("dve", lambda e: e.tensor_copy(out=mds[:], in_=ptm[:]), reads=[ptm], writes=[mds])
                    fw.dma("sp", MD[t].rearrange("(j p) -> j p", p=128), mds[:], reads=[mds], writes=[MDres], kind="o")

            with Phase(nc, fw, f"A{l}") as ph:
                Win = ph.sb("Win", [128, 8, NIN], BF16)
                WC = ph.sb("WC", [128, 3, 8, 512], BF16)
                cwb = ph.sb("cwb", [128, 3, 512], F32)
                qnb = ph.sb("qnb", [128, 64], F32)
                knb = ph.sb("knb", [128, 64], F32)
                cos_t = ph.sb("cos", [128, 32, 32], F32)
                sin_t = ph.sb("sin", [128, 32, 32], F32)
                for kc in range(8):
                    fw.dma("pool", Win[:, kc, :], w_in[l][kc * 128:(kc + 1) * 128, :], writes=[Win], merge=True)
                fw.dma("sp", cwb[:], conv_w[l].partition_broadcast(128), writes=[cwb])
                fw.dma("sp", qnb[:], qn_w[l].partition_broadcast(128), writes=[qnb])
                fw.dma("sp", knb[:], kn_w[l].partition_broadcast(128), writes=[knb])
                fw.dma("sp", cos_t[:], cos_d, writes=[cos_t])
                fw.dma("sp", sin_t[:], sin_d, writes=[sin_t])
                for k in range(3):
                    fw.op("dve", lambda e, k=k: e.tensor_tensor(out=WC[:, k, :, :], in0=Win[:, :, 2304:2816],
                                                               in1=bc(cwb[:, k:k + 1, :], [128, 8, 512]), op=ALU.mult),
                          reads=[Win, cwb], writes=[WC])
                rings = dict(st=ph.ring("st", 4, [128, 4], F32), xn=ph.ring("xn", 2, [128, D], F32),
                             tp=ph.ring("tp", 2, [128, 4, 128], F32, psum=True))
                hcr = ph.ring("hc", 3, [128, D], F32)
                NLIN = 6
                LIN = [ph.sb(f"lin{i}", [128, 8, 130], BF16) for i in range(NLIN)]
                LINH = [Res(f"linh{i}") for i in range(NLIN)]
                pjr = ph.ring("pj", 5, [128, 512], F32, psum=True)
                pgr = ph.ring("pg", 1, [16, 128], F32, psum=True)
                Pcr = ph.ring("Pc", 3, [128, 2304], BF16)
                Pvr = ph.ring("Pv", 2, [128, 512], BF16)
                gsr = ph.ring("gs", 2, [16, 128], F32)
                tA = ph.ring("tA", 2, [128, 512], F32)
                tB = ph.ring("tB", 2, [128, 512], F32)
                tC = ph.ring("tC", 2, [128, 512], F32)
                tD = ph.ring("tD", 2, [128, 512], F32)
                s8r = ph.ring("s8", 4, [128, 16], F32)

                def rope(src, nh, dst_tile, dst_ap, lc):
                    sv = src[:, 0:nh * 64].rearrange("p (h d) -> p h d", d=64)
                    c_ = tC.next()
                    d_ = tD.next()
                    cv = c_[:, 0:nh * 64].rearrange("p (h d) -> p h d", d=64)
                    dv = d_[:, 0:nh * 64].rearrange("p (h d) -> p h d", d=64)
                    ov = dst_ap.rearrange("p (h d) -> p h d", d=64)
                    cb = bc(cos_t[:, lc:lc + 1, :], [128, nh, 32])
                    sb_ = bc(sin_t[:, lc:lc + 1, :], [128, nh, 32])
                    fw.op("pool", lambda e: e.tensor_tensor(out=cv[:, :, 0:32], in0=sv[:, :, 0:32], in1=cb, op=ALU.mult), reads=[src, cos_t], writes=[c_])
                    fw.op("pool", lambda e: e.tensor_tensor(out=cv[:, :, 32:64], in0=sv[:, :, 0:32], in1=sb_, op=ALU.mult), reads=[src, sin_t], writes=[c_])
                    fw.op("dve", lambda e: e.tensor_tensor(out=dv[:, :, 0:32], in0=sv[:, :, 32:64], in1=sb_, op=ALU.mult), reads=[src, sin_t], writes=[d_])
                    fw.op("dve", lambda e: e.tensor_tensor(out=dv[:, :, 32:64], in0=sv[:, :, 32:64], in1=cb, op=ALU.mult), reads=[src, cos_t], writes=[d_])
                    fw.op("pool", lambda e: e.tensor_tensor(out=ov[:, :, 0:32], in0=cv[:, :, 0:32], in1=dv[:, :, 0:32], op=ALU.subtract), reads=[c_, d_], writes=[dst_tile])
                    fw.op("dve", lambda e: e.tensor_tensor(out=ov[:, :, 32:64], in0=cv[:, :, 32:64], in1=dv[:, :, 32:64], op=ALU.add), reads=[c_, d_], writes=[dst_tile])

                def qknorm(pj, col0, nh, wb, dst_tile, dst_ap, c):
                    a_ = tA.next()
                    b_ = tB.next()
                    s8 = s8r.next()
                    n = nh * 64
                    pv = pj[:, col0:col0 + n]
                    fw.op("act", lambda e: e.activation(out=a_[:, 0:n], in_=pv, func=AF.Square), reads=[pj], writes=[a_])
                    fw.op("dve", lambda e: e.tensor_reduce(out=s8[:, 0:nh], in_=a_[:, 0:n].rearrange("p (h d) -> p h d", d=64), axis=AX.X, op=ALU.add),
                          reads=[a_], writes=[s8])
                    fw.op("act", lambda e: e.activation(out=s8[:, 8:8 + nh], in_=s8[:, 0:nh], func=AF.Sqrt, scale=1.0 / 64, bias=cst[:, 0:1]),
                          reads=[s8, cst], writes=[s8])
                    fw.op("dve", lambda e: e.reciprocal(out=s8[:, 0:nh], in_=s8[:, 8:8 + nh]), reads=[s8], writes=[s8])
                    fw.op("dve", lambda e: e.tensor_tensor(out=a_[:, 0:n].rearrange("p (h d) -> p h d", d=64), in0=pv.rearrange("p (h d) -> p h d", d=64),
                                                           in1=bc(s8[:, 0:nh].unsqueeze(2), [128, nh, 64]), op=ALU.mult), reads=[pj, s8], writes=[a_])
                    if c < 2:
                        fw.op("dve", lambda e: e.tensor_tensor(out=dst_ap.rearrange("p (h d) -> p h d", d=64), in0=a_[:, 0:n].rearrange("p (h d) -> p h d", d=64),
                                                               in1=bc(wb[:].unsqueeze(1), [128, nh, 64]), op=ALU.mult), reads=[a_, wb], writes=[dst_tile])
                    else:
                        fw.op("dve", lambda e: e.tensor_tensor(out=b_[:, 0:n].rearrange("p (h d) -> p h d", d=64), in0=a_[:, 0:n].rearrange("p (h d) -> p h d", d=64),
                                                               in1=bc(wb[:].unsqueeze(1), [128, nh, 64]), op=ALU.mult), reads=[a_, wb], writes=[b_])
                        rope(b_, nh, dst_tile, dst_ap, c - 2)

                def proj(lin, n0, n1_, pj, ncols):
                    for kc in range(8):
                        fw.op("pe", lambda e, kc=kc: e.matmul(pj[:, 0:ncols], lhsT=lin[:, kc, 1:129], rhs=Win[:, kc, n0:n1_],
                                                             start=(kc == 0), stop=(kc == 7)), reads=[lin, Win], writes=[pj])

                def front(c):
                    if True:
                        t = 1 if c < 2 else 0
                        lin = LIN[c % NLIN]
                        hc = hcr.next()
                        if l == 0:
                            fw.dma("sp", hc[:], xin[c * 128:(c + 1) * 128, :], writes=[hc])
                        else:
                            fw.dma("sp", hc[:], H[c * 128:(c + 1) * 128, :], reads=[Hres[c]], writes=[hc])
                        st4 = rings["st"].next()
                        xn = rings["xn"].next()
                        fw.op("act", lambda e: e.activation(out=xn[:], in_=hc[:], func=AF.Square, accum_out=st4[:, 0:1]), reads=[hc], writes=[xn, st4])
                        fw.op("act", lambda e: e.activation(out=st4[:, 1:2], in_=st4[:, 0:1], func=AF.Sqrt, scale=1.0 / D, bias=cst[:, 0:1]), reads=[st4, cst], writes=[st4])
                        fw.op("dve", lambda e: e.reciprocal(out=st4[:, 2:3], in_=st4[:, 1:2]), reads=[st4], writes=[st4])
                        fw.op("act", lambda e: e.activation(out=xn[:], in_=hc[:], func=AF.Copy, scale=st4[:, 2:3]), reads=[hc, st4], writes=[xn])
                        for half in range(2):
                            tp = rings["tp"].next()
                            for j in range(4):
                                kc = half * 4 + j
                                fw.op("pe", lambda e, kc=kc, j=j: e.transpose(out=tp[:, j, :], in_=xn[:, kc * 128:(kc + 1) * 128], identity=ident_f[:]),
                                      reads=[xn, ident_f], writes=[tp])
                            for j in range(4):
                                kc = half * 4 + j
                                fw.op("dve", lambda e, kc=kc, j=j: e.tensor_scalar(out=lin[:, kc, 1:129], in0=tp[:, j, :], scalar1=g1T[:, kc, t:t + 1],
                                                                                  scalar2=modT[:, kc, t:t + 1], op0=ALU.mult, op1=ALU.add),
                                      reads=[tp, g1T, modT], writes=[lin])
                        linh = LINH[c % NLIN]
                        if c in (0, 2):
                            fw.op("pool", lambda e: e.memset(lin[:, :, 0:1], 0.0), writes=[linh])
                        else:
                            prev = LIN[(c - 1) % NLIN]
                            fw.op("pool", lambda e: e.tensor_copy(out=lin[:, :, 0:1], in_=prev[:, :, 128:129]), reads=[prev], writes=[linh])
                            fw.op("pool", lambda e: e.tensor_copy(out=prev[:, :, 129:130], in_=lin[:, :, 1:2]), reads=[lin], writes=[LINH[(c - 1) % NLIN]])
                        if c in (1, NCH - 1):
                            fw.op("pool", lambda e: e.memset(lin[:, :, 129:130], 0.0), writes=[linh])
                def back(c):
                    front(c)
                    yield
                    if True:
                        lin = LIN[c % NLIN]
                        Pc = Pcr.next()
                        pj = pjr.next()
                        proj(lin, 0, 512, pj, 512)
                        qknorm(pj, 0, 8, qnb, Pc, Pc[:, PQ:PQ + 512], c)
                        pj = pjr.next()
                        proj(lin, 512, 1024, pj, 512)
                        qknorm(pj, 0, 2, knb, Pc, Pc[:, PK:PK + 128], c)
                        fw.op("act", lambda e, pj=pj: e.activation(out=Pc[:, PV:PV + 384], in_=pj[:, 128:512], func=AF.Copy), reads=[pj], writes=[Pc])
                        yield
                        pj = pjr.next()
                        proj(lin, 1024, 1536, pj, 512)
                        if c < 2:
                            fw.op("act", lambda e, pj=pj: e.activation(out=Pc[:, PRQ:PRQ + 512], in_=pj[:, 0:512], func=AF.Copy), reads=[pj], writes=[Pc])
                        else:
                            b_ = tB.next()
                            fw.op("act", lambda e, pj=pj, b_=b_: e.activation(out=b_[:], in_=pj[:, 0:512], func=AF.Copy), reads=[pj], writes=[b_])
                            rope(b_, 8, Pc, Pc[:, PRQ:PRQ + 512], c - 2)
                        pj = pjr.next()
                        proj(lin, 1536, 2048, pj, 512)
                        fw.op("act", lambda e, pj=pj: e.activation(out=Pc[:, PRG:PRG + 256], in_=pj[:, 0:256], func=AF.Silu), reads=[pj], writes=[Pc])
                        fw.op("act", lambda e, pj=pj: e.activation(out=Pc[:, PMV:PMV + 256], in_=pj[:, 256:512], func=AF.Copy), reads=[pj], writes=[Pc])
                        pj = pjr.next()
                        proj(lin, 2048, 2304, pj, 256)
                        fw.op("act", lambda e, pj=pj: e.activation(out=Pc[:, PMO:PMO + 256], in_=pj[:, 0:256], func=AF.Sigmoid), reads=[pj], writes=[Pc])
                        fw.dma("pool", P[c * 128:(c + 1) * 128, 0:2304], Pc[:], reads=[Pc], writes=[Pres[c]], kind="o")
                        pg = pgr.next()
                        for kc in range(8):
                            fw.op("pe", lambda e, kc=kc: e.matmul(pg[:], lhsT=Win[:, kc, 2816:2832], rhs=lin[:, kc, 1:129], start=(kc == 0), stop=(kc == 7)),
                                  reads=[lin, Win], writes=[pg])
                        gs = gsr.next()
                        fw.op("dve", lambda e: e.tensor_copy(out=gs[:], in_=pg[:]), reads=[pg], writes=[gs])
                        fw.dma("pool", GT[:, c * 128:(c + 1) * 128], gs[:], reads=[gs], writes=[GTres[c]], kind="o")
                    yield
                    cc = c
                    if True:
                        linp = LIN[cc % NLIN]
                        pj = pjr.next()
                        for k in range(3):
                            for kc in range(8):
                                fw.op("pe", lambda e, k=k, kc=kc: e.matmul(pj[:], lhsT=linp[:, kc, k:k + 128], rhs=WC[:, k, kc, :],
                                                                           start=(k == 0 and kc == 0), stop=(k == 2 and kc == 7)),
                                      reads=[linp, LINH[cc % NLIN], WC], writes=[pj])
                        Pv = Pvr.next()
                        fw.op("act", lambda e, pj=pj: e.activation(out=Pv[:], in_=pj[:], func=AF.Silu), reads=[pj], writes=[Pv])
                        fw.dma("pool", P[cc * 128:(cc + 1) * 128, 2304:2816], Pv[:], reads=[Pv], writes=[Pcv[cc]], kind="o")

                gens = []
                for c in list(range(NCH)) + [None]:
                    if c is not None:
                        pump(1)
                        gens.append(back(c))
                    for g_ in list(gens):
                        try:
                            next(g_)
                        except StopIteration:
                            gens.remove(g_)
                while gens:
                    for g_ in list(gens):
                        try:
                            next(g_)
                        except StopIteration:
                            gens.remove(g_)
            if dbg == f"A{l}":
                break

            with Phase(nc, fw, f"B{l}") as ph:
                kT = ph.sb("kT", [128, NTOK], BF16)
                Va = ph.sb("Va", [128, NCH, 2, 128], BF16)
                kvr = ph.ring("kv", 2, [128, 256], BF16)
                tqr = ph.ring("tq", 2, [128, 4, 128], BF16, psum=True)
                fw.op("pool", lambda e: e.memset(Va[:], 1.0), writes=[Va])
                for c in range(NCH):
                    kv = kvr.next()
                    fw.dma("sp", kv[:], P[c * 128:(c + 1) * 128, PK:PK + 256], reads=[Pres[c]], writes=[kv])
                    tk = tqr.next()
                    fw.op("pe", lambda e: e.transpose(out=tk[:, 0, :], in_=kv[:, 0:128], identity=ident_b[:]), reads=[kv, ident_b], writes=[tk])
                    fw.op("dve", lambda e, c=c: e.tensor_copy(out=kT[:, c * 128:(c + 1) * 128], in_=tk[:, 0, :]), reads=[tk], writes=[kT])
                    fw.op("pool", lambda e, c=c: e.tensor_copy(out=Va[:, c, :, 0:64], in_=kv[:, 128:256].rearrange("p (g d) -> p g d", d=64)),
                          reads=[kv], writes=[Va])
                qbr = ph.ring("qb", 2, [128, 512], BF16)
                qTr = ph.ring("qT", 2, [128, 2, 512], BF16)
                for qz in qTr.tiles:
                    fw.op("pool", lambda e, qz=qz: e.memset(qz[:], 0.0), writes=[qz])
                psr = ph.ring("pss", 3, [128, 512], F32, psum=True)
                PTr = ph.ring("PT", 5, [128, 512], BF16)
                oTr = ph.ring("oT", 2, [128, 512], F32, psum=True)
                otr = ph.ring("ot", 1, [128, 4, 128], F32, psum=True)
                oSr = ph.ring("oS", 2, [128, 512], F32)
                pending_epi = []
                recr = ph.ring("rec", 2, [128, 4], F32)
                attr = ph.ring("att", 3, [128, 512], BF16)
                for qb in range(first_out_chunk, NCH):
                    pump(1)
                    keys = [0, 1] if qb < 2 else list(range(NCH))
                    qt = qbr.next()
                    fw.dma("sp", qt[:], P[qb * 128:(qb + 1) * 128, PQ:PQ + 512], reads=[Pres[qb]], writes=[qt])
                    tq = tqr.next()
                    for i in range(4):
                        fw.op("pe", lambda e, i=i: e.transpose(out=tq[:, i, :], in_=qt[:, i * 128:(i + 1) * 128], identity=ident_b[:]),
                              reads=[qt, ident_b], writes=[tq])
                    qT = qTr.next()
                    fw.op("dve", lambda e: e.tensor_copy(out=qT[0:64, 0, :], in_=tq[0:64].rearrange("p a b -> p (a b)")), reads=[tq], writes=[qT])
                    fw.op("dve", lambda e: e.tensor_copy(out=qT[64:128, 1, :], in_=tq[64:128].rearrange("p a b -> p (a b)")), reads=[tq], writes=[qT])
                    att = attr.next()
                    for g in range(2):
                        oT = oTr.next()

                        def smm(kc, g=g):
                            pss = psr.next()
                            fw.op("pe", lambda e: e.matmul(pss[:], lhsT=kT[:, kc * 128:(kc + 1) * 128], rhs=qT[:, g, :], start=True, stop=True),
                                  reads=[kT, qT], writes=[pss])
                            return pss
                        pend = [smm(keys[0])]
                        if len(keys) > 1:
                            pend.append(smm(keys[1]))
                        for ki, kc in enumerate(keys):
                            pss = pend.pop(0)
                            if ki + 2 < len(keys):
                                pend.append(smm(keys[ki + 2]))
                            PT = PTr.next()
                            fw.op("act", lambda e, pss=pss, PT=PT: e.activation(out=PT[:], in_=pss[:], func=AF.Exp, scale=0.125), reads=[pss], writes=[PT])
                            fw.op("pe", lambda e, kc=kc, g=g, PT=PT, oT=oT, ki=ki: e.matmul(
                                oT[:, :], lhsT=Va[:, kc, g, :], rhs=PT[:, :], start=(ki == 0), stop=(ki == len(keys) - 1)), reads=[PT, Va], writes=[oT])
                            if ki == min(3, len(keys) - 1) and pending_epi:
                                pending_epi.pop(0)()

                        def epi(oT=oT, att=att, g=g, qb=qb, lastg=(g == 1)):
                            oS = oSr.next()
                            fw.op("dve", lambda e: e.tensor_copy(out=oS[:], in_=oT[:, :]), reads=[oT], writes=[oS])
                            ot = otr.next()
                            for i in range(4):
                                fw.op("pe", lambda e, i=i: e.transpose(out=ot[:, i, :], in_=oS[:, i * 128:(i + 1) * 128], identity=ident_f[:]),
                                      reads=[oS, ident_f], writes=[ot])
                            rec = recr.next()
                            fw.op("dve", lambda e: e.reciprocal(out=rec[:], in_=ot[:, :, 64]), reads=[ot], writes=[rec])
                            fw.op("dve", lambda e: e.tensor_tensor(
                                out=att[:, g * 256:(g + 1) * 256].rearrange("p (h d) -> p h d", d=64), in0=ot[:, :, 0:64],
                                in1=bc(rec[:].unsqueeze(2), [128, 4, 64]), op=ALU.mult), reads=[ot, rec], writes=[att])
                            if lastg:
                                fw.dma("pool", MIX[qb * 128:(qb + 1) * 128, 0:512], att[:], reads=[att], writes=[MIXres[qb]], kind="o")
                        pending_epi.append(epi)
                while pending_epi:
                    pending_epi.pop(0)()
            if dbg == f"B{l}":
                break

            for kind in ("ret", "ml"):
                if kind == "ret":
                    qcol, kcol, vcol, gcol, ocol = PRQ, PRK, PRV, PRG, 512
                    qres = kres = Pres
                else:
                    qcol, kcol, vcol, gcol, ocol = PMQ, PMK, PMV, PMO, 768
                    qres = kres = Pcv
                with Phase(nc, fw, f"{kind}{l}") as ph:
                    E = [ph.sb(f"E{d_}", [128, 4, NCH], F32) for d_ in range(2)]
                    Fm = [ph.sb(f"F{d_}", [128, 4, NCH], F32) for d_ in range(2)]
                    PRE = [ph.sb(f"PRE{d_}", [64, NCH, 4], F32) for d_ in range(2)]
                    POST = [ph.sb(f"POST{d_}", [64, 4], F32) for d_ in range(2)]
                    nwb = ph.sb("nwb", [128, 256], F32)
                    fw.dma("sp", nwb[:], (ret_norm_w if kind == "ret" else mlstm_norm_w)[l].partition_broadcast(128), writes=[nwb])
                    with Phase(nc, fw, f"{kind}{l}tab") as pt:
                        if kind == "ret":
                            rd = pt.sb("rd", [128, 8], F32)
                            lg = pt.sb("lg", [128, 8], F32)
                            pos = pt.sb("pos", [128, 2], F32)
                            tmp = pt.sb("tmp", [128, 8], F32)
                            tmp2 = pt.sb("tmp2", [128, 8], F32)
                            fw.dma("sp", rd[:], ret_decay[l].partition_broadcast(128), writes=[rd])
                            fw.dma("sp", pos[:], pos_d, writes=[pos])
                            fw.op("act", lambda e: e.activation(out=lg[:], in_=rd[:], func=AF.Exp), reads=[rd], writes=[lg])
                            fw.op("act", lambda e: e.activation(out=lg[:], in_=lg[:], func=AF.Ln, scale=-1.0, bias=cst[:, 1:2]), reads=[lg, cst], writes=[lg])
                            for d_ in range(2):
                                fw.op("dve", lambda e, d_=d_: e.tensor_scalar(out=tmp[:, d_ * 4:d_ * 4 + 4], in0=lg[:, d_ * 4:d_ * 4 + 4], scalar1=pos[:, d_:d_ + 1],
                                                                             scalar2=None, op0=ALU.mult), reads=[lg, pos], writes=[tmp])
                            fw.op("act", lambda e: e.activation(out=tmp2[:], in_=tmp[:], func=AF.Exp), reads=[tmp], writes=[tmp2])
                            for d_ in range(2):
                                fw.op("dve", lambda e, d_=d_: e.tensor_copy(out=Fm[d_][:], in_=bc(tmp2[:, d_ * 4:d_ * 4 + 4].unsqueeze(2), [128, 4, NCH])),
                                      reads=[tmp2], writes=[Fm[d_]])
                            fw.op("act", lambda e: e.activation(out=tmp2[:], in_=tmp[:], func=AF.Exp, scale=-1.0), reads=[tmp], writes=[tmp2])
                            fw.op("dve", lambda e: e.tensor_scalar(out=tmp2[:], in0=tmp2[:], scalar1=0.125, scalar2=None, op0=ALU.mult), reads=[tmp2], writes=[tmp2])
                            for d_ in range(2):
                                fw.op("dve", lambda e, d_=d_: e.tensor_copy(out=E[d_][:], in_=bc(tmp2[:, d_ * 4:d_ * 4 + 4].unsqueeze(2), [128, 4, NCH])),
                                      reads=[tmp2], writes=[E[d_]])
                            fw.op("act", lambda e: e.activation(out=tmp[:], in_=lg[:], func=AF.Exp, scale=128.0), reads=[lg], writes=[tmp])
                            for d_ in range(2):
                                fw.op("dve", lambda e, d_=d_: e.tensor_copy(out=POST[d_][:], in_=tmp[0:64, d_ * 4:d_ * 4 + 4]), reads=[tmp], writes=[POST[d_]])
                                fw.op("dve", lambda e, d_=d_: e.memset(PRE[d_][:], 1.0), writes=[PRE[d_]])
                        else:
                            Gt = pt.sb("Gt", [NCH, 16, 128], F32)
                            gb = pt.sb("gb", [NCH, 16], F32)
                            cs0 = pt.sb("cs0", [NCH, 8, 128], F32)
                            cs1 = pt.sb("cs1", [NCH, 8, 128], F32)
                            t2 = pt.sb("t2", [NCH, 8, 128], F32)
                            U = pt.sb("U", [NCH, 8, 128], F32)
                            NB = pt.sb("NB", [NCH, 8, 128], F32)
                            ub = pt.sb("ub", [NCH, 16], F32)
                            for c in range(NCH):
                                pass
                            fw.dma("sp", Gt[:], GT.rearrange("g (c t) -> c g t", t=128), reads=GTres, writes=[Gt])
                            fw.dma("sp", gb[:], gate_b[l].partition_broadcast(NCH), writes=[gb])
                            for d_ in range(2):
                                fw.op("dve", lambda e, d_=d_: e.memset(POST[d_][:], 1.0), writes=[POST[d_]])
                            fw.op("dve", lambda e: e.tensor_tensor(out=Gt[:], in0=Gt[:], in1=bc(gb[:].unsqueeze(2), [NCH, 16, 128]), op=ALU.add),
                                  reads=[Gt, gb], writes=[Gt])
                            fw.op("act", lambda e: e.activation(out=t2[:], in_=Gt[:, 8:16, :], func=AF.Exp, scale=-1.0), reads=[Gt], writes=[t2])
                            fw.op("act", lambda e: e.activation(out=t2[:], in_=t2[:], func=AF.Ln, scale=1.0, bias=cst[0:NCH, 1:2]), reads=[t2, cst], writes=[t2])
                            src, dst = t2, cs0
                            sh = 1
                            while sh < 128:
                                fw.op("dve", lambda e, src=src, dst=dst, sh=sh: e.tensor_tensor(out=dst[:, :, sh:128], in0=src[:, :, sh:128], in1=src[:, :, 0:128 - sh], op=ALU.add),
                                      reads=[src], writes=[dst])
                                fw.op("dve", lambda e, src=src, dst=dst, sh=sh: e.tensor_copy(out=dst[:, :, 0:sh], in_=src[:, :, 0:sh]), reads=[src], writes=[dst])
                                src = dst
                                dst = cs1 if dst is cs0 else cs0
                                if sh == 1:
                                    pass
                                sh *= 2
                            cs = src
                            other = dst
                            fw.op("dve", lambda e: e.tensor_copy(out=NB[:, 0:4, :], in_=cs[:, 0:4, :]), reads=[cs], writes=[NB])
                            fw.op("dve", lambda e: e.tensor_tensor(out=NB[:, 4:8, :], in0=t2[:, 4:8, :], in1=cs[:, 4:8, :], op=ALU.subtract), reads=[cs, t2], writes=[NB])
                            fw.op("dve", lambda e: e.tensor_tensor(out=NB[:, 4:8, :], in0=NB[:, 4:8, :], in1=bc(cs[:, 4:8, 127:128], [NCH, 4, 128]), op=ALU.add),
                                  reads=[cs, NB], writes=[NB])
                            fw.op("dve", lambda e: e.tensor_tensor(out=U[:], in0=Gt[:, 0:8, :], in1=NB[:], op=ALU.add), reads=[Gt, NB], writes=[U])
                            fw.op("dve", lambda e: e.tensor_reduce(out=ub[:, 0:8], in_=U[:], axis=AX.X, op=ALU.max), reads=[U], writes=[ub])
                            fw.op("dve", lambda e: e.tensor_scalar(out=ub[:, 8:16], in0=cs[:, :, 127], scalar1=-1.0, scalar2=None, op0=ALU.mult), reads=[cs], writes=[ub])
                            pq = pt.ps("pq", [4, 4, NCH], F32)
                            UB = pt.sb("UB", [4, 4, NCH], F32)
                            for qi in range(4):
                                fw.op("pe", lambda e, qi=qi: e.transpose(out=pq[:, qi, :], in_=ub[:, qi * 4:qi * 4 + 4], identity=ident_f[0:NCH, 0:NCH]),
                                      reads=[ub, ident_f], writes=[pq])
                            fw.op("dve", lambda e: e.tensor_copy(out=UB[:], in_=pq[:]), reads=[pq], writes=[UB])
                            mcur = pt.sb("mcur", [4, 2, NCH + 1], F32)
                            Mend = pt.sb("Mend", [4, 2, NCH], F32)
                            dd = pt.sb("dd", [4, 2, NCH], F32)
                            fw.op("dve", lambda e: e.memset(mcur[:], 0.0), writes=[mcur])
                            for d_, order in enumerate((FWD_ORDER, BWD_ORDER)):
                                for idx, c in enumerate(order):
                                    fw.op("dve", lambda e, d_=d_, idx=idx, c=c: e.tensor_tensor(out=Mend[:, d_, c:c + 1], in0=mcur[:, d_, idx:idx + 1],
                                                                                                in1=UB[:, d_, c:c + 1], op=ALU.max), reads=[mcur, UB], writes=[Mend])
                                    fw.op("dve", lambda e, d_=d_, idx=idx, c=c: e.tensor_tensor(out=mcur[:, d_, idx + 1:idx + 2], in0=Mend[:, d_, c:c + 1],
                                                                                                in1=UB[:, 2 + d_, c:c + 1], op=ALU.add), reads=[Mend, UB], writes=[mcur])
                                    fw.op("dve", lambda e, d_=d_, idx=idx, c=c: e.tensor_tensor(out=dd[:, d_, c:c + 1], in0=mcur[:, d_, idx:idx + 1],
                                                                                                in1=Mend[:, d_, c:c + 1], op=ALU.subtract), reads=[mcur, Mend], writes=[dd])
                            SC = pt.sb("SC", [4, 2, NCH], F32)
                            fw.op("act", lambda e: e.activation(out=SC[:], in_=dd[:], func=AF.Exp), reads=[dd], writes=[SC])
                            BD = pt.sb("BD", [4, NCH, 4], F32)
                            ppre = pt.ps("ppre", [64, NCH * 4], F32)
                            for d_ in range(2):
                                fw.op("dve", lambda e, d_=d_: e.tensor_tensor(out=BD[:], in0=bc(SC[:, d_, :].unsqueeze(2), [4, NCH, 4]),
                                                                             in1=bc(ident_f[0:4, 0:4].unsqueeze(1), [4, NCH, 4]), op=ALU.mult),
                                      reads=[SC, ident_f], writes=[BD])
                                fw.op("pe", lambda e: e.matmul(ppre[:], lhsT=ones_f[0:4, 0:64], rhs=BD[:].rearrange("p c h -> p (c h)"), start=True, stop=True),
                                      reads=[ones_f, BD], writes=[ppre])
                                fw.op("dve", lambda e, d_=d_: e.tensor_copy(out=PRE[d_][:].rearrange("p c h -> p (c h)"), in_=ppre[:]), reads=[ppre], writes=[PRE[d_]])
                            pm2 = pt.ps("pm2", [NCH, 8], F32)
                            MT = pt.sb("MT", [NCH, 8], F32)
                            for d_ in range(2):
                                fw.op("pe", lambda e, d_=d_: e.transpose(out=pm2[:, d_ * 4:d_ * 4 + 4], in_=Mend[:, d_, :], identity=ident_f[0:4, 0:4]),
                                      reads=[Mend, ident_f], writes=[pm2])
                            fw.op("dve", lambda e: e.tensor_copy(out=MT[:], in_=pm2[:]), reads=[pm2], writes=[MT])
                            fw.op("dve", lambda e: e.tensor_tensor(out=U[:], in0=U[:], in1=bc(MT[:].unsqueeze(2), [NCH, 8, 128]), op=ALU.subtract), reads=[U, MT], writes=[U])
                            fw.op("act", lambda e: e.activation(out=U[:], in_=U[:], func=AF.Exp), reads=[U], writes=[U])
                            fw.op("dve", lambda e: e.tensor_tensor(out=NB[:], in0=NB[:], in1=bc(MT[:].unsqueeze(2), [NCH, 8, 128]), op=ALU.subtract), reads=[NB, MT], writes=[NB])
                            fw.op("act", lambda e: e.activation(out=NB[:], in_=NB[:], func=AF.Exp), reads=[NB], writes=[NB])
                            ptab = pt.ps("ptab", [128, 4, NCH], F32)
                            for (srcT, dsts, scl) in ((U, E, 0.125), (NB, Fm, 1.0)):
                                for d_ in range(2):
                                    for h in range(4):
                                        fw.op("pe", lambda e, srcT=srcT, d_=d_, h=h: e.transpose(out=ptab[:, h, :], in_=srcT[:, d_ * 4 + h, :], identity=ident_f[0:NCH, 0:NCH]),
                                              reads=[srcT, ident_f], writes=[ptab])
                                    fw.op("dve", lambda e, dsts=dsts, d_=d_, scl=scl: e.tensor_scalar(out=dsts[d_][:], in0=ptab[:], scalar1=scl, scalar2=None, op0=ALU.mult),
                                          reads=[ptab], writes=[dsts[d_]])

                    SBs = ph.sb("SBs", [64, NCH, 4, 65], BF16)
                    ST = [ph.sb(f"ST{d_}", [64, 4, 65], F32) for d_ in range(2)]
                    SP = ph.ring("SP", 2, [64, 4, 65], F32)
                    SFb = ph.ring("SFb", 3, [64, 4, 65], BF16)
                    kvr = ph.ring("kv", 4, [128, 512], BF16)
                    Var = ph.ring("Va", 4, [128, 4, 65], BF16)
                    KEr = ph.ring("KE", 3, [128, 4, 64], BF16)
                    pinc = ph.ring("pinc", 2, [64, 4, 128], F32, psum=True)
                    for d_ in range(2):
                        fw.op("dve", lambda e, d_=d_: e.memset(ST[d_][:], 0.0), writes=[ST[d_]])
                    for va in Var.tiles:
                        fw.op("pool", lambda e, va=va: e.memset(va[:], 1.0), writes=[va])

                    def load_kv(c):
                        kv = kvr.next()
                        fw.dma("sp", kv[:, 0:256], P[c * 128:(c + 1) * 128, kcol:kcol + 256], reads=[kres[c]], writes=[kv], merge=True)
                        fw.dma("sp", kv[:, 256:512], P[c * 128:(c + 1) * 128, vcol:vcol + 256], reads=[Pres[c]], writes=[kv], merge=True)
                        va = Var.next()
                        fw.op("act", lambda e: e.activation(out=va[:, :, 0:64], in_=kv[:, 256:512].rearrange("p (h d) -> p h d", d=64), func=AF.Copy), reads=[kv], writes=[va])
                        return kv, va

                    def state_step(d_, c, kv, va, save_ap=None, save_tile=None):
                        sp = SP.next()
                        fw.op("dve", lambda e: e.tensor_tensor(out=sp[:], in0=ST[d_][:], in1=bc(PRE[d_][:, c, :].unsqueeze(2), [64, 4, 65]), op=ALU.mult),
                              reads=[ST[d_], PRE[d_]], writes=[sp])
                        fw.op("act", lambda e: e.activation(out=save_ap, in_=sp[:], func=AF.Copy), reads=[sp], writes=[save_tile])
                        ke = KEr.next()
                        fw.op("dve", lambda e: e.tensor_tensor(out=ke[:], in0=kv[:, 0:256].rearrange("p (h d) -> p h d", d=64),
                                                               in1=bc(E[d_][:, :, c:c + 1], [128, 4, 64]), op=ALU.mult), reads=[kv, E[d_]], writes=[ke])
                        pi = pinc.next()
                        for h in range(4):
                            fw.op("pe", lambda e, h=h: e.matmul(pi[:, h, 0:65], lhsT=ke[:, h, :], rhs=va[:, h, :], start=True, stop=True),
                                  reads=[ke, va], writes=[pi])
                        fw.op("dve", lambda e: e.tensor_tensor(out=ST[d_][:], in0=sp[:], in1=pi[:, :, 0:65], op=ALU.add), reads=[sp, pi], writes=[ST[d_]])
                        fw.op("dve", lambda e: e.tensor_tensor(out=ST[d_][:], in0=ST[d_][:], in1=bc(POST[d_][:].unsqueeze(2), [64, 4, 65]), op=ALU.mult),
                              reads=[ST[d_], POST[d_]], writes=[ST[d_]])

                    for c in BWD_ORDER:
                        pump(1)
                        kv, va = load_kv(c)
                        state_step(1, c, kv, va, save_ap=SBs[:, c, :, :], save_tile=SBs)

                    qgr = ph.ring("qg", 5, [128, 512], BF16)
                    ptq = ph.ring("ptq", 1, [64, 8, 128], BF16, psum=True)
                    qkT = ph.ring("qkT", 3, [64, 8, 128], BF16)
                    psc = ph.ring("psc", 1, [128, 4, 128], F32, psum=True)
                    tmpS = ph.ring("tmpS", 2, [128, 4, 128], F32)
                    SSr = [ph.ring(f"SS{d_}", 3, [128, 4, 128], BF16) for d_ in range(2)]
                    pR = [ph.ring(f"pR{d_}", 2, [128, 4, 128], F32, psum=True) for d_ in range(2)]
                    s4 = ph.ring("s4", 12, [128, 8], F32)
                    hh = ph.ring("hh", 16, [128, 4, 64], F32)
                    mo_r = ph.ring("mixo", 3, [128, 256], BF16)
                    def chunk_gen(c):
                        pump(1)
                        kv, va = load_kv(c)
                        sfb = SFb.next()
                        do_out = c >= first_out_chunk
                        if do_out:
                            qg = qgr.next()
                            fw.dma("sp", qg[:, 0:256], P[c * 128:(c + 1) * 128, qcol:qcol + 256], reads=[qres[c]], writes=[qg], merge=True)
                            fw.dma("sp", qg[:, 256:512], P[c * 128:(c + 1) * 128, gcol:gcol + 256], reads=[Pres[c]], writes=[qg], merge=True)
                        state_step(0, c, kv, va, save_ap=sfb[:], save_tile=sfb)
                        if not do_out:
                            return
                        tq = ptq.next()
                        for h in range(4):
                            fw.op("pe", lambda e, h=h: e.transpose(out=tq[:, h, :], in_=qg[:, h * 64:(h + 1) * 64], identity=ident_b[:]), reads=[qg, ident_b], writes=[tq])
                            fw.op("pe", lambda e, h=h: e.transpose(out=tq[:, 4 + h, :], in_=kv[:, h * 64:(h + 1) * 64], identity=ident_b[:]), reads=[kv, ident_b], writes=[tq])
                        qk = qkT.next()
                        fw.op("act", lambda e: e.activation(out=qk[:], in_=tq[:], func=AF.Copy), reads=[tq], writes=[qk])
                        ps_ = psc.next()
                        for h in range(4):
                            fw.op("pe", lambda e, h=h: e.matmul(ps_[:, h, :], lhsT=qk[:, 4 + h, :], rhs=qk[:, h, :], start=True, stop=True), reads=[qk], writes=[ps_])
                        sss = []
                        for d_ in range(2):
                            ts_ = tmpS.next()
                            ss = SSr[d_].next()
                            fw.op("dve", lambda e, d_=d_, ts_=ts_: e.tensor_tensor(out=ts_[:], in0=ps_[:], in1=bc(E[d_][:, :, c:c + 1], [128, 4, 128]), op=ALU.mult),
                                  reads=[ps_, E[d_]], writes=[ts_])
                            mk_ = maskF if d_ == 0 else maskB
                            fw.op("pool", lambda e, ts_=ts_, ss=ss, mk_=mk_: e.tensor_tensor(out=ss[:], in0=ts_[:], in1=bc(mk_[:].unsqueeze(1), [128, 4, 128]), op=ALU.mult),
                                  reads=[ts_, mk_], writes=[ss])
                            sss.append(ss)
                        yield
                        Rs = []
                        for d_ in range(2):
                            ss = sss[d_]
                            R = pR[d_].next()
                            for h in range(4):
                                fw.op("pe", lambda e, h=h, ss=ss, R=R: e.matmul(R[:, h, 0:65], lhsT=ss[:, h, :], rhs=va[:, h, :], start=True, stop=False),
                                      reads=[ss, va], writes=[R])
                                if d_ == 0:
                                    fw.op("pe", lambda e, h=h, R=R: e.matmul(R[:, h, 0:65], lhsT=qk[:, h, :], rhs=sfb[:, h, :], start=False, stop=True),
                                          reads=[qk, sfb], writes=[R])
                                else:
                                    fw.op("pe", lambda e, h=h, R=R: e.matmul(R[:, h, 0:65], lhsT=qk[:, h, :], rhs=SBs[:, c, h, :], start=False, stop=True),
                                          reads=[qk, SBs], writes=[R])
                            Rs.append(R)
                        yield
                        hsum = hh.next()
                        hd = []
                        for d_ in range(2):
                            R = Rs[d_]
                            hx = hh.next()
                            if kind == "ml":
                                den = s4.next()
                                fw.op("act", lambda e, R=R, den=den: e.activation(out=den[:, 0:4], in_=R[:, :, 64], func=AF.Abs), reads=[R], writes=[den])
                                fw.op("dve", lambda e, d_=d_, den=den: e.tensor_tensor(out=den[:, 0:4], in0=den[:, 0:4], in1=Fm[d_][:, :, c], op=ALU.max), reads=[den, Fm[d_]], writes=[den])
                                fw.op("dve", lambda e, den=den: e.reciprocal(out=den[:, 4:8], in_=den[:, 0:4]), reads=[den], writes=[den])
                                fw.op("dve", lambda e, R=R, den=den, hx=hx: e.tensor_tensor(out=hx[:], in0=R[:, :, 0:64], in1=bc(den[:, 4:8].unsqueeze(2), [128, 4, 64]), op=ALU.mult),
                                      reads=[R, den], writes=[hx])
                            else:
                                fw.op("dve", lambda e, R=R, d_=d_, hx=hx: e.tensor_tensor(out=hx[:], in0=R[:, :, 0:64], in1=bc(Fm[d_][:, :, c:c + 1], [128, 4, 64]), op=ALU.mult),
                                      reads=[R, Fm[d_]], writes=[hx])
                            hd.append(hx)
                        fw.op("pool", lambda e: e.tensor_tensor(out=hsum[:], in0=hd[0][:], in1=hd[1][:], op=ALU.add), reads=[hd[0], hd[1]], writes=[hsum])
                        gv = qg[:, 256:512].rearrange("p (h d) -> p h d", d=64)
                        if kind == "ml":
                            fw.op("pool", lambda e: e.tensor_tensor(out=hsum[:], in0=hsum[:], in1=gv, op=ALU.mult), reads=[hsum, qg], writes=[hsum])
                        yield
                        stt = s4.next()
                        xc = hh.next()
                        sq = hd[0]
                        fw.op("dve", lambda e: e.tensor_reduce(out=stt[:, 0:4], in_=hsum[:], axis=AX.X, op=ALU.add), reads=[hsum], writes=[stt])
                        fw.op("dve", lambda e: e.tensor_scalar(out=stt[:, 0:4], in0=stt[:, 0:4], scalar1=-1.0 / 64, scalar2=None, op0=ALU.mult), reads=[stt], writes=[stt])
                        fw.op("dve", lambda e: e.tensor_tensor(out=xc[:], in0=hsum[:], in1=bc(stt[:, 0:4].unsqueeze(2), [128, 4, 64]), op=ALU.add), reads=[hsum, stt], writes=[xc])
                        fw.op("act", lambda e: e.activation(out=sq[:], in_=xc[:], func=AF.Square), reads=[xc], writes=[sq])
                        fw.op("dve", lambda e: e.tensor_reduce(out=stt[:, 4:8], in_=sq[:], axis=AX.X, op=ALU.add), reads=[sq], writes=[stt])
                        fw.op("act", lambda e: e.activation(out=stt[:, 0:4], in_=stt[:, 4:8], func=AF.Sqrt, scale=1.0 / 64, bias=cst[:, 0:1]), reads=[stt, cst], writes=[stt])
                        fw.op("dve", lambda e: e.reciprocal(out=stt[:, 4:8], in_=stt[:, 0:4]), reads=[stt], writes=[stt])
                        fw.op("dve", lambda e: e.tensor_tensor(out=xc[:], in0=xc[:], in1=bc(stt[:, 4:8].unsqueeze(2), [128, 4, 64]), op=ALU.mult), reads=[xc, stt], writes=[xc])
                        mo_ = mo_r.next()
                        mv_ = mo_[:].rearrange("p (h d) -> p h d", d=64)
                        nv = nwb[:].rearrange("p (h d) -> p h d", d=64)
                        if kind == "ml":
                            fw.op("pool", lambda e: e.tensor_tensor(out=mv_, in0=xc[:], in1=nv, op=ALU.mult), reads=[xc, nwb], writes=[mo_])
                        else:
                            fw.op("pool", lambda e: e.tensor_tensor(out=xc[:], in0=xc[:], in1=nv, op=ALU.mult), reads=[xc, nwb], writes=[xc])
                            fw.op("pool", lambda e: e.tensor_tensor(out=mv_, in0=xc[:], in1=gv, op=ALU.mult), reads=[xc, qg], writes=[mo_])
                        fw.dma("pool", MIX[c * 128:(c + 1) * 128, ocol:ocol + 256], mo_[:], reads=[mo_], writes=[MIXres[c]], kind="o")

                    gens = []
                    for c in FWD_ORDER + [None]:
                        if c is not None:
                            gens.append(chunk_gen(c))
                        for g_ in list(gens):
                            try:
                                next(g_)
                            except StopIteration:
                                gens.remove(g_)
                    for g_ in gens:
                        for _ in g_:
                            pass
                if dbg == f"{kind}{l}":
                    break
            if dbg in (f"ret{l}", f"ml{l}"):
                break

            with Phase(nc, fw, f"E{l}") as ph:
                Wo = ph.sb("Wo", [128, 8, D], BF16)
                for kc in range(8):
                    fw.dma("pool", Wo[:, kc, :], w_out[l][kc * 128:(kc + 1) * 128, :], writes=[Wo], merge=True)
                gbc = [ph.sb(f"gbc{t}", [128, D], F32) for t in range(2)]
                for t in range(2):
                    fw.dma("sp", gbc[t][:], MD[t, 2 * D:3 * D].partition_broadcast(128), reads=[MDres], writes=[gbc[t]])
                mxr = ph.ring("mx", 2, [128, D], BF16)
                ptx = ph.ring("ptx", 2, [128, 8, 128], BF16, psum=True)
                mTr = ph.ring("mT", 2, [128, 8, 128], BF16)
                pyr = ph.ring("py", 4, [128, 512], F32, psum=True)
                hcr = ph.ring("hc", 3, [128, D], F32)
                tyr = ph.ring("ty", 2, [128, D], F32)
                for c in range(first_out_chunk, NCH):
                    pump(1)
                    t = 1 if c < 2 else 0
                    mx = mxr.next()
                    fw.dma("sp", mx[:], MIX[c * 128:(c + 1) * 128, :], reads=[MIXres[c]], writes=[mx])
                    hc = hcr.next()
                    if l == 0:
                        fw.dma("sp", hc[:], xin[c * 128:(c + 1) * 128, :], writes=[hc])
                    else:
                        fw.dma("sp", hc[:], H[c * 128:(c + 1) * 128, :], reads=[Hres[c]], writes=[hc])
                    tx = ptx.next()
                    for kc in range(8):
                        fw.op("pe", lambda e, kc=kc: e.transpose(out=tx[:, kc, :], in_=mx[:, kc * 128:(kc + 1) * 128], identity=ident_b[:]), reads=[mx, ident_b], writes=[tx])
                    mT = mTr.next()
                    fw.op("act", lambda e: e.activation(out=mT[:], in_=tx[:], func=AF.Copy), reads=[tx], writes=[mT])
                    ty = tyr.next()
                    for n in range(2):
                        py = pyr.next()
                        for kc in range(8):
                            fw.op("pe", lambda e, kc=kc, n=n, py=py: e.matmul(py[:], lhsT=mT[:, kc, :], rhs=Wo[:, kc, n * 512:(n + 1) * 512], start=(kc == 0), stop=(kc == 7)),
                                  reads=[mT, Wo], writes=[py])
                        fw.op("dve", lambda e, n=n, py=py: e.tensor_tensor(out=ty[:, n * 512:(n + 1) * 512], in0=py[:], in1=gbc[t][:, n * 512:(n + 1) * 512], op=ALU.mult),
                              reads=[py, gbc[t]], writes=[ty])
                    fw.op("pool", lambda e: e.tensor_tensor(out=hc[:], in0=hc[:], in1=ty[:], op=ALU.add), reads=[hc, ty], writes=[hc])
                    fw.dma("pool", H[c * 128:(c + 1) * 128, :], hc[:], reads=[hc], writes=[Hres[c]], kind="o")
            if dbg == f"E{l}":
                break

            cfg = FF_CFG[l % 2]
            pump(0, upto=cfg["last_idx"])
            moe = (l % 2 == 1)
            nffc, nblk, nexp = cfg["nffc"], cfg["nblk"], cfg["nexp"]
            if moe and not DENSE_MOE:
                with Phase(nc, fw, f"M{l}") as pm_:
                    SEL1 = pm_.sb("SEL1", [128, 32, 8], F32)
                    SEL2 = pm_.sb("SEL2", [128, 32, 8], F32)
                    GWS = pm_.sb("GWS", [128, 32, 8], F32)
                    RANK = pm_.sb("RANK", [128, 32, 8], F32)
                    BASE = pm_.sb("BASE", [128, 32, 8], F32)
                    SLOT_i = pm_.sb("SLOTi", [128, 2, 32], I32)
                    GWK = pm_.sb("GWK", [128, 2, 32], F32)
                    BE_i = pm_.sb("BEi", [128, NTL], I32)
                    IDXW = pm_.sb("IDXW", [128, NTL, 8], I32)
                    gbc = pm_.sb("gbc", [128, D], F32)
                    fw.dma("sp", gbc[:], MD[0, 5 * D:6 * D].partition_broadcast(128), reads=[MDres], writes=[gbc])
                    with Phase(nc, fw, f"MR{l}") as ph:
                        zt = ph.sb("zt", [128, 4, D], BF16)
                        fw.op("pool", lambda e: e.memset(zt[:], 0.0), writes=[zt])
                        for j in range(NTL):
                            fw.dma("sp", XS[j * 512:(j + 1) * 512, :].rearrange("(s p) d -> p s d", p=128), zt[:], reads=[zt], writes=[XSres], holder=zt, kind="o", merge=True)
                        ATS = ph.sb("ATS", [128, 32, D], BF16)
                        g2bc = ph.sb("g2bc", [128, D], F32)
                        sh2bc = ph.sb("sh2bc", [128, D], F32)
                        n2bc = ph.sb("n2bc", [128, D], F32)
                        Wr = ph.sb("Wr", [128, 8, 8], F32)
                        rbb = ph.sb("rbb", [128, 8], F32)
                        Ust = ph.sb("Ust", [128, 128], F32)
                        run = ph.sb("run", [128, 8], F32)
                        grid = ph.sb("grid", [128, 24], F32)
                        fw.dma("sp", g2bc[:], MD[0, 4 * D:5 * D].partition_broadcast(128), reads=[MDres], writes=[g2bc])
                        fw.dma("sp", sh2bc[:], MD[0, 3 * D:4 * D].partition_broadcast(128), reads=[MDres], writes=[sh2bc])
                        fw.dma("sp", n2bc[:], n2row[l].partition_broadcast(128), writes=[n2bc])
                        fw.dma("sp", Wr[:], router_w.rearrange("(kc p) e -> p kc e", p=128), writes=[Wr])
                        fw.dma("sp", rbb[:], router_b[0].partition_broadcast(128), writes=[rbb])
                        fw.dma("sp", grid[:], grid_d, writes=[grid])
                        fw.op("dve", lambda e: e.tensor_scalar(out=g2bc[:], in0=g2bc[:], scalar1=1.0, scalar2=None, op0=ALU.add), reads=[g2bc], writes=[g2bc])
                        fw.op("dve", lambda e: e.tensor_tensor(out=g2bc[:], in0=g2bc[:], in1=n2bc[:], op=ALU.mult), reads=[g2bc, n2bc], writes=[g2bc])
                        fw.op("dve", lambda e: e.tensor_tensor(out=Ust[:], in0=maskF[:], in1=ident_f[:], op=ALU.subtract), reads=[maskF, ident_f], writes=[Ust])
                        fw.op("dve", lambda e: e.memset(run[:], 0.0), writes=[run])
                        str_ = ph.ring("st", 4, [128, 4], F32)
                        xnr = ph.ring("xn", 2, [128, D], F32)
                        tpr = ph.ring("tp", 2, [128, 4, 128], F32, psum=True)
                        hcr = ph.ring("hc", 2, [128, D], F32)
                        tmr = ph.ring("tm", 2, [128, D], F32)
                        a32r = ph.ring("a32", 2, [128, 8, 128], F32)
                        LG = ph.sb("LG", [128, 32, 8], F32)
                        plr = ph.ring("pl", 2, [128, 64], F32, psum=True)
                        for s_ in range(32):
                            c = 2 + s_
                            hc = hcr.next()
                            fw.dma("sp", hc[:], H[c * 128:(c + 1) * 128, :], reads=[Hres[c]], writes=[hc])
                            st4 = str_.next()
                            xn = xnr.next()
                            fw.op("act", lambda e: e.activation(out=xn[:], in_=hc[:], func=AF.Square, accum_out=st4[:, 0:1]), reads=[hc], writes=[xn, st4])
                            fw.op("act", lambda e: e.activation(out=st4[:, 1:2], in_=st4[:, 0:1], func=AF.Sqrt, scale=1.0 / D, bias=cst[:, 0:1]), reads=[st4, cst], writes=[st4])
                            fw.op("dve", lambda e: e.reciprocal(out=st4[:, 2:3], in_=st4[:, 1:2]), reads=[st4], writes=[st4])
                            fw.op("act", lambda e: e.activation(out=xn[:], in_=hc[:], func=AF.Copy, scale=st4[:, 2:3]), reads=[hc, st4], writes=[xn])
                            tm = tmr.next()
                            fw.op("pool", lambda e: e.tensor_tensor(out=tm[:], in0=xn[:], in1=g2bc[:], op=ALU.mult), reads=[xn, g2bc], writes=[tm])
                            fw.op("pool", lambda e, s_=s_: e.tensor_tensor(out=ATS[:, s_, :], in0=tm[:], in1=sh2bc[:], op=ALU.add), reads=[tm, sh2bc], writes=[ATS])
                            a32 = a32r.next()
                            for half in range(2):
                                tp = tpr.next()
                                for j in range(4):
                                    kc = half * 4 + j
                                    fw.op("pe", lambda e, kc=kc, j=j: e.transpose(out=tp[:, j, :], in_=xn[:, kc * 128:(kc + 1) * 128], identity=ident_f[:]),
                                          reads=[xn, ident_f], writes=[tp])
                                for j in range(4):
                                    kc = half * 4 + j
                                    fw.op("dve", lambda e, kc=kc, j=j: e.tensor_scalar(out=a32[:, kc, :], in0=tp[:, j, :], scalar1=g2T[:, kc, 0:1],
                                                                                      scalar2=modT[:, 24 + kc, 0:1], op0=ALU.mult, op1=ALU.add),
                                          reads=[tp, g2T, modT], writes=[a32])
                            pl = plr.next()
                            for kc in range(8):
                                fw.op("pe", lambda e, kc=kc: e.matmul(pl[:, 0:8], lhsT=a32[:, kc, :], rhs=Wr[:, kc, :], start=(kc == 0), stop=(kc == 7)),
                                      reads=[a32, Wr], writes=[pl])
                            fw.op("dve", lambda e, s_=s_: e.tensor_tensor(out=LG[:, s_, :], in0=pl[:, 0:8], in1=rbb[:], op=ALU.add), reads=[pl, rbb], writes=[LG])
                        m12 = ph.sb("m12", [128, 4, 32], F32)
                        L2 = ph.sb("L2", [128, 32, 8], F32)
                        SEL = ph.sb("SEL", [128, 32, 8], F32)
                        EX = ph.sb("EX", [128, 32, 8], F32)
                        CN0 = ph.sb("CN0", [128, 32, 8], F32)
                        CN1 = ph.sb("CN1", [128, 32, 8], F32)
                        CN2 = ph.sb("CN2", [128, 32, 8], F32)
                        prk = ph.ps("prk", [128, 32, 8], F32)
                        pcn = ph.ps("pcn", [128, 32, 8], F32)
                        fw.op("dve", lambda e: e.tensor_reduce(out=m12[:, 0, :], in_=LG[:], axis=AX.X, op=ALU.max), reads=[LG], writes=[m12])
                        fw.op("dve", lambda e: e.tensor_tensor(out=SEL1[:], in0=LG[:], in1=bc(m12[:, 0, :].unsqueeze(2), [128, 32, 8]), op=ALU.is_ge), reads=[LG, m12], writes=[SEL1])
                        fw.op("dve", lambda e: e.scalar_tensor_tensor(out=L2[:], in0=SEL1[:], scalar=-1e30, in1=LG[:], op0=ALU.mult, op1=ALU.add), reads=[SEL1, LG], writes=[L2])
                        fw.op("dve", lambda e: e.tensor_reduce(out=m12[:, 1, :], in_=L2[:], axis=AX.X, op=ALU.max), reads=[L2], writes=[m12])
                        fw.op("dve", lambda e: e.tensor_tensor(out=SEL[:], in0=LG[:], in1=bc(m12[:, 1, :].unsqueeze(2), [128, 32, 8]), op=ALU.is_ge), reads=[LG, m12], writes=[SEL])
                        fw.op("dve", lambda e: e.tensor_tensor(out=SEL2[:], in0=SEL[:], in1=SEL1[:], op=ALU.subtract), reads=[SEL, SEL1], writes=[SEL2])
                        fw.op("dve", lambda e: e.tensor_tensor(out=EX[:], in0=LG[:], in1=bc(m12[:, 0, :].unsqueeze(2), [128, 32, 8]), op=ALU.subtract), reads=[LG, m12], writes=[EX])
                        fw.op("act", lambda e: e.activation(out=EX[:], in_=EX[:], func=AF.Exp), reads=[EX], writes=[EX])
                        fw.op("dve", lambda e: e.tensor_tensor(out=EX[:], in0=EX[:], in1=SEL[:], op=ALU.mult), reads=[EX, SEL], writes=[EX])
                        fw.op("dve", lambda e: e.tensor_reduce(out=m12[:, 2, :], in_=EX[:], axis=AX.X, op=ALU.add), reads=[EX], writes=[m12])
                        fw.op("dve", lambda e: e.reciprocal(out=m12[:, 3, :], in_=m12[:, 2, :]), reads=[m12], writes=[m12])
                        fw.op("dve", lambda e: e.tensor_tensor(out=GWS[:], in0=EX[:], in1=bc(m12[:, 3, :].unsqueeze(2), [128, 32, 8]), op=ALU.mult), reads=[EX, m12], writes=[GWS])
                        for s_ in range(32):
                            fw.op("pe", lambda e, s_=s_: e.matmul(prk[:, s_, :], lhsT=Ust[:], rhs=SEL[:, s_, :], start=True, stop=True), reads=[Ust, SEL], writes=[prk])
                            fw.op("pe", lambda e, s_=s_: e.matmul(pcn[:, s_, :], lhsT=ones_f[:], rhs=SEL[:, s_, :], start=True, stop=True), reads=[ones_f, SEL], writes=[pcn])
                        fw.op("dve", lambda e: e.tensor_copy(out=RANK[:], in_=prk[:]), reads=[prk], writes=[RANK])
                        fw.op("dve", lambda e: e.tensor_copy(out=CN0[:], in_=pcn[:]), reads=[pcn], writes=[CN0])
                        srcT, dstT = CN0, CN1
                        sh = 1
                        while sh < 32:
                            fw.op("dve", lambda e, srcT=srcT, dstT=dstT, sh=sh: e.tensor_tensor(out=dstT[:, sh:32, :], in0=srcT[:, sh:32, :], in1=srcT[:, 0:32 - sh, :], op=ALU.add), reads=[srcT], writes=[dstT])
                            fw.op("dve", lambda e, srcT=srcT, dstT=dstT, sh=sh: e.tensor_copy(out=dstT[:, 0:sh, :], in_=srcT[:, 0:sh, :]), reads=[srcT], writes=[dstT])
                            srcT = dstT
                            dstT = CN2 if dstT is CN1 else CN1
                            sh *= 2
                        fw.op("dve", lambda e: e.tensor_tensor(out=BASE[:], in0=srcT[:], in1=CN0[:], op=ALU.subtract), reads=[srcT, CN0], writes=[BASE])
                        fw.op("dve", lambda e: e.tensor_copy(out=run[:], in_=srcT[:, 31, :]), reads=[srcT], writes=[run])
                        w8 = ph.sb("w8", [128, 8, 8], F32)
                        w24 = ph.sb("w24", [128, NTL, 8], F32)
                        v8 = ph.sb("v8", [128, 6, 8], F32)
                        bef = ph.sb("bef", [128, NTL], F32)
                        SLF = ph.sb("SLF", [128, 32, 8], F32)
                        w32 = ph.sb("w32", [128, 32, 8], F32)
                        s2f = ph.sb("s2f", [128, 2, 32], F32)
                        fw.op("dve", lambda e: e.tensor_tensor(out=w8[:], in0=bc(run[:].unsqueeze(2), [128, 8, 8]), in1=bc(grid[:, 0:8].unsqueeze(1), [128, 8, 8]), op=ALU.is_gt),
                              reads=[run, grid], writes=[w8])
                        fw.op("dve", lambda e: e.tensor_reduce(out=v8[:, 0, :], in_=w8[:], axis=AX.X, op=ALU.add), reads=[w8], writes=[v8])
                        fw.op("dve", lambda e: e.tensor_scalar(out=v8[:, 0, :], in0=v8[:, 0, :], scalar1=512.0, scalar2=None, op0=ALU.mult), reads=[v8], writes=[v8])
                        srcI, dstI = 0, 1
                        for sh in (1, 2, 4):
                            fw.op("dve", lambda e, srcI=srcI, dstI=dstI, sh=sh: e.tensor_tensor(out=v8[:, dstI, sh:8], in0=v8[:, srcI, sh:8], in1=v8[:, srcI, 0:8 - sh], op=ALU.add), reads=[v8], writes=[v8])
                            fw.op("dve", lambda e, srcI=srcI, dstI=dstI, sh=sh: e.tensor_copy(out=v8[:, dstI, 0:sh], in_=v8[:, srcI, 0:sh]), reads=[v8], writes=[v8])
                            srcI = dstI
                            dstI = 2 if dstI == 1 else 1
                        PE_ = srcI
                        fw.op("dve", lambda e: e.tensor_tensor(out=v8[:, 4, :], in0=v8[:, PE_, :], in1=v8[:, 0, :], op=ALU.subtract), reads=[v8], writes=[v8])
                        fw.op("dve", lambda e: e.tensor_tensor(out=w24[:], in0=bc(v8[:, PE_:PE_ + 1, :], [128, NTL, 8]), in1=bc(grid[:].unsqueeze(2), [128, NTL, 8]), op=ALU.is_le),
                              reads=[v8, grid], writes=[w24])
                        fw.op("dve", lambda e: e.tensor_reduce(out=bef[:], in_=w24[:], axis=AX.X, op=ALU.add), reads=[w24], writes=[bef])
                        fw.op("dve", lambda e: e.tensor_scalar(out=bef[:], in0=bef[:], scalar1=7.0, scalar2=None, op0=ALU.min), reads=[bef], writes=[bef])
                        fw.op("dve", lambda e: e.tensor_copy(out=BE_i[:], in_=bef[:]), reads=[bef], writes=[BE_i])
                        posw = ph.sb("posw", [128, 2], F32)
                        idf = ph.sb("idf", [128, NTL, 8], F32)
                        fw.dma("sp", posw[:], pos_d, writes=[posw])
                        fw.op("dve", lambda e: e.tensor_scalar(out=posw[:, 1:2], in0=posw[:, 0:1], scalar1=-1.0, scalar2=None, op0=ALU.add), reads=[posw], writes=[posw])
                        fw.op("dve", lambda e: e.tensor_scalar(out=bef[:], in0=bef[:], scalar1=896.0, scalar2=posw[:, 1:2], op0=ALU.mult, op1=ALU.add), reads=[bef, posw], writes=[bef])
                        for b_ in range(7):
                            fw.op("dve", lambda e, b_=b_: e.tensor_scalar(out=idf[:, :, b_], in0=bef[:], scalar1=float(128 * b_), scalar2=None, op0=ALU.add), reads=[bef], writes=[idf])
                        fw.op("dve", lambda e: e.tensor_copy(out=IDXW[:, :, 0:7], in_=idf[:, :, 0:7]), reads=[idf], writes=[IDXW])
                        fw.op("dve", lambda e: e.tensor_tensor(out=SLF[:], in0=RANK[:], in1=BASE[:], op=ALU.add), reads=[RANK, BASE], writes=[SLF])
                        fw.op("dve", lambda e: e.tensor_tensor(out=SLF[:], in0=SLF[:], in1=bc(v8[:, 4:5, :], [128, 32, 8]), op=ALU.add), reads=[SLF, v8], writes=[SLF])
                        for k, SELk in enumerate((SEL1, SEL2)):
                            fw.op("dve", lambda e, SELk=SELk: e.tensor_tensor(out=w32[:], in0=SLF[:], in1=SELk[:], op=ALU.mult), reads=[SLF, SELk], writes=[w32])
                            fw.op("dve", lambda e, k=k: e.tensor_reduce(out=s2f[:, k, :], in_=w32[:], axis=AX.X, op=ALU.add), reads=[w32], writes=[s2f])
                            fw.op("dve", lambda e, SELk=SELk: e.tensor_tensor(out=w32[:], in0=GWS[:], in1=SELk[:], op=ALU.mult), reads=[GWS, SELk], writes=[w32])
                            fw.op("dve", lambda e, k=k: e.tensor_reduce(out=GWK[:, k, :], in_=w32[:], axis=AX.X, op=ALU.add), reads=[w32], writes=[GWK])
                        fw.op("dve", lambda e: e.tensor_scalar(out=s2f[:], in0=s2f[:], scalar1=float(NSLOT - 1), scalar2=0.0, op0=ALU.min, op1=ALU.max), reads=[s2f], writes=[s2f])
                        fw.op("dve", lambda e: e.tensor_copy(out=SLOT_i[:], in_=s2f[:]), reads=[s2f], writes=[SLOT_i])
                        for s_ in range(32):
                            for k in range(2):
                                fw.idma(XS[:, :], ATS[:, s_, :], SLOT_i[:, k, s_:s_ + 1], True, NSLOT - 1, reads=[ATS, SLOT_i], writes=[XSres], holder=ATS, kind="o",
                                        merge=not (s_ == 0 and k == 0))

                    with Phase(nc, fw, f"MC{l}") as ph:
                        xsr = ph.ring("xs", 2, [128, 4, D], BF16)
                        fTr = ph.ring("fT", 2, [128, 8, 512], BF16)
                        tpb = ph.ring("tpb", 2, [128, 8, 128], BF16, psum=True)
                        acc = ph.sb("acc", [128, 4, D], F32)
                        WGr = ph.ring("WG", 3, [128, 8, 512], BF16)
                        WUr = ph.ring("WU", 3, [128, 8, 512], BF16)
                        WDr = ph.ring("WD", 5, [128, 4, D], BF16)
                        pgu = ph.ring("pgu", 4, [128, 512], F32, psum=True)
                        pyr = ph.ring("py", 2, [128, 512], F32, psum=True)
                        sgr = ph.ring("sg", 2, [128, 512], F32)
                        gTr = ph.ring("gT", 5, [128, 4, 512], BF16)

                        def mprologue(j):
                            xs = xsr.next()
                            fw.dma("sp", xs[:], XS[j * 512:(j + 1) * 512, :].rearrange("(s p) d -> p s d", p=128), reads=[XSres], writes=[xs])
                            fT = fTr.next()
                            for sub in range(4):
                                tp = tpb.next()
                                for kc in range(8):
                                    fw.op("pe", lambda e, kc=kc, sub=sub: e.transpose(out=tp[:, kc, :], in_=xs[:, sub, kc * 128:(kc + 1) * 128], identity=ident_b[:]),
                                          reads=[xs, ident_b], writes=[tp])
                                eng = "act" if sub % 2 == 0 else "dve"
                                if eng == "act":
                                    fw.op("act", lambda e, sub=sub, tp=tp: e.activation(out=fT[:, :, sub * 128:(sub + 1) * 128], in_=tp[:], func=AF.Copy), reads=[tp], writes=[fT])
                                else:
                                    fw.op("dve", lambda e, sub=sub, tp=tp: e.tensor_copy(out=fT[:, :, sub * 128:(sub + 1) * 128], in_=tp[:]), reads=[tp], writes=[fT])
                            return fT, None

                        cfgm = FF_CFG[1]
                        nxt_pro = mprologue(0)
                        for j in range(NTL):
                            fT, ev = nxt_pro
                            T = 512

                            def gate_up(b):
                                WG, WU, WDt = WGr.next(), WUr.next(), WDr.next()
                                for (Wt_, key_) in ((WG, "WG"), (WU, "WU"), (WDt, "WD")):
                                    fw.idma(Wt_[:].rearrange("p k n -> p (k n)"), cfgm[key_].rearrange("e b p k n -> (e b p) (k n)"), IDXW[:, j, b:b + 1], False, None,
                                            reads=[cfgm["res"], IDXW], writes=[Wt_], holder=Wt_, kind="i")
                                gT = gTr.next()
                                for jj in range(4):
                                    pg_, pu_ = pgu.next(), pgu.next()
                                    for (pp, W_) in ((pg_, WG), (pu_, WU)):
                                        for kc in range(8):
                                            fw.op("pe", lambda e, kc=kc, jj=jj, pp=pp, W_=W_: e.matmul(pp[:, 0:T], lhsT=W_[:, kc, jj * 128:(jj + 1) * 128], rhs=fT[:, kc, 0:T],
                                                                                                     start=(kc == 0), stop=(kc == 7)), reads=[W_, fT], writes=[pp])
                                    sg = sgr.next()
                                    fw.op("act", lambda e, pg_=pg_, sg=sg: e.activation(out=sg[:, 0:T], in_=pg_[:, 0:T], func=AF.Silu), reads=[pg_], writes=[sg])
                                    fw.op("dve", lambda e, pu_=pu_, sg=sg, jj=jj: e.tensor_tensor(out=gT[:, jj, 0:T], in0=pu_[:, 0:T], in1=sg[:, 0:T], op=ALU.mult), reads=[pu_, sg], writes=[gT])
                                return gT, WDt

                            def down(items, first):
                                for sub in range(4):
                                    for n in range(2):
                                        py = pyr.next()
                                        nmm = len(items) * 4
                                        q_ = 0
                                        for (gT, WDt) in items:
                                            for jj in range(4):
                                                fw.op("pe", lambda e, jj=jj, sub=sub, n=n, py=py, gT=gT, WDt=WDt, q_=q_: e.matmul(
                                                    py[:], lhsT=gT[:, jj, sub * 128:(sub + 1) * 128], rhs=WDt[:, jj, n * 512:(n + 1) * 512],
                                                    start=(q_ == 0), stop=(q_ == nmm - 1)), reads=[gT, WDt], writes=[py])
                                                q_ += 1
                                        av = acc[:, sub, n * 512:(n + 1) * 512]
                                        if first:
                                            fw.op("dve", lambda e, py=py, av=av: e.tensor_copy(out=av, in_=py[:]), reads=[py], writes=[acc])
                                        else:
                                            fw.op("dve", lambda e, py=py, av=av: e.tensor_tensor(out=av, in0=py[:], in1=av, op=ALU.add), reads=[py, acc], writes=[acc])

                            pendl = []
                            isfirst = True
                            for b in range(7):
                                pendl.append(gate_up(b))
                                if len(pendl) == 3:
                                    down(pendl[:2], isfirst)
                                    pendl = pendl[2:]
                                    isfirst = False
                                if b == 2 and j + 1 < NTL:
                                    nxt_pro = mprologue(j + 1)
                            while pendl:
                                down(pendl[:2], isfirst)
                                pendl = pendl[2:]
                                isfirst = False
                            fw.dma("pool", YS[j * 512:(j + 1) * 512, :].rearrange("(s p) d -> p s d", p=128), acc[:], reads=[acc], writes=[YSres], holder=acc, kind="o", merge=True)

                    with Phase(nc, fw, f"MO{l}") as ph:
                        y1r = ph.ring("y1", 4, [128, D], F32)
                        y2r = ph.ring("y2", 4, [128, D], F32)
                        hcr = ph.ring("hc", 4, [128, D], F32)
                        loaded = {}

                        def mo_load(s_):
                            c = 2 + s_
                            y1, y2, hc = y1r.next(), y2r.next(), hcr.next()
                            fw.dma("sp", hc[:], H[c * 128:(c + 1) * 128, :], reads=[Hres[c]], writes=[hc])
                            fw.idma(y1[:, :], YS[:, :], SLOT_i[:, 0, s_:s_ + 1], False, NSLOT - 1, reads=[YSres, SLOT_i], writes=[y1], holder=y1, kind="i")
                            fw.idma(y2[:, :], YS[:, :], SLOT_i[:, 1, s_:s_ + 1], False, NSLOT - 1, reads=[YSres, SLOT_i], writes=[y2], holder=y2, kind="i")
                            loaded[s_] = (y1, y2, hc)

                        def mo_comp(s_):
                            y1, y2, hc = loaded.pop(s_)
                            fw.op("dve", lambda e: e.tensor_scalar(out=y1[:], in0=y1[:], scalar1=GWK[:, 0, s_:s_ + 1], scalar2=None, op0=ALU.mult), reads=[y1, GWK], writes=[y1])
                            fw.op("dve", lambda e: e.scalar_tensor_tensor(out=y1[:], in0=y2[:], scalar=GWK[:, 1, s_:s_ + 1], in1=y1[:], op0=ALU.mult, op1=ALU.add),
                                  reads=[y1, y2, GWK], writes=[y1])
                            fw.op("dve", lambda e: e.tensor_tensor(out=y1[:], in0=y1[:], in1=gbc[:], op=ALU.mult), reads=[y1, gbc], writes=[y1])
                            fw.op("dve", lambda e: e.tensor_tensor(out=y1[:], in0=y1[:], in1=hc[:], op=ALU.add), reads=[y1, hc], writes=[y1])
                            fw.dma("sp", out[s_ * 128:(s_ + 1) * 128, :], y1[:], reads=[y1], writes=[OUTres], holder=y1, kind="o", merge=True)
                        LOOK = 2
                        for s_ in range(32 + LOOK):
                            if s_ < 32:
                                mo_load(s_)
                            if s_ >= LOOK:
                                mo_comp(s_ - LOOK)
                if dbg == f"F{l}":
                    break
                continue
            with Phase(nc, fw, f"F{l}") as ph:
                gbc = [ph.sb(f"gbc{t}", [128, D], F32) for t in range(2)]
                for t in range(2):
                    fw.dma("sp", gbc[t][:], MD[t, 5 * D:6 * D].partition_broadcast(128), reads=[MDres], writes=[gbc[t]])
                if moe:
                    Wr = ph.sb("Wr", [128, 8, 8], F32)
                    rbb = ph.sb("rbb", [128, 8], F32)
                    fw.dma("sp", Wr[:], router_w.rearrange("(kc p) e -> p kc e", p=128), writes=[Wr])
                    fw.dma("sp", rbb[:], router_b[0].partition_broadcast(128), writes=[rbb])
                rings = dict(st=ph.ring("st", 4, [128, 4], F32), xn=ph.ring("xn", 2, [128, D], F32),
                             tp=ph.ring("tp", 2, [128, 4, 128], F32, psum=True))
                hTr = ph.ring("hT", 2, [128, 4, D], F32)
                acc = ph.sb("acc", [128, 4, D], F32)
                fTr = ph.ring("fT", 2, [128, 8, 512], BF16)
                a32 = ph.sb("a32", [128, 8, 128], F32)
                gwr = ph.ring("gw", 2, [128, 4, 8], F32)
                lgt = ph.ring("lgt", 2, [128, 32], F32)
                WGr = ph.ring("WG", 3, [128, 8, cfg["bw"]], BF16)
                WUr = ph.ring("WU", 3, [128, 8, cfg["bw"]], BF16)
                WDr = ph.ring("WD", 5, [128, nffc, D], BF16)
                pgu = ph.ring("pgu", 4, [128, 512], F32, psum=True)
                pyr = ph.ring("py", 2, [128, 512], F32, psum=True)
                sgr = ph.ring("sg", 2, [128, 512], F32)
                gTr = ph.ring("gT", 5, [128, nffc, 512], BF16)
                tiles = ([] if last else [(0, 2)]) + [(2 + 4 * i, 4) for i in range(8)]

                def prologue(c0, nsub):
                    t = 1 if c0 < 2 else 0
                    hT, fT, gw = hTr.next(), fTr.next(), gwr.next()
                    for s in range(nsub):
                        c = c0 + s
                        fw.dma("sp", hT[:, s, :], H[c * 128:(c + 1) * 128, :], reads=[Hres[c]], writes=[hT], merge=(s > 0))
                    for s in range(nsub):
                        st4 = rings["st"].next()
                        xn = rings["xn"].next()
                        fw.op("act", lambda e, s=s: e.activation(out=xn[:], in_=hT[:, s, :], func=AF.Square, accum_out=st4[:, 0:1]), reads=[hT], writes=[xn, st4])
                        fw.op("act", lambda e: e.activation(out=st4[:, 1:2], in_=st4[:, 0:1], func=AF.Sqrt, scale=1.0 / D, bias=cst[:, 0:1]), reads=[st4, cst], writes=[st4])
                        fw.op("dve", lambda e: e.reciprocal(out=st4[:, 2:3], in_=st4[:, 1:2]), reads=[st4], writes=[st4])
                        fw.op("act", lambda e, s=s: e.activation(out=xn[:], in_=hT[:, s, :], func=AF.Copy, scale=st4[:, 2:3]), reads=[hT, st4], writes=[xn])
                        for half in range(2):
                            tp = rings["tp"].next()
                            for j in range(4):
                                kc = half * 4 + j
                                fw.op("pe", lambda e, kc=kc, j=j: e.transpose(out=tp[:, j, :], in_=xn[:, kc * 128:(kc + 1) * 128], identity=ident_f[:]),
                                      reads=[xn, ident_f], writes=[tp])
                            for j in range(4):
                                kc = half * 4 + j
                                fw.op("dve", lambda e, kc=kc, j=j, s=s: e.tensor_scalar(out=fT[:, kc, s * 128:(s + 1) * 128], in0=tp[:, j, :], scalar1=g2T[:, kc, t:t + 1],
                                                                                       scalar2=modT[:, 24 + kc, t:t + 1], op0=ALU.mult, op1=ALU.add),
                                      reads=[tp, g2T, modT], writes=[fT])
                                if moe:
                                    fw.op("dve", lambda e, kc=kc, j=j: e.tensor_scalar(out=a32[:, kc, :], in0=tp[:, j, :], scalar1=g2T[:, kc, t:t + 1],
                                                                                      scalar2=modT[:, 24 + kc, t:t + 1], op0=ALU.mult, op1=ALU.add),
                                          reads=[tp, g2T, modT], writes=[a32])
                        if moe:
                            pl = rings["tp"].next()
                            plv = pl[:].rearrange("p a b -> p (a b)")
                            for kc in range(8):
                                fw.op("pe", lambda e, kc=kc: e.matmul(plv[:, 0:8], lhsT=a32[:, kc, :], rhs=Wr[:, kc, :], start=(kc == 0), stop=(kc == 7)),
                                      reads=[a32, Wr], writes=[pl])
                            lg_ = lgt.next()
                            L = lg_[:, 0:8]
                            fw.op("dve", lambda e: e.tensor_tensor(out=L, in0=plv[:, 0:8], in1=rbb[:], op=ALU.add), reads=[pl, rbb], writes=[lg_])
                            fw.op("dve", lambda e: e.tensor_reduce(out=lg_[:, 24:25], in_=L, axis=AX.X, op=ALU.max), reads=[lg_], writes=[lg_])
                            fw.op("dve", lambda e: e.tensor_scalar(out=lg_[:, 8:16], in0=L, scalar1=lg_[:, 24:25], scalar2=-1e30, op0=ALU.is_ge, op1=ALU.mult), reads=[lg_], writes=[lg_])
                            fw.op("dve", lambda e: e.tensor_tensor(out=lg_[:, 8:16], in0=lg_[:, 8:16], in1=L, op=ALU.add), reads=[lg_], writes=[lg_])
                            fw.op("dve", lambda e: e.tensor_reduce(out=lg_[:, 25:26], in_=lg_[:, 8:16], axis=AX.X, op=ALU.max), reads=[lg_], writes=[lg_])
                            fw.op("dve", lambda e: e.tensor_scalar(out=lg_[:, 8:16], in0=L, scalar1=lg_[:, 25:26], scalar2=None, op0=ALU.is_ge), reads=[lg_], writes=[lg_])
                            fw.op("dve", lambda e: e.tensor_scalar(out=lg_[:, 16:24], in0=L, scalar1=lg_[:, 24:25], scalar2=None, op0=ALU.subtract), reads=[lg_], writes=[lg_])
                            fw.op("act", lambda e: e.activation(out=lg_[:, 16:24], in_=lg_[:, 16:24], func=AF.Exp), reads=[lg_], writes=[lg_])
                            fw.op("dve", lambda e: e.tensor_tensor(out=lg_[:, 16:24], in0=lg_[:, 16:24], in1=lg_[:, 8:16], op=ALU.mult), reads=[lg_], writes=[lg_])
                            fw.op("dve", lambda e: e.tensor_reduce(out=lg_[:, 26:27], in_=lg_[:, 16:24], axis=AX.X, op=ALU.add), reads=[lg_], writes=[lg_])
                            fw.op("dve", lambda e: e.reciprocal(out=lg_[:, 27:28], in_=lg_[:, 26:27]), reads=[lg_], writes=[lg_])
                            fw.op("dve", lambda e, s=s: e.tensor_scalar(out=gw[:, s, :], in0=lg_[:, 16:24], scalar1=lg_[:, 27:28], scalar2=None, op0=ALU.mult), reads=[lg_], writes=[gw])
                    return hT, fT, gw

                blocks = [(ex, b) for ex in range(nexp) for b in range(nblk)]
                nxt_pro = prologue(*tiles[0])
                for ti, (c0, nsub) in enumerate(tiles):
                    t = 1 if c0 < 2 else 0
                    T = nsub * 128
                    hT, fT, gw = nxt_pro

                    def gate_up(ex, b):
                        WG, WU, WDt = WGr.next(), WUr.next(), WDr.next()
                        fw.dma("sp", WG[:], cfg["WG"][ex, b], reads=[cfg["res"]], writes=[WG])
                        fw.dma("sp", WU[:], cfg["WU"][ex, b], reads=[cfg["res"]], writes=[WU])
                        fw.dma("sp", WDt[:], cfg["WD"][ex, b], reads=[cfg["res"]], writes=[WDt])
                        gT = gTr.next()
                        for j in range(nffc):
                            pg_, pu_ = pgu.next(), pgu.next()
                            for (pp, W_) in ((pg_, WG), (pu_, WU)):
                                for kc in range(8):
                                    fw.op("pe", lambda e, kc=kc, j=j, pp=pp, W_=W_: e.matmul(pp[:, 0:T], lhsT=W_[:, kc, j * 128:(j + 1) * 128], rhs=fT[:, kc, 0:T],
                                                                                           start=(kc == 0), stop=(kc == 7)), reads=[W_, fT], writes=[pp])
                            sg = sgr.next()
                            fw.op("act", lambda e, pg_=pg_, sg=sg: e.activation(out=sg[:, 0:T], in_=pg_[:, 0:T], func=AF.Silu), reads=[pg_], writes=[sg])
                            fw.op("dve", lambda e, pu_=pu_, sg=sg, j=j: e.tensor_tensor(out=gT[:, j, 0:T], in0=pu_[:, 0:T], in1=sg[:, 0:T], op=ALU.mult), reads=[pu_, sg], writes=[gT])
                        return gT, WDt

                    def down(ex, items, first):
                        for s in range(nsub):
                            for n in range(2):
                                py = pyr.next()
                                nmm = len(items) * nffc
                                q_ = 0
                                for (gT, WDt) in items:
                                    for j in range(nffc):
                                        fw.op("pe", lambda e, j=j, s=s, n=n, py=py, gT=gT, WDt=WDt, q_=q_: e.matmul(
                                            py[:], lhsT=gT[:, j, s * 128:(s + 1) * 128], rhs=WDt[:, j, n * 512:(n + 1) * 512],
                                            start=(q_ == 0), stop=(q_ == nmm - 1)), reads=[gT, WDt], writes=[py])
                                        q_ += 1
                                av = acc[:, s, n * 512:(n + 1) * 512]
                                if moe:
                                    if first:
                                        fw.op("dve", lambda e, py=py, av=av, s=s: e.tensor_scalar(out=av, in0=py[:], scalar1=gw[:, s, ex:ex + 1], scalar2=None, op0=ALU.mult),
                                              reads=[py, gw], writes=[acc])
                                    else:
                                        fw.op("dve", lambda e, py=py, av=av, s=s: e.scalar_tensor_tensor(out=av, in0=py[:], scalar=gw[:, s, ex:ex + 1], in1=av,
                                                                                                       op0=ALU.mult, op1=ALU.add), reads=[py, gw, acc], writes=[acc])
                                else:
                                    if first:
                                        fw.op("dve", lambda e, py=py, av=av: e.tensor_copy(out=av, in_=py[:]), reads=[py], writes=[acc])
                                    else:
                                        fw.op("dve", lambda e, py=py, av=av: e.tensor_tensor(out=av, in0=py[:], in1=av, op=ALU.add), reads=[py, acc], writes=[acc])

                    GRP = 1 if moe else 2
                    pendl = []
                    isfirst = True
                    for bi, (ex, b) in enumerate(blocks):
                        gT, WDt = gate_up(ex, b)
                        pendl.append((gT, WDt))
                        if len(pendl) == GRP + 1:
                            down(ex if GRP == 1 else 0, pendl[:GRP], isfirst) if GRP > 1 else down(blocks[bi - 1][0], pendl[:1], isfirst)
                            pendl = pendl[GRP:]
                            isfirst = False
                        if bi == min(2, len(blocks) - 1) and ti + 1 < len(tiles):
                            nxt_pro = prologue(*tiles[ti + 1])
                    while pendl:
                        down(blocks[-1][0], pendl[:GRP], isfirst)
                        pendl = pendl[GRP:]
                        isfirst = False
                    for s in range(nsub):
                        c = c0 + s
                        fw.op("pool", lambda e, s=s: e.tensor_tensor(out=acc[:, s, :], in0=acc[:, s, :], in1=gbc[t][:], op=ALU.mult), reads=[acc, gbc[t]], writes=[acc])
                        fw.op("pool", lambda e, s=s: e.tensor_tensor(out=acc[:, s, :], in0=acc[:, s, :], in1=hT[:, s, :], op=ALU.add), reads=[acc, hT], writes=[acc])
                        if last:
                            fw.dma("pool", out[(c - 2) * 128:(c - 1) * 128, :], acc[:, s, :], reads=[acc], writes=[OUTres], holder=acc, kind="o", merge=True)
                        else:
                            fw.dma("pool", H[c * 128:(c + 1) * 128, :], acc[:, s, :], reads=[acc], writes=[Hres[c]], holder=acc, kind="o")
            if dbg == f"F{l}":
                break
        for cfg in FF_CFG:
            r_ = cfg["res"]
            if r_.isem is not None and r_.icnt:
                for k_ in fw.engs:
                    fw._wait(k_, [(r_.isem, r_.icnt)])
        fw.barrier(release=False)
    return nc


_PERM = None


def _w_in_perm():
    q = []
    for i in range(4):
        q += list(range(i * 64, (i + 1) * 64)) + list(range((4 + i) * 64, (5 + i) * 64))
    ak, av, rq, rk, rv, rg, mq, mk, mv, mo, mg = 512, 640, 768, 1024, 1280, 1536, 1792, 2048, 2304, 2560, 2816
    r = lambda a, n: list(range(a, a + n))
    perm = q + r(ak, 128) + r(av, 128) + r(rv, 256) + r(rq, 256) + r(rk, 256) + r(rg, 256) + r(mv, 256) + r(mo, 256) + r(mq, 256) + r(mk, 256) + r(mg, 16)
    assert len(perm) == NIN and len(set(perm)) == NIN
    return np.array(perm)


def _consts():
    ident = np.eye(128, dtype=np.float32)
    s = np.arange(128)[:, None]
    l_ = np.arange(128)[None, :]
    maskF = (s <= l_).astype(np.float32)
    maskB = (s >= l_).astype(np.float32)
    n_freq = 16
    inv_freq = (10000.0 ** (-np.arange(n_freq, dtype=np.float32) / n_freq)).astype(np.float32)
    tok = np.arange(4096)
    row = (tok // 64).astype(np.float32)
    col = (tok % 64).astype(np.float32)
    ang = np.concatenate([row[:, None] * inv_freq, col[:, None] * inv_freq], -1).astype(np.float32)
    cos = np.cos(ang).astype(np.float32).reshape(32, 128, 32).transpose(1, 0, 2)
    sin = np.sin(ang).astype(np.float32).reshape(32, 128, 32).transpose(1, 0, 2)
    pos = np.stack([np.arange(128) + 1.0, 128.0 - np.arange(128)], 1).astype(np.float32)
    grid = np.broadcast_to((np.arange(24, dtype=np.float32) * 512.0)[None, :], (128, 24)).copy()
    return dict(grid512=grid, ident=ident, maskF=maskF, maskB=maskB, cos=np.ascontiguousarray(cos), sin=np.ascontiguousarray(sin), pos=pos)


def make_in_maps(x, c, ctx, c_ctx, mod_w, mod_b, norm1_w, norm2_w, w_in, attn_qn_w, attn_kn_w, ret_decay,
                 ret_norm_w, mlstm_conv_w, mlstm_gate_b, mlstm_norm_w, w_out, ffn_w_gate, ffn_w_up, ffn_w_down,
                 router_w, router_b, moe_w_gate, moe_w_up, moe_w_down, moe_nexp=8):
    f = lambda a: np.ascontiguousarray(np.asarray(a, dtype=np.float32))
    perm = _w_in_perm()
    shared = dict(
        mod_w=f(mod_w),
        modbT=f(np.asarray(mod_b).reshape(DEPTH, 48, 128).transpose(0, 2, 1)),
        n1T=f(np.asarray(norm1_w).reshape(DEPTH, 8, 128).transpose(0, 2, 1)),
        n2T=f(np.asarray(norm2_w).reshape(DEPTH, 8, 128).transpose(0, 2, 1)),
        n2row=f(norm2_w),
        w_in=f(np.asarray(w_in)[:, :, perm]),
        qn_w=f(attn_qn_w), kn_w=f(attn_kn_w),
        ret_decay=f(np.asarray(ret_decay).reshape(DEPTH, 8)),
        ret_norm_w=f(ret_norm_w), conv_w=f(mlstm_conv_w),
        gate_b=f(np.asarray(mlstm_gate_b).reshape(DEPTH, 16)),
        mlstm_norm_w=f(mlstm_norm_w), w_out=f(w_out),
        ffn_wg=f(ffn_w_gate), ffn_wu=f(ffn_w_up), ffn_wd=f(ffn_w_down),
        router_w=f(np.asarray(router_w)[0]), router_b=f(np.asarray(router_b)[0:1]),
        moe_wg=f(np.asarray(moe_w_gate)[0][:moe_nexp]), moe_wu=f(np.asarray(moe_w_up)[0][:moe_nexp]), moe_wd=f(np.asarray(moe_w_down)[0][:moe_nexp]),
    )
    shared.update(_consts())
    x = np.asarray(x); ctx = np.asarray(ctx); c = np.asarray(c); c_ctx = np.asarray(c_ctx)
    maps = []
    for b in range(8):
        m = dict(shared)
        m["xin"] = f(np.concatenate([ctx[b], x[b]], axis=0))
        m["cT"] = f(np.stack([c[b].reshape(8, 128).T, c_ctx.reshape(8, 128).T], axis=-1))
        maps.append(m)
    return maps


def kernel(**inputs):
    nc = build_program()
    maps = make_in_maps(**inputs)
    res = run_bass_kernel_spmd(nc, maps, core_ids=list(range(8)))
    return np.stack([np.asarray(r["out"], dtype=np.float32) for r in res.results], axis=0)
```
